# Optimizing a Trainium2 kernel written in Bass

```python
import math
import jax
import jax.numpy as jnp
from jax import lax
import numpy as np

D_MODEL = 1024
BATCH = 4
SEQ = 8192
DEPTH = 2

DEEPNORM_ALPHA = (2.0 * DEPTH) ** 0.25
DEEPNORM_BETA = (8.0 * DEPTH) ** -0.25
LN_EPS = 1e-5

N_ATTN_HEADS = 8
REL_BUCKETS = 32
REL_MAX_DIST = 2048

A_HEADS = 8
A_HEAD_DIM = D_MODEL // 16
A_WIDTH = A_HEADS * A_HEAD_DIM
A_PATTERNS = ((128, 1), (512, 4), (2048, 16))
A_BLOCK = 128

B_HEADS = 4
B_HEAD_DIM = D_MODEL // 8
B_WIDTH = B_HEADS * B_HEAD_DIM
B_CHUNK = 128
B_CONV = 4

AB_AQ = 0
AB_AK = AB_AQ + A_WIDTH
AB_AV = AB_AK + A_WIDTH
AB_BQ = AB_AV + A_WIDTH
AB_BK = AB_BQ + B_WIDTH
AB_BV = AB_BK + B_WIDTH
AB_BO = AB_BV + B_WIDTH
AB_BI = AB_BO + B_WIDTH
AB_BF = AB_BI + B_HEADS
AB_PROJ = AB_BF + B_HEADS
AB_MIX = A_WIDTH + B_WIDTH

C_HEADS = 8
C_GROUPS = 2
C_HPG = C_HEADS // C_GROUPS
C_HEAD_DIM = D_MODEL // 8
C_WIDTH = C_HEADS * C_HEAD_DIM
CMP_BLOCK = 32
CMP_STRIDE = 16
CMP_HIDDEN = 256
SLC_BLOCK = 64
SLC_TOP_N = 16
WIN = 512
C_QBLOCK = 64
C_OFF_KV = C_WIDTH
C_OFF_GATE = C_OFF_KV + 3 * 2 * C_GROUPS * C_HEAD_DIM
C_PROJ = C_OFF_GATE + 3 * C_HEADS

N_GROUPS = 4
EXPERTS_PER_GROUP = 4
N_EXPERTS = N_GROUPS * EXPERTS_PER_GROUP
EXPERT_HIDDEN = D_MODEL // 2
MOE_TOP_K = 2
MOE_CHUNK = 128

kernel_name = "hybrid_dilated_mlstm_nsa_hmoe_deepnorm"


def layer_norm(x, g, b):
    xf = x.astype(jnp.float32)
    mu = xf.mean(-1, keepdims=True)
    var = jnp.square(xf - mu).mean(-1, keepdims=True)
    y = (xf - mu) * lax.rsqrt(var + LN_EPS) * g.astype(jnp.float32) + b.astype(jnp.float32)
    return y.astype(x.dtype)


def _t5_bucket(dist):
    n = jnp.maximum(dist, 0)
    exact = REL_BUCKETS // 2
    nf = jnp.maximum(n, 1).astype(jnp.float32)
    large = exact + (jnp.log(nf / exact) / math.log(REL_MAX_DIST / exact)
                     * (REL_BUCKETS - exact)).astype(jnp.int32)
    return jnp.where(n < exact, n, jnp.minimum(large, REL_BUCKETS - 1))


def _t5_bias(table, dist):
    return table.astype(jnp.float32)[_t5_bucket(dist)]


def masked_softmax(logits, valid):
    masked = jnp.where(valid, logits.astype(jnp.float32), -jnp.inf)
    m = jnp.max(masked, axis=-1, keepdims=True)
    m = jnp.where(jnp.isfinite(m), m, 0.0)
    p = jnp.exp(masked - m)
    s = p.sum(-1, keepdims=True)
    return p / jnp.maximum(s, 1e-30)


def causal_conv(x, w):
    K, C = w.shape
    return lax.conv_general_dilated(x, w[:, None, :].astype(x.dtype), window_strides=(1,),
                                    padding=[(K - 1, 0)],
                                    dimension_numbers=('NWC', 'WIO', 'NWC'),
                                    feature_group_count=C)


def _dilated_branch(q, k, v, rel_bias, window, dilation):
    Bn, S, H, E = q.shape
    nk = window // dilation
    L = -(-S // dilation)
    nb = -(-L // A_BLOCK)
    Lp = nb * A_BLOCK
    pad = Lp * dilation - S

    def to_res(t):
        t = jnp.pad(t, ((0, 0), (0, pad), (0, 0), (0, 0)))
        t = t.reshape(Bn, Lp, dilation, H, E).transpose(0, 2, 1, 3, 4)
        return t.reshape(Bn, dilation, nb, A_BLOCK, H, E)

    def with_prev(t):
        prev = jnp.pad(t, ((0, 0), (0, 0), (1, 0), (0, 0), (0, 0), (0, 0)))[:, :, :-1]
        return jnp.concatenate([prev, t], axis=3)

    qb = to_res(q)
    kk = with_prev(to_res(k))
    vv = with_prev(to_res(v))
    logits = jnp.einsum('brnqhe,brnkhe->brnhqk', qb, kk).astype(jnp.float32) * (E ** -0.5)
    qi = jnp.arange(A_BLOCK)[:, None]
    ki = jnp.arange(2 * A_BLOCK)[None, :]
    j = qi + A_BLOCK - ki
    valid = (j >= 0) & (j <= nk)
    bias = _t5_bias(rel_bias, jnp.maximum(j, 0) * dilation).transpose(2, 0, 1)
    first = (jnp.arange(nb)[:, None, None] > 0) | (ki[None] >= A_BLOCK)
    valid = valid[None] & first
    logits = jnp.where(valid[None, None, :, None], logits + bias, -jnp.inf)
    m = logits.max(-1, keepdims=True)
    p = jnp.exp(logits - m)
    s = p.sum(-1)
    o = jnp.einsum('brnhqk,brnkhe->brnhqe', p, vv.astype(jnp.float32)) / s[..., None]
    lse = m[..., 0] + jnp.log(s)
    o = o.transpose(0, 2, 4, 1, 3, 5).reshape(Bn, Lp * dilation, H, E)[:, :S]
    lse = lse.transpose(0, 2, 4, 1, 3).reshape(Bn, Lp * dilation, H)[:, :S]
    return o, lse


def dilated_attention(q, k, v, rel_bias):
    outs, lses = [], []
    for window, dilation in A_PATTERNS:
        o, lse = _dilated_branch(q, k, v, rel_bias, window, dilation)
        outs.append(o)
        lses.append(lse)
    wts = jax.nn.softmax(jnp.stack(lses, 0), axis=0)
    return jnp.einsum('pbsh,pbshe->bshe', wts, jnp.stack(outs, 0))


def mlstm(q, k, v, i_pre, f_pre):
    f32 = jnp.float32
    Bn, S, H, dk = q.shape
    dv = v.shape[-1]
    L = B_CHUNK
    nc = S // L

    def chunks(t):
        t = t.astype(f32).reshape((Bn, nc, L) + t.shape[2:])
        return t.transpose((1, 0, 3, 2) + tuple(range(4, t.ndim)))

    qc = chunks(q) * (dk ** -0.5)
    kc, vc = chunks(k), chunks(v)
    ic = chunks(i_pre)
    fc = chunks(jax.nn.log_sigmoid(f_pre.astype(f32)))
    tri = jnp.tril(jnp.ones((L, L), bool))

    def step(carry, inp):
        C, n, m = carry
        qt, kt, vt, it, ft = inp
        b = jnp.cumsum(ft, axis=-1)
        Dm = jnp.where(tri, b[..., :, None] - b[..., None, :] + it[..., None, :], -jnp.inf)
        inter = b + m[..., None]
        m_t = jnp.maximum(inter, Dm.max(-1))
        P = jnp.exp(Dm - m_t[..., None])
        Sqk = jnp.einsum('bhtd,bhsd->bhts', qt, kt) * P
        sc = jnp.exp(inter - m_t)
        num = sc[..., None] * jnp.einsum('bhtd,bhdv->bhtv', qt, C) + jnp.einsum('bhts,bhsv->bhtv', Sqk, vt)
        den = sc * jnp.einsum('bhtd,bhd->bht', qt, n) + Sqk.sum(-1)
        h = num / jnp.maximum(jnp.abs(den), jnp.exp(-m_t))[..., None]
        bL = b[..., -1]
        g = bL[..., None] - b + it
        m_new = jnp.maximum(bL + m, g.max(-1))
        wk = jnp.exp(g - m_new[..., None])
        decay = jnp.exp(bL + m - m_new)
        C_new = decay[..., None, None] * C + jnp.einsum('bhs,bhsd,bhsv->bhdv', wk, kt, vt)
        n_new = decay[..., None] * n + jnp.einsum('bhs,bhsd->bhd', wk, kt)
        return (C_new, n_new, m_new), h

    init = (jnp.zeros((Bn, H, dk, dv), f32), jnp.zeros((Bn, H, dk), f32), jnp.zeros((Bn, H), f32))
    _, hs = lax.scan(step, init, (qc, kc, vc, ic, fc))
    return hs.transpose(1, 0, 3, 2, 4).reshape(Bn, S, H, dv)


def head_norm(h, g):
    mu = h.mean(-1, keepdims=True)
    var = jnp.square(h - mu).mean(-1, keepdims=True)
    y = (h - mu) * lax.rsqrt(var + LN_EPS)
    return y.reshape(h.shape[0], h.shape[1], -1) * g.astype(jnp.float32)


def mixer_ab(x, w_in, gate_b, conv_w, head_g, w_out, rel_bias):
    Bn, S, _ = x.shape
    p = jnp.einsum('bsd,de->bse', x, w_in)
    aq = p[..., AB_AQ:AB_AK].reshape(Bn, S, A_HEADS, A_HEAD_DIM)
    ak = p[..., AB_AK:AB_AV].reshape(Bn, S, A_HEADS, A_HEAD_DIM)
    av = p[..., AB_AV:AB_BQ].reshape(Bn, S, A_HEADS, A_HEAD_DIM)
    qk = jax.nn.silu(causal_conv(p[..., AB_BQ:AB_BV], conv_w))
    bq = qk[..., :B_WIDTH].reshape(Bn, S, B_HEADS, B_HEAD_DIM)
    bk = qk[..., B_WIDTH:].reshape(Bn, S, B_HEADS, B_HEAD_DIM)
    bv = p[..., AB_BV:AB_BO].reshape(Bn, S, B_HEADS, B_HEAD_DIM)
    bo = p[..., AB_BO:AB_BI]
    bi = p[..., AB_BI:AB_BF] + gate_b[:B_HEADS]
    bf = p[..., AB_BF:AB_PROJ] + gate_b[B_HEADS:]
    ya = dilated_attention(aq, ak, av, rel_bias).reshape(Bn, S, A_WIDTH)
    yb = jax.nn.sigmoid(bo.astype(jnp.float32)) * head_norm(mlstm(bq, bk, bv, bi, bf), head_g)
    y = jnp.concatenate([ya.astype(x.dtype), yb.astype(x.dtype)], axis=-1)
    return jnp.einsum('bse,ed->bsd', y, w_out)


def mixer_c(x, w_in, gate_b, cmp_pos, cmp_w1, cmp_w2, w_out, rel_bias):
    f32 = jnp.float32
    Bn, S, _ = x.shape
    G, J, E = C_GROUPS, C_HPG, C_HEAD_DIM
    p = jnp.einsum('bsd,de->bse', x, w_in)
    q = p[..., :C_OFF_KV].reshape(Bn, S, G, J, E).transpose(0, 2, 3, 1, 4).astype(f32) * (E ** -0.5)
    kv = p[..., C_OFF_KV:C_OFF_GATE].reshape(Bn, S, 3, 2, G, E).astype(f32)
    gates = jax.nn.sigmoid((p[..., C_OFF_GATE:] + gate_b).astype(f32)).reshape(Bn, S, 3, G, J)

    n_cmp = (S - CMP_BLOCK) // CMP_STRIDE + 1
    cidx = jnp.arange(n_cmp)[:, None] * CMP_STRIDE + jnp.arange(CMP_BLOCK)[None, :]
    cmp_start, cmp_end = cidx[:, 0], cidx[:, -1]

    def compress(t, pos, w1, w2):
        blocks = t[:, cidx] + pos[None, None, :, None, :]
        flat = blocks.transpose(0, 3, 1, 2, 4).reshape(Bn, G, n_cmp, CMP_BLOCK * E)
        return (jax.nn.gelu(flat @ w1) @ w2).astype(f32)

    k_cmp = compress(kv[:, :, 0, 0], cmp_pos[0], cmp_w1[0], cmp_w2[0])
    v_cmp = compress(kv[:, :, 0, 1], cmp_pos[1], cmp_w1[1], cmp_w2[1])

    n_slc = S // SLC_BLOCK
    top_n = min(SLC_TOP_N, n_slc)
    slc_start = jnp.arange(n_slc) * SLC_BLOCK

    def blocks_of(t):
        return t.transpose(0, 2, 1, 3).reshape(Bn, G, n_slc, SLC_BLOCK * E)

    k_sb, v_sb = blocks_of(kv[:, :, 1, 0]), blocks_of(kv[:, :, 1, 1])
    overlap = ((cmp_start[:, None] < slc_start[None, :] + SLC_BLOCK)
               & (cmp_end[:, None] >= slc_start[None, :])).astype(f32)

    def pad_win(t):
        return jnp.pad(t.transpose(0, 2, 1, 3), ((0, 0), (0, 0), (WIN, 0), (0, 0)))

    k_w, v_w = pad_win(kv[:, :, 2, 0]), pad_win(kv[:, :, 2, 1])

    tb = rel_bias.astype(f32).reshape(REL_BUCKETS, G, J)
    tb_g = tb.transpose(1, 0, 2)
    gsel = jnp.arange(G)[None, :, None, None]

    def grouped_bias(dist2d):
        return tb[_t5_bucket(dist2d)].transpose(2, 3, 0, 1)

    def block(blk):
        s0 = blk * C_QBLOCK
        qpos = s0 + jnp.arange(C_QBLOCK)
        qb = lax.dynamic_slice_in_dim(q, s0, C_QBLOCK, axis=3)
        dist_c = qpos[:, None] - cmp_end[None, :]
        lc = jnp.einsum('bgjqe,bgne->bgjqn', qb, k_cmp) + grouped_bias(dist_c)
        p_c = masked_softmax(lc, dist_c >= 0)
        o_c = jnp.einsum('bgjqn,bgne->bgjqe', p_c, v_cmp)
        imp = jnp.einsum('bgjqn,nm->bgqm', p_c, overlap)
        mblk = jnp.arange(n_slc)[None, :]
        qblk = (qpos // SLC_BLOCK)[:, None]
        forced = (mblk == 0) | (mblk == qblk) | (mblk == qblk - 1)
        score = jnp.where(forced, jnp.inf,
                          jnp.where(slc_start[None, :] <= qpos[:, None], imp, -jnp.inf))
        _, sel = lax.top_k(score, top_n)
        flat_sel = sel.reshape(Bn, G, C_QBLOCK * top_n, 1)
        T = top_n * SLC_BLOCK
        ks = jnp.take_along_axis(k_sb, flat_sel, axis=2).reshape(Bn, G, C_QBLOCK, T, E)
        vs = jnp.take_along_axis(v_sb, flat_sel, axis=2).reshape(Bn, G, C_QBLOCK, T, E)
        kpos_s = (sel[..., None] * SLC_BLOCK + jnp.arange(SLC_BLOCK)).reshape(Bn, G, C_QBLOCK, T)
        dist_s = qpos[None, None, :, None] - kpos_s
        bias_s = jnp.moveaxis(tb_g[gsel, _t5_bucket(dist_s)], -1, 2)
        ls = jnp.einsum('bgjqe,bgqte->bgjqt', qb, ks) + bias_s
        p_s = masked_softmax(ls, (dist_s >= 0)[:, :, None])
        o_s = jnp.einsum('bgjqt,bgqte->bgjqe', p_s, vs)
        kw = lax.dynamic_slice_in_dim(k_w, s0, WIN + C_QBLOCK, axis=2)
        vw = lax.dynamic_slice_in_dim(v_w, s0, WIN + C_QBLOCK, axis=2)
        kpos_w = s0 - WIN + jnp.arange(WIN + C_QBLOCK)
        dist_w = qpos[:, None] - kpos_w[None, :]
        valid_w = (dist_w >= 0) & (dist_w < WIN) & (kpos_w[None, :] >= 0)
        lw = jnp.einsum('bgjqe,bgte->bgjqt', qb, kw) + grouped_bias(dist_w)
        p_w = masked_softmax(lw, valid_w)
        o_w = jnp.einsum('bgjqt,bgte->bgjqe', p_w, vw)
        g = lax.dynamic_slice_in_dim(gates, s0, C_QBLOCK, axis=1).transpose(2, 0, 3, 4, 1)
        return g[0][..., None] * o_c + g[1][..., None] * o_s + g[2][..., None] * o_w

    out = lax.map(block, jnp.arange(S // C_QBLOCK))
    out = out.transpose(1, 0, 4, 2, 3, 5).reshape(Bn, S, C_WIDTH)
    return jnp.einsum('bse,ed->bsd', out.astype(x.dtype), w_out)


def hier_moe(x, wr_g, br_g, wr_e, br_e, w_gate, w_up, w_down):
    f32 = jnp.float32
    Bn, S, D = x.shape
    xt = x.reshape(-1, D)
    g_logits = (xt @ wr_g + br_g).astype(f32)
    g_prob = jax.nn.softmax(g_logits, axis=-1)
    _, g_idx = lax.top_k(g_logits, 1)
    g_w = jnp.take_along_axis(g_prob, g_idx, axis=-1)[:, 0]
    e_all = (jnp.einsum('nd,gde->nge', xt, wr_e) + br_e).astype(f32)
    e_logits = jnp.take_along_axis(e_all, g_idx[:, :, None], axis=1)[:, 0]
    top_v, top_i = lax.top_k(e_logits, MOE_TOP_K)
    e_w = jax.nn.softmax(top_v, axis=-1) * g_w[:, None]
    expert_id = g_idx * EXPERTS_PER_GROUP + top_i
    gates = jnp.einsum('nk,nke->ne', e_w, jax.nn.one_hot(expert_id, N_EXPERTS, dtype=f32))
    wg = w_gate.transpose(1, 0, 2).reshape(D, N_EXPERTS * EXPERT_HIDDEN)
    wu = w_up.transpose(1, 0, 2).reshape(D, N_EXPERTS * EXPERT_HIDDEN)
    wd = w_down.reshape(N_EXPERTS * EXPERT_HIDDEN, D)

    def chunk_fn(args):
        xc, gc = args
        h = jax.nn.silu(xc @ wg) * (xc @ wu)
        h = (h.reshape(-1, N_EXPERTS, EXPERT_HIDDEN) * gc[..., None]).reshape(-1, N_EXPERTS * EXPERT_HIDDEN)
        return h @ wd

    out = lax.map(chunk_fn, (xt.reshape(-1, MOE_CHUNK, D), gates.reshape(-1, MOE_CHUNK, N_EXPERTS)))
    return out.reshape(Bn, S, D).astype(x.dtype)


def setup_inputs(seed: int = 0) -> dict:
    key = jax.random.key(seed)
    ks = iter(jax.random.split(key, 32))

    def nrm(shape, scale):
        return jax.random.normal(next(ks), shape, jnp.float32) * scale

    n_even = (DEPTH + 1) // 2
    n_odd = DEPTH // 2
    D = D_MODEL
    ab_col = np.ones(AB_PROJ, np.float32)
    ab_col[AB_AV:AB_AV + A_WIDTH] = DEEPNORM_BETA
    ab_col[AB_BV:AB_BV + B_WIDTH] = DEEPNORM_BETA
    kv_col = np.ones((3, 2, C_GROUPS * C_HEAD_DIM), np.float32)
    kv_col[:, 1] = DEEPNORM_BETA
    c_col = np.concatenate([np.ones(C_WIDTH, np.float32), kv_col.ravel(), np.ones(3 * C_HEADS, np.float32)])
    return {
        "x": nrm((BATCH, SEQ, D), 1.0),
        "rel_bias": nrm((REL_BUCKETS, N_ATTN_HEADS), 0.2),
        "ln_g": 1.0 + nrm((DEPTH, 2, D), 0.02),
        "ln_b": nrm((DEPTH, 2, D), 0.02),
        "ab_w_in": nrm((n_even, D, AB_PROJ), D ** -0.5) * jnp.asarray(ab_col),
        "ab_gate_b": jnp.concatenate([nrm((n_even, B_HEADS), 0.1),
                                      3.0 + nrm((n_even, B_HEADS), 0.5)], axis=-1),
        "ab_conv": nrm((n_even, B_CONV, 2 * B_WIDTH), B_CONV ** -0.5),
        "ab_head_norm": 1.0 + nrm((n_even, B_WIDTH), 0.02),
        "ab_w_out": nrm((n_even, AB_MIX, D), AB_MIX ** -0.5 * DEEPNORM_BETA),
        "c_w_in": nrm((n_odd, D, C_PROJ), D ** -0.5) * jnp.asarray(c_col),
        "c_gate_b": nrm((n_odd, 3 * C_HEADS), 0.1),
        "c_cmp_pos": nrm((n_odd, 2, CMP_BLOCK, C_HEAD_DIM), 0.1),
        "c_cmp_w1": nrm((n_odd, 2, CMP_BLOCK * C_HEAD_DIM, CMP_HIDDEN), (CMP_BLOCK * C_HEAD_DIM) ** -0.5),
        "c_cmp_w2": nrm((n_odd, 2, CMP_HIDDEN, C_HEAD_DIM), CMP_HIDDEN ** -0.5),
        "c_w_out": nrm((n_odd, C_WIDTH, D), C_WIDTH ** -0.5 * DEEPNORM_BETA),
        "moe_wr_g": nrm((DEPTH, D, N_GROUPS), D ** -0.5),
        "moe_br_g": nrm((DEPTH, N_GROUPS), 0.01),
        "moe_wr_e": nrm((DEPTH, N_GROUPS, D, EXPERTS_PER_GROUP), D ** -0.5),
        "moe_br_e": nrm((DEPTH, N_GROUPS, EXPERTS_PER_GROUP), 0.01),
        "moe_w_gate": nrm((DEPTH, N_EXPERTS, D, EXPERT_HIDDEN), D ** -0.5),
        "moe_w_up": nrm((DEPTH, N_EXPERTS, D, EXPERT_HIDDEN), D ** -0.5),
        "moe_w_down": nrm((DEPTH, N_EXPERTS, EXPERT_HIDDEN, D), EXPERT_HIDDEN ** -0.5 * DEEPNORM_BETA),
    }


def reference(x, rel_bias, ln_g, ln_b, ab_w_in, ab_gate_b, ab_conv, ab_head_norm, ab_w_out,
              c_w_in, c_gate_b, c_cmp_pos, c_cmp_w1, c_cmp_w2, c_w_out,
              moe_wr_g, moe_br_g, moe_wr_e, moe_br_e, moe_w_gate, moe_w_up, moe_w_down):
    h = x
    for layer in range(DEPTH):
        j = layer // 2
        if layer % 2 == 0:
            y = mixer_ab(h, ab_w_in[j], ab_gate_b[j], ab_conv[j], ab_head_norm[j], ab_w_out[j], rel_bias)
        else:
            y = mixer_c(h, c_w_in[j], c_gate_b[j], c_cmp_pos[j], c_cmp_w1[j], c_cmp_w2[j], c_w_out[j], rel_bias)
        h = layer_norm(DEEPNORM_ALPHA * h + y, ln_g[layer, 0], ln_b[layer, 0])
        y = hier_moe(h, moe_wr_g[layer], moe_br_g[layer], moe_wr_e[layer], moe_br_e[layer],
                     moe_w_gate[layer], moe_w_up[layer], moe_w_down[layer])
        h = layer_norm(DEEPNORM_ALPHA * h + y, ln_g[layer, 1], ln_b[layer, 1])
    return h
```

```python
import os
import numpy as np
from contextlib import ExitStack
import concourse.bass as bass
import concourse.mybir as mybir
from concourse.bass_utils import run_bass_kernel_spmd

F32 = mybir.dt.float32
BF16 = mybir.dt.bfloat16
AF = mybir.ActivationFunctionType
ALU = mybir.AluOpType
AX = mybir.AxisListType

D = 1024
ALPHA = (2.0 * 2) ** 0.25
LN_EPS = 1e-5
NEXP = 16
EH = 512


class Buf:
    __slots__ = ("name", "w", "rd", "dsem", "dval", "dkey")

    def __init__(self, name):
        self.name = name
        self.w = None
        self.rd = {}
        self.dsem = None
        self.dval = 0
        self.dkey = None


class Prog:
    def __init__(self, nc, es):
        self.nc = nc
        self.es = es
        self.es_global = es
        self.pfx = ""
        self.eng = dict(pe=nc.tensor, act=nc.scalar, dve=nc.vector, pool=nc.gpsimd, sp=nc.sync)
        self.sem = {k: es.enter_context(nc.semaphore("s_" + k)) for k in self.eng}
        self.cnt = {k: 0 for k in self.eng}
        self.known = {k: {} for k in self.eng}
        self.nsem = 0
        self.outs = []
        self.nbuf = 0
        self.dmasems = {}
        self.issuer = {}
        self.free_sems = []
        self.phase_sems = []
        self.in_phase = False

    def buf(self, name=None):
        self.nbuf += 1
        return Buf(name or f"b{self.nbuf}")

    def sb(self, name, shape, dt):
        return self.es.enter_context(self.nc.sbuf_tensor(self.pfx + name, shape, dt))

    def ps(self, name, shape, dt=F32):
        return self.es.enter_context(self.nc.psum_tensor(self.pfx + name, shape, dt))

    def _collect(self, e, reads, writes):
        deps = {}

        def add(tok, same_ok):
            if tok is None:
                return
            key, sh, val = tok
            if key == e and not same_ok:
                return
            if key in deps and deps[key][1] >= val:
                return
            deps[key] = (sh, val)

        for b in reads:
            add(b.w, e != "pe")
        for b in writes:
            add(b.w, e != "pe")
            for tok in b.rd.values():
                add(tok, e != "pe")
        return deps

    def _wait(self, e, deps):
        kn = self.known[e]
        for key, (sh, val) in deps.items():
            if kn.get(key, 0) >= val:
                continue
            self.eng[e].wait_ge(sh, val)
            kn[key] = val

    def op(self, e, fn, reads=(), writes=()):
        self._wait(e, self._collect(e, reads, writes))
        ins = fn(self.eng[e])
        self.cnt[e] += 1
        ins.then_inc(self.sem[e], 1)
        tok = (e, self.sem[e], self.cnt[e])
        for b in reads:
            b.rd[e] = tok
        for b in writes:
            b.w = tok
            b.rd = {}
        return tok

    def dma(self, q, out, in_, reads=(), writes=(), is_out=False, nowaw=False, **kw):
        wb = writes[0]
        if nowaw:
            saved = wb.w
            if saved is not None and saved[0] == wb.dkey:
                wb.w = None
            deps = self._collect(q, reads, writes)
            wb.w = saved
        else:
            deps = self._collect(q, reads, writes)
        self._wait(q, deps)
        self._ensure_sem(wb)
        ins = self.eng[q].dma_start(out=out, in_=in_, **kw)
        wb.dval += 16
        ins.then_inc(wb.dsem, 16)
        tok = (wb.dkey, wb.dsem, wb.dval)
        self.dmasems[wb.dkey] = tok
        self.issuer[wb.dkey] = q
        for b in reads:
            b.rd[wb.dkey] = tok
        for b in writes:
            b.w = tok
            b.rd = {}
        if is_out and wb not in self.outs:
            self.outs.append(wb)
        return tok

    def _ensure_sem(self, wb):
        if wb.dsem is not None:
            return
        if self.free_sems:
            wb.dkey, wb.dsem, wb.dval = self.free_sems.pop()
        else:
            wb.dkey = f"d{self.nsem}"
            wb.dsem = self.es_global.enter_context(self.nc.semaphore(wb.dkey))
            self.nsem += 1
        if self.in_phase:
            self.phase_sems.append(wb)

    def recycle(self):
        for wb in self.phase_sems:
            self.free_sems.append((wb.dkey, wb.dsem, wb.dval))
            wb.dsem = None
        self.phase_sems = []

    def _tok_async(self, q, wb, ins, inc, reads, writes):
        self._ensure_sem(wb)
        wb.dval += inc
        ins.then_inc(wb.dsem, inc)
        tok = (wb.dkey, wb.dsem, wb.dval)
        self.dmasems[wb.dkey] = tok
        self.issuer[wb.dkey] = q
        for b in reads:
            b.rd[wb.dkey] = tok
        for b in writes:
            b.w = tok
            b.rd = {}
        return tok

    def gather(self, out, in_dram, idx_ap, reads=(), writes=()):
        self._wait("pool", self._collect("pool", reads, writes))
        ins = self.nc.gpsimd.indirect_dma_start(out=out, out_offset=None, in_=in_dram,
                                                in_offset=bass.IndirectOffsetOnAxis(ap=idx_ap, axis=0))
        return self._tok_async("pool", writes[0], ins, 16, reads, writes)

    def all_gather(self, src, dst, groups, reads=(), writes=()):
        self._wait("pool", self._collect("pool", reads, writes))
        ins = self.nc.gpsimd.collective_compute("AllGather", ALU.bypass, replica_groups=groups, ins=[src.opt()], outs=[dst.opt()])
        return self._tok_async("pool", writes[0], ins, 1, reads, writes)

    def finish(self):
        for wb in self.outs:
            if wb.dsem is not None:
                self.eng["sp"].wait_ge(wb.dsem, wb.dval)


def bcast_rows(ap1d_tensor, offset, n, parts=128):
    return bass.AP(ap1d_tensor, offset, [[0, parts], [1, n]])


def emit_tail(P, hmid, hmid_buf, out, out_buf, T, w, layer, ST=1024, banks=None, is_final=True):
    nc = P.nc
    NT = ST // 128
    NK = ST // 512
    ident = P.sb("t_ident", [128, 128], F32)
    ones1 = P.sb("t_ones1", [1, 128], F32)
    brow = P.sb("t_brow", [1, 20], F32)
    wr = P.sb("t_wr", [128, 8, 20], F32)
    gbc = P.sb("t_gbc", [128, D], F32)
    bbc = P.sb("t_bbc", [128, D], F32)
    xin = [P.sb(f"t_xin{i}", [128, D], F32) for i in range(2)]
    acc = P.sb("t_acc", [128, NT, D], F32)
    hT16 = P.sb("t_hT16", [128, 8, ST], BF16)
    hT32 = P.sb("t_hT32", [128, 8, 128], F32)
    gates = P.sb("t_gates", [128, NT, NEXP], F32)
    wgb = [P.sb(f"t_wg{i}", [128, 8, EH], BF16) for i in range(2)]
    wub = [P.sb(f"t_wu{i}", [128, 8, EH], BF16) for i in range(2)]
    wdb = [P.sb(f"t_wd{i}", [128, 4, D], BF16) for i in range(2)]
    sg = [P.sb(f"t_sg{i}", [128, 512], BF16) for i in range(2)]
    hid = [P.sb(f"t_hid{i}", [128, 4, 512], BF16) for i in range(2)]
    obuf = [P.sb(f"t_ob{i}", [128, D], F32) for i in range(2)]
    sm = P.sb("t_sm", [128, 64], F32)
    lg = P.sb("t_lg", [128, 20], F32)
    stats = P.sb("t_stats", [128, 2, 6], F32)

    if banks is None:
        banks = [P.ps(f"t_ps{i}", [128, 512]) for i in range(8)]
    ps_g, ps_u, ps_o, ps_t = banks[0:2], banks[2:4], banks[4:6], banks[6:8]

    B = P.buf
    b_ident, b_ones1, b_brow, b_wr, b_gbc, b_bbc = B(), B(), B(), B(), B(), B()
    b_xin = [B(), B()]
    b_acc = [B() for _ in range(NT)]
    b_hT16 = [B() for _ in range(NT)]
    b_hT32 = B()
    b_gates = [B() for _ in range(NT)]
    b_wg, b_wu, b_wd = [B(), B()], [B(), B()], [B(), B()]
    b_sg = [B(), B()]
    b_hid = [[B() for _ in range(4)] for _ in range(2)]
    b_ob = [B(), B()]
    b_outw = [B(), B()]
    b_sm = B()
    b_lg = B()
    b_stats = B()
    b_psg, b_psu, b_pso, b_pst = [B(), B()], [B(), B()], [B(), B()], [B(), B()]

    P.dma("sp", ident[:], w["ident"][:, :], writes=[b_ident])
    P.dma("sp", brow[:], w["br"][:, :], writes=[b_brow])
    P.dma("sp", wr[:], w["wr"].rearrange("(c p) n -> p c n", p=128), writes=[b_wr])
    P.dma("sp", gbc[:], bcast_rows(w["ln_g"].tensor, w["ln_g"].offset, D), writes=[b_gbc])
    P.dma("sp", bbc[:], bcast_rows(w["ln_b"].tensor, w["ln_b"].offset, D), writes=[b_bbc])
    P.op("dve", lambda e: e.memset(ones1[:], 1.0), writes=[b_ones1])

    nst = T // ST
    wslot = 0
    DBG = ()

    stg = [P.sb(f"t_stg{i}", [128, 4, 512], F32) for i in range(3)]
    b_stg = [B() for _ in range(3)]
    stg_i = [0]

    def load_chunk(dst, bdst, src):
        k = stg_i[0] % 3
        stg_i[0] += 1
        P.dma("sp", stg[k][:], src, writes=[b_stg[k]])
        P.op("pool", lambda en, k=k, dst=dst: en.tensor_copy(dst, stg[k][:]), reads=[b_stg[k]], writes=[bdst])

    def load_expert(e, slot):
        for h in range(2):
            load_chunk(wgb[slot][:, 4 * h:4 * h + 4, :], b_wg[slot],
                       w["wg"][e][512 * h:512 * h + 512, :].rearrange("(c p) h -> p c h", p=128))
        for h in range(2):
            load_chunk(wub[slot][:, 4 * h:4 * h + 4, :], b_wu[slot],
                       w["wu"][e][512 * h:512 * h + 512, :].rearrange("(c p) h -> p c h", p=128))
        for h in range(2):
            load_chunk(wdb[slot][:, :, 512 * h:512 * h + 512], b_wd[slot],
                       w["wd"][e][:, 512 * h:512 * h + 512].rearrange("(c p) h -> p c h", p=128))

    xi = 0
    oi = 0
    for st in range(nst):
        t0 = st * ST
        if "noload" not in DBG:
            load_expert(0, wslot)
        for tt in range(NT):
            xb, bx = xin[xi % 2], b_xin[xi % 2]
            xi += 1
            P.dma("sp", xb[:], hmid[t0 + tt * 128:t0 + (tt + 1) * 128, :], reads=[hmid_buf], writes=[bx])
            P.op("act", lambda e, xb=xb, tt=tt: e.mul(acc[:, tt, :], xb[:], ALPHA), reads=[bx], writes=[b_acc[tt]])
            if 'notr' in DBG:
                continue
            for half in range(2):
                pt, bpt = ps_t[half], b_pst[half]
                for j in range(4):
                    dc = half * 4 + j
                    P.op("pe", lambda e, pt=pt, j=j, dc=dc, xb=xb: e.transpose(
                        pt[:, j * 128:(j + 1) * 128], xb[:, dc * 128:(dc + 1) * 128], ident[:]),
                        reads=[bx, b_ident], writes=[bpt])
                P.op("act", lambda e, pt=pt, half=half: e.copy(
                    hT32[:, half * 4:(half + 1) * 4, :], pt[:].rearrange("p (c t) -> p c t", c=4)),
                    reads=[bpt], writes=[b_hT32])
                P.op("dve", lambda e, pt=pt, half=half, tt=tt: e.tensor_copy(
                    hT16[:, half * 4:(half + 1) * 4, tt * 128:(tt + 1) * 128],
                    hT32[:, half * 4:(half + 1) * 4, :]),
                    reads=[b_hT32], writes=[b_hT16[tt]])
            if 'norouter' in DBG:
                continue
            pr, bpr = ps_o[0], b_pso[0]
            for dc in range(8):
                P.op("pe", lambda e, dc=dc, pr=pr: e.matmul(pr[:, 0:20], hT32[:, dc, :], wr[:, dc, :],
                                                            start=(dc == 0), stop=False),
                     reads=[b_hT32, b_wr], writes=[bpr])
            P.op("pe", lambda e, pr=pr: e.matmul(pr[:, 0:20], ones1[0:1, :], brow[0:1, :], start=False, stop=True),
                 reads=[b_ones1, b_brow], writes=[bpr])
            P.op("act", lambda e, pr=pr: e.copy(lg[:], pr[:, 0:20]), reads=[bpr], writes=[b_lg])
            emit_router(P, lg, b_lg, sm, b_sm, gates, b_gates[tt], tt)
        units = [(e, tk) for e in range(NEXP) for tk in range(NK)]

        def gu(ui):
            e, tk = units[ui]
            slot = (wslot + e) % 2
            hb = ui % 2
            for hc in range(4):
                pg, bpg = ps_g[hc % 2], b_psg[hc % 2]
                pu, bpu = ps_u[hc % 2], b_psu[hc % 2]
                rds = [b_hT16[tk * 4 + q] for q in range(4)]
                for dc in range(8):
                    P.op("pe", lambda en, pg=pg, dc=dc, hc=hc, slot=slot, tk=tk: en.matmul(
                        pg[:], wgb[slot][:, dc, hc * 128:(hc + 1) * 128], hT16[:, dc, tk * 512:(tk + 1) * 512],
                        start=(dc == 0), stop=(dc == 7)), reads=rds + [b_wg[slot]], writes=[bpg])
                for dc in range(8):
                    P.op("pe", lambda en, pu=pu, dc=dc, hc=hc, slot=slot, tk=tk: en.matmul(
                        pu[:], wub[slot][:, dc, hc * 128:(hc + 1) * 128], hT16[:, dc, tk * 512:(tk + 1) * 512],
                        start=(dc == 0), stop=(dc == 7)), reads=rds + [b_wu[slot]], writes=[bpu])
                s, bs = sg[hc % 2], b_sg[hc % 2]
                P.op("act", lambda en, s=s, pg=pg: en.activation(s[:], pg[:], AF.Silu), reads=[bpg], writes=[bs])
                P.op("dve", lambda en, s=s, pu=pu, hb=hb, hc=hc: en.tensor_tensor(
                    hid[hb][:, hc, :], s[:], pu[:], ALU.mult), reads=[bs, bpu], writes=[b_hid[hb][hc]])

        def down(ui):
            e, tk = units[ui]
            slot = (wslot + e) % 2
            hb = ui % 2
            k = 0
            for tq in range(4):
                tt = tk * 4 + tq
                for dh in range(2):
                    po, bpo = ps_o[k % 2], b_pso[k % 2]
                    k += 1
                    for hc in range(4):
                        P.op("pe", lambda en, po=po, hc=hc, tq=tq, dh=dh, hb=hb, slot=slot: en.matmul(
                            po[:], hid[hb][:, hc, tq * 128:(tq + 1) * 128], wdb[slot][:, hc, dh * 512:(dh + 1) * 512],
                            start=(hc == 0), stop=(hc == 3)), reads=[b_hid[hb][hc], b_wd[slot]], writes=[bpo])
                    P.op("dve", lambda en, po=po, tt=tt, dh=dh, e=e: en.scalar_tensor_tensor(
                        acc[:, tt, dh * 512:(dh + 1) * 512], po[:], gates[:, tt, e:e + 1],
                        acc[:, tt, dh * 512:(dh + 1) * 512], ALU.mult, ALU.add),
                        reads=[bpo, b_gates[tt], b_acc[tt]], writes=[b_acc[tt]])

        if "noexp" in DBG:
            units = []
        if "oneexp" in DBG:
            units = units[:2]
        for ui in range(len(units)):
            e, tk = units[ui]
            gu(ui)
            if ui > 0:
                down(ui - 1)
            if tk == 0 and e + 1 < NEXP:
                load_expert(e + 1, (wslot + e + 1) % 2)
        if units:
            down(len(units) - 1)
        wslot = (wslot + NEXP) % 2
        for tt in range(NT):
            ob, bo = obuf[oi % 2], b_ob[oi % 2]
            oi += 1
            if "nolnorm" in DBG:
                P.op("act", lambda e, ob=ob, tt=tt: e.copy(ob[:], acc[:, tt, :]), reads=[b_acc[tt]], writes=[bo])
            else:
                emit_ln(P, acc[:, tt, :], b_acc[tt], ob, bo, gbc, b_gbc, bbc, b_bbc, stats, b_stats, sm, b_sm)
            P.dma("sp", out(t0 + tt * 128), ob[:], reads=[bo], writes=[b_outw[(oi - 1) % 2]], is_out=is_final)


def emit_ln(P, x, bx, ob, bo, gbc, b_gbc, bbc, b_bbc, stats, b_stats, sm, b_sm):
    mv = sm[:, 40:42]
    rstd = sm[:, 42:43]
    nmr = sm[:, 43:44]
    P.op("dve", lambda e: e.bn_stats(stats[:, 0, :], x[:, 0:512]), reads=[bx], writes=[b_stats])
    P.op("dve", lambda e: e.bn_stats(stats[:, 1, :], x[:, 512:1024]), reads=[bx], writes=[b_stats])
    P.op("dve", lambda e: e.bn_aggr(mv, stats[:]), reads=[b_stats], writes=[b_sm])
    P.op("dve", lambda e: e.tensor_scalar(rstd, sm[:, 41:42], LN_EPS, None, ALU.add), reads=[b_sm], writes=[b_sm])
    P.op("act", lambda e: e.sqrt(rstd, rstd), reads=[b_sm], writes=[b_sm])
    P.op("dve", lambda e: e.reciprocal(rstd, rstd), reads=[b_sm], writes=[b_sm])
    P.op("dve", lambda e: e.scalar_tensor_tensor(nmr, sm[:, 40:41], -1.0, rstd, ALU.mult, ALU.mult), reads=[b_sm], writes=[b_sm])
    P.op("act", lambda e: e.activation(ob[:], x, AF.Identity, bias=nmr, scale=rstd), reads=[bx, b_sm], writes=[bo])
    P.op("dve", lambda e: e.tensor_tensor(ob[:], ob[:], gbc[:], ALU.mult), reads=[bo, b_gbc], writes=[bo])
    P.op("dve", lambda e: e.tensor_tensor(ob[:], ob[:], bbc[:], ALU.add), reads=[bo, b_bbc], writes=[bo])


def emit_router(P, lg, b_lg, sm, b_sm, gates, b_g, tt):
    gmax, ngmax, gsum, gw = sm[:, 0:1], sm[:, 1:2], sm[:, 2:3], sm[:, 3:4]
    ohg, gexp, esel, oh1 = sm[:, 4:8], sm[:, 8:12], sm[:, 12:16], sm[:, 16:20]
    em, oh2 = sm[:, 20:24], sm[:, 24:28]
    m1, m2, dd, ed, den, w1, w2 = (sm[:, 28 + i:29 + i] for i in range(7))
    ew = sm[:, 36:40]
    R = [b_lg, b_sm]
    W = [b_sm]

    def dv(fn, reads=R, writes=W):
        P.op("dve", fn, reads=reads, writes=writes)

    dv(lambda e: e.tensor_reduce(gmax, lg[:, 0:4], AX.X, ALU.max))
    dv(lambda e: e.tensor_scalar(ohg, lg[:, 0:4], gmax, None, ALU.is_equal))
    dv(lambda e: e.tensor_scalar(ngmax, gmax, -1.0, None, ALU.mult))
    P.op("act", lambda e: e.activation(gexp, lg[:, 0:4], AF.Exp, bias=ngmax, scale=1.0), reads=R, writes=W)
    dv(lambda e: e.tensor_reduce(gsum, gexp, AX.X, ALU.add))
    dv(lambda e: e.reciprocal(gw, gsum))
    dv(lambda e: e.tensor_scalar(esel, lg[:, 4:8], ohg[:, 0:1], None, ALU.mult))
    for g in range(1, 4):
        dv(lambda e, g=g: e.scalar_tensor_tensor(esel, lg[:, 4 + 4 * g:8 + 4 * g], ohg[:, g:g + 1], esel, ALU.mult, ALU.add))
    dv(lambda e: e.tensor_reduce(m1, esel, AX.X, ALU.max))
    dv(lambda e: e.tensor_scalar(oh1, esel, m1, None, ALU.is_equal))
    dv(lambda e: e.scalar_tensor_tensor(em, oh1, -1e30, esel, ALU.mult, ALU.add))
    dv(lambda e: e.tensor_reduce(m2, em, AX.X, ALU.max))
    dv(lambda e: e.tensor_scalar(oh2, em, m2, None, ALU.is_equal))
    dv(lambda e: e.tensor_tensor(dd, m2, m1, ALU.subtract))
    P.op("act", lambda e: e.activation(ed, dd, AF.Exp), reads=R, writes=W)
    dv(lambda e: e.tensor_scalar(den, ed, 1.0, None, ALU.add))
    dv(lambda e: e.reciprocal(den, den))
    dv(lambda e: e.tensor_tensor(w1, den, gw, ALU.mult))
    dv(lambda e: e.tensor_tensor(w2, w1, ed, ALU.mult))
    dv(lambda e: e.tensor_scalar(ew, oh1, w1, None, ALU.mult))
    dv(lambda e: e.scalar_tensor_tensor(ew, oh2, w2, ew, ALU.mult, ALU.add))
    for g in range(4):
        dv(lambda e, g=g: e.tensor_scalar(gates[:, tt, 4 * g:4 * g + 4], ew, ohg[:, g:g + 1], None, ALU.mult),
           reads=[b_sm], writes=[b_g])


def t5_bucket_np(dist):
    n = np.maximum(dist, 0)
    nf = np.maximum(n, 1).astype(np.float32)
    large = 16 + (np.log(nf / np.float32(16)) / np.float32(np.log(2048 / 16)) * np.float32(16)).astype(np.int32)
    return np.where(n < 16, n, np.minimum(large, 31))


def dil_onehot():
    oh = np.zeros((33, 3, 384), np.float32)
    for di, d in enumerate((1, 4, 16)):
        for jp in range(384):
            j = jp - 127
            if 0 <= j <= 128:
                oh[t5_bucket_np(np.array(j * d)), di, jp] = 1.0
            else:
                oh[32, di, jp] = 1.0
    return oh


class Banks:
    def __init__(self, P):
        self.f = [P.ps(f"bank{i}", [128, 512], F32) for i in range(7)]
        self.bf = [P.buf() for _ in range(7)]
        self.h = P.ps("bankh", [128, 1024], BF16)
        self.bh = P.buf()


def full_barrier(P):
    for q in ("sp", "pool"):
        eq = P.eng[q]
        for key, (k_, sh, val) in list(P.dmasems.items()):
            if P.issuer.get(key) == q and P.known[q].get(key, 0) < val:
                eq.wait_ge(sh, val)
                P.known[q][key] = val
        eq.sem_inc(P.sem[q], 1)
        P.cnt[q] += 1
    for e in P.eng:
        for f in P.eng:
            if f != e and P.cnt[f] > P.known[e].get(f, 0):
                P.eng[e].wait_ge(P.sem[f], P.cnt[f])
                P.known[e][f] = P.cnt[f]
    for e in P.eng:
        for (key, sh, val) in P.dmasems.values():
            P.known[e][key] = max(P.known[e].get(key, 0), val)


class Stager:
    def __init__(self, P, n=3, cols=2048):
        self.P = P
        self.cols = cols
        self.t = [P.sb(f"stg{i}", [128, cols], F32) for i in range(n)]
        self.b = [P.buf() for _ in range(n)]
        self.i = 0

    def load(self, dst3, bdst, src2d, eng="pool"):
        C, W = dst3.shape[1], dst3.shape[2]
        per = max(1, self.cols // W)
        c0 = 0
        while c0 < C:
            c1 = min(C, c0 + per)
            k = self.i % len(self.t)
            self.i += 1
            st = self.t[k][:, 0:(c1 - c0) * W].rearrange("p (c w) -> p c w", w=W)
            self.P.dma("sp", st, src2d[c0 * 128:c1 * 128, :].rearrange("(c p) w -> p c w", p=128), writes=[self.b[k]])
            self.P.op(eng, lambda en, st=st, c0=c0, c1=c1: en.tensor_copy(dst3[:, c0:c1, :], st),
                      reads=[self.b[k]], writes=[bdst])
            c0 = c1


def load_xT(P, BK, x, bx_dram, xin, b_xin, xT, b_xT, ident, b_ident, t0, ntile, ctr):
    for tl in range(ntile):
        k = ctr[0] % 2
        ctr[0] += 1
        xb, bx = xin[k], b_xin[k]
        P.dma("sp", xb[:], x(t0 + tl * 128), reads=[bx_dram], writes=[bx])
        for half in range(2):
            pt, bpt = BK.f[half], BK.bf[half]
            for j in range(4):
                dc = half * 4 + j
                P.op("pe", lambda e, pt=pt, j=j, dc=dc, xb=xb: e.transpose(
                    pt[:, j * 128:(j + 1) * 128], xb[:, dc * 128:(dc + 1) * 128], ident[:]),
                    reads=[bx, b_ident], writes=[bpt])
            eng = "act" if half == 0 else "dve"
            if eng == "act":
                P.op("act", lambda e, pt=pt, half=half, tl=tl: e.copy(
                    xT[:, half * 4:(half + 1) * 4, tl * 128:(tl + 1) * 128], pt[:].rearrange("p (c t) -> p c t", c=4)),
                    reads=[bpt], writes=[b_xT[tl]])
            else:
                P.op("dve", lambda e, pt=pt, half=half, tl=tl: e.tensor_copy(
                    xT[:, half * 4:(half + 1) * 4, tl * 128:(tl + 1) * 128], pt[:].rearrange("p (c t) -> p c t", c=4)),
                    reads=[bpt], writes=[b_xT[tl]])


S_TOT = 8192
NBLK = 4
BLK = 2048


def declare_mixer0(nc, pfx=""):
    din = lambda n, s: nc.dram_tensor(pfx + n, s, F32, kind="ExternalInput").ap()
    return dict(x=din("x", [S_TOT, D]), wA=din("wA", [D, 768]), wB=din("wB", [D, 1028]), gbias=din("gbias", [1, 4]),
                convw=din("convw", [128, 16]), hng=din("hng", [1, 256]), relb=din("relb", [33, 4]), oh=din("oh", [33, 3 * 384]),
                triu=din("triu", [128, 128]),
                scr=nc.dram_tensor(pfx + "scr_e", [128, 12 * 384], F32).ap())


def emit_mixer0(P, io):
    nc = P.nc
    x_d, wA, wB, gbias, convw, hng, relb = io["x"], io["wA"], io["wB"], io["gbias"], io["convw"], io["hng"], io["relb"]
    oh_d, ident_d, triu_d, scr = io["oh"], io["ident"], io["triu"], io["scr"]
    x = lambda t0: x_d[t0:t0 + 128, :]
    ystore = io["ystore"]
    if True:
        B = P.buf
        BK = Banks(P)
        b_x, b_y, b_scr = B(), B(), B()
        ident = P.sb("ident_sb", [128, 128], F32)
        b_ident = B()
        P.dma("sp", ident[:], ident_d[:, :], writes=[b_ident])
        xin = [P.sb(f"xin{i}", [128, D], F32) for i in range(2)]
        b_xin = [B(), B()]
        xT = P.sb("xT", [128, 8, BLK], BF16)
        b_xT = [B() for _ in range(16)]
        stg = Stager(P)
        ctr = [0]
        ones_bf = P.sb("ones_bf", [128, 128], BF16)
        b_ones = B()
        P.op("dve", lambda e: e.memset(ones_bf[:], 1.0), writes=[b_ones])

        with ExitStack() as esA:
            sbA = lambda n, s, d: esA.enter_context(nc.sbuf_tensor(P.pfx + n, s, d))
            Etab = sbA("a_E", [128, 12, 2, 128], BF16)
            with ExitStack() as esT:
                sbA_outer = sbA
                sbA = lambda n, s, d: esT.enter_context(nc.sbuf_tensor(P.pfx + n, s, d))
                tab = sbA("a_tab", [33, 4], F32)
                ohs = sbA("a_oh", [33, 3 * 384], F32)
                tabB = sbA("a_tabB", [33, 128], F32)
                fre = sbA("a_fre", [128, 12 * 384], F32)
                E32 = sbA("a_E32", [128, 12 * 256], F32)
                b_tab, b_oh, b_tabB, b_fre, b_E32, b_E = B(), B(), B(), B(), B(), B()
                P.dma("sp", tab[:], relb[:, :], writes=[b_tab])
                P.dma("sp", ohs[:], oh_d[:, :], writes=[b_oh])
                for h in range(4):
                    P.op("dve", lambda e, h=h: e.tensor_copy(tabB[:], tab[:, h:h + 1].to_broadcast([33, 128])),
                         reads=[b_tab], writes=[b_tabB])
                    for di in range(3):
                        pb, bpb = BK.f[2 + (di % 2)], BK.bf[2 + (di % 2)]
                        P.op("pe", lambda e, pb=pb, di=di: e.matmul(pb[:, 0:384], tabB[:], ohs[:, di * 384:(di + 1) * 384],
                                                                    start=True, stop=True),
                             reads=[b_tabB, b_oh], writes=[bpb])
                        P.op("act", lambda e, pb=pb, di=di, h=h: e.activation(
                            fre[:, (di * 4 + h) * 384:(di * 4 + h + 1) * 384], pb[:, 0:384], AF.Exp),
                            reads=[bpb], writes=[b_fre])
                P.dma("sp", scr[:, :], fre[:], reads=[b_fre], writes=[b_scr])
                L = 12 * 384
                skew = bass.AP(scr.tensor, 127, [[L - 1, 128], [384, 12], [128, 2], [1, 128]])
                P.dma("sp", E32[:].rearrange("p (t s q) -> p t s q", t=12, s=2), skew, reads=[b_scr], writes=[b_E32])
                P.op("dve", lambda e: e.tensor_copy(Etab[:].rearrange("p t s q -> p (t s q)"), E32[:]),
                     reads=[b_E32], writes=[b_E])

                full_barrier(P)
            sbA = sbA_outer
            wAb = sbA("a_w", [128, 8, 384], BF16)
            b_wA = B()
            QT = sbA("a_QT", [128, S_TOT], BF16)
            KT = sbA("a_KT", [128, S_TOT], BF16)
            b_QT = [B() for _ in range(NBLK)]
            b_KT = [B() for _ in range(NBLK)]
            Vd = [sbA(f"a_V{di}", [128, 64, 128], BF16) for di in range(3)]
            b_Vd = [[B() for _ in range(NBLK)] for _ in range(3)]
            numacc = sbA("a_num", [128, BLK], F32)
            denacc = sbA("a_den", [128, BLK], F32)
            b_num, b_den = B(), B()
            Pb = [sbA(f"a_P{i}", [128, 2, 2, 128], BF16) for i in range(2)]
            b_Pb = [B(), B()]
            ybuf = sbA("a_y", [128, BLK], BF16)
            b_yb = B()
            pcount = 0
            for hp in range(2):
                stg.load(wAb[:], b_wA, wA[:, hp * 384:(hp + 1) * 384])
                for blk in range(NBLK):
                    T0 = blk * BLK
                    load_xT(P, BK, x, b_x, xin, b_xin, xT, b_xT, ident, b_ident, T0, 16, ctr)
                    for qtr in range(4):
                        rds = [b_xT[qtr * 4 + i] for i in range(4)] + [b_wA]
                        for sel, (dst, bd) in enumerate(((QT, b_QT[blk]), (KT, b_KT[blk]))):
                            pb, bpb = BK.f[2 + sel], BK.bf[2 + sel]
                            for dc in range(8):
                                P.op("pe", lambda e, pb=pb, dc=dc, sel=sel, qtr=qtr: e.matmul(
                                    pb[:], wAb[:, dc, sel * 128:(sel + 1) * 128], xT[:, dc, qtr * 512:(qtr + 1) * 512],
                                    start=(dc == 0), stop=(dc == 7)), reads=rds, writes=[bpb])
                            if sel == 0:
                                P.op("act", lambda e, pb=pb, dst=dst, T0=T0, qtr=qtr: e.copy(
                                    dst[:, T0 + qtr * 512:T0 + (qtr + 1) * 512], pb[:]), reads=[bpb], writes=[bd])
                            else:
                                P.op("dve", lambda e, pb=pb, dst=dst, T0=T0, qtr=qtr: e.tensor_copy(
                                    dst[:, T0 + qtr * 512:T0 + (qtr + 1) * 512], pb[:]), reads=[bpb], writes=[bd])
                    for di, d in enumerate((1, 4, 16)):
                        npb = 16 // d
                        tiles = [(nl, r) for nl in range(npb) for r in range(d)]
                        for g4 in range(4):
                            pb, bpb = BK.f[2 + (g4 % 2)], BK.bf[2 + (g4 % 2)]
                            for j in range(4):
                                nl, r = tiles[g4 * 4 + j]
                                s0 = r + d * 128 * nl
                                for dc in range(8):
                                    P.op("pe", lambda e, pb=pb, j=j, dc=dc, s0=s0, d=d: e.matmul(
                                        pb[:, j * 128:(j + 1) * 128], xT[:, dc, s0:s0 + 127 * d + 1:d], wAb[:, dc, 256:384],
                                        start=(dc == 0), stop=(dc == 7)), reads=b_xT + [b_wA], writes=[bpb])
                            nl0, r0 = tiles[g4 * 4]
                            idx0 = (blk * npb + nl0) * d + r0
                            P.op("act" if g4 % 2 == 0 else "dve",
                                 (lambda e, pb=pb, di=di, idx0=idx0: e.copy(
                                     Vd[di][:, idx0:idx0 + 4, :], pb[:].rearrange("p (j c) -> p j c", j=4)))
                                 if g4 % 2 == 0 else
                                 (lambda e, pb=pb, di=di, idx0=idx0: e.tensor_copy(
                                     Vd[di][:, idx0:idx0 + 4, :], pb[:].rearrange("p (j c) -> p j c", j=4))),
                                 reads=[bpb], writes=[b_Vd[di][blk]])
                    for di, d in enumerate((1, 4, 16)):
                        npb = 16 // d
                        for nl in range(npb):
                            n = blk * npb + nl
                            nslot = 2 if n > 0 else 1
                            for r in range(d):
                                qs = T0 + r + d * 128 * nl
                                ql = r + d * 128 * nl
                                par = pcount % 2
                                ps_h = [BK.f[2 + 2 * par + h] for h in range(2)]
                                bps_h = [BK.bf[2 + 2 * par + h] for h in range(2)]
                                pb_, bpb_ = Pb[pcount % 2], b_Pb[pcount % 2]
                                pcount += 1
                                kblks = sorted({blk, (qs - d * 128) // BLK}) if n > 0 else [blk]
                                rds = [b_QT[blk]] + [b_KT[kb] for kb in kblks]
                                for h in range(2):
                                    for sl in range(nslot):
                                        ks = qs - sl * d * 128
                                        P.op("pe", lambda e, h=h, sl=sl, ks=ks, qs=qs, d=d, ps_h=ps_h: e.matmul(
                                            ps_h[h][:, sl * 128:(sl + 1) * 128],
                                            KT[h * 64:(h + 1) * 64, ks:ks + 127 * d + 1:d],
                                            QT[h * 64:(h + 1) * 64, qs:qs + 127 * d + 1:d], start=True, stop=True),
                                            reads=rds, writes=[bps_h[h]])
                                for h in range(2):
                                    P.op("act", lambda e, pb_=pb_, h=h, nslot=nslot, ps_h=ps_h: e.activation(
                                        pb_[:, h, 0:nslot, :], ps_h[h][:, 0:nslot * 128].rearrange("p (s q) -> p s q", s=nslot),
                                        AF.Exp, scale=0.125), reads=[bps_h[h]], writes=[bpb_])
                                P.op("dve", lambda e, pb_=pb_, di=di, hp=hp, nslot=nslot: e.tensor_tensor(
                                    pb_[:, :, 0:nslot, :], pb_[:, :, 0:nslot, :],
                                    Etab[:, di * 4 + hp * 2:di * 4 + hp * 2 + 2, 0:nslot, :], ALU.mult),
                                    reads=[bpb_, b_E], writes=[bpb_])
                                pv, bpv = BK.f[6], BK.bf[6]
                                vrd = [b_Vd[di][kb] for kb in kblks]
                                for h in range(2):
                                    for sl in range(nslot):
                                        kidx = (n - sl) * d + r
                                        P.op("pe", lambda e, pv=pv, h=h, sl=sl, kidx=kidx, di=di, pb_=pb_, nslot=nslot: e.matmul(
                                            pv[:, h * 128:(h + 1) * 128], Vd[di][:, kidx, :], pb_[:, h, sl, :],
                                            start=(sl == 0), stop=(sl == nslot - 1)), reads=vrd + [bpb_], writes=[bpv])
                                    for sl in range(nslot):
                                        P.op("pe", lambda e, pv=pv, h=h, sl=sl, pb_=pb_, nslot=nslot: e.matmul(
                                            pv[:, (2 + h) * 128:(3 + h) * 128], ones_bf[:], pb_[:, h, sl, :],
                                            start=(sl == 0), stop=(sl == nslot - 1)), reads=[b_ones, bpb_], writes=[bpv])
                                for h in range(2):
                                    pr = slice(h * 64, (h + 1) * 64)
                                    qc = slice(ql, ql + 127 * d + 1, d)
                                    if di == 0:
                                        P.op("dve", lambda e, pr=pr, qc=qc, h=h: e.tensor_copy(
                                            numacc[pr, qc], pv[pr, h * 128:(h + 1) * 128]), reads=[bpv], writes=[b_num])
                                        P.op("dve", lambda e, pr=pr, qc=qc, h=h: e.tensor_copy(
                                            denacc[pr, qc], pv[pr, (2 + h) * 128:(3 + h) * 128]), reads=[bpv], writes=[b_den])
                                    else:
                                        P.op("dve", lambda e, pr=pr, qc=qc, h=h: e.tensor_tensor(
                                            numacc[pr, qc], numacc[pr, qc], pv[pr, h * 128:(h + 1) * 128], ALU.add),
                                            reads=[bpv, b_num], writes=[b_num])
                                        P.op("dve", lambda e, pr=pr, qc=qc, h=h: e.tensor_tensor(
                                            denacc[pr, qc], denacc[pr, qc], pv[pr, (2 + h) * 128:(3 + h) * 128], ALU.add),
                                            reads=[bpv, b_den], writes=[b_den])
                    P.op("dve", lambda e: e.reciprocal(denacc[:], denacc[:]), reads=[b_den], writes=[b_den])
                    P.op("dve", lambda e: e.tensor_tensor(ybuf[:], numacc[:], denacc[:], ALU.mult),
                         reads=[b_num, b_den], writes=[b_yb])
                    ystore(P, hp, blk, ybuf[:], [b_yb])
            full_barrier(P)

        with ExitStack() as esB:
            sbB = lambda n, s, d: esB.enter_context(nc.sbuf_tensor(P.pfx + n, s, d))
            SC = 128.0 ** -0.5
            wBb = sbB("b_w", [128, 8, 1028], BF16)
            b_wB = B()
            stg.load(wBb[:], b_wB, wB[:, :])
            pc = sbB("b_pc", [128, 4, BLK + 3], F32)
            b_pc = [B() for _ in range(4)]
            ctmp = [sbB(f"b_ct{i}", [128, BLK], F32) for i in range(2)]
            b_ct = [B(), B()]
            QKb = sbB("b_qk", [128, 4, BLK], BF16)
            b_qk = [B() for _ in range(4)]
            cw = sbB("b_cw", [128, 16], F32)
            hngb = sbB("b_hng", [128, 256], F32)
            gbb = sbB("b_gb", [128, 4], F32)
            triu32 = sbB("b_triu32", [128, 128], F32)
            triub = sbB("b_triub", [128, 128], BF16)
            ones32 = sbB("b_ones32", [128, 128], F32)
            identb = sbB("b_identb", [128, 128], BF16)
            b_cst = B()
            P.dma("sp", cw[:], convw[:, :], writes=[b_cst])
            b_c2, b_c3, b_c4 = B(), B(), B()
            P.dma("sp", hngb[:], bcast_rows(hng.tensor, hng.offset, 256), writes=[b_c2])
            P.dma("sp", gbb[:], bcast_rows(gbias.tensor, gbias.offset, 4), writes=[b_c3])
            P.dma("sp", triu32[:], triu_d[:, :], writes=[b_c4])
            b_c5 = B()
            P.op("dve", lambda e: e.tensor_copy(triub[:], triu32[:]), reads=[b_c4], writes=[b_c5])
            P.op("dve", lambda e: e.memset(ones32[:], 1.0), writes=[b_c5])
            P.op("dve", lambda e: e.tensor_copy(identb[:], ident[:]), reads=[b_ident], writes=[b_c5])
            CST = [b_cst, b_c2, b_c3, b_c4, b_c5]
            Vaug = [sbB(f"b_va{i}", [128, 2, 129], BF16) for i in range(2)]
            b_va = [B(), B()]
            sgo = [sbB(f"b_sgo{i}", [128, 256], F32) for i in range(2)]
            b_sgo = [B(), B()]
            smB = [sbB(f"b_sm{i}", [128, 64], F32) for i in range(2)]
            b_smB = [B(), B()]
            Sm = [sbB(f"b_Sm{i}", [128, 128], BF16) for i in range(2)]
            b_Sm = [B(), B()]
            hh = [sbB(f"b_hh{i}", [128, 128], F32) for i in range(2)]
            b_hh = [B(), B()]
            yn32 = [sbB(f"b_yn{i}", [128, 128], F32) for i in range(2)]
            b_yn = [B(), B()]
            ynb = [sbB(f"b_ynb{i}", [128, 128], BF16) for i in range(2)]
            b_ynb = [B(), B()]
            Ktm = [sbB(f"b_ktm{i}", [128, 128], BF16) for i in range(2)]
            b_ktm = [B(), B()]
            Vw = [sbB(f"b_vw{i}", [128, 129], BF16) for i in range(2)]
            b_vw = [B(), B()]
            C32 = sbB("b_C32", [128, 2, 129], F32)
            Cb = sbB("b_Cb", [128, 2, 129], BF16)
            b_C32, b_Cb = [B(), B()], [B(), B()]
            hst = [sbB(f"b_hst{i}", [128, 16], F32) for i in range(2)]
            b_hst = [B(), B()]
            ybufB = sbB("b_yb", [128, 2, BLK], BF16)
            b_ybB = B()
            P.op("dve", lambda e: e.memset(C32[:], 0.0), writes=b_C32)
            P.op("dve", lambda e: e.memset(Cb[:], 0.0), writes=b_Cb)
            for k in range(2):
                P.op("dve", lambda e, k=k: e.memset(Vaug[k][:, :, 128:129], 1.0), writes=[b_va[k]])
            for g in range(4):
                P.op("dve", lambda e, g=g: e.memset(pc[:, g, 0:3], 0.0), writes=[b_pc[g]])
            f0, f1, f2, f3, f4, f5, f6 = BK.f
            bf0, bf1, bf2, bf3, bf4, bf5, bf6 = BK.bf
            cti = 0
            for blk in range(NBLK):
                T0 = blk * BLK
                load_xT(P, BK, x, b_x, xin, b_xin, xT, b_xT, ident, b_ident, T0, 16, ctr)
                for g in range(4):
                    for qtr in range(4):
                        pb, bpb = (f0, bf0) if g % 2 == 0 else (f1, bf1)
                        rds = [b_xT[qtr * 4 + i] for i in range(4)] + [b_wB]
                        for dc in range(8):
                            P.op("pe", lambda e, pb=pb, dc=dc, g=g, qtr=qtr: e.matmul(
                                pb[:], wBb[:, dc, g * 128:(g + 1) * 128], xT[:, dc, qtr * 512:(qtr + 1) * 512],
                                start=(dc == 0), stop=(dc == 7)), reads=rds, writes=[bpb])
                        if g % 2 == 0:
                            P.op("act", lambda e, pb=pb, g=g, qtr=qtr: e.copy(pc[:, g, 3 + qtr * 512:3 + (qtr + 1) * 512], pb[:]),
                                 reads=[bpb], writes=[b_pc[g]])
                        else:
                            P.op("dve", lambda e, pb=pb, g=g, qtr=qtr: e.tensor_copy(pc[:, g, 3 + qtr * 512:3 + (qtr + 1) * 512], pb[:]),
                                 reads=[bpb], writes=[b_pc[g]])
                for g in range(4):
                    ct, bct = ctmp[cti % 2], b_ct[cti % 2]
                    cti += 1
                    P.op("dve", lambda e, ct=ct, g=g: e.tensor_scalar(ct[:], pc[:, g, 3:3 + BLK], cw[:, g * 4 + 3:g * 4 + 4], None, ALU.mult),
                         reads=[b_pc[g], b_cst], writes=[bct])
                    for j in (2, 1, 0):
                        P.op("dve", lambda e, ct=ct, g=g, j=j: e.scalar_tensor_tensor(
                            ct[:], pc[:, g, j:j + BLK], cw[:, g * 4 + j:g * 4 + j + 1], ct[:], ALU.mult, ALU.add),
                            reads=[b_pc[g], b_cst, bct], writes=[bct])
                    P.op("act", lambda e, ct=ct, g=g: e.activation(QKb[:, g, :], ct[:], AF.Silu), reads=[bct], writes=[b_qk[g]])
                    P.op("dve", lambda e, g=g: e.tensor_copy(pc[:, g, 0:3], pc[:, g, BLK:BLK + 3]), reads=[b_pc[g]], writes=[b_pc[g]])
                for ch in range(16):
                    k = ch % 2
                    tsl = slice(ch * 128, (ch + 1) * 128)
                    va, bva, sg_, bsg, sm_, bsm = Vaug[k], b_va[k], sgo[k], b_sgo[k], smB[k], b_smB[k]
                    gt, e1, sp_, cs = sm_[:, 0:4], sm_[:, 4:6], sm_[:, 6:8], sm_[:, 8:12]
                    ti, ws, wsq, eb, ebL = sm_[:, 12:14], sm_[:, 14:16], sm_[:, 16:18], sm_[:, 18:20], sm_[:, 20:22]
                    rdx = [b_xT[ch], b_wB]
                    for (bank, bbank, c0, c1, w0) in ((f2, bf2, 0, 256, 512), (f2, bf2, 256, 260, 1024), (f3, bf3, 0, 256, 768)):
                        for dc in range(8):
                            P.op("pe", lambda e, bank=bank, c0=c0, c1=c1, w0=w0, dc=dc, tsl=tsl: e.matmul(
                                bank[:, c0:c1], xT[:, dc, tsl], wBb[:, dc, w0:w0 + (c1 - c0)],
                                start=(dc == 0), stop=(dc == 7)), reads=rdx, writes=[bbank])
                    P.op("dve", lambda e, va=va: e.tensor_copy(va[:, :, 0:128], f2[:, 0:256].rearrange("p (h c) -> p h c", h=2)),
                         reads=[bf2], writes=[bva])
                    P.op("dve", lambda e, gt=gt: e.tensor_tensor(gt, f2[:, 256:260], gbb[:], ALU.add), reads=[bf2] + CST, writes=[bsm])
                    P.op("act", lambda e, sg_=sg_: e.activation(sg_[:], f3[:, 0:256], AF.Sigmoid), reads=[bf3], writes=[bsg])
                    P.op("act", lambda e, e1=e1, gt=gt: e.activation(e1, gt[:, 2:4], AF.Exp, scale=-1.0), reads=[bsm], writes=[bsm])
                    P.op("act", lambda e, e1=e1, sp_=sp_: e.activation(sp_, e1, AF.Ln, bias=1.0), reads=[bsm], writes=[bsm])
                    P.op("pe", lambda e, sp_=sp_: e.matmul(f3[:, 256:258], triu32[:], sp_, start=True, stop=True), reads=[bsm] + CST, writes=[bf3])
                    P.op("pe", lambda e, sp_=sp_: e.matmul(f3[:, 258:260], ones32[:], sp_, start=True, stop=True), reads=[bsm] + CST, writes=[bf3])
                    P.op("act", lambda e, cs=cs: e.copy(cs, f3[:, 256:260]), reads=[bf3], writes=[bsm])
                    P.op("dve", lambda e, ti=ti, gt=gt, cs=cs: e.tensor_tensor(ti, gt[:, 0:2], cs[:, 0:2], ALU.add), reads=[bsm], writes=[bsm])
                    P.op("act", lambda e, ws=ws, ti=ti: e.activation(ws, ti, AF.Exp), reads=[bsm], writes=[bsm])
                    P.op("act", lambda e, eb=eb, cs=cs: e.activation(eb, cs[:, 0:2], AF.Exp), reads=[bsm], writes=[bsm])
                    P.op("act", lambda e, ebL=ebL, cs=cs: e.activation(ebL, cs[:, 2:4], AF.Exp, scale=-1.0), reads=[bsm], writes=[bsm])
                    P.op("dve", lambda e, wsq=wsq, ws=ws: e.tensor_scalar(wsq, ws, SC, None, ALU.mult), reads=[bsm], writes=[bsm])
                    for h in range(2):
                        st_, bst = hst[h], b_hst[h]
                        P.op("pe", lambda e, h=h, tsl=tsl: e.matmul(f4[:, h * 128:(h + 1) * 128], QKb[:, 2 + h, tsl], QKb[:, h, tsl],
                                                                     start=True, stop=True), reads=[b_qk[h], b_qk[2 + h]], writes=[bf4])
                        P.op("dve", lambda e, h=h, wsq=wsq: e.scalar_tensor_tensor(
                            Sm[h][:], f4[:, h * 128:(h + 1) * 128], wsq[:, h:h + 1], triub[:], ALU.mult, ALU.mult),
                            reads=[bf4, bsm] + CST, writes=[b_Sm[h]])
                        xs_ = f5[:, h * 256:h * 256 + 129]
                        P.op("pe", lambda e, h=h, xs_=xs_, va=va: e.matmul(xs_, Sm[h][:], va[:, h, :], start=True, stop=False),
                             reads=[b_Sm[h], bva], writes=[bf5])
                        P.op("pe", lambda e, h=h, xs_=xs_, tsl=tsl: e.matmul(xs_, QKb[:, h, tsl], Cb[:, h, :], start=False, stop=True),
                             reads=[b_qk[h], b_Cb[h]], writes=[bf5])
                        ax, r_, mv, rstd, nmr = st_[:, 0:1], st_[:, 1:2], st_[:, 2:4], st_[:, 4:5], st_[:, 5:6]
                        stat6 = st_[:, 8:14]
                        P.op("dve", lambda e, ax=ax, h=h: e.tensor_scalar(ax, f5[:, h * 256 + 128:h * 256 + 129], -1.0, None, ALU.mult),
                             reads=[bf5], writes=[bst])
                        P.op("dve", lambda e, ax=ax, h=h: e.tensor_tensor(ax, ax, f5[:, h * 256 + 128:h * 256 + 129], ALU.max),
                             reads=[bf5, bst], writes=[bst])
                        P.op("dve", lambda e, ax=ax, eb=eb, h=h: e.tensor_tensor(ax, ax, eb[:, h:h + 1], ALU.max), reads=[bst, bsm], writes=[bst])
                        P.op("dve", lambda e, ax=ax, r_=r_: e.reciprocal(r_, ax), reads=[bst], writes=[bst])
                        P.op("dve", lambda e, h=h, r_=r_: e.tensor_scalar(hh[h][:], f5[:, h * 256:h * 256 + 128], r_, None, ALU.mult),
                             reads=[bf5, bst], writes=[b_hh[h]])
                        P.op("dve", lambda e, h=h, stat6=stat6: e.bn_stats(stat6, hh[h][:]), reads=[b_hh[h]], writes=[bst])
                        P.op("dve", lambda e, mv=mv, stat6=stat6: e.bn_aggr(mv, stat6), reads=[bst], writes=[bst])
                        P.op("dve", lambda e, rstd=rstd, mv=mv: e.tensor_scalar(rstd, mv[:, 1:2], LN_EPS, None, ALU.add), reads=[bst], writes=[bst])
                        P.op("act", lambda e, rstd=rstd: e.sqrt(rstd, rstd), reads=[bst], writes=[bst])
                        P.op("dve", lambda e, rstd=rstd: e.reciprocal(rstd, rstd), reads=[bst], writes=[bst])
                        P.op("dve", lambda e, nmr=nmr, mv=mv, rstd=rstd: e.scalar_tensor_tensor(nmr, mv[:, 0:1], -1.0, rstd, ALU.mult, ALU.mult),
                             reads=[bst], writes=[bst])
                        P.op("act", lambda e, h=h, nmr=nmr, rstd=rstd: e.activation(yn32[h][:], hh[h][:], AF.Identity, bias=nmr, scale=rstd),
                             reads=[b_hh[h], bst], writes=[b_yn[h]])
                        P.op("pool", lambda e, h=h: e.tensor_tensor(yn32[h][:], yn32[h][:], hngb[:, h * 128:(h + 1) * 128], ALU.mult),
                             reads=[b_yn[h]] + CST, writes=[b_yn[h]])
                        P.op("pool", lambda e, h=h, sg_=sg_: e.tensor_tensor(ynb[h][:], yn32[h][:], sg_[:, h * 128:(h + 1) * 128], ALU.mult),
                             reads=[b_yn[h], bsg], writes=[b_ynb[h]])
                        P.op("pe", lambda e, h=h: e.transpose(BK.h[:, h * 256:h * 256 + 128], ynb[h][:], identb[:]),
                             reads=[b_ynb[h]] + CST, writes=[BK.bh])
                        P.op("act", lambda e, h=h, tsl=tsl: e.copy(ybufB[:, h, tsl], BK.h[:, h * 256:h * 256 + 128]),
                             reads=[BK.bh], writes=[b_ybB])
                        P.op("pe", lambda e, h=h, tsl=tsl: e.transpose(BK.h[:, h * 256 + 128:h * 256 + 256], QKb[:, 2 + h, tsl], identb[:]),
                             reads=[b_qk[2 + h]] + CST, writes=[BK.bh])
                        P.op("act", lambda e, h=h: e.copy(Ktm[h][:], BK.h[:, h * 256 + 128:h * 256 + 256]), reads=[BK.bh], writes=[b_ktm[h]])
                        P.op("dve", lambda e, h=h, va=va, ws=ws: e.tensor_scalar(Vw[h][:], va[:, h, :], ws[:, h:h + 1], None, ALU.mult),
                             reads=[bva, bsm], writes=[b_vw[h]])
                        dps = f6[:, h * 256:h * 256 + 129]
                        P.op("pe", lambda e, h=h, dps=dps: e.matmul(dps, Ktm[h][:], Vw[h][:], start=True, stop=True),
                             reads=[b_ktm[h], b_vw[h]], writes=[bf6])
                        P.op("dve", lambda e, h=h, dps=dps: e.tensor_tensor(C32[:, h, :], dps, C32[:, h, :], ALU.add),
                             reads=[bf6, b_C32[h]], writes=[b_C32[h]])
                        P.op("dve", lambda e, h=h, ebL=ebL: e.tensor_scalar(C32[:, h, :], C32[:, h, :], ebL[:, h:h + 1], None, ALU.mult),
                             reads=[b_C32[h], bsm], writes=[b_C32[h]])
                        P.op("act", lambda e, h=h: e.mul(Cb[:, h, :], C32[:, h, :], SC), reads=[b_C32[h]], writes=[b_Cb[h]])
                for h in range(2):
                    ystore(P, 2 + h, blk, ybufB[:, h, :], [b_ybB])
            full_barrier(P)


TOK = 4096


def declare_post(nc, pfx=""):
    din = lambda n, sh: nc.dram_tensor(pfx + n, sh, F32, kind="ExternalInput").ap()
    return dict(wo=din("wo", [D, D]), ln1g=din("ln1g", [D]), ln1b=din("ln1b", [D]),
                w=dict(wg=din("wg", [16, D, EH]), wu=din("wu", [16, D, EH]), wd=din("wd", [16, EH, D]), wr=din("wr", [D, 20]),
                       br=din("br", [1, 20]), ln_g=din("ln2g", [D]), ln_b=din("ln2b", [D])),
                hmid=nc.dram_tensor(pfx + "hmid_scr", [TOK, D], F32).ap())


def emit_post(P, io):
    nc = P.nc
    wo, ln1g, ln1b, w, hmid = io["wo"], io["ln1g"], io["ln1b"], dict(io["w"]), io["hmid"]
    w["ident"] = io["ident"]
    load_yt, xres, out = io["load_yt"], io["xres"], io["out"]
    if True:
        B = P.buf
        banks = [P.ps(f"bank{i}", [128, 512]) for i in range(8)]
        bbank = [B() for _ in range(8)]
        b_hmid, b_out, b_yT = B(), B(), B()
        b_xres = io.get("b_xres") or B()
        b_hmw = [B(), B()]
        with ExitStack() as esF:
            sbF = lambda n, sh, d: esF.enter_context(nc.sbuf_tensor(P.pfx + n, sh, d))
            stg = Stager(P)
            wob = sbF("f_wo", [128, 8, D], BF16)
            b_wo = B()
            stg.load(wob[:], b_wo, wo[:, :])
            g1 = sbF("f_g1", [128, D], F32)
            b1 = sbF("f_b1", [128, D], F32)
            b_g1, b_b1 = B(), B()
            P.dma("sp", g1[:], bcast_rows(ln1g.tensor, ln1g.offset, D), writes=[b_g1])
            P.dma("sp", b1[:], bcast_rows(ln1b.tensor, ln1b.offset, D), writes=[b_b1])
            yts = [sbF(f"f_yt{i}", [128, 8, 512], BF16) for i in range(2)]
            b_yts = [[B() for _ in range(8)] for _ in range(2)]
            xr = [sbF(f"f_xr{i}", [128, D], F32) for i in range(2)]
            b_xr = [B(), B()]
            z = [sbF(f"f_z{i}", [128, D], F32) for i in range(2)]
            b_z = [B(), B()]
            hm = [sbF(f"f_hm{i}", [128, D], F32) for i in range(2)]
            b_hm = [B(), B()]
            stats = sbF("f_stats", [128, 2, 6], F32)
            sm = sbF("f_sm", [128, 64], F32)
            b_stats, b_sm = B(), B()
            ti = 0
            for t4 in range(TOK // 512):
                yt, byt = yts[t4 % 2], b_yts[t4 % 2]
                load_yt(P, yt, byt, t4)
                for tq in range(4):
                    k = ti % 2
                    ti += 1
                    t0 = t4 * 512 + tq * 128
                    P.dma("sp", xr[k][:], xres(t0), reads=[b_xres], writes=[b_xr[k]])
                    for dh in range(2):
                        pb, bpb = banks[(ti * 2 + dh) % 4], bbank[(ti * 2 + dh) % 4]
                        for ec in range(8):
                            P.op("pe", lambda e, pb=pb, ec=ec, yt=yt, tq=tq, dh=dh: e.matmul(
                                pb[:], yt[:, ec, tq * 128:(tq + 1) * 128], wob[:, ec, dh * 512:(dh + 1) * 512],
                                start=(ec == 0), stop=(ec == 7)), reads=[byt[ec], b_wo], writes=[bpb])
                        P.op("dve", lambda e, pb=pb, k=k, dh=dh: e.scalar_tensor_tensor(
                            z[k][:, dh * 512:(dh + 1) * 512], xr[k][:, dh * 512:(dh + 1) * 512], ALPHA, pb[:], ALU.mult, ALU.add),
                            reads=[bpb, b_xr[k]], writes=[b_z[k]])
                    emit_ln(P, z[k][:], b_z[k], hm[k], b_hm[k], g1, b_g1, b1, b_b1, stats, b_stats, sm, b_sm)
                    P.dma("sp", hmid[t0:t0 + 128, :], hm[k][:], reads=[b_hm[k]], writes=[b_hmw[k]])
            full_barrier(P)
        emit_tail(P, hmid, b_hmid, out, b_out, TOK, w, 0, banks=banks, is_final=io.get("is_final", True))
        full_barrier(P)


def pack_m0(inp, b, c):
    w = inp["ab_w_in"][0]
    cols = []
    for hp in range(2):
        h0 = 4 * c + 2 * hp
        for sec in (0, 512, 1024):
            cols.append(w[:, sec + h0 * 64: sec + (h0 + 2) * 64])
    wA = np.concatenate(cols, axis=1)
    hb = [2 * c, 2 * c + 1]
    BQ, BKK, BV, BO, BI, BF = 1536, 2048, 2560, 3072, 3584, 3588
    wB = np.concatenate([w[:, BQ + h * 128: BQ + (h + 1) * 128] for h in hb] + [w[:, BKK + h * 128: BKK + (h + 1) * 128] for h in hb]
                        + [w[:, BV + h * 128: BV + (h + 1) * 128] for h in hb] + [w[:, BO + h * 128: BO + (h + 1) * 128] for h in hb]
                        + [w[:, BI + h: BI + h + 1] for h in hb] + [w[:, BF + h: BF + h + 1] for h in hb], axis=1)
    gb = inp["ab_gate_b"][0]
    gbias = np.array([[gb[hb[0]], gb[hb[1]], gb[4 + hb[0]], gb[4 + hb[1]]]], np.float32)
    cw = inp["ab_conv"][0]
    convw = np.zeros((128, 4, 4), np.float32)
    for g, off in enumerate([hb[0] * 128, hb[1] * 128, 512 + hb[0] * 128, 512 + hb[1] * 128]):
        convw[:, g, :] = cw[:, off:off + 128].T
    hng = np.concatenate([inp["ab_head_norm"][0][h * 128:(h + 1) * 128] for h in hb])[None]
    relb = np.concatenate([inp["rel_bias"][:, 4 * c:4 * c + 4], np.full((1, 4), -30000.0, np.float32)], axis=0)
    return dict(x=np.ascontiguousarray(inp["x"][b]), wA=np.ascontiguousarray(wA), wB=np.ascontiguousarray(wB), gbias=gbias,
                convw=np.ascontiguousarray(convw.reshape(128, 16)), hng=np.ascontiguousarray(hng),
                relb=np.ascontiguousarray(relb), oh=np.ascontiguousarray(dil_onehot().reshape(33, -1)),
                ident=np.eye(128, dtype=np.float32), triu=np.triu(np.ones((128, 128), np.float32)))


L1 = 5632
OFF1 = 2063


def nsa_consts():
    dist = np.arange(L1) - OFF1
    oh = np.zeros((33, L1), np.float32)
    bk = t5_bucket_np(dist)
    for jp in range(L1):
        if dist[jp] >= 0:
            oh[bk[jp], jp] = 1.0
        else:
            oh[32, jp] = 1.0
    n = np.arange(512)
    m = np.arange(128)
    cs, ce = n * 16, n * 16 + 31
    ov = ((cs[:, None] < m[None, :] * 64 + 64) & (ce[:, None] >= m[None, :] * 64)).astype(np.float32)
    ov[511] = 0.0
    ovl = ov.reshape(4, 128, 128).transpose(1, 0, 2)
    mrow = np.broadcast_to(m[None, :].astype(np.float32), (128, 128))
    k = np.arange(128)
    cst = np.stack([mrow, (mrow >= 1).astype(np.float32), (mrow == 0).astype(np.float32),
                    (k[:, None] > k[None, :]).astype(np.float32)], axis=1)
    qhalf = (np.arange(128) >= 64).astype(np.float32)[:, None]
    return dict(oh1=oh, ovl=np.ascontiguousarray(ovl), cst1=np.ascontiguousarray(cst.astype(np.float32)), qhalf=qhalf)


def declare_mixer1(nc, pfx=""):
    din = lambda n, s: nc.dram_tensor(pfx + n, s, F32, kind="ExternalInput").ap()
    return dict(wQ=din("wQ", [D, 512]), wKV=din("wKV", [D, 768]), wG=din("wG", [D, 12]), gb=din("gb", [1, 12]),
                posT=din("posT", [128, 64]), w1=din("w1", [2, 4096, 256]), w2=din("w2", [2, 256, 128]), relb=din("relb", [33, 4]),
                oh1=din("oh1", [33, L1]), ovl=din("ovl", [128, 512]), cst1=din("cst1", [128, 512]), qhalf=din("qhalf", [128, 1]),
                fsc=nc.dram_tensor(pfx + "frep_scr", [128, 4 * L1], BF16).ap())


def emit_mixer1(P, io):
    nc = P.nc
    wQ, wKV, wG, gbd, posT, w1d, w2d, relb = io["wQ"], io["wKV"], io["wG"], io["gb"], io["posT"], io["w1"], io["w2"], io["relb"]
    oh_d, ovl_d, cst_d, qh_d, ident_d, fsc = io["oh1"], io["ovl"], io["cst1"], io["qhalf"], io["ident"], io["fsc"]
    x, ostore = io["x"], io["ostore"]
    NT = S_TOT // 128
    SCQ = 128.0 ** -0.5
    if True:
        B = P.buf
        BK = Banks(P)
        f0, f1, f2, f3, f4, f5, f6 = BK.f
        bf0, bf1, bf2, bf3, bf4, bf5, bf6 = BK.bf
        b_x, b_o, b_fsc = io.get("b_x") or B(), B(), B()
        ident = P.sb("ident_sb", [128, 128], F32)
        identb = P.sb("identb", [128, 128], BF16)
        ones_bf = P.sb("ones_bf", [128, 128], BF16)
        b_ident, b_cb = B(), B()
        P.dma("sp", ident[:], ident_d[:, :], writes=[b_ident])
        P.op("dve", lambda e: e.tensor_copy(identb[:], ident[:]), reads=[b_ident], writes=[b_cb])
        P.op("dve", lambda e: e.memset(ones_bf[:], 1.0), writes=[b_cb])
        xin = [P.sb(f"xin{i}", [128, D], F32) for i in range(2)]
        b_xin = [B(), B()]
        stg = Stager(P, n=2)
        ctr = [0]
        wQb = P.sb("wQb", [128, 8, 512], BF16)
        wGb = P.sb("wGb", [128, 8, 12], BF16)
        b_wQ, b_wG = B(), B()
        stg.load(wQb[:], b_wQ, wQ[:, :])
        stg.load(wGb[:], b_wG, wG[:, :])
        KsT = P.sb("KsT", [128, S_TOT], BF16)
        KwT = P.sb("KwT", [128, S_TOT], BF16)
        Vs = P.sb("Vs", [128, NT, 129], BF16)
        Vw = P.sb("Vw", [128, NT, 129], BF16)
        b_KsT, b_KwT, b_Vs, b_Vw = B(), B(), B(), B()
        kcT = P.sb("kcT", [128, 512], BF16)
        rhsC = P.sb("rhsC", [128, 4, 256], BF16)
        b_kcT, b_rhsC = B(), B()
        P.op("dve", lambda e: e.memset(Vs[:, :, 128:129], 1.0), writes=[b_Vs])
        P.op("dve", lambda e: e.memset(Vw[:, :, 128:129], 1.0), writes=[b_Vw])
        with ExitStack() as es12:
            sb12 = lambda n, s, d: es12.enter_context(nc.sbuf_tensor(P.pfx + n, s, d))
            KcT = sb12("KcT", [128, 2, S_TOT], BF16)
            b_KcT = [B(), B()]
            with ExitStack() as es1:
                sb1 = lambda n, s, d: es1.enter_context(nc.sbuf_tensor(P.pfx + n, s, d))
                hT = sb1("hT", [128, 8, BLK], BF16)
                b_hT = [B() for _ in range(16)]
                wKVb = sb1("wKVb", [128, 8, 768], BF16)
                b_wKV = B()
                stg.load(wKVb[:], b_wKV, wKV[:, :])
                for blk in range(NBLK):
                    T0 = blk * BLK
                    load_xT(P, BK, x, b_x, xin, b_xin, hT, b_hT, ident, b_ident, T0, 16, ctr)
                    fm = [(0, KcT[:, 0, :], b_KcT[0]), (1, KcT[:, 1, :], b_KcT[1]), (2, KsT[:], b_KsT), (4, KwT[:], b_KwT)]
                    for fi, (wi, dst, bd) in enumerate(fm):
                        for qtr in range(4):
                            pb, bpb = (f2, bf2) if (fi + qtr) % 2 == 0 else (f3, bf3)
                            rds = [b_hT[qtr * 4 + i] for i in range(4)] + [b_wKV]
                            for dc in range(8):
                                P.op("pe", lambda e, pb=pb, dc=dc, wi=wi, qtr=qtr: e.matmul(
                                    pb[:], wKVb[:, dc, wi * 128:(wi + 1) * 128], hT[:, dc, qtr * 512:(qtr + 1) * 512],
                                    start=(dc == 0), stop=(dc == 7)), reads=rds, writes=[bpb])
                            if (fi + qtr) % 2 == 0:
                                P.op("act", lambda e, pb=pb, dst=dst, T0=T0, qtr=qtr: e.copy(
                                    dst[:, T0 + qtr * 512:T0 + (qtr + 1) * 512], pb[:]), reads=[bpb], writes=[bd])
                            else:
                                P.op("dve", lambda e, pb=pb, dst=dst, T0=T0, qtr=qtr: e.tensor_copy(
                                    dst[:, T0 + qtr * 512:T0 + (qtr + 1) * 512], pb[:]), reads=[bpb], writes=[bd])
                    for tl in range(16):
                        for (wi, dst, bd, pb, bpb, eng) in ((3, Vs, b_Vs, f4, bf4, "act"), (5, Vw, b_Vw, f5, bf5, "dve")):
                            for dc in range(8):
                                P.op("pe", lambda e, pb=pb, dc=dc, wi=wi, tl=tl: e.matmul(
                                    pb[:, 0:128], hT[:, dc, tl * 128:(tl + 1) * 128], wKVb[:, dc, wi * 128:(wi + 1) * 128],
                                    start=(dc == 0), stop=(dc == 7)), reads=[b_hT[tl], b_wKV], writes=[bpb])
                            if eng == "act":
                                P.op("act", lambda e, pb=pb, dst=dst, tl=tl, blk=blk: e.copy(dst[:, blk * 16 + tl, 0:128], pb[:, 0:128]),
                                     reads=[bpb], writes=[bd])
                            else:
                                P.op("dve", lambda e, pb=pb, dst=dst, tl=tl, blk=blk: e.tensor_copy(dst[:, blk * 16 + tl, 0:128], pb[:, 0:128]),
                                     reads=[bpb], writes=[bd])
                full_barrier(P)
            with ExitStack() as es2:
                sb2 = lambda n, s, d: es2.enter_context(nc.sbuf_tensor(P.pfx + n, s, d))
                w1b = sb2("w1b", [128, 32, 256], BF16)
                w2b = sb2("w2b", [128, 2, 128], BF16)
                posb = sb2("posb", [128, 64], BF16)
                pos32 = sb2("pos32", [128, 64], F32)
                ovl32 = sb2("ovl32", [128, 512], F32)
                pbias = sb2("pbias", [128, 2], F32)
                xg = sb2("xg", [128, 512], F32)
                ug = sb2("ug", [128, 512], F32)
                hid = sb2("hid_sb", [128, 2, 512], BF16)
                b_w1, b_w2, b_pos, b_ovl, b_pb, b_xg, b_ug, b_hid = B(), B(), B(), B(), B(), B(), B(), [B(), B()]
                P.dma("sp", pos32[:], posT[:, :], writes=[b_pos])
                P.op("dve", lambda e: e.tensor_copy(posb[:], pos32[:]), reads=[b_pos], writes=[b_pos])
                P.dma("sp", ovl32[:], ovl_d[:, :], writes=[b_ovl])
                P.op("dve", lambda e: e.tensor_copy(rhsC[:, :, 128:256], ovl32[:].rearrange("p (c m) -> p c m", c=4)),
                     reads=[b_ovl], writes=[b_rhsC])
                P.op("dve", lambda e: e.memset(hid[:, :, 511:512], 0.0), writes=b_hid)
                for kv in range(2):
                    stg.load(w1b[:], b_w1, w1d[kv])
                    stg.load(w2b[:], b_w2, w2d[kv])
                    for hc in range(2):
                        for p_ in range(32):
                            P.op("pe", lambda e, hc=hc, p_=p_, kv=kv: e.matmul(
                                f6[:, 0:1], w1b[:, p_, hc * 128:(hc + 1) * 128], posb[:, kv * 32 + p_:kv * 32 + p_ + 1],
                                start=(p_ == 0), stop=(p_ == 31)), reads=[b_w1, b_pos], writes=[bf6])
                        P.op("act", lambda e, hc=hc: e.copy(pbias[:, hc:hc + 1], f6[:, 0:1]), reads=[bf6], writes=[b_pb])
                        for p_ in range(32):
                            P.op("pe", lambda e, hc=hc, p_=p_, kv=kv: e.matmul(
                                f2[:, 0:511], w1b[:, p_, hc * 128:(hc + 1) * 128], KcT[:, kv, p_:p_ + 16 * 510 + 1:16],
                                start=(p_ == 0), stop=(p_ == 31)), reads=[b_w1, b_KcT[kv]], writes=[bf2])
                        P.op("act", lambda e, hc=hc: e.activation(xg[:, 0:511], f2[:, 0:511], AF.Identity, bias=pbias[:, hc:hc + 1], scale=1.0),
                             reads=[bf2, b_pb], writes=[b_xg])
                        P.op("dve", lambda e: e.tensor_tensor(ug[:, 0:511], xg[:, 0:511], xg[:, 0:511], ALU.mult), reads=[b_xg], writes=[b_ug])
                        P.op("dve", lambda e: e.tensor_scalar(ug[:, 0:511], ug[:, 0:511], 0.044715, 1.0, ALU.mult, ALU.add), reads=[b_ug], writes=[b_ug])
                        P.op("dve", lambda e: e.tensor_tensor(ug[:, 0:511], ug[:, 0:511], xg[:, 0:511], ALU.mult), reads=[b_ug, b_xg], writes=[b_ug])
                        P.op("act", lambda e: e.activation(ug[:, 0:511], ug[:, 0:511], AF.Sigmoid, scale=1.5957691216057308), reads=[b_ug], writes=[b_ug])
                        P.op("dve", lambda e, hc=hc: e.tensor_tensor(hid[:, hc, 0:511], xg[:, 0:511], ug[:, 0:511], ALU.mult),
                             reads=[b_ug, b_xg], writes=[b_hid[hc]])
                    if kv == 0:
                        for hc in range(2):
                            P.op("pe", lambda e, hc=hc: e.matmul(f3[:, 0:512], w2b[:, hc, :], hid[:, hc, :], start=(hc == 0), stop=(hc == 1)),
                                 reads=[b_w2] + b_hid, writes=[bf3])
                        P.op("act", lambda e: e.copy(kcT[:], f3[:, 0:512]), reads=[bf3], writes=[b_kcT])
                    else:
                        for c in range(4):
                            for hc in range(2):
                                P.op("pe", lambda e, hc=hc, c=c: e.matmul(f3[:, c * 128:(c + 1) * 128], hid[:, hc, c * 128:(c + 1) * 128], w2b[:, hc, :],
                                                                          start=(hc == 0), stop=(hc == 1)), reads=[b_w2] + b_hid, writes=[bf3])
                        P.op("act", lambda e: e.copy(rhsC[:, :, 0:128], f3[:].rearrange("p (c m) -> p c m", c=4)), reads=[bf3], writes=[b_rhsC])
                full_barrier(P)
        with ExitStack() as es3:
            sb3 = lambda n, s, d: es3.enter_context(nc.sbuf_tensor(P.pfx + n, s, d))
            tab = sb3("tab", [33, 4], F32)
            tabB = sb3("tabB", [33, 128], F32)
            ohp = [sb3(f"ohp{i}", [33, 512], F32) for i in range(2)]
            frp = [sb3(f"frp{i}", [128, 512], BF16) for i in range(2)]
            b_tab, b_tabB, b_ohp, b_frp = B(), B(), [B(), B()], [B(), B()]
            b_fscw = [B(), B()]
            P.dma("sp", tab[:], relb[:, :], writes=[b_tab])
            ki = 0
            for h in range(4):
                P.op("dve", lambda e, h=h: e.tensor_copy(tabB[:], tab[:, h:h + 1].to_broadcast([33, 128])), reads=[b_tab], writes=[b_tabB])
                for pc_ in range(L1 // 512):
                    k = ki % 2
                    ki += 1
                    P.dma("sp", ohp[k][:], oh_d[:, pc_ * 512:(pc_ + 1) * 512], writes=[b_ohp[k]])
                    pb, bpb = (f2, bf2) if k == 0 else (f3, bf3)
                    P.op("pe", lambda e, pb=pb, k=k: e.matmul(pb[:], tabB[:], ohp[k][:], start=True, stop=True), reads=[b_tabB, b_ohp[k]], writes=[bpb])
                    P.op("act", lambda e, pb=pb, k=k: e.activation(frp[k][:], pb[:], AF.Exp), reads=[bpb], writes=[b_frp[k]])
                    P.dma("sp", fsc[:, h * L1 + pc_ * 512:h * L1 + (pc_ + 1) * 512], frp[k][:], reads=[b_frp[k]], writes=[b_fscw[k]])
            full_barrier(P)
            RS = 4 * L1
            Esel = sb3("Esel", [128, 14, 4, 128], BF16)
            Ew4 = sb3("Ew4", [128, 4, 128], BF16)
            b_Esel = B()
            for dl in range(14):
                P.dma("sp", Esel[:, dl], bass.AP(fsc.tensor, 128 * dl + OFF1, [[RS - 1, 128], [L1, 4], [1, 128]]),
                      reads=[b_fsc], writes=[b_Esel])
            cst = sb3("cst", [128, 4, 128], F32)
            qhalf = sb3("qhalf_sb", [128, 1], F32)
            gbb = sb3("gbb", [128, 12], F32)
            m4b = sb3("m4b", [128, 128], BF16)
            b_cst = B()
            P.dma("sp", cst[:], cst_d[:, :].rearrange("p (c m) -> p c m", c=4), writes=[b_cst])
            b_q2, b_g2 = B(), B()
            P.dma("sp", qhalf[:], qh_d[:, :], writes=[b_q2])
            P.dma("sp", gbb[:], bcast_rows(gbd.tensor, gbd.offset, 12), writes=[b_g2])
            CST = [b_cst, b_q2, b_g2, b_cb, b_Esel]
            P.op("dve", lambda e: e.tensor_copy(m4b[:], cst[:, 3, :]), reads=[b_cst], writes=[b_Esel])
            P.op("dve", lambda e: e.tensor_tensor(Ew4[:], Esel[:, 4], m4b[:].unsqueeze(1).to_broadcast([128, 4, 128]), ALU.mult),
                 reads=[b_Esel], writes=[b_Esel])
            mrow, mge1, meq0 = cst[:, 0, :], cst[:, 1, :], cst[:, 2, :]
            Expand = sb3("Expand", [128, NT, 128], BF16)
            for Tk in range(NT):
                P.op("pool", lambda e, Tk=Tk: e.tensor_copy(Expand[:, Tk, :].rearrange("m (h k) -> m h k", h=2),
                                                            identb[:, 2 * Tk:2 * Tk + 2].unsqueeze(2).to_broadcast([128, 2, 64])),
                     reads=[b_cb], writes=[b_Esel])
            hTt = sb3("hTt", [128, 8, 128], BF16)
            b_hTt = [B()]
            QT = [sb3(f"QT{i}", [128, 4, 128], BF16) for i in range(2)]
            b_QT = [B(), B()]
            gsg = sb3("gsg", [128, 12], F32)
            b_gsg = B()
            ecb = [sb3(f"ecb{i}", [128, 4, 128], BF16) for i in range(2)]
            b_ecb = [B(), B()]
            psb = [sb3(f"psb{i}", [128, 4, 128], BF16) for i in range(3)]
            b_psb = [B(), B(), B()]
            ocb = sb3("ocb", [128, 4, 128], F32)
            b_ocb = B()
            sm = sb3("sm3", [128, 64], F32)
            b_sm = B()
            imp = sb3("imp", [128, 128], F32)
            M1 = sb3("M1", [128, 128], F32)
            Fm = sb3("Fm", [128, 128], F32)
            sa = sb3("sa", [128, 128], F32)
            rp = sb3("rp", [128, 128], F32)
            sel = sb3("sel", [128, 128], F32)
            nsb = sb3("nsb", [128, 128], BF16)
            nsT = sb3("nsT", [128, 128], BF16)
            mx = sb3("mx", [128, 16], F32)
            b_imp, b_M1, b_Fm, b_sa, b_rp, b_sel, b_nsb, b_nsT, b_mx = (B() for _ in range(9))
            b_ow = [B(), B()]
            ob = sb3("ob", [128, 4, 128], BF16)
            oTt = [sb3(f"oTt{i}", [128, 4, 128], BF16) for i in range(2)]
            b_ob, b_oTt = B(), [B(), B()]
            si = 0
            for T in range(NT):
                k2 = T % 2
                load_xT(P, BK, x, b_x, xin, b_xin, hTt, b_hTt, ident, b_ident, T * 128, 1, ctr)
                for j in range(4):
                    for dc in range(8):
                        P.op("pe", lambda e, j=j, dc=dc: e.matmul(f6[:, j * 128:(j + 1) * 128], wQb[:, dc, j * 128:(j + 1) * 128], hTt[:, dc, :],
                                                                  start=(dc == 0), stop=(dc == 7)), reads=[b_hTt[0], b_wQ], writes=[bf6])
                qt, bqt = QT[k2], b_QT[k2]
                P.op("act", lambda e, qt=qt: e.mul(qt[:].rearrange("p j q -> p (j q)"), f6[:], SCQ), reads=[bf6], writes=[bqt])
                for dc in range(8):
                    P.op("pe", lambda e, dc=dc: e.matmul(f4[:, 16:28], hTt[:, dc, :], wGb[:, dc, :], start=(dc == 0), stop=(dc == 7)),
                         reads=[b_hTt[0], b_wG], writes=[bf4])
                P.op("dve", lambda e: e.tensor_tensor(gsg[:], f4[:, 16:28], gbb[:], ALU.add), reads=[bf4] + CST, writes=[b_gsg])
                P.op("act", lambda e: e.activation(gsg[:], gsg[:], AF.Sigmoid), reads=[b_gsg], writes=[b_gsg])
                qrhs = qt[:].rearrange("p j q -> p (j q)")
                ncmp = (8 * T + 6) // 128 + 1
                for c in range(ncmp):
                    dl = min(T - 16 * c, 28)
                    ke = si % 2
                    kp = si % 3
                    si += 1
                    P.dma("sp", ecb[ke][:], bass.AP(fsc.tensor, 128 * dl - 31 + OFF1, [[RS - 16, 128], [L1, 4], [1, 128]]),
                          reads=[b_fsc], writes=[b_ecb[ke]])
                    P.op("pe", lambda e, c=c, qrhs=qrhs: e.matmul(f6[:], kcT[:, c * 128:(c + 1) * 128], qrhs, start=True, stop=True),
                         reads=[b_kcT, bqt], writes=[bf6])
                    pp, bpp = psb[kp], b_psb[kp]
                    P.op("act", lambda e, pp=pp: e.activation(pp[:].rearrange("p j q -> p (j q)"), f6[:], AF.Exp), reads=[bf6], writes=[bpp])
                    P.op("dve", lambda e, pp=pp, ke=ke: e.tensor_tensor(pp[:], pp[:], ecb[ke][:], ALU.mult), reads=[bpp, b_ecb[ke]], writes=[bpp])
                    for j in range(4):
                        fb, bfb = (f2, bf2) if j < 2 else (f3, bf3)
                        P.op("pe", lambda e, fb=fb, j=j, c=c, pp=pp, ncmp=ncmp: e.matmul(
                            fb[:, (j % 2) * 256:(j % 2) * 256 + 256], pp[:, j, :], rhsC[:, c, :],
                            start=(c == 0 and j % 2 == 0), stop=(c == ncmp - 1 and j % 2 == 1)),
                            reads=[bpp, b_rhsC], writes=[bfb])
                        P.op("pe", lambda e, j=j, c=c, pp=pp, ncmp=ncmp: e.matmul(
                            f4[:, j:j + 1], pp[:, j, :], ones_bf[:, 0:1], start=(c == 0 and j == 0), stop=(c == ncmp - 1 and j == 3)),
                            reads=[bpp, b_cb], writes=[bf4])
                rc, cc = sm[:, 0:4], sm[:, 4:8]
                P.op("dve", lambda e, rc=rc: e.tensor_scalar(rc, f4[:, 0:4], 1e-30, None, ALU.max), reads=[bf4], writes=[b_sm])
                P.op("dve", lambda e, rc=rc: e.reciprocal(rc, rc), reads=[b_sm], writes=[b_sm])
                P.op("dve", lambda e, rc=rc, cc=cc: e.tensor_tensor(cc, rc, gsg[:, 0:4], ALU.mult), reads=[b_sm, b_gsg], writes=[b_sm])
                for j in range(4):
                    fb, bfb = (f2, bf2) if j < 2 else (f3, bf3)
                    o0 = (j % 2) * 256
                    P.op("dve", lambda e, fb=fb, j=j, o0=o0, cc=cc: e.tensor_scalar(ocb[:, j, :], fb[:, o0:o0 + 128], cc[:, j:j + 1], None, ALU.mult),
                         reads=[bfb, b_sm], writes=[b_ocb])
                    if j == 0:
                        P.op("dve", lambda e, fb=fb, o0=o0, rc=rc: e.tensor_scalar(imp[:], fb[:, o0 + 128:o0 + 256], rc[:, 0:1], None, ALU.mult),
                             reads=[bfb, b_sm], writes=[b_imp])
                    else:
                        P.op("dve", lambda e, fb=fb, o0=o0, rc=rc, j=j: e.scalar_tensor_tensor(
                            imp[:], fb[:, o0 + 128:o0 + 256], rc[:, j:j + 1], imp[:], ALU.mult, ALU.add), reads=[bfb, b_sm, b_imp], writes=[b_imp])
                qb, qb1, qb2, tau = sm[:, 8:9], sm[:, 9:10], sm[:, 10:11], sm[:, 11:12]
                P.op("dve", lambda e, qb=qb, T=T: e.tensor_scalar(qb, qhalf[:], float(2 * T), None, ALU.add), reads=CST, writes=[b_sm])
                P.op("dve", lambda e, qb=qb, qb1=qb1: e.tensor_scalar(qb1, qb, -1.0, None, ALU.add), reads=[b_sm], writes=[b_sm])
                P.op("dve", lambda e, qb=qb, qb2=qb2: e.tensor_scalar(qb2, qb, -2.0, None, ALU.add), reads=[b_sm], writes=[b_sm])
                P.op("dve", lambda e, qb2=qb2: e.tensor_scalar(M1[:], mrow, qb2, None, ALU.is_le), reads=[b_sm] + CST, writes=[b_M1])
                P.op("dve", lambda e: e.tensor_tensor(M1[:], M1[:], mge1, ALU.mult), reads=[b_M1] + CST, writes=[b_M1])
                P.op("dve", lambda e, qb=qb: e.tensor_scalar(Fm[:], mrow, qb, None, ALU.is_equal), reads=[b_sm] + CST, writes=[b_Fm])
                P.op("dve", lambda e, qb1=qb1: e.tensor_scalar(sel[:], mrow, qb1, None, ALU.is_equal), reads=[b_sm] + CST, writes=[b_sel])
                P.op("dve", lambda e: e.tensor_tensor(Fm[:], Fm[:], sel[:], ALU.add), reads=[b_Fm, b_sel], writes=[b_Fm])
                P.op("dve", lambda e: e.tensor_tensor(Fm[:], Fm[:], meq0, ALU.max), reads=[b_Fm] + CST, writes=[b_Fm])
                P.op("dve", lambda e: e.tensor_tensor(sa[:], imp[:], M1[:], ALU.mult), reads=[b_imp, b_M1], writes=[b_sa])
                P.op("dve", lambda e: e.max(mx[:, 0:8], sa[:]), reads=[b_sa], writes=[b_mx])
                P.op("dve", lambda e: e.match_replace(rp[:], mx[:, 0:8], sa[:], -1.0), reads=[b_sa, b_mx], writes=[b_rp])
                P.op("dve", lambda e: e.max(mx[:, 8:16], rp[:]), reads=[b_rp], writes=[b_mx])
                P.op("dve", lambda e: e.tensor_scalar(sel[:], sa[:], mx[:, 12:13], None, ALU.is_ge), reads=[b_sa, b_mx], writes=[b_sel])
                P.op("dve", lambda e: e.tensor_tensor(sel[:], sel[:], M1[:], ALU.mult), reads=[b_sel, b_M1], writes=[b_sel])
                P.op("dve", lambda e: e.tensor_tensor(sel[:], sel[:], Fm[:], ALU.max), reads=[b_sel, b_Fm], writes=[b_sel])
                P.op("dve", lambda e: e.tensor_scalar(nsb[:], sel[:], -1.0, 30000.0, ALU.add, ALU.mult), reads=[b_sel], writes=[b_nsb])
                P.op("pe", lambda e: e.transpose(BK.h[:, 512:640], nsb[:], identb[:]), reads=[b_nsb, b_cb], writes=[BK.bh])
                P.op("act", lambda e: e.copy(nsT[:], BK.h[:, 512:640]), reads=[BK.bh], writes=[b_nsT])
                nsrhs = nsT[:].unsqueeze(1).to_broadcast([128, 4, 128])
                for br, (KT_, bKT, V_, bV, fa, bfa, fb_, bfb_, tks) in enumerate((
                        (KsT, b_KsT, Vs, b_Vs, f2, bf2, f3, bf3, list(range(0, T + 1))),
                        (KwT, b_KwT, Vw, b_Vw, f4, bf4, f5, bf5, list(range(max(0, T - 4), T + 1))))):
                    for ii, Tk in enumerate(tks):
                        dl = T - Tk
                        kp = si % 3
                        ks_ = si % 2
                        si += 1
                        st, bst = (f0, bf0) if ks_ == 0 else (f1, bf1)
                        P.op("pe", lambda e, st=st, KT_=KT_, Tk=Tk, qrhs=qrhs, br=br: e.matmul(
                            st[:], KT_[:, Tk * 128:(Tk + 1) * 128], qrhs, start=True, stop=(br == 1)),
                            reads=[bKT, bqt], writes=[bst])
                        if br == 0:
                            P.op("pe", lambda e, st=st, Tk=Tk, nsrhs=nsrhs: e.matmul(
                                st[:].rearrange("p (j q) -> p j q", j=4), Expand[:, Tk, :], nsrhs, start=False, stop=True),
                                reads=[b_nsT, b_Esel], writes=[bst])
                        pp, bpp = psb[kp], b_psb[kp]
                        P.op("act", lambda e, pp=pp, st=st: e.activation(pp[:].rearrange("p j q -> p (j q)"), st[:], AF.Exp), reads=[bst], writes=[bpp])
                        if br == 1 and dl == 4:
                            et = Ew4[:]
                        else:
                            et = Esel[:, min(dl, 13)]
                        P.op("dve", lambda e, pp=pp, et=et: e.tensor_tensor(pp[:], pp[:], et, ALU.mult), reads=[bpp, b_Esel], writes=[bpp])
                        for j in range(4):
                            fo, bfo = (fa, bfa) if j < 2 else (fb_, bfb_)
                            P.op("pe", lambda e, fo=fo, j=j, pp=pp, V_=V_, Tk=Tk, ii=ii, n=len(tks): e.matmul(
                                fo[:, (j % 2) * 129:(j % 2) * 129 + 129], pp[:, j, :], V_[:, Tk, :],
                                start=(ii == 0 and j % 2 == 0), stop=(ii == n - 1 and j % 2 == 1)),
                                reads=[bpp, bV], writes=[bfo])
                    for j in range(4):
                        fo, bfo = (fa, bfa) if j < 2 else (fb_, bfb_)
                        P.op("dve", lambda e, fo=fo, j=j, br=br: e.tensor_scalar(
                            sm[:, 16 + br * 4 + j:17 + br * 4 + j], fo[:, (j % 2) * 129 + 128:(j % 2) * 129 + 129], 1e-30, None, ALU.max),
                            reads=[bfo], writes=[b_sm])
                cf = sm[:, 16:24]
                P.op("dve", lambda e, cf=cf: e.reciprocal(cf, cf), reads=[b_sm], writes=[b_sm])
                P.op("dve", lambda e, cf=cf: e.tensor_tensor(cf, cf, gsg[:, 4:12], ALU.mult), reads=[b_sm, b_gsg], writes=[b_sm])
                for j in range(4):
                    fs_, bfs = (f2, bf2) if j < 2 else (f3, bf3)
                    fw_, bfw = (f4, bf4) if j < 2 else (f5, bf5)
                    o0 = (j % 2) * 129
                    P.op("dve", lambda e, fs_=fs_, j=j, o0=o0, cf=cf: e.scalar_tensor_tensor(
                        ocb[:, j, :], fs_[:, o0:o0 + 128], cf[:, j:j + 1], ocb[:, j, :], ALU.mult, ALU.add),
                        reads=[bfs, b_sm, b_ocb], writes=[b_ocb])
                    P.op("dve", lambda e, fw_=fw_, j=j, o0=o0, cf=cf: e.scalar_tensor_tensor(
                        ob[:, j, :], fw_[:, o0:o0 + 128], cf[:, 4 + j:5 + j], ocb[:, j, :], ALU.mult, ALU.add),
                        reads=[bfw, b_sm, b_ocb], writes=[b_ob])
                for j in range(4):
                    P.op("pe", lambda e, j=j: e.transpose(BK.h[:, j * 128:(j + 1) * 128], ob[:, j, :], identb[:]), reads=[b_ob, b_cb], writes=[BK.bh])
                ot, bot = oTt[k2], b_oTt[k2]
                P.op("act", lambda e, ot=ot: e.copy(ot[:].rearrange("p j q -> p (j q)"), BK.h[:, 0:512]), reads=[BK.bh], writes=[bot])
                ostore(P, T, ot[:], [bot], k2)
            full_barrier(P)


C_OFF_KV_, C_OFF_GATE_ = 1024, 2560


def pack_m1(inp, h1b, g):
    w = inp["c_w_in"][0]
    wQ = w[:, g * 512:(g + 1) * 512]
    kvcols = []
    for br in range(3):
        for kv in range(2):
            o = C_OFF_KV_ + ((br * 2 + kv) * 2 + g) * 128
            kvcols.append(w[:, o:o + 128])
    wKV = np.concatenate(kvcols, axis=1)
    gcols = [C_OFF_GATE_ + br * 8 + g * 4 + j for br in range(3) for j in range(4)]
    wG = w[:, gcols]
    gb = inp["c_gate_b"][0][[c - C_OFF_GATE_ for c in gcols]][None]
    pos = inp["c_cmp_pos"][0]
    posT = np.concatenate([pos[0].T, pos[1].T], axis=1)
    relb = np.concatenate([inp["rel_bias"][:, 4 * g:4 * g + 4], np.full((1, 4), -30000.0, np.float32)], axis=0)
    c = nsa_consts()
    return dict(x=h1b, wQ=np.ascontiguousarray(wQ), wKV=np.ascontiguousarray(wKV), wG=np.ascontiguousarray(wG),
                gb=np.ascontiguousarray(gb.astype(np.float32)), posT=np.ascontiguousarray(posT), w1=inp["c_cmp_w1"][0], w2=inp["c_cmp_w2"][0],
                relb=np.ascontiguousarray(relb), oh1=c["oh1"], ovl=np.ascontiguousarray(c["ovl"].reshape(128, 512)),
                cst1=np.ascontiguousarray(c["cst1"].reshape(128, 512)), qhalf=c["qhalf"], ident=np.eye(128, dtype=np.float32))


U32 = mybir.dt.uint32
GROUPS = [[0, 1], [2, 3], [4, 5], [6, 7]]


def build_fused():
    nc = bass.Bass("TRN2", target_bir_lowering=False)
    ident_d = nc.dram_tensor("ident", [128, 128], F32, kind="ExternalInput").ap()
    gidx_d = nc.dram_tensor("gidx", [128, 16], U32, kind="ExternalInput").ap()
    xres_d = nc.dram_tensor("xres", [TOK, D], F32, kind="ExternalInput").ap()
    out_d = nc.dram_tensor("out", [TOK, D], F32, kind="ExternalOutput").ap()
    io_m0 = declare_mixer0(nc, "m0_")
    io_p0 = declare_post(nc, "p0_")
    io_m1 = declare_mixer1(nc, "m1_")
    io_p1 = declare_post(nc, "p1_")
    y0src = [nc.dram_tensor(f"y0src{i}", [2048, 512], BF16).ap() for i in range(4)]
    y0dst = [nc.dram_tensor(f"y0dst{i}", [4096, 512], BF16).ap() for i in range(4)]
    o1src = [nc.dram_tensor(f"o1src{i}", [2048, 512], BF16).ap() for i in range(4)]
    o1dst = [nc.dram_tensor(f"o1dst{i}", [4096, 512], BF16).ap() for i in range(4)]
    h1src = [nc.dram_tensor(f"h1src{i}", [512, D], F32).ap() for i in range(8)]
    h1dst = [nc.dram_tensor(f"h1dst{i}", [1024, D], F32).ap() for i in range(8)]
    with ExitStack() as es:
        P = Prog(nc, es)
        B = P.buf
        gidx = P.sb("gidx_sb", [128, 16], U32)
        b_gidx = B()
        P.dma("sp", gidx[:], gidx_d[:, :], writes=[b_gidx])

        def phase(pfx, fn, io):
            with ExitStack() as pes:
                P.es = pes
                P.pfx = pfx
                P.in_phase = True
                fn(P, io)
                full_barrier(P)
                P.recycle()
                P.in_phase = False
            P.es = es

        def ag(srcs, dsts):
            P.in_phase = True
            for a, d in zip(srcs, dsts):
                P.all_gather(a, d, GROUPS, reads=[], writes=[B()])
            full_barrier(P)
            P.recycle()
            P.in_phase = False

        def row_store(dst_list, nbuf=4):
            bufs = [[B() for _ in range(nbuf)] for _ in dst_list]
            cnt = [0]

            def ystore(P_, i, blk, sb, reads):
                h, tq = blk // 2, (blk % 2) * 4
                r0 = (h * 8 + tq) * 128
                dst = dst_list[i][r0:r0 + 512, :].rearrange("(t p) c -> p t c", p=128)
                P_.dma("sp", dst, sb.rearrange("p (t c) -> p t c", t=4), reads=reads, writes=[bufs[i][cnt[0] % nbuf]])
                cnt[0] += 1
            return ystore

        def make_load_yt(dsts):
            def load_yt(P_, yt, byt, t4):
                for ec in range(8):
                    i, r = ec // 2, ec % 2
                    P_.gather(yt[:, ec, :], dsts[i], gidx[:, r * 8 + t4:r * 8 + t4 + 1], reads=[b_gidx], writes=[byt[ec]])
            return load_yt

        io_m0.update(ident=ident_d, ystore=row_store(y0src))
        phase("m0_", emit_mixer0, io_m0)
        ag(y0src, y0dst)
        io_p0.update(ident=ident_d, load_yt=make_load_yt(y0dst), xres=lambda t0: xres_d[t0:t0 + 128, :],
                     out=lambda t0: h1src[t0 // 512][t0 % 512:t0 % 512 + 128, :], is_final=False)
        phase("p0_", emit_post, io_p0)
        ag(h1src, h1dst)
        o1bufs = [[B(), B()] for _ in range(4)]

        def ostore(P_, T, ot, reads, slot):
            h, t4 = T // 32, (T % 32) // 4
            r0 = (h * 8 + t4) * 128
            c0 = (T % 4) * 128
            for j in range(4):
                P_.dma("sp", o1src[j][r0:r0 + 128, c0:c0 + 128], ot[:, j, :], reads=reads, writes=[o1bufs[j][slot]])

        def h1tile(t0):
            k, rr, tt = (t0 % TOK) // 512, t0 // TOK, t0 % 512
            return h1dst[k][rr * 512 + tt:rr * 512 + tt + 128, :]

        io_m1.update(ident=ident_d, x=h1tile, ostore=ostore)
        phase("m1_", emit_mixer1, io_m1)
        ag(o1src, o1dst)
        io_p1.update(ident=ident_d, load_yt=make_load_yt(o1dst), xres=lambda t0: h1src[t0 // 512][t0 % 512:t0 % 512 + 128, :],
                     out=lambda t0: out_d[t0:t0 + 128, :], is_final=True)
        phase("p1_", emit_post, io_p1)
        P.finish()
    return nc


def pack_post(inp, layer, wo_perm, pfx):
    wr = np.concatenate([inp["moe_wr_g"][layer]] + [inp["moe_wr_e"][layer][g] for g in range(4)], axis=1)
    br = np.concatenate([inp["moe_br_g"][layer], inp["moe_br_e"][layer].reshape(-1)])[None]
    d = dict(wo=np.ascontiguousarray(wo_perm),
             ln1g=np.ascontiguousarray(inp["ln_g"][layer, 0]), ln1b=np.ascontiguousarray(inp["ln_b"][layer, 0]),
             ln2g=np.ascontiguousarray(inp["ln_g"][layer, 1]), ln2b=np.ascontiguousarray(inp["ln_b"][layer, 1]),
             wg=inp["moe_w_gate"][layer], wu=inp["moe_w_up"][layer], wd=inp["moe_w_down"][layer],
             wr=np.ascontiguousarray(wr.astype(np.float32)), br=np.ascontiguousarray(br.astype(np.float32)))
    return {pfx + k: v for k, v in d.items()}


def kernel(**inputs):
    inp = {k: np.asarray(v) for k, v in inputs.items()}
    nb = inp["x"].shape[0]
    cores = list(range(2 * nb))
    wo0 = inp["ab_w_out"][0]
    ch0 = [(2 * r + i) if i < 2 else (4 + 2 * r + (i - 2)) for i in range(4) for r in range(2)]
    wo0p = np.concatenate([wo0[c * 128:(c + 1) * 128] for c in ch0], axis=0)
    wo1 = inp["c_w_out"][0]
    ch1 = [4 * r + i for i in range(4) for r in range(2)]
    wo1p = np.concatenate([wo1[c * 128:(c + 1) * 128] for c in ch1], axis=0)
    post0 = pack_post(inp, 0, wo0p, "p0_")
    post1 = pack_post(inp, 1, wo1p, "p1_")
    ident = np.eye(128, dtype=np.float32)
    maps = []
    for b in range(nb):
        for c in range(2):
            m = dict(ident=ident, xres=np.ascontiguousarray(inp["x"][b, TOK * c:TOK * (c + 1)]))
            p = np.arange(128, dtype=np.uint32)[:, None]
            rt = np.arange(16, dtype=np.uint32)[None, :]
            m["gidx"] = np.ascontiguousarray(((rt // 8) * 2048 + c * 1024 + (rt % 8) * 128 + p).astype(np.uint32))
            m0 = pack_m0(inp, b, c)
            m0.pop("ident")
            m.update({"m0_" + k: v for k, v in m0.items()})
            m1 = pack_m1(inp, None, c)
            m1.pop("ident")
            m1.pop("x")
            m.update({"m1_" + k: v for k, v in m1.items()})
            m.update(post0)
            m.update(post1)
            maps.append(m)
    nc = build_fused()
    res = run_bass_kernel_spmd(nc, maps, core_ids=cores).results
    out = np.stack([np.concatenate([np.asarray(res[b * 2 + c]["out"]) for c in range(2)], axis=0) for b in range(nb)])
    return out.astype(np.float32)
```

```python
import os
import numpy as np
from contextlib import ExitStack
import concourse.bass as bass
import concourse.mybir as mybir
from concourse.bass_utils import run_bass_kernel_spmd

F32 = mybir.dt.float32
BF16 = mybir.dt.bfloat16
AF = mybir.ActivationFunctionType
ALU = mybir.AluOpType
AX = mybir.AxisListType

D = 1024
ALPHA = (2.0 * 2) ** 0.25
LN_EPS = 1e-5
NEXP = 16
EH = 512


class Buf:
    __slots__ = ("name", "w", "rd", "dsem", "dval", "dkey")

    def __init__(self, name):
        self.name = name
        self.w = None
        self.rd = {}
        self.dsem = None
        self.dval = 0
        self.dkey = None


class Prog:
    def __init__(self, nc, es):
        self.nc = nc
        self.es = es
        self.es_global = es
        self.pfx = ""
        self.eng = dict(pe=nc.tensor, act=nc.scalar, dve=nc.vector, pool=nc.gpsimd, sp=nc.sync)
        self.sem = {k: es.enter_context(nc.semaphore("s_" + k)) for k in self.eng}
        self.cnt = {k: 0 for k in self.eng}
        self.known = {k: {} for k in self.eng}
        self.nsem = 0
        self.outs = []
        self.nbuf = 0
        self.dmasems = {}
        self.issuer = {}
        self.free_sems = []
        self.phase_sems = []
        self.in_phase = False

    def buf(self, name=None):
        self.nbuf += 1
        return Buf(name or f"b{self.nbuf}")

    def sb(self, name, shape, dt):
        return self.es.enter_context(self.nc.sbuf_tensor(self.pfx + name, shape, dt))

    def ps(self, name, shape, dt=F32):
        return self.es.enter_context(self.nc.psum_tensor(self.pfx + name, shape, dt))

    def _collect(self, e, reads, writes):
        deps = {}

        def add(tok, same_ok):
            if tok is None:
                return
            key, sh, val = tok
            if key == e and not same_ok:
                return
            if key in deps and deps[key][1] >= val:
                return
            deps[key] = (sh, val)

        for b in reads:
            add(b.w, e != "pe")
        for b in writes:
            add(b.w, e != "pe")
            for tok in b.rd.values():
                add(tok, e != "pe")
        return deps

    def _wait(self, e, deps):
        kn = self.known[e]
        for key, (sh, val) in deps.items():
            if kn.get(key, 0) >= val:
                continue
            self.eng[e].wait_ge(sh, val)
            kn[key] = val

    def op(self, e, fn, reads=(), writes=()):
        self._wait(e, self._collect(e, reads, writes))
        ins = fn(self.eng[e])
        self.cnt[e] += 1
        ins.then_inc(self.sem[e], 1)
        tok = (e, self.sem[e], self.cnt[e])
        for b in reads:
            b.rd[e] = tok
        for b in writes:
            b.w = tok
            b.rd = {}
        return tok

    def dma(self, q, out, in_, reads=(), writes=(), is_out=False, nowaw=False, **kw):
        wb = writes[0]
        if nowaw:
            saved = wb.w
            if saved is not None and saved[0] == wb.dkey:
                wb.w = None
            deps = self._collect(q, reads, writes)
            wb.w = saved
        else:
            deps = self._collect(q, reads, writes)
        self._wait(q, deps)
        self._ensure_sem(wb)
        ins = self.eng[q].dma_start(out=out, in_=in_, **kw)
        wb.dval += 16
        ins.then_inc(wb.dsem, 16)
        tok = (wb.dkey, wb.dsem, wb.dval)
        self.dmasems[wb.dkey] = tok
        self.issuer[wb.dkey] = q
        for b in reads:
            b.rd[wb.dkey] = tok
        for b in writes:
            b.w = tok
            b.rd = {}
        if is_out and wb not in self.outs:
            self.outs.append(wb)
        return tok

    def _ensure_sem(self, wb):
        if wb.dsem is not None:
            return
        if self.free_sems:
            wb.dkey, wb.dsem, wb.dval = self.free_sems.pop()
        else:
            wb.dkey = f"d{self.nsem}"
            wb.dsem = self.es_global.enter_context(self.nc.semaphore(wb.dkey))
            self.nsem += 1
        if self.in_phase:
            self.phase_sems.append(wb)

    def recycle(self):
        for wb in self.phase_sems:
            self.free_sems.append((wb.dkey, wb.dsem, wb.dval))
            wb.dsem = None
        self.phase_sems = []

    def _tok_async(self, q, wb, ins, inc, reads, writes):
        self._ensure_sem(wb)
        wb.dval += inc
        ins.then_inc(wb.dsem, inc)
        tok = (wb.dkey, wb.dsem, wb.dval)
        self.dmasems[wb.dkey] = tok
        self.issuer[wb.dkey] = q
        for b in reads:
            b.rd[wb.dkey] = tok
        for b in writes:
            b.w = tok
            b.rd = {}
        return tok

    def gather(self, out, in_dram, idx_ap, reads=(), writes=()):
        self._wait("pool", self._collect("pool", reads, writes))
        ins = self.nc.gpsimd.indirect_dma_start(out=out, out_offset=None, in_=in_dram,
                                                in_offset=bass.IndirectOffsetOnAxis(ap=idx_ap, axis=0))
        return self._tok_async("pool", writes[0], ins, 16, reads, writes)

    def all_gather(self, src, dst, groups, reads=(), writes=()):
        self._wait("pool", self._collect("pool", reads, writes))
        ins = self.nc.gpsimd.collective_compute("AllGather", ALU.bypass, replica_groups=groups, ins=[src.opt()], outs=[dst.opt()])
        return self._tok_async("pool", writes[0], ins, 1, reads, writes)

    def finish(self):
        for wb in self.outs:
            if wb.dsem is not None:
                self.eng["sp"].wait_ge(wb.dsem, wb.dval)


def bcast_rows(ap1d_tensor, offset, n, parts=128):
    return bass.AP(ap1d_tensor, offset, [[0, parts], [1, n]])


def emit_tail(P, hmid, hmid_buf, out, out_buf, T, w, layer, ST=1024, banks=None, is_final=True):
    nc = P.nc
    NT = ST // 128
    NK = ST // 512
    ident = P.sb("t_ident", [128, 128], F32)
    ones1 = P.sb("t_ones1", [1, 128], F32)
    brow = P.sb("t_brow", [1, 20], F32)
    wr = P.sb("t_wr", [128, 8, 20], F32)
    gbc = P.sb("t_gbc", [128, D], F32)
    bbc = P.sb("t_bbc", [128, D], F32)
    xin = [P.sb(f"t_xin{i}", [128, D], F32) for i in range(2)]
    acc = P.sb("t_acc", [128, NT, D], F32)
    hT16 = P.sb("t_hT16", [128, 8, ST], BF16)
    hT32 = P.sb("t_hT32", [128, 8, 128], F32)
    gates = P.sb("t_gates", [128, NT, NEXP], F32)
    wgb = [P.sb(f"t_wg{i}", [128, 8, EH], BF16) for i in range(2)]
    wub = [P.sb(f"t_wu{i}", [128, 8, EH], BF16) for i in range(2)]
    wdb = [P.sb(f"t_wd{i}", [128, 4, D], BF16) for i in range(2)]
    sg = [P.sb(f"t_sg{i}", [128, 512], BF16) for i in range(2)]
    hid = [P.sb(f"t_hid{i}", [128, 4, 512], BF16) for i in range(2)]
    obuf = [P.sb(f"t_ob{i}", [128, D], F32) for i in range(2)]
    sm = P.sb("t_sm", [128, 64], F32)
    lg = P.sb("t_lg", [128, 20], F32)
    stats = P.sb("t_stats", [128, 2, 6], F32)

    if banks is None:
        banks = [P.ps(f"t_ps{i}", [128, 512]) for i in range(8)]
    ps_g, ps_u, ps_o, ps_t = banks[0:2], banks[2:4], banks[4:6], banks[6:8]

    B = P.buf
    b_ident, b_ones1, b_brow, b_wr, b_gbc, b_bbc = B(), B(), B(), B(), B(), B()
    b_xin = [B(), B()]
    b_acc = [B() for _ in range(NT)]
    b_hT16 = [B() for _ in range(NT)]
    b_hT32 = B()
    b_gates = [B() for _ in range(NT)]
    b_wg, b_wu, b_wd = [B(), B()], [B(), B()], [B(), B()]
    b_sg = [B(), B()]
    b_hid = [[B() for _ in range(4)] for _ in range(2)]
    b_ob = [B(), B()]
    b_outw = [B(), B()]
    b_sm = B()
    b_lg = B()
    b_stats = B()
    b_psg, b_psu, b_pso, b_pst = [B(), B()], [B(), B()], [B(), B()], [B(), B()]

    P.dma("sp", ident[:], w["ident"][:, :], writes=[b_ident])
    P.dma("sp", brow[:], w["br"][:, :], writes=[b_brow])
    P.dma("sp", wr[:], w["wr"].rearrange("(c p) n -> p c n", p=128), writes=[b_wr])
    P.dma("sp", gbc[:], bcast_rows(w["ln_g"].tensor, w["ln_g"].offset, D), writes=[b_gbc])
    P.dma("sp", bbc[:], bcast_rows(w["ln_b"].tensor, w["ln_b"].offset, D), writes=[b_bbc])
    P.op("dve", lambda e: e.memset(ones1[:], 1.0), writes=[b_ones1])

    nst = T // ST
    wslot = 0
    DBG = ()

    stg = [P.sb(f"t_stg{i}", [128, 4, 512], F32) for i in range(3)]
    b_stg = [B() for _ in range(3)]
    stg_i = [0]

    def load_chunk(dst, bdst, src):
        k = stg_i[0] % 3
        stg_i[0] += 1
        P.dma("sp", stg[k][:], src, writes=[b_stg[k]])
        P.op("act", lambda en, k=k, dst=dst: en.copy(dst, stg[k][:]), reads=[b_stg[k]], writes=[bdst])

    def load_expert(e, slot):
        for h in range(2):
            load_chunk(wgb[slot][:, 4 * h:4 * h + 4, :], b_wg[slot],
                       w["wg"][e][512 * h:512 * h + 512, :].rearrange("(c p) h -> p c h", p=128))
        for h in range(2):
            load_chunk(wub[slot][:, 4 * h:4 * h + 4, :], b_wu[slot],
                       w["wu"][e][512 * h:512 * h + 512, :].rearrange("(c p) h -> p c h", p=128))
        for h in range(2):
            load_chunk(wdb[slot][:, :, 512 * h:512 * h + 512], b_wd[slot],
                       w["wd"][e][:, 512 * h:512 * h + 512].rearrange("(c p) h -> p c h", p=128))

    xi = 0
    oi = 0
    for st in range(nst):
        t0 = st * ST
        if "noload" not in DBG:
            load_expert(0, wslot)
        for tt in range(NT):
            xb, bx = xin[xi % 2], b_xin[xi % 2]
            xi += 1
            P.dma("sp", xb[:], hmid[t0 + tt * 128:t0 + (tt + 1) * 128, :], reads=[hmid_buf], writes=[bx])
            P.op("act", lambda e, xb=xb, tt=tt: e.mul(acc[:, tt, :], xb[:], ALPHA), reads=[bx], writes=[b_acc[tt]])
            if 'notr' in DBG:
                continue
            for half in range(2):
                pt, bpt = ps_t[half], b_pst[half]
                for j in range(4):
                    dc = half * 4 + j
                    P.op("pe", lambda e, pt=pt, j=j, dc=dc, xb=xb: e.transpose(
                        pt[:, j * 128:(j + 1) * 128], xb[:, dc * 128:(dc + 1) * 128], ident[:]),
                        reads=[bx, b_ident], writes=[bpt])
                P.op("act", lambda e, pt=pt, half=half: e.copy(
                    hT32[:, half * 4:(half + 1) * 4, :], pt[:].rearrange("p (c t) -> p c t", c=4)),
                    reads=[bpt], writes=[b_hT32])
                P.op("dve", lambda e, pt=pt, half=half, tt=tt: e.tensor_copy(
                    hT16[:, half * 4:(half + 1) * 4, tt * 128:(tt + 1) * 128],
                    hT32[:, half * 4:(half + 1) * 4, :]),
                    reads=[b_hT32], writes=[b_hT16[tt]])
            if 'norouter' in DBG:
                continue
            pr, bpr = ps_o[0], b_pso[0]
            for dc in range(8):
                P.op("pe", lambda e, dc=dc, pr=pr: e.matmul(pr[:, 0:20], hT32[:, dc, :], wr[:, dc, :],
                                                            start=(dc == 0), stop=False),
                     reads=[b_hT32, b_wr], writes=[bpr])
            P.op("pe", lambda e, pr=pr: e.matmul(pr[:, 0:20], ones1[0:1, :], brow[0:1, :], start=False, stop=True),
                 reads=[b_ones1, b_brow], writes=[bpr])
            P.op("act", lambda e, pr=pr: e.copy(lg[:], pr[:, 0:20]), reads=[bpr], writes=[b_lg])
            emit_router(P, lg, b_lg, sm, b_sm, gates, b_gates[tt], tt)
        units = [(e, tk) for e in range(NEXP) for tk in range(NK)]

        def gu(ui):
            e, tk = units[ui]
            slot = (wslot + e) % 2
            hb = ui % 2
            for hc in range(4):
                pg, bpg = ps_g[hc % 2], b_psg[hc % 2]
                pu, bpu = ps_u[hc % 2], b_psu[hc % 2]
                rds = [b_hT16[tk * 4 + q] for q in range(4)]
                for dc in range(8):
                    P.op("pe", lambda en, pg=pg, dc=dc, hc=hc, slot=slot, tk=tk: en.matmul(
                        pg[:], wgb[slot][:, dc, hc * 128:(hc + 1) * 128], hT16[:, dc, tk * 512:(tk + 1) * 512],
                        start=(dc == 0), stop=(dc == 7)), reads=rds + [b_wg[slot]], writes=[bpg])
                for dc in range(8):
                    P.op("pe", lambda en, pu=pu, dc=dc, hc=hc, slot=slot, tk=tk: en.matmul(
                        pu[:], wub[slot][:, dc, hc * 128:(hc + 1) * 128], hT16[:, dc, tk * 512:(tk + 1) * 512],
                        start=(dc == 0), stop=(dc == 7)), reads=rds + [b_wu[slot]], writes=[bpu])
                s, bs = sg[hc % 2], b_sg[hc % 2]
                P.op("act", lambda en, s=s, pg=pg: en.activation(s[:], pg[:], AF.Silu), reads=[bpg], writes=[bs])
                P.op("dve", lambda en, s=s, pu=pu, hb=hb, hc=hc: en.tensor_tensor(
                    hid[hb][:, hc, :], s[:], pu[:], ALU.mult), reads=[bs, bpu], writes=[b_hid[hb][hc]])

        def down(ui):
            e, tk = units[ui]
            slot = (wslot + e) % 2
            hb = ui % 2
            k = 0
            for tq in range(4):
                tt = tk * 4 + tq
                for dh in range(2):
                    po, bpo = ps_o[k % 2], b_pso[k % 2]
                    k += 1
                    for hc in range(4):
                        P.op("pe", lambda en, po=po, hc=hc, tq=tq, dh=dh, hb=hb, slot=slot: en.matmul(
                            po[:], hid[hb][:, hc, tq * 128:(tq + 1) * 128], wdb[slot][:, hc, dh * 512:(dh + 1) * 512],
                            start=(hc == 0), stop=(hc == 3)), reads=[b_hid[hb][hc], b_wd[slot]], writes=[bpo])
                    P.op("dve", lambda en, po=po, tt=tt, dh=dh, e=e: en.scalar_tensor_tensor(
                        acc[:, tt, dh * 512:(dh + 1) * 512], po[:], gates[:, tt, e:e + 1],
                        acc[:, tt, dh * 512:(dh + 1) * 512], ALU.mult, ALU.add),
                        reads=[bpo, b_gates[tt], b_acc[tt]], writes=[b_acc[tt]])

        if "noexp" in DBG:
            units = []
        if "oneexp" in DBG:
            units = units[:2]
        for ui in range(len(units)):
            e, tk = units[ui]
            gu(ui)
            if ui > 0:
                down(ui - 1)
            if tk == 0 and e + 1 < NEXP:
                load_expert(e + 1, (wslot + e + 1) % 2)
        if units:
            down(len(units) - 1)
        wslot = (wslot + NEXP) % 2
        for tt in range(NT):
            ob, bo = obuf[oi % 2], b_ob[oi % 2]
            oi += 1
            if "nolnorm" in DBG:
                P.op("act", lambda e, ob=ob, tt=tt: e.copy(ob[:], acc[:, tt, :]), reads=[b_acc[tt]], writes=[bo])
            else:
                emit_ln(P, acc[:, tt, :], b_acc[tt], ob, bo, gbc, b_gbc, bbc, b_bbc, stats, b_stats, sm, b_sm)
            P.dma("sp", out(t0 + tt * 128), ob[:], reads=[bo], writes=[b_outw[(oi - 1) % 2]], is_out=is_final)


def emit_ln(P, x, bx, ob, bo, gbc, b_gbc, bbc, b_bbc, stats, b_stats, sm, b_sm):
    mv = sm[:, 40:42]
    rstd = sm[:, 42:43]
    nmr = sm[:, 43:44]
    P.op("dve", lambda e: e.bn_stats(stats[:, 0, :], x[:, 0:512]), reads=[bx], writes=[b_stats])
    P.op("dve", lambda e: e.bn_stats(stats[:, 1, :], x[:, 512:1024]), reads=[bx], writes=[b_stats])
    P.op("dve", lambda e: e.bn_aggr(mv, stats[:]), reads=[b_stats], writes=[b_sm])
    P.op("dve", lambda e: e.tensor_scalar(rstd, sm[:, 41:42], LN_EPS, None, ALU.add), reads=[b_sm], writes=[b_sm])
    P.op("act", lambda e: e.sqrt(rstd, rstd), reads=[b_sm], writes=[b_sm])
    P.op("dve", lambda e: e.reciprocal(rstd, rstd), reads=[b_sm], writes=[b_sm])
    P.op("dve", lambda e: e.scalar_tensor_tensor(nmr, sm[:, 40:41], -1.0, rstd, ALU.mult, ALU.mult), reads=[b_sm], writes=[b_sm])
    P.op("act", lambda e: e.activation(ob[:], x, AF.Identity, bias=nmr, scale=rstd), reads=[bx, b_sm], writes=[bo])
    P.op("dve", lambda e: e.tensor_tensor(ob[:], ob[:], gbc[:], ALU.mult), reads=[bo, b_gbc], writes=[bo])
    P.op("dve", lambda e: e.tensor_tensor(ob[:], ob[:], bbc[:], ALU.add), reads=[bo, b_bbc], writes=[bo])


def emit_router(P, lg, b_lg, sm, b_sm, gates, b_g, tt):
    gmax, ngmax, gsum, gw = sm[:, 0:1], sm[:, 1:2], sm[:, 2:3], sm[:, 3:4]
    ohg, gexp, esel, oh1 = sm[:, 4:8], sm[:, 8:12], sm[:, 12:16], sm[:, 16:20]
    em, oh2 = sm[:, 20:24], sm[:, 24:28]
    m1, m2, dd, ed, den, w1, w2 = (sm[:, 28 + i:29 + i] for i in range(7))
    ew = sm[:, 36:40]
    R = [b_lg, b_sm]
    W = [b_sm]

    def dv(fn, reads=R, writes=W):
        P.op("dve", fn, reads=reads, writes=writes)

    dv(lambda e: e.tensor_reduce(gmax, lg[:, 0:4], AX.X, ALU.max))
    dv(lambda e: e.tensor_scalar(ohg, lg[:, 0:4], gmax, None, ALU.is_equal))
    dv(lambda e: e.tensor_scalar(ngmax, gmax, -1.0, None, ALU.mult))
    P.op("act", lambda e: e.activation(gexp, lg[:, 0:4], AF.Exp, bias=ngmax, scale=1.0), reads=R, writes=W)
    dv(lambda e: e.tensor_reduce(gsum, gexp, AX.X, ALU.add))
    dv(lambda e: e.reciprocal(gw, gsum))
    dv(lambda e: e.tensor_scalar(esel, lg[:, 4:8], ohg[:, 0:1], None, ALU.mult))
    for g in range(1, 4):
        dv(lambda e, g=g: e.scalar_tensor_tensor(esel, lg[:, 4 + 4 * g:8 + 4 * g], ohg[:, g:g + 1], esel, ALU.mult, ALU.add))
    dv(lambda e: e.tensor_reduce(m1, esel, AX.X, ALU.max))
    dv(lambda e: e.tensor_scalar(oh1, esel, m1, None, ALU.is_equal))
    dv(lambda e: e.scalar_tensor_tensor(em, oh1, -1e30, esel, ALU.mult, ALU.add))
    dv(lambda e: e.tensor_reduce(m2, em, AX.X, ALU.max))
    dv(lambda e: e.tensor_scalar(oh2, em, m2, None, ALU.is_equal))
    dv(lambda e: e.tensor_tensor(dd, m2, m1, ALU.subtract))
    P.op("act", lambda e: e.activation(ed, dd, AF.Exp), reads=R, writes=W)
    dv(lambda e: e.tensor_scalar(den, ed, 1.0, None, ALU.add))
    dv(lambda e: e.reciprocal(den, den))
    dv(lambda e: e.tensor_tensor(w1, den, gw, ALU.mult))
    dv(lambda e: e.tensor_tensor(w2, w1, ed, ALU.mult))
    dv(lambda e: e.tensor_scalar(ew, oh1, w1, None, ALU.mult))
    dv(lambda e: e.scalar_tensor_tensor(ew, oh2, w2, ew, ALU.mult, ALU.add))
    for g in range(4):
        dv(lambda e, g=g: e.tensor_scalar(gates[:, tt, 4 * g:4 * g + 4], ew, ohg[:, g:g + 1], None, ALU.mult),
           reads=[b_sm], writes=[b_g])


def t5_bucket_np(dist):
    n = np.maximum(dist, 0)
    nf = np.maximum(n, 1).astype(np.float32)
    large = 16 + (np.log(nf / np.float32(16)) / np.float32(np.log(2048 / 16)) * np.float32(16)).astype(np.int32)
    return np.where(n < 16, n, np.minimum(large, 31))


def dil_onehot():
    oh = np.zeros((33, 3, 384), np.float32)
    for di, d in enumerate((1, 4, 16)):
        for jp in range(384):
            j = jp - 127
            if 0 <= j <= 128:
                oh[t5_bucket_np(np.array(j * d)), di, jp] = 1.0
            else:
                oh[32, di, jp] = 1.0
    return oh


class Banks:
    def __init__(self, P):
        self.f = [P.ps(f"bank{i}", [128, 512], F32) for i in range(7)]
        self.bf = [P.buf() for _ in range(7)]
        self.h = P.ps("bankh", [128, 1024], BF16)
        self.bh = P.buf()


def full_barrier(P):
    for q in ("sp", "pool"):
        eq = P.eng[q]
        for key, (k_, sh, val) in list(P.dmasems.items()):
            if P.issuer.get(key) == q and P.known[q].get(key, 0) < val:
                eq.wait_ge(sh, val)
                P.known[q][key] = val
        eq.sem_inc(P.sem[q], 1)
        P.cnt[q] += 1
    for e in P.eng:
        for f in P.eng:
            if f != e and P.cnt[f] > P.known[e].get(f, 0):
                P.eng[e].wait_ge(P.sem[f], P.cnt[f])
                P.known[e][f] = P.cnt[f]
    for e in P.eng:
        for (key, sh, val) in P.dmasems.values():
            P.known[e][key] = max(P.known[e].get(key, 0), val)


class Stager:
    def __init__(self, P, n=3, cols=2048):
        self.P = P
        self.cols = cols
        self.t = [P.sb(f"stg{i}", [128, cols], F32) for i in range(n)]
        self.b = [P.buf() for _ in range(n)]
        self.i = 0

    def load(self, dst3, bdst, src2d, eng="act"):
        C, W = dst3.shape[1], dst3.shape[2]
        per = max(1, self.cols // W)
        c0 = 0
        while c0 < C:
            c1 = min(C, c0 + per)
            k = self.i % len(self.t)
            self.i += 1
            st = self.t[k][:, 0:(c1 - c0) * W].rearrange("p (c w) -> p c w", w=W)
            self.P.dma("sp", st, src2d[c0 * 128:c1 * 128, :].rearrange("(c p) w -> p c w", p=128), writes=[self.b[k]])
            self.P.op(eng, lambda en, st=st, c0=c0, c1=c1: en.copy(dst3[:, c0:c1, :], st),
                      reads=[self.b[k]], writes=[bdst])
            c0 = c1


def load_xT(P, BK, x, bx_dram, xin, b_xin, xT, b_xT, ident, b_ident, t0, ntile, ctr):
    for tl in range(ntile):
        k = ctr[0] % 2
        ctr[0] += 1
        xb, bx = xin[k], b_xin[k]
        P.dma("sp", xb[:], x(t0 + tl * 128), reads=[bx_dram], writes=[bx])
        for half in range(2):
            pt, bpt = BK.f[half], BK.bf[half]
            for j in range(4):
                dc = half * 4 + j
                P.op("pe", lambda e, pt=pt, j=j, dc=dc, xb=xb: e.transpose(
                    pt[:, j * 128:(j + 1) * 128], xb[:, dc * 128:(dc + 1) * 128], ident[:]),
                    reads=[bx, b_ident], writes=[bpt])
            eng = "act" if half == 0 else "dve"
            if eng == "act":
                P.op("act", lambda e, pt=pt, half=half, tl=tl: e.copy(
                    xT[:, half * 4:(half + 1) * 4, tl * 128:(tl + 1) * 128], pt[:].rearrange("p (c t) -> p c t", c=4)),
                    reads=[bpt], writes=[b_xT[tl]])
            else:
                P.op("dve", lambda e, pt=pt, half=half, tl=tl: e.tensor_copy(
                    xT[:, half * 4:(half + 1) * 4, tl * 128:(tl + 1) * 128], pt[:].rearrange("p (c t) -> p c t", c=4)),
                    reads=[bpt], writes=[b_xT[tl]])


S_TOT = 8192
NBLK = 4
BLK = 2048


def declare_mixer0(nc, pfx=""):
    din = lambda n, s: nc.dram_tensor(pfx + n, s, F32, kind="ExternalInput").ap()
    return dict(x=din("x", [S_TOT, D]), wA=din("wA", [D, 768]), wB=din("wB", [D, 1028]), gbias=din("gbias", [1, 4]),
                convw=din("convw", [128, 16]), hng=din("hng", [1, 256]), relb=din("relb", [33, 4]), oh=din("oh", [33, 3 * 384]),
                triu=din("triu", [128, 128]),
                scr=nc.dram_tensor(pfx + "scr_e", [128, 12 * 384], F32).ap())


def emit_mixer0(P, io):
    nc = P.nc
    x_d, wA, wB, gbias, convw, hng, relb = io["x"], io["wA"], io["wB"], io["gbias"], io["convw"], io["hng"], io["relb"]
    oh_d, ident_d, triu_d, scr = io["oh"], io["ident"], io["triu"], io["scr"]
    x = lambda t0: x_d[t0:t0 + 128, :]
    ystore = io["ystore"]
    if True:
        B = P.buf
        BK = Banks(P)
        b_x, b_y, b_scr = B(), B(), B()
        ident = P.sb("ident_sb", [128, 128], F32)
        b_ident = B()
        P.dma("sp", ident[:], ident_d[:, :], writes=[b_ident])
        xin = [P.sb(f"xin{i}", [128, D], F32) for i in range(2)]
        b_xin = [B(), B()]
        xT = P.sb("xT", [128, 8, BLK], BF16)
        b_xT = [B() for _ in range(16)]
        stg = Stager(P)
        ctr = [0]
        ones_bf = P.sb("ones_bf", [128, 128], BF16)
        b_ones = B()
        P.op("dve", lambda e: e.memset(ones_bf[:], 1.0), writes=[b_ones])

        with ExitStack() as esA:
            sbA = lambda n, s, d: esA.enter_context(nc.sbuf_tensor(P.pfx + n, s, d))
            Etab = sbA("a_E", [128, 12, 2, 128], BF16)
            with ExitStack() as esT:
                sbA_outer = sbA
                sbA = lambda n, s, d: esT.enter_context(nc.sbuf_tensor(P.pfx + n, s, d))
                tab = sbA("a_tab", [33, 4], F32)
                ohs = sbA("a_oh", [33, 3 * 384], F32)
                tabB = sbA("a_tabB", [33, 128], F32)
                fre = sbA("a_fre", [128, 12 * 384], F32)
                E32 = sbA("a_E32", [128, 12 * 256], F32)
                b_tab, b_oh, b_tabB, b_fre, b_E32, b_E = B(), B(), B(), B(), B(), B()
                P.dma("sp", tab[:], relb[:, :], writes=[b_tab])
                P.dma("sp", ohs[:], oh_d[:, :], writes=[b_oh])
                for h in range(4):
                    P.op("dve", lambda e, h=h: e.tensor_copy(tabB[:], tab[:, h:h + 1].to_broadcast([33, 128])),
                         reads=[b_tab], writes=[b_tabB])
                    for di in range(3):
                        pb, bpb = BK.f[2 + (di % 2)], BK.bf[2 + (di % 2)]
                        P.op("pe", lambda e, pb=pb, di=di: e.matmul(pb[:, 0:384], tabB[:], ohs[:, di * 384:(di + 1) * 384],
                                                                    start=True, stop=True),
                             reads=[b_tabB, b_oh], writes=[bpb])
                        P.op("act", lambda e, pb=pb, di=di, h=h: e.activation(
                            fre[:, (di * 4 + h) * 384:(di * 4 + h + 1) * 384], pb[:, 0:384], AF.Exp),
                            reads=[bpb], writes=[b_fre])
                P.dma("sp", scr[:, :], fre[:], reads=[b_fre], writes=[b_scr])
                L = 12 * 384
                skew = bass.AP(scr.tensor, 127, [[L - 1, 128], [384, 12], [128, 2], [1, 128]])
                P.dma("sp", E32[:].rearrange("p (t s q) -> p t s q", t=12, s=2), skew, reads=[b_scr], writes=[b_E32])
                P.op("dve", lambda e: e.tensor_copy(Etab[:].rearrange("p t s q -> p (t s q)"), E32[:]),
                     reads=[b_E32], writes=[b_E])

                full_barrier(P)
            sbA = sbA_outer
            wAb = sbA("a_w", [128, 8, 384], BF16)
            b_wA = B()
            QT = sbA("a_QT", [128, S_TOT], BF16)
            KT = sbA("a_KT", [128, S_TOT], BF16)
            b_QT = [B() for _ in range(NBLK)]
            b_KT = [B() for _ in range(NBLK)]
            Vd = [sbA(f"a_V{di}", [128, 64, 128], BF16) for di in range(3)]
            b_Vd = [[B() for _ in range(NBLK)] for _ in range(3)]
            numacc = sbA("a_num", [128, BLK], F32)
            denacc = sbA("a_den", [128, BLK], F32)
            b_num, b_den = B(), B()
            Pb = [sbA(f"a_P{i}", [128, 2, 2, 128], BF16) for i in range(2)]
            b_Pb = [B(), B()]
            ybuf = sbA("a_y", [128, BLK], BF16)
            b_yb = B()
            pcount = 0
            for hp in range(2):
                stg.load(wAb[:], b_wA, wA[:, hp * 384:(hp + 1) * 384])
                for blk in range(NBLK):
                    T0 = blk * BLK
                    load_xT(P, BK, x, b_x, xin, b_xin, xT, b_xT, ident, b_ident, T0, 16, ctr)
                    for qtr in range(4):
                        rds = [b_xT[qtr * 4 + i] for i in range(4)] + [b_wA]
                        for sel, (dst, bd) in enumerate(((QT, b_QT[blk]), (KT, b_KT[blk]))):
                            pb, bpb = BK.f[2 + sel], BK.bf[2 + sel]
                            for dc in range(8):
                                P.op("pe", lambda e, pb=pb, dc=dc, sel=sel, qtr=qtr: e.matmul(
                                    pb[:], wAb[:, dc, sel * 128:(sel + 1) * 128], xT[:, dc, qtr * 512:(qtr + 1) * 512],
                                    start=(dc == 0), stop=(dc == 7)), reads=rds, writes=[bpb])
                            if sel == 0:
                                P.op("act", lambda e, pb=pb, dst=dst, T0=T0, qtr=qtr: e.copy(
                                    dst[:, T0 + qtr * 512:T0 + (qtr + 1) * 512], pb[:]), reads=[bpb], writes=[bd])
                            else:
                                P.op("dve", lambda e, pb=pb, dst=dst, T0=T0, qtr=qtr: e.tensor_copy(
                                    dst[:, T0 + qtr * 512:T0 + (qtr + 1) * 512], pb[:]), reads=[bpb], writes=[bd])
                    for di, d in enumerate((1, 4, 16)):
                        npb = 16 // d
                        tiles = [(nl, r) for nl in range(npb) for r in range(d)]
                        for g4 in range(4):
                            pb, bpb = BK.f[2 + (g4 % 2)], BK.bf[2 + (g4 % 2)]
                            for j in range(4):
                                nl, r = tiles[g4 * 4 + j]
                                s0 = r + d * 128 * nl
                                for dc in range(8):
                                    P.op("pe", lambda e, pb=pb, j=j, dc=dc, s0=s0, d=d: e.matmul(
                                        pb[:, j * 128:(j + 1) * 128], xT[:, dc, s0:s0 + 127 * d + 1:d], wAb[:, dc, 256:384],
                                        start=(dc == 0), stop=(dc == 7)), reads=b_xT + [b_wA], writes=[bpb])
                            nl0, r0 = tiles[g4 * 4]
                            idx0 = (blk * npb + nl0) * d + r0
                            P.op("act" if g4 % 2 == 0 else "dve",
                                 (lambda e, pb=pb, di=di, idx0=idx0: e.copy(
                                     Vd[di][:, idx0:idx0 + 4, :], pb[:].rearrange("p (j c) -> p j c", j=4)))
                                 if g4 % 2 == 0 else
                                 (lambda e, pb=pb, di=di, idx0=idx0: e.tensor_copy(
                                     Vd[di][:, idx0:idx0 + 4, :], pb[:].rearrange("p (j c) -> p j c", j=4))),
                                 reads=[bpb], writes=[b_Vd[di][blk]])
                    for di, d in enumerate((1, 4, 16)):
                        npb = 16 // d
                        for nl in range(npb):
                            n = blk * npb + nl
                            nslot = 2 if n > 0 else 1
                            for r in range(d):
                                qs = T0 + r + d * 128 * nl
                                ql = r + d * 128 * nl
                                par = pcount % 2
                                ps_h = [BK.f[2 + 2 * par + h] for h in range(2)]
                                bps_h = [BK.bf[2 + 2 * par + h] for h in range(2)]
                                pb_, bpb_ = Pb[pcount % 2], b_Pb[pcount % 2]
                                pcount += 1
                                kblks = sorted({blk, (qs - d * 128) // BLK}) if n > 0 else [blk]
                                rds = [b_QT[blk]] + [b_KT[kb] for kb in kblks]
                                for h in range(2):
                                    for sl in range(nslot):
                                        ks = qs - sl * d * 128
                                        P.op("pe", lambda e, h=h, sl=sl, ks=ks, qs=qs, d=d, ps_h=ps_h: e.matmul(
                                            ps_h[h][:, sl * 128:(sl + 1) * 128],
                                            KT[h * 64:(h + 1) * 64, ks:ks + 127 * d + 1:d],
                                            QT[h * 64:(h + 1) * 64, qs:qs + 127 * d + 1:d], start=True, stop=True),
                                            reads=rds, writes=[bps_h[h]])
                                for h in range(2):
                                    P.op("act", lambda e, pb_=pb_, h=h, nslot=nslot, ps_h=ps_h: e.activation(
                                        pb_[:, h, 0:nslot, :], ps_h[h][:, 0:nslot * 128].rearrange("p (s q) -> p s q", s=nslot),
                                        AF.Exp, scale=0.125), reads=[bps_h[h]], writes=[bpb_])
                                P.op("dve", lambda e, pb_=pb_, di=di, hp=hp, nslot=nslot: e.tensor_tensor(
                                    pb_[:, :, 0:nslot, :], pb_[:, :, 0:nslot, :],
                                    Etab[:, di * 4 + hp * 2:di * 4 + hp * 2 + 2, 0:nslot, :], ALU.mult),
                                    reads=[bpb_, b_E], writes=[bpb_])
                                pv, bpv = BK.f[6], BK.bf[6]
                                vrd = [b_Vd[di][kb] for kb in kblks]
                                for h in range(2):
                                    for sl in range(nslot):
                                        kidx = (n - sl) * d + r
                                        P.op("pe", lambda e, pv=pv, h=h, sl=sl, kidx=kidx, di=di, pb_=pb_, nslot=nslot: e.matmul(
                                            pv[:, h * 128:(h + 1) * 128], Vd[di][:, kidx, :], pb_[:, h, sl, :],
                                            start=(sl == 0), stop=(sl == nslot - 1)), reads=vrd + [bpb_], writes=[bpv])
                                    for sl in range(nslot):
                                        P.op("pe", lambda e, pv=pv, h=h, sl=sl, pb_=pb_, nslot=nslot: e.matmul(
                                            pv[:, (2 + h) * 128:(3 + h) * 128], ones_bf[:], pb_[:, h, sl, :],
                                            start=(sl == 0), stop=(sl == nslot - 1)), reads=[b_ones, bpb_], writes=[bpv])
                                for h in range(2):
                                    pr = slice(h * 64, (h + 1) * 64)
                                    qc = slice(ql, ql + 127 * d + 1, d)
                                    if di == 0:
                                        P.op("dve", lambda e, pr=pr, qc=qc, h=h: e.tensor_copy(
                                            numacc[pr, qc], pv[pr, h * 128:(h + 1) * 128]), reads=[bpv], writes=[b_num])
                                        P.op("dve", lambda e, pr=pr, qc=qc, h=h: e.tensor_copy(
                                            denacc[pr, qc], pv[pr, (2 + h) * 128:(3 + h) * 128]), reads=[bpv], writes=[b_den])
                                    else:
                                        P.op("dve", lambda e, pr=pr, qc=qc, h=h: e.tensor_tensor(
                                            numacc[pr, qc], numacc[pr, qc], pv[pr, h * 128:(h + 1) * 128], ALU.add),
                                            reads=[bpv, b_num], writes=[b_num])
                                        P.op("dve", lambda e, pr=pr, qc=qc, h=h: e.tensor_tensor(
                                            denacc[pr, qc], denacc[pr, qc], pv[pr, (2 + h) * 128:(3 + h) * 128], ALU.add),
                                            reads=[bpv, b_den], writes=[b_den])
                    P.op("dve", lambda e: e.reciprocal(denacc[:], denacc[:]), reads=[b_den], writes=[b_den])
                    P.op("dve", lambda e: e.tensor_tensor(ybuf[:], numacc[:], denacc[:], ALU.mult),
                         reads=[b_num, b_den], writes=[b_yb])
                    ystore(P, hp, blk, ybuf[:], [b_yb])
            full_barrier(P)

        with ExitStack() as esB:
            sbB = lambda n, s, d: esB.enter_context(nc.sbuf_tensor(P.pfx + n, s, d))
            SC = 128.0 ** -0.5
            wBb = sbB("b_w", [128, 8, 1028], BF16)
            b_wB = B()
            stg.load(wBb[:], b_wB, wB[:, :])
            pc = sbB("b_pc", [128, 4, BLK + 3], F32)
            b_pc = [B() for _ in range(4)]
            ctmp = [sbB(f"b_ct{i}", [128, BLK], F32) for i in range(2)]
            b_ct = [B(), B()]
            QKb = sbB("b_qk", [128, 4, BLK], BF16)
            b_qk = [B() for _ in range(4)]
            cw = sbB("b_cw", [128, 16], F32)
            hngb = sbB("b_hng", [128, 256], F32)
            gbb = sbB("b_gb", [128, 4], F32)
            triu32 = sbB("b_triu32", [128, 128], F32)
            triub = sbB("b_triub", [128, 128], BF16)
            ones32 = sbB("b_ones32", [128, 128], F32)
            identb = sbB("b_identb", [128, 128], BF16)
            b_cst = B()
            P.dma("sp", cw[:], convw[:, :], writes=[b_cst])
            b_c2, b_c3, b_c4 = B(), B(), B()
            P.dma("sp", hngb[:], bcast_rows(hng.tensor, hng.offset, 256), writes=[b_c2])
            P.dma("sp", gbb[:], bcast_rows(gbias.tensor, gbias.offset, 4), writes=[b_c3])
            P.dma("sp", triu32[:], triu_d[:, :], writes=[b_c4])
            b_c5 = B()
            P.op("dve", lambda e: e.tensor_copy(triub[:], triu32[:]), reads=[b_c4], writes=[b_c5])
            P.op("dve", lambda e: e.memset(ones32[:], 1.0), writes=[b_c5])
            P.op("dve", lambda e: e.tensor_copy(identb[:], ident[:]), reads=[b_ident], writes=[b_c5])
            CST = [b_cst, b_c2, b_c3, b_c4, b_c5]
            Vaug = [sbB(f"b_va{i}", [128, 2, 129], BF16) for i in range(2)]
            b_va = [B(), B()]
            sgo = [sbB(f"b_sgo{i}", [128, 256], F32) for i in range(2)]
            b_sgo = [B(), B()]
            smB = [sbB(f"b_sm{i}", [128, 64], F32) for i in range(2)]
            b_smB = [B(), B()]
            Sm = [sbB(f"b_Sm{i}", [128, 128], BF16) for i in range(2)]
            b_Sm = [B(), B()]
            hh = [sbB(f"b_hh{i}", [128, 128], F32) for i in range(2)]
            b_hh = [B(), B()]
            yn32 = [sbB(f"b_yn{i}", [128, 128], F32) for i in range(2)]
            b_yn = [B(), B()]
            ynb = [sbB(f"b_ynb{i}", [128, 128], BF16) for i in range(2)]
            b_ynb = [B(), B()]
            Ktm = [sbB(f"b_ktm{i}", [128, 128], BF16) for i in range(2)]
            b_ktm = [B(), B()]
            Vw = [sbB(f"b_vw{i}", [128, 129], BF16) for i in range(2)]
            b_vw = [B(), B()]
            C32 = sbB("b_C32", [128, 2, 129], F32)
            Cb = sbB("b_Cb", [128, 2, 129], BF16)
            b_C32, b_Cb = [B(), B()], [B(), B()]
            hst = [sbB(f"b_hst{i}", [128, 16], F32) for i in range(2)]
            b_hst = [B(), B()]
            ybufB = sbB("b_yb", [128, 2, BLK], BF16)
            b_ybB = B()
            P.op("dve", lambda e: e.memset(C32[:], 0.0), writes=b_C32)
            P.op("dve", lambda e: e.memset(Cb[:], 0.0), writes=b_Cb)
            for k in range(2):
                P.op("dve", lambda e, k=k: e.memset(Vaug[k][:, :, 128:129], 1.0), writes=[b_va[k]])
            for g in range(4):
                P.op("dve", lambda e, g=g: e.memset(pc[:, g, 0:3], 0.0), writes=[b_pc[g]])
            f0, f1, f2, f3, f4, f5, f6 = BK.f
            bf0, bf1, bf2, bf3, bf4, bf5, bf6 = BK.bf
            cti = 0
            for blk in range(NBLK):
                T0 = blk * BLK
                load_xT(P, BK, x, b_x, xin, b_xin, xT, b_xT, ident, b_ident, T0, 16, ctr)
                for g in range(4):
                    for qtr in range(4):
                        pb, bpb = (f0, bf0) if g % 2 == 0 else (f1, bf1)
                        rds = [b_xT[qtr * 4 + i] for i in range(4)] + [b_wB]
                        for dc in range(8):
                            P.op("pe", lambda e, pb=pb, dc=dc, g=g, qtr=qtr: e.matmul(
                                pb[:], wBb[:, dc, g * 128:(g + 1) * 128], xT[:, dc, qtr * 512:(qtr + 1) * 512],
                                start=(dc == 0), stop=(dc == 7)), reads=rds, writes=[bpb])
                        if g % 2 == 0:
                            P.op("act", lambda e, pb=pb, g=g, qtr=qtr: e.copy(pc[:, g, 3 + qtr * 512:3 + (qtr + 1) * 512], pb[:]),
                                 reads=[bpb], writes=[b_pc[g]])
                        else:
                            P.op("dve", lambda e, pb=pb, g=g, qtr=qtr: e.tensor_copy(pc[:, g, 3 + qtr * 512:3 + (qtr + 1) * 512], pb[:]),
                                 reads=[bpb], writes=[b_pc[g]])
                for g in range(4):
                    ct, bct = ctmp[cti % 2], b_ct[cti % 2]
                    cti += 1
                    P.op("dve", lambda e, ct=ct, g=g: e.tensor_scalar(ct[:], pc[:, g, 3:3 + BLK], cw[:, g * 4 + 3:g * 4 + 4], None, ALU.mult),
                         reads=[b_pc[g], b_cst], writes=[bct])
                    for j in (2, 1, 0):
                        P.op("dve", lambda e, ct=ct, g=g, j=j: e.scalar_tensor_tensor(
                            ct[:], pc[:, g, j:j + BLK], cw[:, g * 4 + j:g * 4 + j + 1], ct[:], ALU.mult, ALU.add),
                            reads=[b_pc[g], b_cst, bct], writes=[bct])
                    P.op("act", lambda e, ct=ct, g=g: e.activation(QKb[:, g, :], ct[:], AF.Silu), reads=[bct], writes=[b_qk[g]])
                    P.op("dve", lambda e, g=g: e.tensor_copy(pc[:, g, 0:3], pc[:, g, BLK:BLK + 3]), reads=[b_pc[g]], writes=[b_pc[g]])
                for ch in range(16):
                    k = ch % 2
                    tsl = slice(ch * 128, (ch + 1) * 128)
                    va, bva, sg_, bsg, sm_, bsm = Vaug[k], b_va[k], sgo[k], b_sgo[k], smB[k], b_smB[k]
                    gt, e1, sp_, cs = sm_[:, 0:4], sm_[:, 4:6], sm_[:, 6:8], sm_[:, 8:12]
                    ti, ws, wsq, eb, ebL = sm_[:, 12:14], sm_[:, 14:16], sm_[:, 16:18], sm_[:, 18:20], sm_[:, 20:22]
                    rdx = [b_xT[ch], b_wB]
                    for (bank, bbank, c0, c1, w0) in ((f2, bf2, 0, 256, 512), (f2, bf2, 256, 260, 1024), (f3, bf3, 0, 256, 768)):
                        for dc in range(8):
                            P.op("pe", lambda e, bank=bank, c0=c0, c1=c1, w0=w0, dc=dc, tsl=tsl: e.matmul(
                                bank[:, c0:c1], xT[:, dc, tsl], wBb[:, dc, w0:w0 + (c1 - c0)],
                                start=(dc == 0), stop=(dc == 7)), reads=rdx, writes=[bbank])
                    P.op("dve", lambda e, va=va: e.tensor_copy(va[:, :, 0:128], f2[:, 0:256].rearrange("p (h c) -> p h c", h=2)),
                         reads=[bf2], writes=[bva])
                    P.op("dve", lambda e, gt=gt: e.tensor_tensor(gt, f2[:, 256:260], gbb[:], ALU.add), reads=[bf2] + CST, writes=[bsm])
                    P.op("act", lambda e, sg_=sg_: e.activation(sg_[:], f3[:, 0:256], AF.Sigmoid), reads=[bf3], writes=[bsg])
                    P.op("act", lambda e, e1=e1, gt=gt: e.activation(e1, gt[:, 2:4], AF.Exp, scale=-1.0), reads=[bsm], writes=[bsm])
                    P.op("act", lambda e, e1=e1, sp_=sp_: e.activation(sp_, e1, AF.Ln, bias=1.0), reads=[bsm], writes=[bsm])
                    P.op("pe", lambda e, sp_=sp_: e.matmul(f3[:, 256:258], triu32[:], sp_, start=True, stop=True), reads=[bsm] + CST, writes=[bf3])
                    P.op("pe", lambda e, sp_=sp_: e.matmul(f3[:, 258:260], ones32[:], sp_, start=True, stop=True), reads=[bsm] + CST, writes=[bf3])
                    P.op("act", lambda e, cs=cs: e.copy(cs, f3[:, 256:260]), reads=[bf3], writes=[bsm])
                    P.op("dve", lambda e, ti=ti, gt=gt, cs=cs: e.tensor_tensor(ti, gt[:, 0:2], cs[:, 0:2], ALU.add), reads=[bsm], writes=[bsm])
                    P.op("act", lambda e, ws=ws, ti=ti: e.activation(ws, ti, AF.Exp), reads=[bsm], writes=[bsm])
                    P.op("act", lambda e, eb=eb, cs=cs: e.activation(eb, cs[:, 0:2], AF.Exp), reads=[bsm], writes=[bsm])
                    P.op("act", lambda e, ebL=ebL, cs=cs: e.activation(ebL, cs[:, 2:4], AF.Exp, scale=-1.0), reads=[bsm], writes=[bsm])
                    P.op("dve", lambda e, wsq=wsq, ws=ws: e.tensor_scalar(wsq, ws, SC, None, ALU.mult), reads=[bsm], writes=[bsm])
                    for h in range(2):
                        st_, bst = hst[h], b_hst[h]
                        P.op("pe", lambda e, h=h, tsl=tsl: e.matmul(f4[:, h * 128:(h + 1) * 128], QKb[:, 2 + h, tsl], QKb[:, h, tsl],
                                                                     start=True, stop=True), reads=[b_qk[h], b_qk[2 + h]], writes=[bf4])
                        P.op("dve", lambda e, h=h, wsq=wsq: e.scalar_tensor_tensor(
                            Sm[h][:], f4[:, h * 128:(h + 1) * 128], wsq[:, h:h + 1], triub[:], ALU.mult, ALU.mult),
                            reads=[bf4, bsm] + CST, writes=[b_Sm[h]])
                        xs_ = f5[:, h * 256:h * 256 + 129]
                        P.op("pe", lambda e, h=h, xs_=xs_, va=va: e.matmul(xs_, Sm[h][:], va[:, h, :], start=True, stop=False),
                             reads=[b_Sm[h], bva], writes=[bf5])
                        P.op("pe", lambda e, h=h, xs_=xs_, tsl=tsl: e.matmul(xs_, QKb[:, h, tsl], Cb[:, h, :], start=False, stop=True),
                             reads=[b_qk[h], b_Cb[h]], writes=[bf5])
                        ax, r_, mv, rstd, nmr = st_[:, 0:1], st_[:, 1:2], st_[:, 2:4], st_[:, 4:5], st_[:, 5:6]
                        stat6 = st_[:, 8:14]
                        P.op("dve", lambda e, ax=ax, h=h: e.tensor_scalar(ax, f5[:, h * 256 + 128:h * 256 + 129], -1.0, None, ALU.mult),
                             reads=[bf5], writes=[bst])
                        P.op("dve", lambda e, ax=ax, h=h: e.tensor_tensor(ax, ax, f5[:, h * 256 + 128:h * 256 + 129], ALU.max),
                             reads=[bf5, bst], writes=[bst])
                        P.op("dve", lambda e, ax=ax, eb=eb, h=h: e.tensor_tensor(ax, ax, eb[:, h:h + 1], ALU.max), reads=[bst, bsm], writes=[bst])
                        P.op("dve", lambda e, ax=ax, r_=r_: e.reciprocal(r_, ax), reads=[bst], writes=[bst])
                        P.op("dve", lambda e, h=h, r_=r_: e.tensor_scalar(hh[h][:], f5[:, h * 256:h * 256 + 128], r_, None, ALU.mult),
                             reads=[bf5, bst], writes=[b_hh[h]])
                        P.op("dve", lambda e, h=h, stat6=stat6: e.bn_stats(stat6, hh[h][:]), reads=[b_hh[h]], writes=[bst])
                        P.op("dve", lambda e, mv=mv, stat6=stat6: e.bn_aggr(mv, stat6), reads=[bst], writes=[bst])
                        P.op("dve", lambda e, rstd=rstd, mv=mv: e.tensor_scalar(rstd, mv[:, 1:2], LN_EPS, None, ALU.add), reads=[bst], writes=[bst])
                        P.op("act", lambda e, rstd=rstd: e.sqrt(rstd, rstd), reads=[bst], writes=[bst])
                        P.op("dve", lambda e, rstd=rstd: e.reciprocal(rstd, rstd), reads=[bst], writes=[bst])
                        P.op("dve", lambda e, nmr=nmr, mv=mv, rstd=rstd: e.scalar_tensor_tensor(nmr, mv[:, 0:1], -1.0, rstd, ALU.mult, ALU.mult),
                             reads=[bst], writes=[bst])
                        P.op("act", lambda e, h=h, nmr=nmr, rstd=rstd: e.activation(yn32[h][:], hh[h][:], AF.Identity, bias=nmr, scale=rstd),
                             reads=[b_hh[h], bst], writes=[b_yn[h]])
                        P.op("dve", lambda e, h=h: e.tensor_tensor(yn32[h][:], yn32[h][:], hngb[:, h * 128:(h + 1) * 128], ALU.mult),
                             reads=[b_yn[h]] + CST, writes=[b_yn[h]])
                        P.op("dve", lambda e, h=h, sg_=sg_: e.tensor_tensor(ynb[h][:], yn32[h][:], sg_[:, h * 128:(h + 1) * 128], ALU.mult),
                             reads=[b_yn[h], bsg], writes=[b_ynb[h]])
                        P.op("pe", lambda e, h=h: e.transpose(BK.h[:, h * 256:h * 256 + 128], ynb[h][:], identb[:]),
                             reads=[b_ynb[h]] + CST, writes=[BK.bh])
                        P.op("act", lambda e, h=h, tsl=tsl: e.copy(ybufB[:, h, tsl], BK.h[:, h * 256:h * 256 + 128]),
                             reads=[BK.bh], writes=[b_ybB])
                        P.op("pe", lambda e, h=h, tsl=tsl: e.transpose(BK.h[:, h * 256 + 128:h * 256 + 256], QKb[:, 2 + h, tsl], identb[:]),
                             reads=[b_qk[2 + h]] + CST, writes=[BK.bh])
                        P.op("act", lambda e, h=h: e.copy(Ktm[h][:], BK.h[:, h * 256 + 128:h * 256 + 256]), reads=[BK.bh], writes=[b_ktm[h]])
                        P.op("dve", lambda e, h=h, va=va, ws=ws: e.tensor_scalar(Vw[h][:], va[:, h, :], ws[:, h:h + 1], None, ALU.mult),
                             reads=[bva, bsm], writes=[b_vw[h]])
                        dps = f6[:, h * 256:h * 256 + 129]
                        P.op("pe", lambda e, h=h, dps=dps: e.matmul(dps, Ktm[h][:], Vw[h][:], start=True, stop=True),
                             reads=[b_ktm[h], b_vw[h]], writes=[bf6])
                        P.op("dve", lambda e, h=h, dps=dps: e.tensor_tensor(C32[:, h, :], dps, C32[:, h, :], ALU.add),
                             reads=[bf6, b_C32[h]], writes=[b_C32[h]])
                        P.op("dve", lambda e, h=h, ebL=ebL: e.tensor_scalar(C32[:, h, :], C32[:, h, :], ebL[:, h:h + 1], None, ALU.mult),
                             reads=[b_C32[h], bsm], writes=[b_C32[h]])
                        P.op("act", lambda e, h=h: e.mul(Cb[:, h, :], C32[:, h, :], SC), reads=[b_C32[h]], writes=[b_Cb[h]])
                for h in range(2):
                    ystore(P, 2 + h, blk, ybufB[:, h, :], [b_ybB])
            full_barrier(P)


TOK = 4096


def declare_post(nc, pfx=""):
    din = lambda n, sh: nc.dram_tensor(pfx + n, sh, F32, kind="ExternalInput").ap()
    return dict(wo=din("wo", [D, D]), ln1g=din("ln1g", [D]), ln1b=din("ln1b", [D]),
                w=dict(wg=din("wg", [16, D, EH]), wu=din("wu", [16, D, EH]), wd=din("wd", [16, EH, D]), wr=din("wr", [D, 20]),
                       br=din("br", [1, 20]), ln_g=din("ln2g", [D]), ln_b=din("ln2b", [D])),
                hmid=nc.dram_tensor(pfx + "hmid_scr", [TOK, D], F32).ap())


def emit_post(P, io):
    nc = P.nc
    wo, ln1g, ln1b, w, hmid = io["wo"], io["ln1g"], io["ln1b"], dict(io["w"]), io["hmid"]
    w["ident"] = io["ident"]
    load_yt, xres, out = io["load_yt"], io["xres"], io["out"]
    if True:
        B = P.buf
        banks = [P.ps(f"bank{i}", [128, 512]) for i in range(8)]
        bbank = [B() for _ in range(8)]
        b_hmid, b_out, b_yT = B(), B(), B()
        b_xres = io.get("b_xres") or B()
        b_hmw = [B(), B()]
        with ExitStack() as esF:
            sbF = lambda n, sh, d: esF.enter_context(nc.sbuf_tensor(P.pfx + n, sh, d))
            stg = Stager(P)
            wob = sbF("f_wo", [128, 8, D], BF16)
            b_wo = B()
            stg.load(wob[:], b_wo, wo[:, :])
            g1 = sbF("f_g1", [128, D], F32)
            b1 = sbF("f_b1", [128, D], F32)
            b_g1, b_b1 = B(), B()
            P.dma("sp", g1[:], bcast_rows(ln1g.tensor, ln1g.offset, D), writes=[b_g1])
            P.dma("sp", b1[:], bcast_rows(ln1b.tensor, ln1b.offset, D), writes=[b_b1])
            yts = [sbF(f"f_yt{i}", [128, 8, 512], BF16) for i in range(2)]
            b_yts = [[B() for _ in range(8)] for _ in range(2)]
            xr = [sbF(f"f_xr{i}", [128, D], F32) for i in range(2)]
            b_xr = [B(), B()]
            z = [sbF(f"f_z{i}", [128, D], F32) for i in range(2)]
            b_z = [B(), B()]
            hm = [sbF(f"f_hm{i}", [128, D], F32) for i in range(2)]
            b_hm = [B(), B()]
            stats = sbF("f_stats", [128, 2, 6], F32)
            sm = sbF("f_sm", [128, 64], F32)
            b_stats, b_sm = B(), B()
            ti = 0
            for t4 in range(TOK // 512):
                yt, byt = yts[t4 % 2], b_yts[t4 % 2]
                load_yt(P, yt, byt, t4)
                for tq in range(4):
                    k = ti % 2
                    ti += 1
                    t0 = t4 * 512 + tq * 128
                    P.dma("sp", xr[k][:], xres(t0), reads=[b_xres], writes=[b_xr[k]])
                    for dh in range(2):
                        pb, bpb = banks[(ti * 2 + dh) % 4], bbank[(ti * 2 + dh) % 4]
                        for ec in range(8):
                            P.op("pe", lambda e, pb=pb, ec=ec, yt=yt, tq=tq, dh=dh: e.matmul(
                                pb[:], yt[:, ec, tq * 128:(tq + 1) * 128], wob[:, ec, dh * 512:(dh + 1) * 512],
                                start=(ec == 0), stop=(ec == 7)), reads=[byt[ec], b_wo], writes=[bpb])
                        P.op("dve", lambda e, pb=pb, k=k, dh=dh: e.scalar_tensor_tensor(
                            z[k][:, dh * 512:(dh + 1) * 512], xr[k][:, dh * 512:(dh + 1) * 512], ALPHA, pb[:], ALU.mult, ALU.add),
                            reads=[bpb, b_xr[k]], writes=[b_z[k]])
                    emit_ln(P, z[k][:], b_z[k], hm[k], b_hm[k], g1, b_g1, b1, b_b1, stats, b_stats, sm, b_sm)
                    P.dma("sp", hmid[t0:t0 + 128, :], hm[k][:], reads=[b_hm[k]], writes=[b_hmw[k]])
            full_barrier(P)
        emit_tail(P, hmid, b_hmid, out, b_out, TOK, w, 0, banks=banks, is_final=io.get("is_final", True))
        full_barrier(P)


def pack_m0(inp, b, c):
    w = inp["ab_w_in"][0]
    cols = []
    for hp in range(2):
        h0 = 4 * c + 2 * hp
        for sec in (0, 512, 1024):
            cols.append(w[:, sec + h0 * 64: sec + (h0 + 2) * 64])
    wA = np.concatenate(cols, axis=1)
    hb = [2 * c, 2 * c + 1]
    BQ, BKK, BV, BO, BI, BF = 1536, 2048, 2560, 3072, 3584, 3588
    wB = np.concatenate([w[:, BQ + h * 128: BQ + (h + 1) * 128] for h in hb] + [w[:, BKK + h * 128: BKK + (h + 1) * 128] for h in hb]
                        + [w[:, BV + h * 128: BV + (h + 1) * 128] for h in hb] + [w[:, BO + h * 128: BO + (h + 1) * 128] for h in hb]
                        + [w[:, BI + h: BI + h + 1] for h in hb] + [w[:, BF + h: BF + h + 1] for h in hb], axis=1)
    gb = inp["ab_gate_b"][0]
    gbias = np.array([[gb[hb[0]], gb[hb[1]], gb[4 + hb[0]], gb[4 + hb[1]]]], np.float32)
    cw = inp["ab_conv"][0]
    convw = np.zeros((128, 4, 4), np.float32)
    for g, off in enumerate([hb[0] * 128, hb[1] * 128, 512 + hb[0] * 128, 512 + hb[1] * 128]):
        convw[:, g, :] = cw[:, off:off + 128].T
    hng = np.concatenate([inp["ab_head_norm"][0][h * 128:(h + 1) * 128] for h in hb])[None]
    relb = np.concatenate([inp["rel_bias"][:, 4 * c:4 * c + 4], np.full((1, 4), -30000.0, np.float32)], axis=0)
    return dict(x=np.ascontiguousarray(inp["x"][b]), wA=np.ascontiguousarray(wA), wB=np.ascontiguousarray(wB), gbias=gbias,
                convw=np.ascontiguousarray(convw.reshape(128, 16)), hng=np.ascontiguousarray(hng),
                relb=np.ascontiguousarray(relb), oh=np.ascontiguousarray(dil_onehot().reshape(33, -1)),
                ident=np.eye(128, dtype=np.float32), triu=np.triu(np.ones((128, 128), np.float32)))


L1 = 5632
OFF1 = 2063


def nsa_consts():
    dist = np.arange(L1) - OFF1
    oh = np.zeros((33, L1), np.float32)
    bk = t5_bucket_np(dist)
    for jp in range(L1):
        if dist[jp] >= 0:
            oh[bk[jp], jp] = 1.0
        else:
            oh[32, jp] = 1.0
    n = np.arange(512)
    m = np.arange(128)
    cs, ce = n * 16, n * 16 + 31
    ov = ((cs[:, None] < m[None, :] * 64 + 64) & (ce[:, None] >= m[None, :] * 64)).astype(np.float32)
    ov[511] = 0.0
    ovl = ov.reshape(4, 128, 128).transpose(1, 0, 2)
    mrow = np.broadcast_to(m[None, :].astype(np.float32), (128, 128))
    k = np.arange(128)
    cst = np.stack([mrow, (mrow >= 1).astype(np.float32), (mrow == 0).astype(np.float32),
                    (k[:, None] > k[None, :]).astype(np.float32)], axis=1)
    qhalf = (np.arange(128) >= 64).astype(np.float32)[:, None]
    return dict(oh1=oh, ovl=np.ascontiguousarray(ovl), cst1=np.ascontiguousarray(cst.astype(np.float32)), qhalf=qhalf)


def declare_mixer1(nc, pfx=""):
    din = lambda n, s: nc.dram_tensor(pfx + n, s, F32, kind="ExternalInput").ap()
    return dict(wQ=din("wQ", [D, 512]), wKV=din("wKV", [D, 768]), wG=din("wG", [D, 12]), gb=din("gb", [1, 12]),
                posT=din("posT", [128, 64]), w1=din("w1", [2, 4096, 256]), w2=din("w2", [2, 256, 128]), relb=din("relb", [33, 4]),
                oh1=din("oh1", [33, L1]), ovl=din("ovl", [128, 512]), cst1=din("cst1", [128, 512]), qhalf=din("qhalf", [128, 1]),
                fsc=nc.dram_tensor(pfx + "frep_scr", [128, 4 * L1], BF16).ap())


def emit_mixer1(P, io):
    nc = P.nc
    wQ, wKV, wG, gbd, posT, w1d, w2d, relb = io["wQ"], io["wKV"], io["wG"], io["gb"], io["posT"], io["w1"], io["w2"], io["relb"]
    oh_d, ovl_d, cst_d, qh_d, ident_d, fsc = io["oh1"], io["ovl"], io["cst1"], io["qhalf"], io["ident"], io["fsc"]
    x, ostore = io["x"], io["ostore"]
    NT = S_TOT // 128
    SCQ = 128.0 ** -0.5
    if True:
        B = P.buf
        BK = Banks(P)
        f0, f1, f2, f3, f4, f5, f6 = BK.f
        bf0, bf1, bf2, bf3, bf4, bf5, bf6 = BK.bf
        b_x, b_o, b_fsc = io.get("b_x") or B(), B(), B()
        ident = P.sb("ident_sb", [128, 128], F32)
        identb = P.sb("identb", [128, 128], BF16)
        ones_bf = P.sb("ones_bf", [128, 128], BF16)
        b_ident, b_cb = B(), B()
        P.dma("sp", ident[:], ident_d[:, :], writes=[b_ident])
        P.op("dve", lambda e: e.tensor_copy(identb[:], ident[:]), reads=[b_ident], writes=[b_cb])
        P.op("dve", lambda e: e.memset(ones_bf[:], 1.0), writes=[b_cb])
        xin = [P.sb(f"xin{i}", [128, D], F32) for i in range(2)]
        b_xin = [B(), B()]
        stg = Stager(P, n=2)
        ctr = [0]
        wQb = P.sb("wQb", [128, 8, 512], BF16)
        wGb = P.sb("wGb", [128, 8, 12], BF16)
        b_wQ, b_wG = B(), B()
        stg.load(wQb[:], b_wQ, wQ[:, :])
        stg.load(wGb[:], b_wG, wG[:, :])
        KsT = P.sb("KsT", [128, S_TOT], BF16)
        KwT = P.sb("KwT", [128, S_TOT], BF16)
        Vs = P.sb("Vs", [128, NT, 129], BF16)
        Vw = P.sb("Vw", [128, NT, 129], BF16)
        b_KsT, b_KwT, b_Vs, b_Vw = B(), B(), B(), B()
        kcT = P.sb("kcT", [128, 512], BF16)
        rhsC = P.sb("rhsC", [128, 4, 256], BF16)
        b_kcT, b_rhsC = B(), B()
        P.op("dve", lambda e: e.memset(Vs[:, :, 128:129], 1.0), writes=[b_Vs])
        P.op("dve", lambda e: e.memset(Vw[:, :, 128:129], 1.0), writes=[b_Vw])
        with ExitStack() as es12:
            sb12 = lambda n, s, d: es12.enter_context(nc.sbuf_tensor(P.pfx + n, s, d))
            KcT = sb12("KcT", [128, 2, S_TOT], BF16)
            b_KcT = [B(), B()]
            with ExitStack() as es1:
                sb1 = lambda n, s, d: es1.enter_context(nc.sbuf_tensor(P.pfx + n, s, d))
                hT = sb1("hT", [128, 8, BLK], BF16)
                b_hT = [B() for _ in range(16)]
                wKVb = sb1("wKVb", [128, 8, 768], BF16)
                b_wKV = B()
                stg.load(wKVb[:], b_wKV, wKV[:, :])
                for blk in range(NBLK):
                    T0 = blk * BLK
                    load_xT(P, BK, x, b_x, xin, b_xin, hT, b_hT, ident, b_ident, T0, 16, ctr)
                    fm = [(0, KcT[:, 0, :], b_KcT[0]), (1, KcT[:, 1, :], b_KcT[1]), (2, KsT[:], b_KsT), (4, KwT[:], b_KwT)]
                    for fi, (wi, dst, bd) in enumerate(fm):
                        for qtr in range(4):
                            pb, bpb = (f2, bf2) if (fi + qtr) % 2 == 0 else (f3, bf3)
                            rds = [b_hT[qtr * 4 + i] for i in range(4)] + [b_wKV]
                            for dc in range(8):
                                P.op("pe", lambda e, pb=pb, dc=dc, wi=wi, qtr=qtr: e.matmul(
                                    pb[:], wKVb[:, dc, wi * 128:(wi + 1) * 128], hT[:, dc, qtr * 512:(qtr + 1) * 512],
                                    start=(dc == 0), stop=(dc == 7)), reads=rds, writes=[bpb])
                            if (fi + qtr) % 2 == 0:
                                P.op("act", lambda e, pb=pb, dst=dst, T0=T0, qtr=qtr: e.copy(
                                    dst[:, T0 + qtr * 512:T0 + (qtr + 1) * 512], pb[:]), reads=[bpb], writes=[bd])
                            else:
                                P.op("dve", lambda e, pb=pb, dst=dst, T0=T0, qtr=qtr: e.tensor_copy(
                                    dst[:, T0 + qtr * 512:T0 + (qtr + 1) * 512], pb[:]), reads=[bpb], writes=[bd])
                    for tl in range(16):
                        for (wi, dst, bd, pb, bpb, eng) in ((3, Vs, b_Vs, f4, bf4, "act"), (5, Vw, b_Vw, f5, bf5, "dve")):
                            for dc in range(8):
                                P.op("pe", lambda e, pb=pb, dc=dc, wi=wi, tl=tl: e.matmul(
                                    pb[:, 0:128], hT[:, dc, tl * 128:(tl + 1) * 128], wKVb[:, dc, wi * 128:(wi + 1) * 128],
                                    start=(dc == 0), stop=(dc == 7)), reads=[b_hT[tl], b_wKV], writes=[bpb])
                            if eng == "act":
                                P.op("act", lambda e, pb=pb, dst=dst, tl=tl, blk=blk: e.copy(dst[:, blk * 16 + tl, 0:128], pb[:, 0:128]),
                                     reads=[bpb], writes=[bd])
                            else:
                                P.op("dve", lambda e, pb=pb, dst=dst, tl=tl, blk=blk: e.tensor_copy(dst[:, blk * 16 + tl, 0:128], pb[:, 0:128]),
                                     reads=[bpb], writes=[bd])
                full_barrier(P)
            with ExitStack() as es2:
                sb2 = lambda n, s, d: es2.enter_context(nc.sbuf_tensor(P.pfx + n, s, d))
                w1b = sb2("w1b", [128, 32, 256], BF16)
                w2b = sb2("w2b", [128, 2, 128], BF16)
                posb = sb2("posb", [128, 64], BF16)
                pos32 = sb2("pos32", [128, 64], F32)
                ovl32 = sb2("ovl32", [128, 512], F32)
                pbias = sb2("pbias", [128, 2], F32)
                xg = sb2("xg", [128, 512], F32)
                ug = sb2("ug", [128, 512], F32)
                hid = sb2("hid_sb", [128, 2, 512], BF16)
                b_w1, b_w2, b_pos, b_ovl, b_pb, b_xg, b_ug, b_hid = B(), B(), B(), B(), B(), B(), B(), [B(), B()]
                P.dma("sp", pos32[:], posT[:, :], writes=[b_pos])
                P.op("dve", lambda e: e.tensor_copy(posb[:], pos32[:]), reads=[b_pos], writes=[b_pos])
                P.dma("sp", ovl32[:], ovl_d[:, :], writes=[b_ovl])
                P.op("dve", lambda e: e.tensor_copy(rhsC[:, :, 128:256], ovl32[:].rearrange("p (c m) -> p c m", c=4)),
                     reads=[b_ovl], writes=[b_rhsC])
                P.op("dve", lambda e: e.memset(hid[:, :, 511:512], 0.0), writes=b_hid)
                for kv in range(2):
                    stg.load(w1b[:], b_w1, w1d[kv])
                    stg.load(w2b[:], b_w2, w2d[kv])
                    for hc in range(2):
                        for p_ in range(32):
                            P.op("pe", lambda e, hc=hc, p_=p_, kv=kv: e.matmul(
                                f6[:, 0:1], w1b[:, p_, hc * 128:(hc + 1) * 128], posb[:, kv * 32 + p_:kv * 32 + p_ + 1],
                                start=(p_ == 0), stop=(p_ == 31)), reads=[b_w1, b_pos], writes=[bf6])
                        P.op("act", lambda e, hc=hc: e.copy(pbias[:, hc:hc + 1], f6[:, 0:1]), reads=[bf6], writes=[b_pb])
                        for p_ in range(32):
                            P.op("pe", lambda e, hc=hc, p_=p_, kv=kv: e.matmul(
                                f2[:, 0:511], w1b[:, p_, hc * 128:(hc + 1) * 128], KcT[:, kv, p_:p_ + 16 * 510 + 1:16],
                                start=(p_ == 0), stop=(p_ == 31)), reads=[b_w1, b_KcT[kv]], writes=[bf2])
                        P.op("act", lambda e, hc=hc: e.activation(xg[:, 0:511], f2[:, 0:511], AF.Identity, bias=pbias[:, hc:hc + 1], scale=1.0),
                             reads=[bf2, b_pb], writes=[b_xg])
                        P.op("dve", lambda e: e.tensor_tensor(ug[:, 0:511], xg[:, 0:511], xg[:, 0:511], ALU.mult), reads=[b_xg], writes=[b_ug])
                        P.op("dve", lambda e: e.tensor_scalar(ug[:, 0:511], ug[:, 0:511], 0.044715, 1.0, ALU.mult, ALU.add), reads=[b_ug], writes=[b_ug])
                        P.op("dve", lambda e: e.tensor_tensor(ug[:, 0:511], ug[:, 0:511], xg[:, 0:511], ALU.mult), reads=[b_ug, b_xg], writes=[b_ug])
                        P.op("act", lambda e: e.activation(ug[:, 0:511], ug[:, 0:511], AF.Sigmoid, scale=1.5957691216057308), reads=[b_ug], writes=[b_ug])
                        P.op("dve", lambda e, hc=hc: e.tensor_tensor(hid[:, hc, 0:511], xg[:, 0:511], ug[:, 0:511], ALU.mult),
                             reads=[b_ug, b_xg], writes=[b_hid[hc]])
                    if kv == 0:
                        for hc in range(2):
                            P.op("pe", lambda e, hc=hc: e.matmul(f3[:, 0:512], w2b[:, hc, :], hid[:, hc, :], start=(hc == 0), stop=(hc == 1)),
                                 reads=[b_w2] + b_hid, writes=[bf3])
                        P.op("act", lambda e: e.copy(kcT[:], f3[:, 0:512]), reads=[bf3], writes=[b_kcT])
                    else:
                        for c in range(4):
                            for hc in range(2):
                                P.op("pe", lambda e, hc=hc, c=c: e.matmul(f3[:, c * 128:(c + 1) * 128], hid[:, hc, c * 128:(c + 1) * 128], w2b[:, hc, :],
                                                                          start=(hc == 0), stop=(hc == 1)), reads=[b_w2] + b_hid, writes=[bf3])
                        P.op("act", lambda e: e.copy(rhsC[:, :, 0:128], f3[:].rearrange("p (c m) -> p c m", c=4)), reads=[bf3], writes=[b_rhsC])
                full_barrier(P)
        with ExitStack() as es3:
            sb3 = lambda n, s, d: es3.enter_context(nc.sbuf_tensor(P.pfx + n, s, d))
            tab = sb3("tab", [33, 4], F32)
            tabB = sb3("tabB", [33, 128], F32)
            ohp = [sb3(f"ohp{i}", [33, 512], F32) for i in range(2)]
            frp = [sb3(f"frp{i}", [128, 512], BF16) for i in range(2)]
            b_tab, b_tabB, b_ohp, b_frp = B(), B(), [B(), B()], [B(), B()]
            b_fscw = [B(), B()]
            P.dma("sp", tab[:], relb[:, :], writes=[b_tab])
            ki = 0
            for h in range(4):
                P.op("dve", lambda e, h=h: e.tensor_copy(tabB[:], tab[:, h:h + 1].to_broadcast([33, 128])), reads=[b_tab], writes=[b_tabB])
                for pc_ in range(L1 // 512):
                    k = ki % 2
                    ki += 1
                    P.dma("sp", ohp[k][:], oh_d[:, pc_ * 512:(pc_ + 1) * 512], writes=[b_ohp[k]])
                    pb, bpb = (f2, bf2) if k == 0 else (f3, bf3)
                    P.op("pe", lambda e, pb=pb, k=k: e.matmul(pb[:], tabB[:], ohp[k][:], start=True, stop=True), reads=[b_tabB, b_ohp[k]], writes=[bpb])
                    P.op("act", lambda e, pb=pb, k=k: e.activation(frp[k][:], pb[:], AF.Exp), reads=[bpb], writes=[b_frp[k]])
                    P.dma("sp", fsc[:, h * L1 + pc_ * 512:h * L1 + (pc_ + 1) * 512], frp[k][:], reads=[b_frp[k]], writes=[b_fscw[k]])
            full_barrier(P)
            RS = 4 * L1
            Esel = sb3("Esel", [128, 14, 4, 128], BF16)
            Ew4 = sb3("Ew4", [128, 4, 128], BF16)
            b_Esel = B()
            for dl in range(14):
                P.dma("sp", Esel[:, dl], bass.AP(fsc.tensor, 128 * dl + OFF1, [[RS - 1, 128], [L1, 4], [1, 128]]),
                      reads=[b_fsc], writes=[b_Esel])
            cst = sb3("cst", [128, 4, 128], F32)
            qhalf = sb3("qhalf_sb", [128, 1], F32)
            gbb = sb3("gbb", [128, 12], F32)
            m4b = sb3("m4b", [128, 128], BF16)
            b_cst = B()
            P.dma("sp", cst[:], cst_d[:, :].rearrange("p (c m) -> p c m", c=4), writes=[b_cst])
            b_q2, b_g2 = B(), B()
            P.dma("sp", qhalf[:], qh_d[:, :], writes=[b_q2])
            P.dma("sp", gbb[:], bcast_rows(gbd.tensor, gbd.offset, 12), writes=[b_g2])
            CST = [b_cst, b_q2, b_g2, b_cb, b_Esel]
            P.op("dve", lambda e: e.tensor_copy(m4b[:], cst[:, 3, :]), reads=[b_cst], writes=[b_Esel])
            P.op("dve", lambda e: e.tensor_tensor(Ew4[:], Esel[:, 4], m4b[:].unsqueeze(1).to_broadcast([128, 4, 128]), ALU.mult),
                 reads=[b_Esel], writes=[b_Esel])
            mrow, mge1, meq0 = cst[:, 0, :], cst[:, 1, :], cst[:, 2, :]
            Expand = sb3("Expand", [128, NT, 128], BF16)
            for Tk in range(NT):
                P.op("dve", lambda e, Tk=Tk: e.tensor_copy(Expand[:, Tk, :].rearrange("m (h k) -> m h k", h=2),
                                                            identb[:, 2 * Tk:2 * Tk + 2].unsqueeze(2).to_broadcast([128, 2, 64])),
                     reads=[b_cb], writes=[b_Esel])
            hTt = sb3("hTt", [128, 8, 128], BF16)
            b_hTt = [B()]
            QT = [sb3(f"QT{i}", [128, 4, 128], BF16) for i in range(2)]
            b_QT = [B(), B()]
            gsg = sb3("gsg", [128, 12], F32)
            b_gsg = B()
            ecb = [sb3(f"ecb{i}", [128, 4, 128], BF16) for i in range(2)]
            b_ecb = [B(), B()]
            psb = [sb3(f"psb{i}", [128, 4, 128], BF16) for i in range(3)]
            b_psb = [B(), B(), B()]
            ocb = sb3("ocb", [128, 4, 128], F32)
            b_ocb = B()
            sm = sb3("sm3", [128, 64], F32)
            b_sm = B()
            imp = sb3("imp", [128, 128], F32)
            M1 = sb3("M1", [128, 128], F32)
            Fm = sb3("Fm", [128, 128], F32)
            sa = sb3("sa", [128, 128], F32)
            rp = sb3("rp", [128, 128], F32)
            sel = sb3("sel", [128, 128], F32)
            nsb = sb3("nsb", [128, 128], BF16)
            nsT = sb3("nsT", [128, 128], BF16)
            mx = sb3("mx", [128, 16], F32)
            b_imp, b_M1, b_Fm, b_sa, b_rp, b_sel, b_nsb, b_nsT, b_mx = (B() for _ in range(9))
            b_ow = [B(), B()]
            ob = sb3("ob", [128, 4, 128], BF16)
            oTt = [sb3(f"oTt{i}", [128, 4, 128], BF16) for i in range(2)]
            b_ob, b_oTt = B(), [B(), B()]
            si = 0
            for T in range(NT):
                k2 = T % 2
                load_xT(P, BK, x, b_x, xin, b_xin, hTt, b_hTt, ident, b_ident, T * 128, 1, ctr)
                for j in range(4):
                    for dc in range(8):
                        P.op("pe", lambda e, j=j, dc=dc: e.matmul(f6[:, j * 128:(j + 1) * 128], wQb[:, dc, j * 128:(j + 1) * 128], hTt[:, dc, :],
                                                                  start=(dc == 0), stop=(dc == 7)), reads=[b_hTt[0], b_wQ], writes=[bf6])
                qt, bqt = QT[k2], b_QT[k2]
                P.op("act", lambda e, qt=qt: e.mul(qt[:].rearrange("p j q -> p (j q)"), f6[:], SCQ), reads=[bf6], writes=[bqt])
                for dc in range(8):
                    P.op("pe", lambda e, dc=dc: e.matmul(f4[:, 16:28], hTt[:, dc, :], wGb[:, dc, :], start=(dc == 0), stop=(dc == 7)),
                         reads=[b_hTt[0], b_wG], writes=[bf4])
                P.op("dve", lambda e: e.tensor_tensor(gsg[:], f4[:, 16:28], gbb[:], ALU.add), reads=[bf4] + CST, writes=[b_gsg])
                P.op("act", lambda e: e.activation(gsg[:], gsg[:], AF.Sigmoid), reads=[b_gsg], writes=[b_gsg])
                qrhs = qt[:].rearrange("p j q -> p (j q)")
                ncmp = (8 * T + 6) // 128 + 1
                for c in range(ncmp):
                    dl = min(T - 16 * c, 28)
                    ke = si % 2
                    kp = si % 3
                    si += 1
                    P.dma("sp", ecb[ke][:], bass.AP(fsc.tensor, 128 * dl - 31 + OFF1, [[RS - 16, 128], [L1, 4], [1, 128]]),
                          reads=[b_fsc], writes=[b_ecb[ke]])
                    P.op("pe", lambda e, c=c, qrhs=qrhs: e.matmul(f6[:], kcT[:, c * 128:(c + 1) * 128], qrhs, start=True, stop=True),
                         reads=[b_kcT, bqt], writes=[bf6])
                    pp, bpp = psb[kp], b_psb[kp]
                    P.op("act", lambda e, pp=pp: e.activation(pp[:].rearrange("p j q -> p (j q)"), f6[:], AF.Exp), reads=[bf6], writes=[bpp])
                    P.op("dve", lambda e, pp=pp, ke=ke: e.tensor_tensor(pp[:], pp[:], ecb[ke][:], ALU.mult), reads=[bpp, b_ecb[ke]], writes=[bpp])
                    for j in range(4):
                        fb, bfb = (f2, bf2) if j < 2 else (f3, bf3)
                        P.op("pe", lambda e, fb=fb, j=j, c=c, pp=pp, ncmp=ncmp: e.matmul(
                            fb[:, (j % 2) * 256:(j % 2) * 256 + 256], pp[:, j, :], rhsC[:, c, :],
                            start=(c == 0 and j % 2 == 0), stop=(c == ncmp - 1 and j % 2 == 1)),
                            reads=[bpp, b_rhsC], writes=[bfb])
                        P.op("pe", lambda e, j=j, c=c, pp=pp, ncmp=ncmp: e.matmul(
                            f4[:, j:j + 1], pp[:, j, :], ones_bf[:, 0:1], start=(c == 0 and j == 0), stop=(c == ncmp - 1 and j == 3)),
                            reads=[bpp, b_cb], writes=[bf4])
                rc, cc = sm[:, 0:4], sm[:, 4:8]
                P.op("dve", lambda e, rc=rc: e.tensor_scalar(rc, f4[:, 0:4], 1e-30, None, ALU.max), reads=[bf4], writes=[b_sm])
                P.op("dve", lambda e, rc=rc: e.reciprocal(rc, rc), reads=[b_sm], writes=[b_sm])
                P.op("dve", lambda e, rc=rc, cc=cc: e.tensor_tensor(cc, rc, gsg[:, 0:4], ALU.mult), reads=[b_sm, b_gsg], writes=[b_sm])
                for j in range(4):
                    fb, bfb = (f2, bf2) if j < 2 else (f3, bf3)
                    o0 = (j % 2) * 256
                    P.op("dve", lambda e, fb=fb, j=j, o0=o0, cc=cc: e.tensor_scalar(ocb[:, j, :], fb[:, o0:o0 + 128], cc[:, j:j + 1], None, ALU.mult),
                         reads=[bfb, b_sm], writes=[b_ocb])
                    if j == 0:
                        P.op("dve", lambda e, fb=fb, o0=o0, rc=rc: e.tensor_scalar(imp[:], fb[:, o0 + 128:o0 + 256], rc[:, 0:1], None, ALU.mult),
                             reads=[bfb, b_sm], writes=[b_imp])
                    else:
                        P.op("dve", lambda e, fb=fb, o0=o0, rc=rc, j=j: e.scalar_tensor_tensor(
                            imp[:], fb[:, o0 + 128:o0 + 256], rc[:, j:j + 1], imp[:], ALU.mult, ALU.add), reads=[bfb, b_sm, b_imp], writes=[b_imp])
                qb, qb1, qb2, tau = sm[:, 8:9], sm[:, 9:10], sm[:, 10:11], sm[:, 11:12]
                P.op("dve", lambda e, qb=qb, T=T: e.tensor_scalar(qb, qhalf[:], float(2 * T), None, ALU.add), reads=CST, writes=[b_sm])
                P.op("dve", lambda e, qb=qb, qb1=qb1: e.tensor_scalar(qb1, qb, -1.0, None, ALU.add), reads=[b_sm], writes=[b_sm])
                P.op("dve", lambda e, qb=qb, qb2=qb2: e.tensor_scalar(qb2, qb, -2.0, None, ALU.add), reads=[b_sm], writes=[b_sm])
                P.op("dve", lambda e, qb2=qb2: e.tensor_scalar(M1[:], mrow, qb2, None, ALU.is_le), reads=[b_sm] + CST, writes=[b_M1])
                P.op("dve", lambda e: e.tensor_tensor(M1[:], M1[:], mge1, ALU.mult), reads=[b_M1] + CST, writes=[b_M1])
                P.op("dve", lambda e, qb=qb: e.tensor_scalar(Fm[:], mrow, qb, None, ALU.is_equal), reads=[b_sm] + CST, writes=[b_Fm])
                P.op("dve", lambda e, qb1=qb1: e.tensor_scalar(sel[:], mrow, qb1, None, ALU.is_equal), reads=[b_sm] + CST, writes=[b_sel])
                P.op("dve", lambda e: e.tensor_tensor(Fm[:], Fm[:], sel[:], ALU.add), reads=[b_Fm, b_sel], writes=[b_Fm])
                P.op("dve", lambda e: e.tensor_tensor(Fm[:], Fm[:], meq0, ALU.max), reads=[b_Fm] + CST, writes=[b_Fm])
                P.op("dve", lambda e: e.tensor_tensor(sa[:], imp[:], M1[:], ALU.mult), reads=[b_imp, b_M1], writes=[b_sa])
                P.op("dve", lambda e: e.max(mx[:, 0:8], sa[:]), reads=[b_sa], writes=[b_mx])
                P.op("dve", lambda e: e.match_replace(rp[:], mx[:, 0:8], sa[:], -1.0), reads=[b_sa, b_mx], writes=[b_rp])
                P.op("dve", lambda e: e.max(mx[:, 8:16], rp[:]), reads=[b_rp], writes=[b_mx])
                P.op("dve", lambda e: e.tensor_scalar(sel[:], sa[:], mx[:, 12:13], None, ALU.is_ge), reads=[b_sa, b_mx], writes=[b_sel])
                P.op("dve", lambda e: e.tensor_tensor(sel[:], sel[:], M1[:], ALU.mult), reads=[b_sel, b_M1], writes=[b_sel])
                P.op("dve", lambda e: e.tensor_tensor(sel[:], sel[:], Fm[:], ALU.max), reads=[b_sel, b_Fm], writes=[b_sel])
                P.op("dve", lambda e: e.tensor_scalar(nsb[:], sel[:], -1.0, 30000.0, ALU.add, ALU.mult), reads=[b_sel], writes=[b_nsb])
                P.op("pe", lambda e: e.transpose(BK.h[:, 512:640], nsb[:], identb[:]), reads=[b_nsb, b_cb], writes=[BK.bh])
                P.op("act", lambda e: e.copy(nsT[:], BK.h[:, 512:640]), reads=[BK.bh], writes=[b_nsT])
                nsrhs = nsT[:].unsqueeze(1).to_broadcast([128, 4, 128])
                for br, (KT_, bKT, V_, bV, fa, bfa, fb_, bfb_, tks) in enumerate((
                        (KsT, b_KsT, Vs, b_Vs, f2, bf2, f3, bf3, list(range(0, T + 1))),
                        (KwT, b_KwT, Vw, b_Vw, f4, bf4, f5, bf5, list(range(max(0, T - 4), T + 1))))):
                    for ii, Tk in enumerate(tks):
                        dl = T - Tk
                        kp = si % 3
                        ks_ = si % 2
                        si += 1
                        st, bst = (f0, bf0) if ks_ == 0 else (f1, bf1)
                        P.op("pe", lambda e, st=st, KT_=KT_, Tk=Tk, qrhs=qrhs, br=br: e.matmul(
                            st[:], KT_[:, Tk * 128:(Tk + 1) * 128], qrhs, start=True, stop=(br == 1)),
                            reads=[bKT, bqt], writes=[bst])
                        if br == 0:
                            P.op("pe", lambda e, st=st, Tk=Tk, nsrhs=nsrhs: e.matmul(
                                st[:].rearrange("p (j q) -> p j q", j=4), Expand[:, Tk, :], nsrhs, start=False, stop=True),
                                reads=[b_nsT, b_Esel], writes=[bst])
                        pp, bpp = psb[kp], b_psb[kp]
                        P.op("act", lambda e, pp=pp, st=st: e.activation(pp[:].rearrange("p j q -> p (j q)"), st[:], AF.Exp), reads=[bst], writes=[bpp])
                        if br == 1 and dl == 4:
                            et = Ew4[:]
                        else:
                            et = Esel[:, min(dl, 13)]
                        P.op("dve", lambda e, pp=pp, et=et: e.tensor_tensor(pp[:], pp[:], et, ALU.mult), reads=[bpp, b_Esel], writes=[bpp])
                        for j in range(4):
                            fo, bfo = (fa, bfa) if j < 2 else (fb_, bfb_)
                            P.op("pe", lambda e, fo=fo, j=j, pp=pp, V_=V_, Tk=Tk, ii=ii, n=len(tks): e.matmul(
                                fo[:, (j % 2) * 129:(j % 2) * 129 + 129], pp[:, j, :], V_[:, Tk, :],
                                start=(ii == 0 and j % 2 == 0), stop=(ii == n - 1 and j % 2 == 1)),
                                reads=[bpp, bV], writes=[bfo])
                    for j in range(4):
                        fo, bfo = (fa, bfa) if j < 2 else (fb_, bfb_)
                        P.op("dve", lambda e, fo=fo, j=j, br=br: e.tensor_scalar(
                            sm[:, 16 + br * 4 + j:17 + br * 4 + j], fo[:, (j % 2) * 129 + 128:(j % 2) * 129 + 129], 1e-30, None, ALU.max),
                            reads=[bfo], writes=[b_sm])
                cf = sm[:, 16:24]
                P.op("dve", lambda e, cf=cf: e.reciprocal(cf, cf), reads=[b_sm], writes=[b_sm])
                P.op("dve", lambda e, cf=cf: e.tensor_tensor(cf, cf, gsg[:, 4:12], ALU.mult), reads=[b_sm, b_gsg], writes=[b_sm])
                for j in range(4):
                    fs_, bfs = (f2, bf2) if j < 2 else (f3, bf3)
                    fw_, bfw = (f4, bf4) if j < 2 else (f5, bf5)
                    o0 = (j % 2) * 129
                    P.op("dve", lambda e, fs_=fs_, j=j, o0=o0, cf=cf: e.scalar_tensor_tensor(
                        ocb[:, j, :], fs_[:, o0:o0 + 128], cf[:, j:j + 1], ocb[:, j, :], ALU.mult, ALU.add),
                        reads=[bfs, b_sm, b_ocb], writes=[b_ocb])
                    P.op("dve", lambda e, fw_=fw_, j=j, o0=o0, cf=cf: e.scalar_tensor_tensor(
                        ob[:, j, :], fw_[:, o0:o0 + 128], cf[:, 4 + j:5 + j], ocb[:, j, :], ALU.mult, ALU.add),
                        reads=[bfw, b_sm, b_ocb], writes=[b_ob])
                for j in range(4):
                    P.op("pe", lambda e, j=j: e.transpose(BK.h[:, j * 128:(j + 1) * 128], ob[:, j, :], identb[:]), reads=[b_ob, b_cb], writes=[BK.bh])
                ot, bot = oTt[k2], b_oTt[k2]
                P.op("act", lambda e, ot=ot: e.copy(ot[:].rearrange("p j q -> p (j q)"), BK.h[:, 0:512]), reads=[BK.bh], writes=[bot])
                ostore(P, T, ot[:], [bot], k2)
            full_barrier(P)


C_OFF_KV_, C_OFF_GATE_ = 1024, 2560


def pack_m1(inp, h1b, g):
    w = inp["c_w_in"][0]
    wQ = w[:, g * 512:(g + 1) * 512]
    kvcols = []
    for br in range(3):
        for kv in range(2):
            o = C_OFF_KV_ + ((br * 2 + kv) * 2 + g) * 128
            kvcols.append(w[:, o:o + 128])
    wKV = np.concatenate(kvcols, axis=1)
    gcols = [C_OFF_GATE_ + br * 8 + g * 4 + j for br in range(3) for j in range(4)]
    wG = w[:, gcols]
    gb = inp["c_gate_b"][0][[c - C_OFF_GATE_ for c in gcols]][None]
    pos = inp["c_cmp_pos"][0]
    posT = np.concatenate([pos[0].T, pos[1].T], axis=1)
    relb = np.concatenate([inp["rel_bias"][:, 4 * g:4 * g + 4], np.full((1, 4), -30000.0, np.float32)], axis=0)
    c = nsa_consts()
    return dict(x=h1b, wQ=np.ascontiguousarray(wQ), wKV=np.ascontiguousarray(wKV), wG=np.ascontiguousarray(wG),
                gb=np.ascontiguousarray(gb.astype(np.float32)), posT=np.ascontiguousarray(posT), w1=inp["c_cmp_w1"][0], w2=inp["c_cmp_w2"][0],
                relb=np.ascontiguousarray(relb), oh1=c["oh1"], ovl=np.ascontiguousarray(c["ovl"].reshape(128, 512)),
                cst1=np.ascontiguousarray(c["cst1"].reshape(128, 512)), qhalf=c["qhalf"], ident=np.eye(128, dtype=np.float32))


U32 = mybir.dt.uint32
GROUPS = [[0, 1], [2, 3], [4, 5], [6, 7]]


def build_fused():
    nc = bass.Bass("TRN2", target_bir_lowering=False)
    ident_d = nc.dram_tensor("ident", [128, 128], F32, kind="ExternalInput").ap()
    gidx_d = nc.dram_tensor("gidx", [128, 16], U32, kind="ExternalInput").ap()
    xres_d = nc.dram_tensor("xres", [TOK, D], F32, kind="ExternalInput").ap()
    out_d = nc.dram_tensor("out", [TOK, D], F32, kind="ExternalOutput").ap()
    io_m0 = declare_mixer0(nc, "m0_")
    io_p0 = declare_post(nc, "p0_")
    io_m1 = declare_mixer1(nc, "m1_")
    io_p1 = declare_post(nc, "p1_")
    y0src = [nc.dram_tensor(f"y0src{i}", [2048, 512], BF16).ap() for i in range(4)]
    y0dst = [nc.dram_tensor(f"y0dst{i}", [4096, 512], BF16).ap() for i in range(4)]
    o1src = [nc.dram_tensor(f"o1src{i}", [2048, 512], BF16).ap() for i in range(4)]
    o1dst = [nc.dram_tensor(f"o1dst{i}", [4096, 512], BF16).ap() for i in range(4)]
    h1src = [nc.dram_tensor(f"h1src{i}", [512, D], F32).ap() for i in range(8)]
    h1dst = [nc.dram_tensor(f"h1dst{i}", [1024, D], F32).ap() for i in range(8)]
    with ExitStack() as es:
        P = Prog(nc, es)
        B = P.buf
        gidx = P.sb("gidx_sb", [128, 16], U32)
        b_gidx = B()
        P.dma("sp", gidx[:], gidx_d[:, :], writes=[b_gidx])

        def phase(pfx, fn, io):
            with ExitStack() as pes:
                P.es = pes
                P.pfx = pfx
                P.in_phase = True
                fn(P, io)
                full_barrier(P)
                P.recycle()
                P.in_phase = False
            P.es = es

        def ag(srcs, dsts):
            P.in_phase = True
            for a, d in zip(srcs, dsts):
                P.all_gather(a, d, GROUPS, reads=[], writes=[B()])
            full_barrier(P)
            P.recycle()
            P.in_phase = False

        def row_store(dst_list, nbuf=4):
            bufs = [[B() for _ in range(nbuf)] for _ in dst_list]
            cnt = [0]

            def ystore(P_, i, blk, sb, reads):
                h, tq = blk // 2, (blk % 2) * 4
                r0 = (h * 8 + tq) * 128
                dst = dst_list[i][r0:r0 + 512, :].rearrange("(t p) c -> p t c", p=128)
                P_.dma("sp", dst, sb.rearrange("p (t c) -> p t c", t=4), reads=reads, writes=[bufs[i][cnt[0] % nbuf]])
                cnt[0] += 1
            return ystore

        def make_load_yt(dsts):
            def load_yt(P_, yt, byt, t4):
                for ec in range(8):
                    i, r = ec // 2, ec % 2
                    P_.gather(yt[:, ec, :], dsts[i], gidx[:, r * 8 + t4:r * 8 + t4 + 1], reads=[b_gidx], writes=[byt[ec]])
            return load_yt

        io_m0.update(ident=ident_d, ystore=row_store(y0src))
        phase("m0_", emit_mixer0, io_m0)
        ag(y0src, y0dst)
        io_p0.update(ident=ident_d, load_yt=make_load_yt(y0dst), xres=lambda t0: xres_d[t0:t0 + 128, :],
                     out=lambda t0: h1src[t0 // 512][t0 % 512:t0 % 512 + 128, :], is_final=False)
        phase("p0_", emit_post, io_p0)
        ag(h1src, h1dst)
        o1bufs = [[B(), B()] for _ in range(4)]

        def ostore(P_, T, ot, reads, slot):
            h, t4 = T // 32, (T % 32) // 4
            r0 = (h * 8 + t4) * 128
            c0 = (T % 4) * 128
            for j in range(4):
                P_.dma("sp", o1src[j][r0:r0 + 128, c0:c0 + 128], ot[:, j, :], reads=reads, writes=[o1bufs[j][slot]])

        def h1tile(t0):
            k, rr, tt = (t0 % TOK) // 512, t0 // TOK, t0 % 512
            return h1dst[k][rr * 512 + tt:rr * 512 + tt + 128, :]

        io_m1.update(ident=ident_d, x=h1tile, ostore=ostore)
        phase("m1_", emit_mixer1, io_m1)
        ag(o1src, o1dst)
        io_p1.update(ident=ident_d, load_yt=make_load_yt(o1dst), xres=lambda t0: h1src[t0 // 512][t0 % 512:t0 % 512 + 128, :],
                     out=lambda t0: out_d[t0:t0 + 128, :], is_final=True)
        phase("p1_", emit_post, io_p1)
        P.finish()
    return nc


def pack_post(inp, layer, wo_perm, pfx):
    wr = np.concatenate([inp["moe_wr_g"][layer]] + [inp["moe_wr_e"][layer][g] for g in range(4)], axis=1)
    br = np.concatenate([inp["moe_br_g"][layer], inp["moe_br_e"][layer].reshape(-1)])[None]
    d = dict(wo=np.ascontiguousarray(wo_perm),
             ln1g=np.ascontiguousarray(inp["ln_g"][layer, 0]), ln1b=np.ascontiguousarray(inp["ln_b"][layer, 0]),
             ln2g=np.ascontiguousarray(inp["ln_g"][layer, 1]), ln2b=np.ascontiguousarray(inp["ln_b"][layer, 1]),
             wg=inp["moe_w_gate"][layer], wu=inp["moe_w_up"][layer], wd=inp["moe_w_down"][layer],
             wr=np.ascontiguousarray(wr.astype(np.float32)), br=np.ascontiguousarray(br.astype(np.float32)))
    return {pfx + k: v for k, v in d.items()}


def kernel(**inputs):
    inp = {k: np.asarray(v) for k, v in inputs.items()}
    nb = inp["x"].shape[0]
    cores = list(range(2 * nb))
    wo0 = inp["ab_w_out"][0]
    ch0 = [(2 * r + i) if i < 2 else (4 + 2 * r + (i - 2)) for i in range(4) for r in range(2)]
    wo0p = np.concatenate([wo0[c * 128:(c + 1) * 128] for c in ch0], axis=0)
    wo1 = inp["c_w_out"][0]
    ch1 = [4 * r + i for i in range(4) for r in range(2)]
    wo1p = np.concatenate([wo1[c * 128:(c + 1) * 128] for c in ch1], axis=0)
    post0 = pack_post(inp, 0, wo0p, "p0_")
    post1 = pack_post(inp, 1, wo1p, "p1_")
    ident = np.eye(128, dtype=np.float32)
    maps = []
    for b in range(nb):
        for c in range(2):
            m = dict(ident=ident, xres=np.ascontiguousarray(inp["x"][b, TOK * c:TOK * (c + 1)]))
            p = np.arange(128, dtype=np.uint32)[:, None]
            rt = np.arange(16, dtype=np.uint32)[None, :]
            m["gidx"] = np.ascontiguousarray(((rt // 8) * 2048 + c * 1024 + (rt % 8) * 128 + p).astype(np.uint32))
            m0 = pack_m0(inp, b, c)
            m0.pop("ident")
            m.update({"m0_" + k: v for k, v in m0.items()})
            m1 = pack_m1(inp, None, c)
            m1.pop("ident")
            m1.pop("x")
            m.update({"m1_" + k: v for k, v in m1.items()})
            m.update(post0)
            m.update(post1)
            maps.append(m)
    nc = build_fused()
    res = run_bass_kernel_spmd(nc, maps, core_ids=cores).results
    out = np.stack([np.concatenate([np.asarray(res[b * 2 + c]["out"]) for c in range(2)], axis=0) for b in range(nb)])
    return out.astype(np.float32)
```

```python
import os
import numpy as np
from contextlib import ExitStack
import concourse.bass as bass
import concourse.mybir as mybir
from concourse.bass_utils import run_bass_kernel_spmd

F32 = mybir.dt.float32
BF16 = mybir.dt.bfloat16
AF = mybir.ActivationFunctionType
ALU = mybir.AluOpType
AX = mybir.AxisListType

D = 1024
ALPHA = (2.0 * 2) ** 0.25
LN_EPS = 1e-5
NEXP = 16
EH = 512


class Buf:
    __slots__ = ("name", "w", "rd", "dsem", "dval", "dkey")

    def __init__(self, name):
        self.name = name
        self.w = None
        self.rd = {}
        self.dsem = None
        self.dval = 0
        self.dkey = None


class Prog:
    def __init__(self, nc, es):
        self.nc = nc
        self.es = es
        self.es_global = es
        self.pfx = ""
        self.eng = dict(pe=nc.tensor, act=nc.scalar, dve=nc.vector, pool=nc.gpsimd, sp=nc.sync)
        self.sem = {k: es.enter_context(nc.semaphore("s_" + k)) for k in self.eng}
        self.cnt = {k: 0 for k in self.eng}
        self.known = {k: {} for k in self.eng}
        self.nsem = 0
        self.outs = []
        self.nbuf = 0
        self.dmasems = {}
        self.issuer = {}
        self.free_sems = []
        self.phase_sems = []
        self.in_phase = False

    def buf(self, name=None):
        self.nbuf += 1
        return Buf(name or f"b{self.nbuf}")

    def sb(self, name, shape, dt):
        return self.es.enter_context(self.nc.sbuf_tensor(self.pfx + name, shape, dt))

    def ps(self, name, shape, dt=F32):
        return self.es.enter_context(self.nc.psum_tensor(self.pfx + name, shape, dt))

    def _collect(self, e, reads, writes):
        deps = {}

        def add(tok, same_ok):
            if tok is None:
                return
            key, sh, val = tok
            if key == e and not same_ok:
                return
            if key in deps and deps[key][1] >= val:
                return
            deps[key] = (sh, val)

        for b in reads:
            add(b.w, e != "pe")
        for b in writes:
            add(b.w, e != "pe")
            for tok in b.rd.values():
                add(tok, e != "pe")
        return deps

    def _wait(self, e, deps):
        kn = self.known[e]
        for key, (sh, val) in deps.items():
            if kn.get(key, 0) >= val:
                continue
            self.eng[e].wait_ge(sh, val)
            kn[key] = val

    def op(self, e, fn, reads=(), writes=()):
        self._wait(e, self._collect(e, reads, writes))
        ins = fn(self.eng[e])
        self.cnt[e] += 1
        ins.then_inc(self.sem[e], 1)
        tok = (e, self.sem[e], self.cnt[e])
        for b in reads:
            b.rd[e] = tok
        for b in writes:
            b.w = tok
            b.rd = {}
        return tok

    def dma(self, q, out, in_, reads=(), writes=(), is_out=False, nowaw=False, **kw):
        wb = writes[0]
        if nowaw:
            saved = wb.w
            if saved is not None and saved[0] == wb.dkey:
                wb.w = None
            deps = self._collect(q, reads, writes)
            wb.w = saved
        else:
            deps = self._collect(q, reads, writes)
        self._wait(q, deps)
        self._ensure_sem(wb)
        ins = self.eng[q].dma_start(out=out, in_=in_, **kw)
        wb.dval += 16
        ins.then_inc(wb.dsem, 16)
        tok = (wb.dkey, wb.dsem, wb.dval)
        self.dmasems[wb.dkey] = tok
        self.issuer[wb.dkey] = q
        for b in reads:
            b.rd[wb.dkey] = tok
        for b in writes:
            b.w = tok
            b.rd = {}
        if is_out and wb not in self.outs:
            self.outs.append(wb)
        return tok

    def _ensure_sem(self, wb):
        if wb.dsem is not None:
            return
        if self.free_sems:
            wb.dkey, wb.dsem, wb.dval = self.free_sems.pop()
        else:
            wb.dkey = f"d{self.nsem}"
            wb.dsem = self.es_global.enter_context(self.nc.semaphore(wb.dkey))
            self.nsem += 1
        if self.in_phase:
            self.phase_sems.append(wb)

    def recycle(self):
        for wb in self.phase_sems:
            self.free_sems.append((wb.dkey, wb.dsem, wb.dval))
            wb.dsem = None
        self.phase_sems = []

    def _tok_async(self, q, wb, ins, inc, reads, writes):
        self._ensure_sem(wb)
        wb.dval += inc
        ins.then_inc(wb.dsem, inc)
        tok = (wb.dkey, wb.dsem, wb.dval)
        self.dmasems[wb.dkey] = tok
        self.issuer[wb.dkey] = q
        for b in reads:
            b.rd[wb.dkey] = tok
        for b in writes:
            b.w = tok
            b.rd = {}
        return tok

    def gather(self, out, in_dram, idx_ap, reads=(), writes=()):
        self._wait("pool", self._collect("pool", reads, writes))
        ins = self.nc.gpsimd.indirect_dma_start(out=out, out_offset=None, in_=in_dram,
                                                in_offset=bass.IndirectOffsetOnAxis(ap=idx_ap, axis=0))
        return self._tok_async("pool", writes[0], ins, 16, reads, writes)

    def all_gather(self, src, dst, groups, reads=(), writes=()):
        self._wait("pool", self._collect("pool", reads, writes))
        ins = self.nc.gpsimd.collective_compute("AllGather", ALU.bypass, replica_groups=groups, ins=[src.opt()], outs=[dst.opt()])
        return self._tok_async("pool", writes[0], ins, 1, reads, writes)

    def finish(self):
        for wb in self.outs:
            if wb.dsem is not None:
                self.eng["sp"].wait_ge(wb.dsem, wb.dval)


def bcast_rows(ap1d_tensor, offset, n, parts=128):
    return bass.AP(ap1d_tensor, offset, [[0, parts], [1, n]])


def emit_tail(P, hmid, hmid_buf, out, out_buf, T, w, layer, ST=1024, banks=None, is_final=True):
    nc = P.nc
    NT = ST // 128
    NK = ST // 512
    ident = P.sb("t_ident", [128, 128], F32)
    ones1 = P.sb("t_ones1", [1, 128], F32)
    brow = P.sb("t_brow", [1, 20], F32)
    wr = P.sb("t_wr", [128, 8, 20], F32)
    gbc = P.sb("t_gbc", [128, D], F32)
    bbc = P.sb("t_bbc", [128, D], F32)
    xin = [P.sb(f"t_xin{i}", [128, D], F32) for i in range(2)]
    acc = P.sb("t_acc", [128, NT, D], F32)
    hT16 = P.sb("t_hT16", [128, 8, ST], BF16)
    hT32 = P.sb("t_hT32", [128, 8, 128], F32)
    gates = P.sb("t_gates", [128, NT, NEXP], F32)
    wgb = [P.sb(f"t_wg{i}", [128, 8, EH], BF16) for i in range(2)]
    wub = [P.sb(f"t_wu{i}", [128, 8, EH], BF16) for i in range(2)]
    wdb = [P.sb(f"t_wd{i}", [128, 4, D], BF16) for i in range(2)]
    sg = [P.sb(f"t_sg{i}", [128, 512], BF16) for i in range(2)]
    hid = [P.sb(f"t_hid{i}", [128, 4, 512], BF16) for i in range(2)]
    obuf = [P.sb(f"t_ob{i}", [128, D], F32) for i in range(2)]
    sm = P.sb("t_sm", [128, 64], F32)
    lg = P.sb("t_lg", [128, 20], F32)
    stats = P.sb("t_stats", [128, 2, 6], F32)

    if banks is None:
        banks = [P.ps(f"t_ps{i}", [128, 512]) for i in range(8)]
    ps_g, ps_u, ps_o, ps_t = banks[0:2], banks[2:4], banks[4:6], banks[6:8]

    B = P.buf
    b_ident, b_ones1, b_brow, b_wr, b_gbc, b_bbc = B(), B(), B(), B(), B(), B()
    b_xin = [B(), B()]
    b_acc = [B() for _ in range(NT)]
    b_hT16 = [B() for _ in range(NT)]
    b_hT32 = B()
    b_gates = [B() for _ in range(NT)]
    b_wg, b_wu, b_wd = [B(), B()], [B(), B()], [B(), B()]
    b_sg = [B(), B()]
    b_hid = [[B() for _ in range(4)] for _ in range(2)]
    b_ob = [B(), B()]
    b_outw = [B(), B()]
    b_sm = B()
    b_lg = B()
    b_stats = B()
    b_psg, b_psu, b_pso, b_pst = [B(), B()], [B(), B()], [B(), B()], [B(), B()]

    P.dma("sp", ident[:], w["ident"][:, :], writes=[b_ident])
    P.dma("sp", brow[:], w["br"][:, :], writes=[b_brow])
    P.dma("sp", wr[:], w["wr"].rearrange("(c p) n -> p c n", p=128), writes=[b_wr])
    P.dma("sp", gbc[:], bcast_rows(w["ln_g"].tensor, w["ln_g"].offset, D), writes=[b_gbc])
    P.dma("sp", bbc[:], bcast_rows(w["ln_b"].tensor, w["ln_b"].offset, D), writes=[b_bbc])
    P.op("dve", lambda e: e.memset(ones1[:], 1.0), writes=[b_ones1])

    nst = T // ST
    wslot = 0
    DBG = ()

    stg = [P.sb(f"t_stg{i}", [128, 4, 512], F32) for i in range(3)]
    b_stg = [B() for _ in range(3)]
    stg_i = [0]

    def load_chunk(dst, bdst, src):
        k = stg_i[0] % 3
        stg_i[0] += 1
        P.dma("sp", stg[k][:], src, writes=[b_stg[k]])
        P.op("act", lambda en, k=k, dst=dst: en.copy(dst, stg[k][:]), reads=[b_stg[k]], writes=[bdst])

    def load_expert(e, slot):
        for h in range(2):
            load_chunk(wgb[slot][:, 4 * h:4 * h + 4, :], b_wg[slot],
                       w["wg"][e][512 * h:512 * h + 512, :].rearrange("(c p) h -> p c h", p=128))
        for h in range(2):
            load_chunk(wub[slot][:, 4 * h:4 * h + 4, :], b_wu[slot],
                       w["wu"][e][512 * h:512 * h + 512, :].rearrange("(c p) h -> p c h", p=128))
        for h in range(2):
            load_chunk(wdb[slot][:, :, 512 * h:512 * h + 512], b_wd[slot],
                       w["wd"][e][:, 512 * h:512 * h + 512].rearrange("(c p) h -> p c h", p=128))

    xi = 0
    oi = 0
    for st in range(nst):
        t0 = st * ST
        if "noload" not in DBG:
            load_expert(0, wslot)
        for tt in range(NT):
            xb, bx = xin[xi % 2], b_xin[xi % 2]
            xi += 1
            P.dma("sp", xb[:], hmid[t0 + tt * 128:t0 + (tt + 1) * 128, :], reads=[hmid_buf], writes=[bx])
            P.op("act", lambda e, xb=xb, tt=tt: e.mul(acc[:, tt, :], xb[:], ALPHA), reads=[bx], writes=[b_acc[tt]])
            if 'notr' in DBG:
                continue
            for half in range(2):
                pt, bpt = ps_t[half], b_pst[half]
                for j in range(4):
                    dc = half * 4 + j
                    P.op("pe", lambda e, pt=pt, j=j, dc=dc, xb=xb: e.transpose(
                        pt[:, j * 128:(j + 1) * 128], xb[:, dc * 128:(dc + 1) * 128], ident[:]),
                        reads=[bx, b_ident], writes=[bpt])
                P.op("act", lambda e, pt=pt, half=half: e.copy(
                    hT32[:, half * 4:(half + 1) * 4, :], pt[:].rearrange("p (c t) -> p c t", c=4)),
                    reads=[bpt], writes=[b_hT32])
                P.op("dve", lambda e, pt=pt, half=half, tt=tt: e.tensor_copy(
                    hT16[:, half * 4:(half + 1) * 4, tt * 128:(tt + 1) * 128],
                    hT32[:, half * 4:(half + 1) * 4, :]),
                    reads=[b_hT32], writes=[b_hT16[tt]])
            if 'norouter' in DBG:
                continue
            pr, bpr = ps_o[0], b_pso[0]
            for dc in range(8):
                P.op("pe", lambda e, dc=dc, pr=pr: e.matmul(pr[:, 0:20], hT32[:, dc, :], wr[:, dc, :],
                                                            start=(dc == 0), stop=False),
                     reads=[b_hT32, b_wr], writes=[bpr])
            P.op("pe", lambda e, pr=pr: e.matmul(pr[:, 0:20], ones1[0:1, :], brow[0:1, :], start=False, stop=True),
                 reads=[b_ones1, b_brow], writes=[bpr])
            P.op("act", lambda e, pr=pr: e.copy(lg[:], pr[:, 0:20]), reads=[bpr], writes=[b_lg])
            emit_router(P, lg, b_lg, sm, b_sm, gates, b_gates[tt], tt)
        units = [(e, tk) for e in range(NEXP) for tk in range(NK)]

        def gu(ui):
            e, tk = units[ui]
            slot = (wslot + e) % 2
            hb = ui % 2
            for hc in range(4):
                pg, bpg = ps_g[hc % 2], b_psg[hc % 2]
                pu, bpu = ps_u[hc % 2], b_psu[hc % 2]
                rds = [b_hT16[tk * 4 + q] for q in range(4)]
                for dc in range(8):
                    P.op("pe", lambda en, pg=pg, dc=dc, hc=hc, slot=slot, tk=tk: en.matmul(
                        pg[:], wgb[slot][:, dc, hc * 128:(hc + 1) * 128], hT16[:, dc, tk * 512:(tk + 1) * 512],
                        start=(dc == 0), stop=(dc == 7)), reads=rds + [b_wg[slot]], writes=[bpg])
                for dc in range(8):
                    P.op("pe", lambda en, pu=pu, dc=dc, hc=hc, slot=slot, tk=tk: en.matmul(
                        pu[:], wub[slot][:, dc, hc * 128:(hc + 1) * 128], hT16[:, dc, tk * 512:(tk + 1) * 512],
                        start=(dc == 0), stop=(dc == 7)), reads=rds + [b_wu[slot]], writes=[bpu])
                s, bs = sg[hc % 2], b_sg[hc % 2]
                P.op("act", lambda en, s=s, pg=pg: en.activation(s[:], pg[:], AF.Silu), reads=[bpg], writes=[bs])
                P.op("dve", lambda en, s=s, pu=pu, hb=hb, hc=hc: en.tensor_tensor(
                    hid[hb][:, hc, :], s[:], pu[:], ALU.mult), reads=[bs, bpu], writes=[b_hid[hb][hc]])

        def down(ui):
            e, tk = units[ui]
            slot = (wslot + e) % 2
            hb = ui % 2
            k = 0
            for tq in range(4):
                tt = tk * 4 + tq
                for dh in range(2):
                    po, bpo = ps_o[k % 2], b_pso[k % 2]
                    k += 1
                    for hc in range(4):
                        P.op("pe", lambda en, po=po, hc=hc, tq=tq, dh=dh, hb=hb, slot=slot: en.matmul(
                            po[:], hid[hb][:, hc, tq * 128:(tq + 1) * 128], wdb[slot][:, hc, dh * 512:(dh + 1) * 512],
                            start=(hc == 0), stop=(hc == 3)), reads=[b_hid[hb][hc], b_wd[slot]], writes=[bpo])
                    P.op("dve", lambda en, po=po, tt=tt, dh=dh, e=e: en.scalar_tensor_tensor(
                        acc[:, tt, dh * 512:(dh + 1) * 512], po[:], gates[:, tt, e:e + 1],
                        acc[:, tt, dh * 512:(dh + 1) * 512], ALU.mult, ALU.add),
                        reads=[bpo, b_gates[tt], b_acc[tt]], writes=[b_acc[tt]])

        if "noexp" in DBG:
            units = []
        if "oneexp" in DBG:
            units = units[:2]
        for ui in range(len(units)):
            e, tk = units[ui]
            gu(ui)
            if ui > 0:
                down(ui - 1)
            if tk == 0 and e + 1 < NEXP:
                load_expert(e + 1, (wslot + e + 1) % 2)
        if units:
            down(len(units) - 1)
        wslot = (wslot + NEXP) % 2
        for tt in range(NT):
            ob, bo = obuf[oi % 2], b_ob[oi % 2]
            oi += 1
            if "nolnorm" in DBG:
                P.op("act", lambda e, ob=ob, tt=tt: e.copy(ob[:], acc[:, tt, :]), reads=[b_acc[tt]], writes=[bo])
            else:
                emit_ln(P, acc[:, tt, :], b_acc[tt], ob, bo, gbc, b_gbc, bbc, b_bbc, stats, b_stats, sm, b_sm)
            P.dma("sp", out(t0 + tt * 128), ob[:], reads=[bo], writes=[b_outw[(oi - 1) % 2]], is_out=is_final)


def emit_ln(P, x, bx, ob, bo, gbc, b_gbc, bbc, b_bbc, stats, b_stats, sm, b_sm):
    mv = sm[:, 40:42]
    rstd = sm[:, 42:43]
    nmr = sm[:, 43:44]
    P.op("dve", lambda e: e.bn_stats(stats[:, 0, :], x[:, 0:512]), reads=[bx], writes=[b_stats])
    P.op("dve", lambda e: e.bn_stats(stats[:, 1, :], x[:, 512:1024]), reads=[bx], writes=[b_stats])
    P.op("dve", lambda e: e.bn_aggr(mv, stats[:]), reads=[b_stats], writes=[b_sm])
    P.op("dve", lambda e: e.tensor_scalar(rstd, sm[:, 41:42], LN_EPS, None, ALU.add), reads=[b_sm], writes=[b_sm])
    P.op("act", lambda e: e.sqrt(rstd, rstd), reads=[b_sm], writes=[b_sm])
    P.op("dve", lambda e: e.reciprocal(rstd, rstd), reads=[b_sm], writes=[b_sm])
    P.op("dve", lambda e: e.scalar_tensor_tensor(nmr, sm[:, 40:41], -1.0, rstd, ALU.mult, ALU.mult), reads=[b_sm], writes=[b_sm])
    P.op("act", lambda e: e.activation(ob[:], x, AF.Identity, bias=nmr, scale=rstd), reads=[bx, b_sm], writes=[bo])
    P.op("dve", lambda e: e.tensor_tensor(ob[:], ob[:], gbc[:], ALU.mult), reads=[bo, b_gbc], writes=[bo])
    P.op("dve", lambda e: e.tensor_tensor(ob[:], ob[:], bbc[:], ALU.add), reads=[bo, b_bbc], writes=[bo])


def emit_router(P, lg, b_lg, sm, b_sm, gates, b_g, tt):
    gmax, ngmax, gsum, gw = sm[:, 0:1], sm[:, 1:2], sm[:, 2:3], sm[:, 3:4]
    ohg, gexp, esel, oh1 = sm[:, 4:8], sm[:, 8:12], sm[:, 12:16], sm[:, 16:20]
    em, oh2 = sm[:, 20:24], sm[:, 24:28]
    m1, m2, dd, ed, den, w1, w2 = (sm[:, 28 + i:29 + i] for i in range(7))
    ew = sm[:, 36:40]
    R = [b_lg, b_sm]
    W = [b_sm]

    def dv(fn, reads=R, writes=W):
        P.op("dve", fn, reads=reads, writes=writes)

    dv(lambda e: e.tensor_reduce(gmax, lg[:, 0:4], AX.X, ALU.max))
    dv(lambda e: e.tensor_scalar(ohg, lg[:, 0:4], gmax, None, ALU.is_equal))
    dv(lambda e: e.tensor_scalar(ngmax, gmax, -1.0, None, ALU.mult))
    P.op("act", lambda e: e.activation(gexp, lg[:, 0:4], AF.Exp, bias=ngmax, scale=1.0), reads=R, writes=W)
    dv(lambda e: e.tensor_reduce(gsum, gexp, AX.X, ALU.add))
    dv(lambda e: e.reciprocal(gw, gsum))
    dv(lambda e: e.tensor_scalar(esel, lg[:, 4:8], ohg[:, 0:1], None, ALU.mult))
    for g in range(1, 4):
        dv(lambda e, g=g: e.scalar_tensor_tensor(esel, lg[:, 4 + 4 * g:8 + 4 * g], ohg[:, g:g + 1], esel, ALU.mult, ALU.add))
    dv(lambda e: e.tensor_reduce(m1, esel, AX.X, ALU.max))
    dv(lambda e: e.tensor_scalar(oh1, esel, m1, None, ALU.is_equal))
    dv(lambda e: e.scalar_tensor_tensor(em, oh1, -1e30, esel, ALU.mult, ALU.add))
    dv(lambda e: e.tensor_reduce(m2, em, AX.X, ALU.max))
    dv(lambda e: e.tensor_scalar(oh2, em, m2, None, ALU.is_equal))
    dv(lambda e: e.tensor_tensor(dd, m2, m1, ALU.subtract))
    P.op("act", lambda e: e.activation(ed, dd, AF.Exp), reads=R, writes=W)
    dv(lambda e: e.tensor_scalar(den, ed, 1.0, None, ALU.add))
    dv(lambda e: e.reciprocal(den, den))
    dv(lambda e: e.tensor_tensor(w1, den, gw, ALU.mult))
    dv(lambda e: e.tensor_tensor(w2, w1, ed, ALU.mult))
    dv(lambda e: e.tensor_scalar(ew, oh1, w1, None, ALU.mult))
    dv(lambda e: e.scalar_tensor_tensor(ew, oh2, w2, ew, ALU.mult, ALU.add))
    for g in range(4):
        dv(lambda e, g=g: e.tensor_scalar(gates[:, tt, 4 * g:4 * g + 4], ew, ohg[:, g:g + 1], None, ALU.mult),
           reads=[b_sm], writes=[b_g])


def t5_bucket_np(dist):
    n = np.maximum(dist, 0)
    nf = np.maximum(n, 1).astype(np.float32)
    large = 16 + (np.log(nf / np.float32(16)) / np.float32(np.log(2048 / 16)) * np.float32(16)).astype(np.int32)
    return np.where(n < 16, n, np.minimum(large, 31))


def dil_onehot():
    oh = np.zeros((33, 3, 384), np.float32)
    for di, d in enumerate((1, 4, 16)):
        for jp in range(384):
            j = jp - 127
            if 0 <= j <= 128:
                oh[t5_bucket_np(np.array(j * d)), di, jp] = 1.0
            else:
                oh[32, di, jp] = 1.0
    return oh


class Banks:
    def __init__(self, P):
        self.f = [P.ps(f"bank{i}", [128, 512], F32) for i in range(7)]
        self.bf = [P.buf() for _ in range(7)]
        self.h = P.ps("bankh", [128, 1024], BF16)
        self.bh = P.buf()


def full_barrier(P):
    for q in ("sp", "pool"):
        eq = P.eng[q]
        for key, (k_, sh, val) in list(P.dmasems.items()):
            if P.issuer.get(key) == q and P.known[q].get(key, 0) < val:
                eq.wait_ge(sh, val)
                P.known[q][key] = val
        eq.sem_inc(P.sem[q], 1)
        P.cnt[q] += 1
    for e in P.eng:
        for f in P.eng:
            if f != e and P.cnt[f] > P.known[e].get(f, 0):
                P.eng[e].wait_ge(P.sem[f], P.cnt[f])
                P.known[e][f] = P.cnt[f]
    for e in P.eng:
        for (key, sh, val) in P.dmasems.values():
            P.known[e][key] = max(P.known[e].get(key, 0), val)


class Pipe:
    def __init__(self):
        self.pending = None

    def step(self, fn):
        prev, self.pending = self.pending, fn
        if prev is not None:
            prev()

    def flush(self):
        if self.pending is not None:
            self.pending()
            self.pending = None


class Stager:
    def __init__(self, P, n=3, cols=2048):
        self.P = P
        self.cols = cols
        self.t = [P.sb(f"stg{i}", [128, cols], F32) for i in range(n)]
        self.b = [P.buf() for _ in range(n)]
        self.i = 0

    def load(self, dst3, bdst, src2d, eng="act"):
        C, W = dst3.shape[1], dst3.shape[2]
        per = max(1, self.cols // W)
        c0 = 0
        while c0 < C:
            c1 = min(C, c0 + per)
            k = self.i % len(self.t)
            self.i += 1
            st = self.t[k][:, 0:(c1 - c0) * W].rearrange("p (c w) -> p c w", w=W)
            self.P.dma("sp", st, src2d[c0 * 128:c1 * 128, :].rearrange("(c p) w -> p c w", p=128), writes=[self.b[k]])
            self.P.op(eng, lambda en, st=st, c0=c0, c1=c1: en.copy(dst3[:, c0:c1, :], st),
                      reads=[self.b[k]], writes=[bdst])
            c0 = c1


def load_xT(P, BK, x, bx_dram, xin, b_xin, xT, b_xT, ident, b_ident, t0, ntile, ctr):
    for tl in range(ntile):
        k = ctr[0] % 2
        ctr[0] += 1
        xb, bx = xin[k], b_xin[k]
        P.dma("sp", xb[:], x(t0 + tl * 128), reads=[bx_dram], writes=[bx])
        for half in range(2):
            pt, bpt = BK.f[half], BK.bf[half]
            for j in range(4):
                dc = half * 4 + j
                P.op("pe", lambda e, pt=pt, j=j, dc=dc, xb=xb: e.transpose(
                    pt[:, j * 128:(j + 1) * 128], xb[:, dc * 128:(dc + 1) * 128], ident[:]),
                    reads=[bx, b_ident], writes=[bpt])
            eng = "act" if half == 0 else "dve"
            if eng == "act":
                P.op("act", lambda e, pt=pt, half=half, tl=tl: e.copy(
                    xT[:, half * 4:(half + 1) * 4, tl * 128:(tl + 1) * 128], pt[:].rearrange("p (c t) -> p c t", c=4)),
                    reads=[bpt], writes=[b_xT[tl]])
            else:
                P.op("dve", lambda e, pt=pt, half=half, tl=tl: e.tensor_copy(
                    xT[:, half * 4:(half + 1) * 4, tl * 128:(tl + 1) * 128], pt[:].rearrange("p (c t) -> p c t", c=4)),
                    reads=[bpt], writes=[b_xT[tl]])


S_TOT = 8192
NBLK = 4
BLK = 2048


def declare_mixer0(nc, pfx=""):
    din = lambda n, s: nc.dram_tensor(pfx + n, s, F32, kind="ExternalInput").ap()
    return dict(x=din("x", [S_TOT, D]), wA=din("wA", [D, 768]), wB=din("wB", [D, 1028]), gbias=din("gbias", [1, 4]),
                convw=din("convw", [128, 16]), hng=din("hng", [1, 256]), relb=din("relb", [33, 4]), oh=din("oh", [33, 3 * 384]),
                triu=din("triu", [128, 128]),
                scr=nc.dram_tensor(pfx + "scr_e", [128, 12 * 384], F32).ap())


def emit_mixer0(P, io):
    nc = P.nc
    x_d, wA, wB, gbias, convw, hng, relb = io["x"], io["wA"], io["wB"], io["gbias"], io["convw"], io["hng"], io["relb"]
    oh_d, ident_d, triu_d, scr = io["oh"], io["ident"], io["triu"], io["scr"]
    x = lambda t0: x_d[t0:t0 + 128, :]
    ystore = io["ystore"]
    if True:
        B = P.buf
        BK = Banks(P)
        b_x, b_y, b_scr = B(), B(), B()
        ident = P.sb("ident_sb", [128, 128], F32)
        b_ident = B()
        P.dma("sp", ident[:], ident_d[:, :], writes=[b_ident])
        xin = [P.sb(f"xin{i}", [128, D], F32) for i in range(2)]
        b_xin = [B(), B()]
        xT = P.sb("xT", [128, 8, BLK], BF16)
        b_xT = [B() for _ in range(16)]
        stg = Stager(P)
        ctr = [0]
        ones_bf = P.sb("ones_bf", [128, 128], BF16)
        b_ones = B()
        P.op("dve", lambda e: e.memset(ones_bf[:], 1.0), writes=[b_ones])

        with ExitStack() as esA:
            sbA = lambda n, s, d: esA.enter_context(nc.sbuf_tensor(P.pfx + n, s, d))
            Etab = sbA("a_E", [128, 12, 2, 128], BF16)
            with ExitStack() as esT:
                sbA_outer = sbA
                sbA = lambda n, s, d: esT.enter_context(nc.sbuf_tensor(P.pfx + n, s, d))
                tab = sbA("a_tab", [33, 4], F32)
                ohs = sbA("a_oh", [33, 3 * 384], F32)
                tabB = sbA("a_tabB", [33, 128], F32)
                fre = sbA("a_fre", [128, 12 * 384], F32)
                E32 = sbA("a_E32", [128, 12 * 256], F32)
                b_tab, b_oh, b_tabB, b_fre, b_E32, b_E = B(), B(), B(), B(), B(), B()
                P.dma("sp", tab[:], relb[:, :], writes=[b_tab])
                P.dma("sp", ohs[:], oh_d[:, :], writes=[b_oh])
                for h in range(4):
                    P.op("dve", lambda e, h=h: e.tensor_copy(tabB[:], tab[:, h:h + 1].to_broadcast([33, 128])),
                         reads=[b_tab], writes=[b_tabB])
                    for di in range(3):
                        pb, bpb = BK.f[2 + (di % 2)], BK.bf[2 + (di % 2)]
                        P.op("pe", lambda e, pb=pb, di=di: e.matmul(pb[:, 0:384], tabB[:], ohs[:, di * 384:(di + 1) * 384],
                                                                    start=True, stop=True),
                             reads=[b_tabB, b_oh], writes=[bpb])
                        P.op("act", lambda e, pb=pb, di=di, h=h: e.activation(
                            fre[:, (di * 4 + h) * 384:(di * 4 + h + 1) * 384], pb[:, 0:384], AF.Exp),
                            reads=[bpb], writes=[b_fre])
                P.dma("sp", scr[:, :], fre[:], reads=[b_fre], writes=[b_scr])
                L = 12 * 384
                skew = bass.AP(scr.tensor, 127, [[L - 1, 128], [384, 12], [128, 2], [1, 128]])
                P.dma("sp", E32[:].rearrange("p (t s q) -> p t s q", t=12, s=2), skew, reads=[b_scr], writes=[b_E32])
                P.op("dve", lambda e: e.tensor_copy(Etab[:].rearrange("p t s q -> p (t s q)"), E32[:]),
                     reads=[b_E32], writes=[b_E])

                full_barrier(P)
            sbA = sbA_outer
            wAb = sbA("a_w", [128, 8, 384], BF16)
            b_wA = B()
            QT = sbA("a_QT", [128, S_TOT], BF16)
            KT = sbA("a_KT", [128, S_TOT], BF16)
            b_QT = [B() for _ in range(NBLK)]
            b_KT = [B() for _ in range(NBLK)]
            Vd = [sbA(f"a_V{di}", [128, 64, 128], BF16) for di in range(3)]
            b_Vd = [[B() for _ in range(NBLK)] for _ in range(3)]
            numacc = sbA("a_num", [128, BLK], F32)
            denacc = sbA("a_den", [128, BLK], F32)
            b_num, b_den = B(), B()
            Pb = [sbA(f"a_P{i}", [128, 2, 2, 128], BF16) for i in range(2)]
            b_Pb = [B(), B()]
            ybuf = sbA("a_y", [128, BLK], BF16)
            b_yb = B()
            pcount = 0
            pipeA = Pipe()
            for hp in range(2):
                stg.load(wAb[:], b_wA, wA[:, hp * 384:(hp + 1) * 384])
                for blk in range(NBLK):
                    T0 = blk * BLK
                    load_xT(P, BK, x, b_x, xin, b_xin, xT, b_xT, ident, b_ident, T0, 16, ctr)
                    for qtr in range(4):
                        rds = [b_xT[qtr * 4 + i] for i in range(4)] + [b_wA]
                        for sel, (dst, bd) in enumerate(((QT, b_QT[blk]), (KT, b_KT[blk]))):
                            pb, bpb = BK.f[2 + sel], BK.bf[2 + sel]
                            for dc in range(8):
                                P.op("pe", lambda e, pb=pb, dc=dc, sel=sel, qtr=qtr: e.matmul(
                                    pb[:], wAb[:, dc, sel * 128:(sel + 1) * 128], xT[:, dc, qtr * 512:(qtr + 1) * 512],
                                    start=(dc == 0), stop=(dc == 7)), reads=rds, writes=[bpb])
                            if sel == 0:
                                P.op("act", lambda e, pb=pb, dst=dst, T0=T0, qtr=qtr: e.copy(
                                    dst[:, T0 + qtr * 512:T0 + (qtr + 1) * 512], pb[:]), reads=[bpb], writes=[bd])
                            else:
                                P.op("dve", lambda e, pb=pb, dst=dst, T0=T0, qtr=qtr: e.tensor_copy(
                                    dst[:, T0 + qtr * 512:T0 + (qtr + 1) * 512], pb[:]), reads=[bpb], writes=[bd])
                    for di, d in enumerate((1, 4, 16)):
                        npb = 16 // d
                        tiles = [(nl, r) for nl in range(npb) for r in range(d)]
                        for g4 in range(4):
                            pb, bpb = BK.f[2 + (g4 % 2)], BK.bf[2 + (g4 % 2)]
                            for j in range(4):
                                nl, r = tiles[g4 * 4 + j]
                                s0 = r + d * 128 * nl
                                for dc in range(8):
                                    P.op("pe", lambda e, pb=pb, j=j, dc=dc, s0=s0, d=d: e.matmul(
                                        pb[:, j * 128:(j + 1) * 128], xT[:, dc, s0:s0 + 127 * d + 1:d], wAb[:, dc, 256:384],
                                        start=(dc == 0), stop=(dc == 7)), reads=b_xT + [b_wA], writes=[bpb])
                            nl0, r0 = tiles[g4 * 4]
                            idx0 = (blk * npb + nl0) * d + r0
                            P.op("act" if g4 % 2 == 0 else "dve",
                                 (lambda e, pb=pb, di=di, idx0=idx0: e.copy(
                                     Vd[di][:, idx0:idx0 + 4, :], pb[:].rearrange("p (j c) -> p j c", j=4)))
                                 if g4 % 2 == 0 else
                                 (lambda e, pb=pb, di=di, idx0=idx0: e.tensor_copy(
                                     Vd[di][:, idx0:idx0 + 4, :], pb[:].rearrange("p (j c) -> p j c", j=4))),
                                 reads=[bpb], writes=[b_Vd[di][blk]])
                    for di, d in enumerate((1, 4, 16)):
                        npb = 16 // d
                        for nl in range(npb):
                            n = blk * npb + nl
                            nslot = 2 if n > 0 else 1
                            for r in range(d):
                                qs = T0 + r + d * 128 * nl
                                ql = r + d * 128 * nl
                                par = pcount % 2
                                ps_h = [BK.f[2 + 2 * par + h] for h in range(2)]
                                bps_h = [BK.bf[2 + 2 * par + h] for h in range(2)]
                                pb_, bpb_ = Pb[pcount % 2], b_Pb[pcount % 2]
                                pcount += 1
                                kblks = sorted({blk, (qs - d * 128) // BLK}) if n > 0 else [blk]
                                rds = [b_QT[blk]] + [b_KT[kb] for kb in kblks]
                                for h in range(2):
                                    for sl in range(nslot):
                                        ks = qs - sl * d * 128
                                        P.op("pe", lambda e, h=h, sl=sl, ks=ks, qs=qs, d=d, ps_h=ps_h: e.matmul(
                                            ps_h[h][:, sl * 128:(sl + 1) * 128],
                                            KT[h * 64:(h + 1) * 64, ks:ks + 127 * d + 1:d],
                                            QT[h * 64:(h + 1) * 64, qs:qs + 127 * d + 1:d], start=True, stop=True),
                                            reads=rds, writes=[bps_h[h]])
                                for h in range(2):
                                    P.op("act", lambda e, pb_=pb_, h=h, nslot=nslot, ps_h=ps_h: e.activation(
                                        pb_[:, h, 0:nslot, :], ps_h[h][:, 0:nslot * 128].rearrange("p (s q) -> p s q", s=nslot),
                                        AF.Exp, scale=0.125), reads=[bps_h[h]], writes=[bpb_])
                                P.op("dve", lambda e, pb_=pb_, di=di, hp=hp, nslot=nslot: e.tensor_tensor(
                                    pb_[:, :, 0:nslot, :], pb_[:, :, 0:nslot, :],
                                    Etab[:, di * 4 + hp * 2:di * 4 + hp * 2 + 2, 0:nslot, :], ALU.mult),
                                    reads=[bpb_, b_E], writes=[bpb_])
                                def stageB(di=di, d=d, n=n, r=r, nslot=nslot, pb_=pb_, bpb_=bpb_, kblks=kblks, ql=ql, u=pcount):
                                    pv, bpv = (BK.f[6], BK.bf[6]) if u % 2 == 0 else (BK.f[0], BK.bf[0])
                                    vrd = [b_Vd[di][kb] for kb in kblks]
                                    for h in range(2):
                                        for sl in range(nslot):
                                            kidx = (n - sl) * d + r
                                            P.op("pe", lambda e, pv=pv, h=h, sl=sl, kidx=kidx, di=di, pb_=pb_, nslot=nslot: e.matmul(
                                                pv[:, h * 128:(h + 1) * 128], Vd[di][:, kidx, :], pb_[:, h, sl, :],
                                                start=(sl == 0), stop=(sl == nslot - 1)), reads=vrd + [bpb_], writes=[bpv])
                                        for sl in range(nslot):
                                            P.op("pe", lambda e, pv=pv, h=h, sl=sl, pb_=pb_, nslot=nslot: e.matmul(
                                                pv[:, (2 + h) * 128:(3 + h) * 128], ones_bf[:], pb_[:, h, sl, :],
                                                start=(sl == 0), stop=(sl == nslot - 1)), reads=[b_ones, bpb_], writes=[bpv])
                                    for h in range(2):
                                        pr = slice(h * 64, (h + 1) * 64)
                                        qc = slice(ql, ql + 127 * d + 1, d)
                                        if di == 0:
                                            P.op("dve", lambda e, pr=pr, qc=qc, h=h: e.tensor_copy(
                                                numacc[pr, qc], pv[pr, h * 128:(h + 1) * 128]), reads=[bpv], writes=[b_num])
                                            P.op("dve", lambda e, pr=pr, qc=qc, h=h: e.tensor_copy(
                                                denacc[pr, qc], pv[pr, (2 + h) * 128:(3 + h) * 128]), reads=[bpv], writes=[b_den])
                                        else:
                                            P.op("dve", lambda e, pr=pr, qc=qc, h=h: e.tensor_tensor(
                                                numacc[pr, qc], numacc[pr, qc], pv[pr, h * 128:(h + 1) * 128], ALU.add),
                                                reads=[bpv, b_num], writes=[b_num])
                                            P.op("dve", lambda e, pr=pr, qc=qc, h=h: e.tensor_tensor(
                                                denacc[pr, qc], denacc[pr, qc], pv[pr, (2 + h) * 128:(3 + h) * 128], ALU.add),
                                                reads=[bpv, b_den], writes=[b_den])
                                pipeA.step(stageB)
                    pipeA.flush()
                    P.op("dve", lambda e: e.reciprocal(denacc[:], denacc[:]), reads=[b_den], writes=[b_den])
                    P.op("dve", lambda e: e.tensor_tensor(ybuf[:], numacc[:], denacc[:], ALU.mult),
                         reads=[b_num, b_den], writes=[b_yb])
                    ystore(P, hp, blk, ybuf[:], [b_yb])
            full_barrier(P)

        with ExitStack() as esB:
            sbB = lambda n, s, d: esB.enter_context(nc.sbuf_tensor(P.pfx + n, s, d))
            SC = 128.0 ** -0.5
            wBb = sbB("b_w", [128, 8, 1028], BF16)
            b_wB = B()
            stg.load(wBb[:], b_wB, wB[:, :])
            pc = sbB("b_pc", [128, 4, BLK + 3], F32)
            b_pc = [B() for _ in range(4)]
            ctmp = [sbB(f"b_ct{i}", [128, BLK], F32) for i in range(2)]
            b_ct = [B(), B()]
            QKb = sbB("b_qk", [128, 4, BLK], BF16)
            b_qk = [B() for _ in range(4)]
            cw = sbB("b_cw", [128, 16], F32)
            hngb = sbB("b_hng", [128, 256], F32)
            gbb = sbB("b_gb", [128, 4], F32)
            triu32 = sbB("b_triu32", [128, 128], F32)
            triub = sbB("b_triub", [128, 128], BF16)
            ones32 = sbB("b_ones32", [128, 128], F32)
            identb = sbB("b_identb", [128, 128], BF16)
            b_cst = B()
            P.dma("sp", cw[:], convw[:, :], writes=[b_cst])
            b_c2, b_c3, b_c4 = B(), B(), B()
            P.dma("sp", hngb[:], bcast_rows(hng.tensor, hng.offset, 256), writes=[b_c2])
            P.dma("sp", gbb[:], bcast_rows(gbias.tensor, gbias.offset, 4), writes=[b_c3])
            P.dma("sp", triu32[:], triu_d[:, :], writes=[b_c4])
            b_c5 = B()
            P.op("dve", lambda e: e.tensor_copy(triub[:], triu32[:]), reads=[b_c4], writes=[b_c5])
            P.op("dve", lambda e: e.memset(ones32[:], 1.0), writes=[b_c5])
            P.op("dve", lambda e: e.tensor_copy(identb[:], ident[:]), reads=[b_ident], writes=[b_c5])
            CST = [b_cst, b_c2, b_c3, b_c4, b_c5]
            Vaug = [sbB(f"b_va{i}", [128, 2, 129], BF16) for i in range(2)]
            b_va = [B(), B()]
            sgo = [sbB(f"b_sgo{i}", [128, 256], F32) for i in range(2)]
            b_sgo = [B(), B()]
            smB = [sbB(f"b_sm{i}", [128, 64], F32) for i in range(2)]
            b_smB = [B(), B()]
            Sm = [sbB(f"b_Sm{i}", [128, 128], BF16) for i in range(2)]
            b_Sm = [B(), B()]
            hh = [sbB(f"b_hh{i}", [128, 128], F32) for i in range(2)]
            b_hh = [B(), B()]
            yn32 = [sbB(f"b_yn{i}", [128, 128], F32) for i in range(2)]
            b_yn = [B(), B()]
            ynb = [sbB(f"b_ynb{i}", [128, 128], BF16) for i in range(2)]
            b_ynb = [B(), B()]
            Ktm = [sbB(f"b_ktm{i}", [128, 128], BF16) for i in range(2)]
            b_ktm = [B(), B()]
            Vw = [sbB(f"b_vw{i}", [128, 129], BF16) for i in range(2)]
            b_vw = [B(), B()]
            C32 = sbB("b_C32", [128, 2, 129], F32)
            Cb = sbB("b_Cb", [128, 2, 129], BF16)
            b_C32, b_Cb = [B(), B()], [B(), B()]
            hst = [sbB(f"b_hst{i}", [128, 16], F32) for i in range(2)]
            b_hst = [B(), B()]
            ybufB = sbB("b_yb", [128, 2, BLK], BF16)
            b_ybB = B()
            P.op("dve", lambda e: e.memset(C32[:], 0.0), writes=b_C32)
            P.op("dve", lambda e: e.memset(Cb[:], 0.0), writes=b_Cb)
            for k in range(2):
                P.op("dve", lambda e, k=k: e.memset(Vaug[k][:, :, 128:129], 1.0), writes=[b_va[k]])
            for g in range(4):
                P.op("dve", lambda e, g=g: e.memset(pc[:, g, 0:3], 0.0), writes=[b_pc[g]])
            f0, f1, f2, f3, f4, f5, f6 = BK.f
            bf0, bf1, bf2, bf3, bf4, bf5, bf6 = BK.bf
            cti = 0
            for blk in range(NBLK):
                T0 = blk * BLK
                load_xT(P, BK, x, b_x, xin, b_xin, xT, b_xT, ident, b_ident, T0, 16, ctr)
                for g in range(4):
                    for qtr in range(4):
                        pb, bpb = (f0, bf0) if g % 2 == 0 else (f1, bf1)
                        rds = [b_xT[qtr * 4 + i] for i in range(4)] + [b_wB]
                        for dc in range(8):
                            P.op("pe", lambda e, pb=pb, dc=dc, g=g, qtr=qtr: e.matmul(
                                pb[:], wBb[:, dc, g * 128:(g + 1) * 128], xT[:, dc, qtr * 512:(qtr + 1) * 512],
                                start=(dc == 0), stop=(dc == 7)), reads=rds, writes=[bpb])
                        if g % 2 == 0:
                            P.op("act", lambda e, pb=pb, g=g, qtr=qtr: e.copy(pc[:, g, 3 + qtr * 512:3 + (qtr + 1) * 512], pb[:]),
                                 reads=[bpb], writes=[b_pc[g]])
                        else:
                            P.op("dve", lambda e, pb=pb, g=g, qtr=qtr: e.tensor_copy(pc[:, g, 3 + qtr * 512:3 + (qtr + 1) * 512], pb[:]),
                                 reads=[bpb], writes=[b_pc[g]])
                for g in range(4):
                    ct, bct = ctmp[cti % 2], b_ct[cti % 2]
                    cti += 1
                    P.op("dve", lambda e, ct=ct, g=g: e.tensor_scalar(ct[:], pc[:, g, 3:3 + BLK], cw[:, g * 4 + 3:g * 4 + 4], None, ALU.mult),
                         reads=[b_pc[g], b_cst], writes=[bct])
                    for j in (2, 1, 0):
                        P.op("dve", lambda e, ct=ct, g=g, j=j: e.scalar_tensor_tensor(
                            ct[:], pc[:, g, j:j + BLK], cw[:, g * 4 + j:g * 4 + j + 1], ct[:], ALU.mult, ALU.add),
                            reads=[b_pc[g], b_cst, bct], writes=[bct])
                    P.op("act", lambda e, ct=ct, g=g: e.activation(QKb[:, g, :], ct[:], AF.Silu), reads=[bct], writes=[b_qk[g]])
                    P.op("dve", lambda e, g=g: e.tensor_copy(pc[:, g, 0:3], pc[:, g, BLK:BLK + 3]), reads=[b_pc[g]], writes=[b_pc[g]])
                for ch in range(16):
                    k = ch % 2
                    tsl = slice(ch * 128, (ch + 1) * 128)
                    va, bva, sg_, bsg, sm_, bsm = Vaug[k], b_va[k], sgo[k], b_sgo[k], smB[k], b_smB[k]
                    gt, e1, sp_, cs = sm_[:, 0:4], sm_[:, 4:6], sm_[:, 6:8], sm_[:, 8:12]
                    ti, ws, wsq, eb, ebL = sm_[:, 12:14], sm_[:, 14:16], sm_[:, 16:18], sm_[:, 18:20], sm_[:, 20:22]
                    rdx = [b_xT[ch], b_wB]
                    for (bank, bbank, c0, c1, w0) in ((f2, bf2, 0, 256, 512), (f2, bf2, 256, 260, 1024), (f3, bf3, 0, 256, 768)):
                        for dc in range(8):
                            P.op("pe", lambda e, bank=bank, c0=c0, c1=c1, w0=w0, dc=dc, tsl=tsl: e.matmul(
                                bank[:, c0:c1], xT[:, dc, tsl], wBb[:, dc, w0:w0 + (c1 - c0)],
                                start=(dc == 0), stop=(dc == 7)), reads=rdx, writes=[bbank])
                    P.op("dve", lambda e, va=va: e.tensor_copy(va[:, :, 0:128], f2[:, 0:256].rearrange("p (h c) -> p h c", h=2)),
                         reads=[bf2], writes=[bva])
                    P.op("dve", lambda e, gt=gt: e.tensor_tensor(gt, f2[:, 256:260], gbb[:], ALU.add), reads=[bf2] + CST, writes=[bsm])
                    P.op("act", lambda e, sg_=sg_: e.activation(sg_[:], f3[:, 0:256], AF.Sigmoid), reads=[bf3], writes=[bsg])
                    P.op("act", lambda e, e1=e1, gt=gt: e.activation(e1, gt[:, 2:4], AF.Exp, scale=-1.0), reads=[bsm], writes=[bsm])
                    P.op("act", lambda e, e1=e1, sp_=sp_: e.activation(sp_, e1, AF.Ln, bias=1.0), reads=[bsm], writes=[bsm])
                    P.op("pe", lambda e, sp_=sp_: e.matmul(f3[:, 256:258], triu32[:], sp_, start=True, stop=True), reads=[bsm] + CST, writes=[bf3])
                    P.op("pe", lambda e, sp_=sp_: e.matmul(f3[:, 258:260], ones32[:], sp_, start=True, stop=True), reads=[bsm] + CST, writes=[bf3])
                    P.op("act", lambda e, cs=cs: e.copy(cs, f3[:, 256:260]), reads=[bf3], writes=[bsm])
                    P.op("dve", lambda e, ti=ti, gt=gt, cs=cs: e.tensor_tensor(ti, gt[:, 0:2], cs[:, 0:2], ALU.add), reads=[bsm], writes=[bsm])
                    P.op("act", lambda e, ws=ws, ti=ti: e.activation(ws, ti, AF.Exp), reads=[bsm], writes=[bsm])
                    P.op("act", lambda e, eb=eb, cs=cs: e.activation(eb, cs[:, 0:2], AF.Exp), reads=[bsm], writes=[bsm])
                    P.op("act", lambda e, ebL=ebL, cs=cs: e.activation(ebL, cs[:, 2:4], AF.Exp, scale=-1.0), reads=[bsm], writes=[bsm])
                    P.op("dve", lambda e, wsq=wsq, ws=ws: e.tensor_scalar(wsq, ws, SC, None, ALU.mult), reads=[bsm], writes=[bsm])
                    for h in range(2):
                        st_, bst = hst[h], b_hst[h]
                        P.op("pe", lambda e, h=h, tsl=tsl: e.matmul(f4[:, h * 128:(h + 1) * 128], QKb[:, 2 + h, tsl], QKb[:, h, tsl],
                                                                     start=True, stop=True), reads=[b_qk[h], b_qk[2 + h]], writes=[bf4])
                        P.op("dve", lambda e, h=h, wsq=wsq: e.scalar_tensor_tensor(
                            Sm[h][:], f4[:, h * 128:(h + 1) * 128], wsq[:, h:h + 1], triub[:], ALU.mult, ALU.mult),
                            reads=[bf4, bsm] + CST, writes=[b_Sm[h]])
                        xs_ = f5[:, h * 256:h * 256 + 129]
                        P.op("pe", lambda e, h=h, xs_=xs_, va=va: e.matmul(xs_, Sm[h][:], va[:, h, :], start=True, stop=False),
                             reads=[b_Sm[h], bva], writes=[bf5])
                        P.op("pe", lambda e, h=h, xs_=xs_, tsl=tsl: e.matmul(xs_, QKb[:, h, tsl], Cb[:, h, :], start=False, stop=True),
                             reads=[b_qk[h], b_Cb[h]], writes=[bf5])
                        ax, r_, mv, rstd, nmr = st_[:, 0:1], st_[:, 1:2], st_[:, 2:4], st_[:, 4:5], st_[:, 5:6]
                        stat6 = st_[:, 8:14]
                        P.op("dve", lambda e, ax=ax, h=h: e.tensor_scalar(ax, f5[:, h * 256 + 128:h * 256 + 129], -1.0, None, ALU.mult),
                             reads=[bf5], writes=[bst])
                        P.op("dve", lambda e, ax=ax, h=h: e.tensor_tensor(ax, ax, f5[:, h * 256 + 128:h * 256 + 129], ALU.max),
                             reads=[bf5, bst], writes=[bst])
                        P.op("dve", lambda e, ax=ax, eb=eb, h=h: e.tensor_tensor(ax, ax, eb[:, h:h + 1], ALU.max), reads=[bst, bsm], writes=[bst])
                        P.op("dve", lambda e, ax=ax, r_=r_: e.reciprocal(r_, ax), reads=[bst], writes=[bst])
                        P.op("dve", lambda e, h=h, r_=r_: e.tensor_scalar(hh[h][:], f5[:, h * 256:h * 256 + 128], r_, None, ALU.mult),
                             reads=[bf5, bst], writes=[b_hh[h]])
                        P.op("dve", lambda e, h=h, stat6=stat6: e.bn_stats(stat6, hh[h][:]), reads=[b_hh[h]], writes=[bst])
                        P.op("dve", lambda e, mv=mv, stat6=stat6: e.bn_aggr(mv, stat6), reads=[bst], writes=[bst])
                        P.op("dve", lambda e, rstd=rstd, mv=mv: e.tensor_scalar(rstd, mv[:, 1:2], LN_EPS, None, ALU.add), reads=[bst], writes=[bst])
                        P.op("act", lambda e, rstd=rstd: e.sqrt(rstd, rstd), reads=[bst], writes=[bst])
                        P.op("dve", lambda e, rstd=rstd: e.reciprocal(rstd, rstd), reads=[bst], writes=[bst])
                        P.op("dve", lambda e, nmr=nmr, mv=mv, rstd=rstd: e.scalar_tensor_tensor(nmr, mv[:, 0:1], -1.0, rstd, ALU.mult, ALU.mult),
                             reads=[bst], writes=[bst])
                        P.op("act", lambda e, h=h, nmr=nmr, rstd=rstd: e.activation(yn32[h][:], hh[h][:], AF.Identity, bias=nmr, scale=rstd),
                             reads=[b_hh[h], bst], writes=[b_yn[h]])
                        P.op("dve", lambda e, h=h: e.tensor_tensor(yn32[h][:], yn32[h][:], hngb[:, h * 128:(h + 1) * 128], ALU.mult),
                             reads=[b_yn[h]] + CST, writes=[b_yn[h]])
                        P.op("dve", lambda e, h=h, sg_=sg_: e.tensor_tensor(ynb[h][:], yn32[h][:], sg_[:, h * 128:(h + 1) * 128], ALU.mult),
                             reads=[b_yn[h], bsg], writes=[b_ynb[h]])
                        P.op("pe", lambda e, h=h: e.transpose(BK.h[:, h * 256:h * 256 + 128], ynb[h][:], identb[:]),
                             reads=[b_ynb[h]] + CST, writes=[BK.bh])
                        P.op("act", lambda e, h=h, tsl=tsl: e.copy(ybufB[:, h, tsl], BK.h[:, h * 256:h * 256 + 128]),
                             reads=[BK.bh], writes=[b_ybB])
                        P.op("pe", lambda e, h=h, tsl=tsl: e.transpose(BK.h[:, h * 256 + 128:h * 256 + 256], QKb[:, 2 + h, tsl], identb[:]),
                             reads=[b_qk[2 + h]] + CST, writes=[BK.bh])
                        P.op("act", lambda e, h=h: e.copy(Ktm[h][:], BK.h[:, h * 256 + 128:h * 256 + 256]), reads=[BK.bh], writes=[b_ktm[h]])
                        P.op("dve", lambda e, h=h, va=va, ws=ws: e.tensor_scalar(Vw[h][:], va[:, h, :], ws[:, h:h + 1], None, ALU.mult),
                             reads=[bva, bsm], writes=[b_vw[h]])
                        dps = f6[:, h * 256:h * 256 + 129]
                        P.op("pe", lambda e, h=h, dps=dps: e.matmul(dps, Ktm[h][:], Vw[h][:], start=True, stop=True),
                             reads=[b_ktm[h], b_vw[h]], writes=[bf6])
                        P.op("dve", lambda e, h=h, dps=dps: e.tensor_tensor(C32[:, h, :], dps, C32[:, h, :], ALU.add),
                             reads=[bf6, b_C32[h]], writes=[b_C32[h]])
                        P.op("dve", lambda e, h=h, ebL=ebL: e.tensor_scalar(C32[:, h, :], C32[:, h, :], ebL[:, h:h + 1], None, ALU.mult),
                             reads=[b_C32[h], bsm], writes=[b_C32[h]])
                        P.op("act", lambda e, h=h: e.mul(Cb[:, h, :], C32[:, h, :], SC), reads=[b_C32[h]], writes=[b_Cb[h]])
                for h in range(2):
                    ystore(P, 2 + h, blk, ybufB[:, h, :], [b_ybB])
            full_barrier(P)


TOK = 4096


def declare_post(nc, pfx=""):
    din = lambda n, sh: nc.dram_tensor(pfx + n, sh, F32, kind="ExternalInput").ap()
    return dict(wo=din("wo", [D, D]), ln1g=din("ln1g", [D]), ln1b=din("ln1b", [D]),
                w=dict(wg=din("wg", [16, D, EH]), wu=din("wu", [16, D, EH]), wd=din("wd", [16, EH, D]), wr=din("wr", [D, 20]),
                       br=din("br", [1, 20]), ln_g=din("ln2g", [D]), ln_b=din("ln2b", [D])),
                hmid=nc.dram_tensor(pfx + "hmid_scr", [TOK, D], F32).ap())


def emit_post(P, io):
    nc = P.nc
    wo, ln1g, ln1b, w, hmid = io["wo"], io["ln1g"], io["ln1b"], dict(io["w"]), io["hmid"]
    w["ident"] = io["ident"]
    load_yt, xres, out = io["load_yt"], io["xres"], io["out"]
    if True:
        B = P.buf
        banks = [P.ps(f"bank{i}", [128, 512]) for i in range(8)]
        bbank = [B() for _ in range(8)]
        b_hmid, b_out, b_yT = B(), B(), B()
        b_xres = io.get("b_xres") or B()
        b_hmw = [B(), B()]
        with ExitStack() as esF:
            sbF = lambda n, sh, d: esF.enter_context(nc.sbuf_tensor(P.pfx + n, sh, d))
            stg = Stager(P)
            wob = sbF("f_wo", [128, 8, D], BF16)
            b_wo = B()
            stg.load(wob[:], b_wo, wo[:, :])
            g1 = sbF("f_g1", [128, D], F32)
            b1 = sbF("f_b1", [128, D], F32)
            b_g1, b_b1 = B(), B()
            P.dma("sp", g1[:], bcast_rows(ln1g.tensor, ln1g.offset, D), writes=[b_g1])
            P.dma("sp", b1[:], bcast_rows(ln1b.tensor, ln1b.offset, D), writes=[b_b1])
            yts = [sbF(f"f_yt{i}", [128, 8, 512], BF16) for i in range(2)]
            b_yts = [[B() for _ in range(8)] for _ in range(2)]
            xr = [sbF(f"f_xr{i}", [128, D], F32) for i in range(2)]
            b_xr = [B(), B()]
            z = [sbF(f"f_z{i}", [128, D], F32) for i in range(2)]
            b_z = [B(), B()]
            hm = [sbF(f"f_hm{i}", [128, D], F32) for i in range(2)]
            b_hm = [B(), B()]
            stats = sbF("f_stats", [128, 2, 6], F32)
            sm = sbF("f_sm", [128, 64], F32)
            b_stats, b_sm = B(), B()
            ti = 0
            for t4 in range(TOK // 512):
                yt, byt = yts[t4 % 2], b_yts[t4 % 2]
                load_yt(P, yt, byt, t4)
                for tq in range(4):
                    k = ti % 2
                    ti += 1
                    t0 = t4 * 512 + tq * 128
                    P.dma("sp", xr[k][:], xres(t0), reads=[b_xres], writes=[b_xr[k]])
                    for dh in range(2):
                        pb, bpb = banks[(ti * 2 + dh) % 4], bbank[(ti * 2 + dh) % 4]
                        for ec in range(8):
                            P.op("pe", lambda e, pb=pb, ec=ec, yt=yt, tq=tq, dh=dh: e.matmul(
                                pb[:], yt[:, ec, tq * 128:(tq + 1) * 128], wob[:, ec, dh * 512:(dh + 1) * 512],
                                start=(ec == 0), stop=(ec == 7)), reads=[byt[ec], b_wo], writes=[bpb])
                        P.op("dve", lambda e, pb=pb, k=k, dh=dh: e.scalar_tensor_tensor(
                            z[k][:, dh * 512:(dh + 1) * 512], xr[k][:, dh * 512:(dh + 1) * 512], ALPHA, pb[:], ALU.mult, ALU.add),
                            reads=[bpb, b_xr[k]], writes=[b_z[k]])
                    emit_ln(P, z[k][:], b_z[k], hm[k], b_hm[k], g1, b_g1, b1, b_b1, stats, b_stats, sm, b_sm)
                    P.dma("sp", hmid[t0:t0 + 128, :], hm[k][:], reads=[b_hm[k]], writes=[b_hmw[k]])
            full_barrier(P)
        emit_tail(P, hmid, b_hmid, out, b_out, TOK, w, 0, banks=banks, is_final=io.get("is_final", True))
        full_barrier(P)


def pack_m0(inp, b, c):
    w = inp["ab_w_in"][0]
    cols = []
    for hp in range(2):
        h0 = 4 * c + 2 * hp
        for sec in (0, 512, 1024):
            cols.append(w[:, sec + h0 * 64: sec + (h0 + 2) * 64])
    wA = np.concatenate(cols, axis=1)
    hb = [2 * c, 2 * c + 1]
    BQ, BKK, BV, BO, BI, BF = 1536, 2048, 2560, 3072, 3584, 3588
    wB = np.concatenate([w[:, BQ + h * 128: BQ + (h + 1) * 128] for h in hb] + [w[:, BKK + h * 128: BKK + (h + 1) * 128] for h in hb]
                        + [w[:, BV + h * 128: BV + (h + 1) * 128] for h in hb] + [w[:, BO + h * 128: BO + (h + 1) * 128] for h in hb]
                        + [w[:, BI + h: BI + h + 1] for h in hb] + [w[:, BF + h: BF + h + 1] for h in hb], axis=1)
    gb = inp["ab_gate_b"][0]
    gbias = np.array([[gb[hb[0]], gb[hb[1]], gb[4 + hb[0]], gb[4 + hb[1]]]], np.float32)
    cw = inp["ab_conv"][0]
    convw = np.zeros((128, 4, 4), np.float32)
    for g, off in enumerate([hb[0] * 128, hb[1] * 128, 512 + hb[0] * 128, 512 + hb[1] * 128]):
        convw[:, g, :] = cw[:, off:off + 128].T
    hng = np.concatenate([inp["ab_head_norm"][0][h * 128:(h + 1) * 128] for h in hb])[None]
    relb = np.concatenate([inp["rel_bias"][:, 4 * c:4 * c + 4], np.full((1, 4), -30000.0, np.float32)], axis=0)
    return dict(x=np.ascontiguousarray(inp["x"][b]), wA=np.ascontiguousarray(wA), wB=np.ascontiguousarray(wB), gbias=gbias,
                convw=np.ascontiguousarray(convw.reshape(128, 16)), hng=np.ascontiguousarray(hng),
                relb=np.ascontiguousarray(relb), oh=np.ascontiguousarray(dil_onehot().reshape(33, -1)),
                ident=np.eye(128, dtype=np.float32), triu=np.triu(np.ones((128, 128), np.float32)))


L1 = 5632
OFF1 = 2063


def nsa_consts():
    dist = np.arange(L1) - OFF1
    oh = np.zeros((33, L1), np.float32)
    bk = t5_bucket_np(dist)
    for jp in range(L1):
        if dist[jp] >= 0:
            oh[bk[jp], jp] = 1.0
        else:
            oh[32, jp] = 1.0
    n = np.arange(512)
    m = np.arange(128)
    cs, ce = n * 16, n * 16 + 31
    ov = ((cs[:, None] < m[None, :] * 64 + 64) & (ce[:, None] >= m[None, :] * 64)).astype(np.float32)
    ov[511] = 0.0
    ovl = ov.reshape(4, 128, 128).transpose(1, 0, 2)
    mrow = np.broadcast_to(m[None, :].astype(np.float32), (128, 128))
    k = np.arange(128)
    cst = np.stack([mrow, (mrow >= 1).astype(np.float32), (mrow == 0).astype(np.float32),
                    (k[:, None] > k[None, :]).astype(np.float32)], axis=1)
    qhalf = (np.arange(128) >= 64).astype(np.float32)[:, None]
    return dict(oh1=oh, ovl=np.ascontiguousarray(ovl), cst1=np.ascontiguousarray(cst.astype(np.float32)), qhalf=qhalf)


def declare_mixer1(nc, pfx=""):
    din = lambda n, s: nc.dram_tensor(pfx + n, s, F32, kind="ExternalInput").ap()
    return dict(wQ=din("wQ", [D, 512]), wKV=din("wKV", [D, 768]), wG=din("wG", [D, 12]), gb=din("gb", [1, 12]),
                posT=din("posT", [128, 64]), w1=din("w1", [2, 4096, 256]), w2=din("w2", [2, 256, 128]), relb=din("relb", [33, 4]),
                oh1=din("oh1", [33, L1]), ovl=din("ovl", [128, 512]), cst1=din("cst1", [128, 512]), qhalf=din("qhalf", [128, 1]),
                fsc=nc.dram_tensor(pfx + "frep_scr", [128, 4 * L1], BF16).ap())


def emit_mixer1(P, io):
    nc = P.nc
    wQ, wKV, wG, gbd, posT, w1d, w2d, relb = io["wQ"], io["wKV"], io["wG"], io["gb"], io["posT"], io["w1"], io["w2"], io["relb"]
    oh_d, ovl_d, cst_d, qh_d, ident_d, fsc = io["oh1"], io["ovl"], io["cst1"], io["qhalf"], io["ident"], io["fsc"]
    x, ostore = io["x"], io["ostore"]
    NT = S_TOT // 128
    SCQ = 128.0 ** -0.5
    if True:
        B = P.buf
        BK = Banks(P)
        f0, f1, f2, f3, f4, f5, f6 = BK.f
        bf0, bf1, bf2, bf3, bf4, bf5, bf6 = BK.bf
        b_x, b_o, b_fsc = io.get("b_x") or B(), B(), B()
        ident = P.sb("ident_sb", [128, 128], F32)
        identb = P.sb("identb", [128, 128], BF16)
        ones_bf = P.sb("ones_bf", [128, 128], BF16)
        b_ident, b_cb = B(), B()
        P.dma("sp", ident[:], ident_d[:, :], writes=[b_ident])
        P.op("dve", lambda e: e.tensor_copy(identb[:], ident[:]), reads=[b_ident], writes=[b_cb])
        P.op("dve", lambda e: e.memset(ones_bf[:], 1.0), writes=[b_cb])
        xin = [P.sb(f"xin{i}", [128, D], F32) for i in range(2)]
        b_xin = [B(), B()]
        stg = Stager(P, n=2)
        ctr = [0]
        wQb = P.sb("wQb", [128, 8, 512], BF16)
        wGb = P.sb("wGb", [128, 8, 12], BF16)
        b_wQ, b_wG = B(), B()
        stg.load(wQb[:], b_wQ, wQ[:, :])
        stg.load(wGb[:], b_wG, wG[:, :])
        KsT = P.sb("KsT", [128, S_TOT], BF16)
        KwT = P.sb("KwT", [128, S_TOT], BF16)
        Vs = P.sb("Vs", [128, NT, 129], BF16)
        Vw = P.sb("Vw", [128, NT, 129], BF16)
        b_KsT, b_KwT, b_Vs, b_Vw = B(), B(), B(), B()
        kcT = P.sb("kcT", [128, 512], BF16)
        rhsC = P.sb("rhsC", [128, 4, 256], BF16)
        b_kcT, b_rhsC = B(), B()
        P.op("dve", lambda e: e.memset(Vs[:, :, 128:129], 1.0), writes=[b_Vs])
        P.op("dve", lambda e: e.memset(Vw[:, :, 128:129], 1.0), writes=[b_Vw])
        with ExitStack() as es12:
            sb12 = lambda n, s, d: es12.enter_context(nc.sbuf_tensor(P.pfx + n, s, d))
            KcT = sb12("KcT", [128, 2, S_TOT], BF16)
            b_KcT = [B(), B()]
            with ExitStack() as es1:
                sb1 = lambda n, s, d: es1.enter_context(nc.sbuf_tensor(P.pfx + n, s, d))
                hT = sb1("hT", [128, 8, BLK], BF16)
                b_hT = [B() for _ in range(16)]
                wKVb = sb1("wKVb", [128, 8, 768], BF16)
                b_wKV = B()
                stg.load(wKVb[:], b_wKV, wKV[:, :])
                for blk in range(NBLK):
                    T0 = blk * BLK
                    load_xT(P, BK, x, b_x, xin, b_xin, hT, b_hT, ident, b_ident, T0, 16, ctr)
                    fm = [(0, KcT[:, 0, :], b_KcT[0]), (1, KcT[:, 1, :], b_KcT[1]), (2, KsT[:], b_KsT), (4, KwT[:], b_KwT)]
                    for fi, (wi, dst, bd) in enumerate(fm):
                        for qtr in range(4):
                            pb, bpb = (f2, bf2) if (fi + qtr) % 2 == 0 else (f3, bf3)
                            rds = [b_hT[qtr * 4 + i] for i in range(4)] + [b_wKV]
                            for dc in range(8):
                                P.op("pe", lambda e, pb=pb, dc=dc, wi=wi, qtr=qtr: e.matmul(
                                    pb[:], wKVb[:, dc, wi * 128:(wi + 1) * 128], hT[:, dc, qtr * 512:(qtr + 1) * 512],
                                    start=(dc == 0), stop=(dc == 7)), reads=rds, writes=[bpb])
                            if (fi + qtr) % 2 == 0:
                                P.op("act", lambda e, pb=pb, dst=dst, T0=T0, qtr=qtr: e.copy(
                                    dst[:, T0 + qtr * 512:T0 + (qtr + 1) * 512], pb[:]), reads=[bpb], writes=[bd])
                            else:
                                P.op("dve", lambda e, pb=pb, dst=dst, T0=T0, qtr=qtr: e.tensor_copy(
                                    dst[:, T0 + qtr * 512:T0 + (qtr + 1) * 512], pb[:]), reads=[bpb], writes=[bd])
                    for tl in range(16):
                        for (wi, dst, bd, pb, bpb, eng) in ((3, Vs, b_Vs, f4, bf4, "act"), (5, Vw, b_Vw, f5, bf5, "dve")):
                            for dc in range(8):
                                P.op("pe", lambda e, pb=pb, dc=dc, wi=wi, tl=tl: e.matmul(
                                    pb[:, 0:128], hT[:, dc, tl * 128:(tl + 1) * 128], wKVb[:, dc, wi * 128:(wi + 1) * 128],
                                    start=(dc == 0), stop=(dc == 7)), reads=[b_hT[tl], b_wKV], writes=[bpb])
                            if eng == "act":
                                P.op("act", lambda e, pb=pb, dst=dst, tl=tl, blk=blk: e.copy(dst[:, blk * 16 + tl, 0:128], pb[:, 0:128]),
                                     reads=[bpb], writes=[bd])
                            else:
                                P.op("dve", lambda e, pb=pb, dst=dst, tl=tl, blk=blk: e.tensor_copy(dst[:, blk * 16 + tl, 0:128], pb[:, 0:128]),
                                     reads=[bpb], writes=[bd])
                full_barrier(P)
            with ExitStack() as es2:
                sb2 = lambda n, s, d: es2.enter_context(nc.sbuf_tensor(P.pfx + n, s, d))
                w1b = sb2("w1b", [128, 32, 256], BF16)
                w2b = sb2("w2b", [128, 2, 128], BF16)
                posb = sb2("posb", [128, 64], BF16)
                pos32 = sb2("pos32", [128, 64], F32)
                ovl32 = sb2("ovl32", [128, 512], F32)
                pbias = sb2("pbias", [128, 2], F32)
                xg = sb2("xg", [128, 512], F32)
                ug = sb2("ug", [128, 512], F32)
                hid = sb2("hid_sb", [128, 2, 512], BF16)
                b_w1, b_w2, b_pos, b_ovl, b_pb, b_xg, b_ug, b_hid = B(), B(), B(), B(), B(), B(), B(), [B(), B()]
                P.dma("sp", pos32[:], posT[:, :], writes=[b_pos])
                P.op("dve", lambda e: e.tensor_copy(posb[:], pos32[:]), reads=[b_pos], writes=[b_pos])
                P.dma("sp", ovl32[:], ovl_d[:, :], writes=[b_ovl])
                P.op("dve", lambda e: e.tensor_copy(rhsC[:, :, 128:256], ovl32[:].rearrange("p (c m) -> p c m", c=4)),
                     reads=[b_ovl], writes=[b_rhsC])
                P.op("dve", lambda e: e.memset(hid[:, :, 511:512], 0.0), writes=b_hid)
                for kv in range(2):
                    stg.load(w1b[:], b_w1, w1d[kv])
                    stg.load(w2b[:], b_w2, w2d[kv])
                    for hc in range(2):
                        for p_ in range(32):
                            P.op("pe", lambda e, hc=hc, p_=p_, kv=kv: e.matmul(
                                f6[:, 0:1], w1b[:, p_, hc * 128:(hc + 1) * 128], posb[:, kv * 32 + p_:kv * 32 + p_ + 1],
                                start=(p_ == 0), stop=(p_ == 31)), reads=[b_w1, b_pos], writes=[bf6])
                        P.op("act", lambda e, hc=hc: e.copy(pbias[:, hc:hc + 1], f6[:, 0:1]), reads=[bf6], writes=[b_pb])
                        for p_ in range(32):
                            P.op("pe", lambda e, hc=hc, p_=p_, kv=kv: e.matmul(
                                f2[:, 0:511], w1b[:, p_, hc * 128:(hc + 1) * 128], KcT[:, kv, p_:p_ + 16 * 510 + 1:16],
                                start=(p_ == 0), stop=(p_ == 31)), reads=[b_w1, b_KcT[kv]], writes=[bf2])
                        P.op("act", lambda e, hc=hc: e.activation(xg[:, 0:511], f2[:, 0:511], AF.Identity, bias=pbias[:, hc:hc + 1], scale=1.0),
                             reads=[bf2, b_pb], writes=[b_xg])
                        P.op("dve", lambda e: e.tensor_tensor(ug[:, 0:511], xg[:, 0:511], xg[:, 0:511], ALU.mult), reads=[b_xg], writes=[b_ug])
                        P.op("dve", lambda e: e.tensor_scalar(ug[:, 0:511], ug[:, 0:511], 0.044715, 1.0, ALU.mult, ALU.add), reads=[b_ug], writes=[b_ug])
                        P.op("dve", lambda e: e.tensor_tensor(ug[:, 0:511], ug[:, 0:511], xg[:, 0:511], ALU.mult), reads=[b_ug, b_xg], writes=[b_ug])
                        P.op("act", lambda e: e.activation(ug[:, 0:511], ug[:, 0:511], AF.Sigmoid, scale=1.5957691216057308), reads=[b_ug], writes=[b_ug])
                        P.op("dve", lambda e, hc=hc: e.tensor_tensor(hid[:, hc, 0:511], xg[:, 0:511], ug[:, 0:511], ALU.mult),
                             reads=[b_ug, b_xg], writes=[b_hid[hc]])
                    if kv == 0:
                        for hc in range(2):
                            P.op("pe", lambda e, hc=hc: e.matmul(f3[:, 0:512], w2b[:, hc, :], hid[:, hc, :], start=(hc == 0), stop=(hc == 1)),
                                 reads=[b_w2] + b_hid, writes=[bf3])
                        P.op("act", lambda e: e.copy(kcT[:], f3[:, 0:512]), reads=[bf3], writes=[b_kcT])
                    else:
                        for c in range(4):
                            for hc in range(2):
                                P.op("pe", lambda e, hc=hc, c=c: e.matmul(f3[:, c * 128:(c + 1) * 128], hid[:, hc, c * 128:(c + 1) * 128], w2b[:, hc, :],
                                                                          start=(hc == 0), stop=(hc == 1)), reads=[b_w2] + b_hid, writes=[bf3])
                        P.op("act", lambda e: e.copy(rhsC[:, :, 0:128], f3[:].rearrange("p (c m) -> p c m", c=4)), reads=[bf3], writes=[b_rhsC])
                full_barrier(P)
        with ExitStack() as es3:
            sb3 = lambda n, s, d: es3.enter_context(nc.sbuf_tensor(P.pfx + n, s, d))
            tab = sb3("tab", [33, 4], F32)
            tabB = sb3("tabB", [33, 128], F32)
            ohp = [sb3(f"ohp{i}", [33, 512], F32) for i in range(2)]
            frp = [sb3(f"frp{i}", [128, 512], BF16) for i in range(2)]
            b_tab, b_tabB, b_ohp, b_frp = B(), B(), [B(), B()], [B(), B()]
            b_fscw = [B(), B()]
            P.dma("sp", tab[:], relb[:, :], writes=[b_tab])
            ki = 0
            for h in range(4):
                P.op("dve", lambda e, h=h: e.tensor_copy(tabB[:], tab[:, h:h + 1].to_broadcast([33, 128])), reads=[b_tab], writes=[b_tabB])
                for pc_ in range(L1 // 512):
                    k = ki % 2
                    ki += 1
                    P.dma("sp", ohp[k][:], oh_d[:, pc_ * 512:(pc_ + 1) * 512], writes=[b_ohp[k]])
                    pb, bpb = (f2, bf2) if k == 0 else (f3, bf3)
                    P.op("pe", lambda e, pb=pb, k=k: e.matmul(pb[:], tabB[:], ohp[k][:], start=True, stop=True), reads=[b_tabB, b_ohp[k]], writes=[bpb])
                    P.op("act", lambda e, pb=pb, k=k: e.activation(frp[k][:], pb[:], AF.Exp), reads=[bpb], writes=[b_frp[k]])
                    P.dma("sp", fsc[:, h * L1 + pc_ * 512:h * L1 + (pc_ + 1) * 512], frp[k][:], reads=[b_frp[k]], writes=[b_fscw[k]])
            full_barrier(P)
            RS = 4 * L1
            Esel = sb3("Esel", [128, 14, 4, 128], BF16)
            Ew4 = sb3("Ew4", [128, 4, 128], BF16)
            b_Esel = B()
            for dl in range(14):
                P.dma("sp", Esel[:, dl], bass.AP(fsc.tensor, 128 * dl + OFF1, [[RS - 1, 128], [L1, 4], [1, 128]]),
                      reads=[b_fsc], writes=[b_Esel])
            cst = sb3("cst", [128, 4, 128], F32)
            qhalf = sb3("qhalf_sb", [128, 1], F32)
            gbb = sb3("gbb", [128, 12], F32)
            m4b = sb3("m4b", [128, 128], BF16)
            b_cst = B()
            P.dma("sp", cst[:], cst_d[:, :].rearrange("p (c m) -> p c m", c=4), writes=[b_cst])
            b_q2, b_g2 = B(), B()
            P.dma("sp", qhalf[:], qh_d[:, :], writes=[b_q2])
            P.dma("sp", gbb[:], bcast_rows(gbd.tensor, gbd.offset, 12), writes=[b_g2])
            CST = [b_cst, b_q2, b_g2, b_cb, b_Esel]
            P.op("dve", lambda e: e.tensor_copy(m4b[:], cst[:, 3, :]), reads=[b_cst], writes=[b_Esel])
            P.op("dve", lambda e: e.tensor_tensor(Ew4[:], Esel[:, 4], m4b[:].unsqueeze(1).to_broadcast([128, 4, 128]), ALU.mult),
                 reads=[b_Esel], writes=[b_Esel])
            mrow, mge1, meq0 = cst[:, 0, :], cst[:, 1, :], cst[:, 2, :]
            Expand = sb3("Expand", [128, NT, 128], BF16)
            for Tk in range(NT):
                P.op("dve", lambda e, Tk=Tk: e.tensor_copy(Expand[:, Tk, :].rearrange("m (h k) -> m h k", h=2),
                                                            identb[:, 2 * Tk:2 * Tk + 2].unsqueeze(2).to_broadcast([128, 2, 64])),
                     reads=[b_cb], writes=[b_Esel])
            hTt = sb3("hTt", [128, 8, 128], BF16)
            b_hTt = [B()]
            QT = [sb3(f"QT{i}", [128, 4, 128], BF16) for i in range(2)]
            b_QT = [B(), B()]
            gsg = sb3("gsg", [128, 12], F32)
            b_gsg = B()
            ecb = [sb3(f"ecb{i}", [128, 4, 128], BF16) for i in range(2)]
            b_ecb = [B(), B()]
            psb = [sb3(f"psb{i}", [128, 4, 128], BF16) for i in range(3)]
            b_psb = [B(), B(), B()]
            ocb = sb3("ocb", [128, 4, 128], F32)
            b_ocb = B()
            sm = sb3("sm3", [128, 64], F32)
            b_sm = B()
            imp = sb3("imp", [128, 128], F32)
            M1 = sb3("M1", [128, 128], F32)
            Fm = sb3("Fm", [128, 128], F32)
            sa = sb3("sa", [128, 128], F32)
            rp = sb3("rp", [128, 128], F32)
            sel = sb3("sel", [128, 128], F32)
            nsb = sb3("nsb", [128, 128], BF16)
            nsT = sb3("nsT", [128, 128], BF16)
            mx = sb3("mx", [128, 16], F32)
            b_imp, b_M1, b_Fm, b_sa, b_rp, b_sel, b_nsb, b_nsT, b_mx = (B() for _ in range(9))
            b_ow = [B(), B()]
            ob = sb3("ob", [128, 4, 128], BF16)
            oTt = [sb3(f"oTt{i}", [128, 4, 128], BF16) for i in range(2)]
            b_ob, b_oTt = B(), [B(), B()]
            si = 0
            pipe1 = Pipe()
            for T in range(NT):
                k2 = T % 2
                load_xT(P, BK, x, b_x, xin, b_xin, hTt, b_hTt, ident, b_ident, T * 128, 1, ctr)
                for j in range(4):
                    for dc in range(8):
                        P.op("pe", lambda e, j=j, dc=dc: e.matmul(f6[:, j * 128:(j + 1) * 128], wQb[:, dc, j * 128:(j + 1) * 128], hTt[:, dc, :],
                                                                  start=(dc == 0), stop=(dc == 7)), reads=[b_hTt[0], b_wQ], writes=[bf6])
                qt, bqt = QT[k2], b_QT[k2]
                P.op("act", lambda e, qt=qt: e.mul(qt[:].rearrange("p j q -> p (j q)"), f6[:], SCQ), reads=[bf6], writes=[bqt])
                for dc in range(8):
                    P.op("pe", lambda e, dc=dc: e.matmul(f4[:, 16:28], hTt[:, dc, :], wGb[:, dc, :], start=(dc == 0), stop=(dc == 7)),
                         reads=[b_hTt[0], b_wG], writes=[bf4])
                P.op("dve", lambda e: e.tensor_tensor(gsg[:], f4[:, 16:28], gbb[:], ALU.add), reads=[bf4] + CST, writes=[b_gsg])
                P.op("act", lambda e: e.activation(gsg[:], gsg[:], AF.Sigmoid), reads=[b_gsg], writes=[b_gsg])
                qrhs = qt[:].rearrange("p j q -> p (j q)")
                ncmp = (8 * T + 6) // 128 + 1
                for c in range(ncmp):
                    dl = min(T - 16 * c, 28)
                    ke = si % 2
                    kp = si % 3
                    si += 1
                    P.dma("sp", ecb[ke][:], bass.AP(fsc.tensor, 128 * dl - 31 + OFF1, [[RS - 16, 128], [L1, 4], [1, 128]]),
                          reads=[b_fsc], writes=[b_ecb[ke]])
                    P.op("pe", lambda e, c=c, qrhs=qrhs: e.matmul(f6[:], kcT[:, c * 128:(c + 1) * 128], qrhs, start=True, stop=True),
                         reads=[b_kcT, bqt], writes=[bf6])
                    pp, bpp = psb[kp], b_psb[kp]
                    P.op("act", lambda e, pp=pp: e.activation(pp[:].rearrange("p j q -> p (j q)"), f6[:], AF.Exp), reads=[bf6], writes=[bpp])
                    P.op("dve", lambda e, pp=pp, ke=ke: e.tensor_tensor(pp[:], pp[:], ecb[ke][:], ALU.mult), reads=[bpp, b_ecb[ke]], writes=[bpp])
                    for j in range(4):
                        fb, bfb = (f2, bf2) if j < 2 else (f3, bf3)
                        P.op("pe", lambda e, fb=fb, j=j, c=c, pp=pp, ncmp=ncmp: e.matmul(
                            fb[:, (j % 2) * 256:(j % 2) * 256 + 256], pp[:, j, :], rhsC[:, c, :],
                            start=(c == 0 and j % 2 == 0), stop=(c == ncmp - 1 and j % 2 == 1)),
                            reads=[bpp, b_rhsC], writes=[bfb])
                        P.op("pe", lambda e, j=j, c=c, pp=pp, ncmp=ncmp: e.matmul(
                            f4[:, j:j + 1], pp[:, j, :], ones_bf[:, 0:1], start=(c == 0 and j == 0), stop=(c == ncmp - 1 and j == 3)),
                            reads=[bpp, b_cb], writes=[bf4])
                rc, cc = sm[:, 0:4], sm[:, 4:8]
                P.op("dve", lambda e, rc=rc: e.tensor_scalar(rc, f4[:, 0:4], 1e-30, None, ALU.max), reads=[bf4], writes=[b_sm])
                P.op("dve", lambda e, rc=rc: e.reciprocal(rc, rc), reads=[b_sm], writes=[b_sm])
                P.op("dve", lambda e, rc=rc, cc=cc: e.tensor_tensor(cc, rc, gsg[:, 0:4], ALU.mult), reads=[b_sm, b_gsg], writes=[b_sm])
                for j in range(4):
                    fb, bfb = (f2, bf2) if j < 2 else (f3, bf3)
                    o0 = (j % 2) * 256
                    P.op("dve", lambda e, fb=fb, j=j, o0=o0, cc=cc: e.tensor_scalar(ocb[:, j, :], fb[:, o0:o0 + 128], cc[:, j:j + 1], None, ALU.mult),
                         reads=[bfb, b_sm], writes=[b_ocb])
                    if j == 0:
                        P.op("dve", lambda e, fb=fb, o0=o0, rc=rc: e.tensor_scalar(imp[:], fb[:, o0 + 128:o0 + 256], rc[:, 0:1], None, ALU.mult),
                             reads=[bfb, b_sm], writes=[b_imp])
                    else:
                        P.op("dve", lambda e, fb=fb, o0=o0, rc=rc, j=j: e.scalar_tensor_tensor(
                            imp[:], fb[:, o0 + 128:o0 + 256], rc[:, j:j + 1], imp[:], ALU.mult, ALU.add), reads=[bfb, b_sm, b_imp], writes=[b_imp])
                qb, qb1, qb2, tau = sm[:, 8:9], sm[:, 9:10], sm[:, 10:11], sm[:, 11:12]
                P.op("dve", lambda e, qb=qb, T=T: e.tensor_scalar(qb, qhalf[:], float(2 * T), None, ALU.add), reads=CST, writes=[b_sm])
                P.op("dve", lambda e, qb=qb, qb1=qb1: e.tensor_scalar(qb1, qb, -1.0, None, ALU.add), reads=[b_sm], writes=[b_sm])
                P.op("dve", lambda e, qb=qb, qb2=qb2: e.tensor_scalar(qb2, qb, -2.0, None, ALU.add), reads=[b_sm], writes=[b_sm])
                P.op("dve", lambda e, qb2=qb2: e.tensor_scalar(M1[:], mrow, qb2, None, ALU.is_le), reads=[b_sm] + CST, writes=[b_M1])
                P.op("dve", lambda e: e.tensor_tensor(M1[:], M1[:], mge1, ALU.mult), reads=[b_M1] + CST, writes=[b_M1])
                P.op("dve", lambda e, qb=qb: e.tensor_scalar(Fm[:], mrow, qb, None, ALU.is_equal), reads=[b_sm] + CST, writes=[b_Fm])
                P.op("dve", lambda e, qb1=qb1: e.tensor_scalar(sel[:], mrow, qb1, None, ALU.is_equal), reads=[b_sm] + CST, writes=[b_sel])
                P.op("dve", lambda e: e.tensor_tensor(Fm[:], Fm[:], sel[:], ALU.add), reads=[b_Fm, b_sel], writes=[b_Fm])
                P.op("dve", lambda e: e.tensor_tensor(Fm[:], Fm[:], meq0, ALU.max), reads=[b_Fm] + CST, writes=[b_Fm])
                P.op("dve", lambda e: e.tensor_tensor(sa[:], imp[:], M1[:], ALU.mult), reads=[b_imp, b_M1], writes=[b_sa])
                P.op("dve", lambda e: e.max(mx[:, 0:8], sa[:]), reads=[b_sa], writes=[b_mx])
                P.op("dve", lambda e: e.match_replace(rp[:], mx[:, 0:8], sa[:], -1.0), reads=[b_sa, b_mx], writes=[b_rp])
                P.op("dve", lambda e: e.max(mx[:, 8:16], rp[:]), reads=[b_rp], writes=[b_mx])
                P.op("dve", lambda e: e.tensor_scalar(sel[:], sa[:], mx[:, 12:13], None, ALU.is_ge), reads=[b_sa, b_mx], writes=[b_sel])
                P.op("dve", lambda e: e.tensor_tensor(sel[:], sel[:], M1[:], ALU.mult), reads=[b_sel, b_M1], writes=[b_sel])
                P.op("dve", lambda e: e.tensor_tensor(sel[:], sel[:], Fm[:], ALU.max), reads=[b_sel, b_Fm], writes=[b_sel])
                P.op("dve", lambda e: e.tensor_scalar(nsb[:], sel[:], -1.0, 30000.0, ALU.add, ALU.mult), reads=[b_sel], writes=[b_nsb])
                P.op("pe", lambda e: e.transpose(BK.h[:, 512:640], nsb[:], identb[:]), reads=[b_nsb, b_cb], writes=[BK.bh])
                P.op("act", lambda e: e.copy(nsT[:], BK.h[:, 512:640]), reads=[BK.bh], writes=[b_nsT])
                nsrhs = nsT[:].unsqueeze(1).to_broadcast([128, 4, 128])
                for br, (KT_, bKT, V_, bV, fa, bfa, fb_, bfb_, tks) in enumerate((
                        (KsT, b_KsT, Vs, b_Vs, f2, bf2, f3, bf3, list(range(0, T + 1))),
                        (KwT, b_KwT, Vw, b_Vw, f4, bf4, f5, bf5, list(range(max(0, T - 4), T + 1))))):
                    for ii, Tk in enumerate(tks):
                        dl = T - Tk
                        kp = si % 3
                        ks_ = si % 2
                        si += 1
                        st, bst = (f0, bf0) if ks_ == 0 else (f1, bf1)
                        P.op("pe", lambda e, st=st, KT_=KT_, Tk=Tk, qrhs=qrhs, br=br: e.matmul(
                            st[:], KT_[:, Tk * 128:(Tk + 1) * 128], qrhs, start=True, stop=(br == 1)),
                            reads=[bKT, bqt], writes=[bst])
                        if br == 0:
                            P.op("pe", lambda e, st=st, Tk=Tk, nsrhs=nsrhs: e.matmul(
                                st[:].rearrange("p (j q) -> p j q", j=4), Expand[:, Tk, :], nsrhs, start=False, stop=True),
                                reads=[b_nsT, b_Esel], writes=[bst])
                        pp, bpp = psb[kp], b_psb[kp]
                        P.op("act", lambda e, pp=pp, st=st: e.activation(pp[:].rearrange("p j q -> p (j q)"), st[:], AF.Exp), reads=[bst], writes=[bpp])
                        if br == 1 and dl == 4:
                            et = Ew4[:]
                        else:
                            et = Esel[:, min(dl, 13)]
                        P.op("dve", lambda e, pp=pp, et=et: e.tensor_tensor(pp[:], pp[:], et, ALU.mult), reads=[bpp, b_Esel], writes=[bpp])
                        def stageB(fa=fa, bfa=bfa, fb_=fb_, bfb_=bfb_, pp=pp, bpp=bpp, V_=V_, bV=bV, Tk=Tk, ii=ii, n=len(tks)):
                            for j in range(4):
                                fo, bfo = (fa, bfa) if j < 2 else (fb_, bfb_)
                                P.op("pe", lambda e, fo=fo, j=j: e.matmul(
                                    fo[:, (j % 2) * 129:(j % 2) * 129 + 129], pp[:, j, :], V_[:, Tk, :],
                                    start=(ii == 0 and j % 2 == 0), stop=(ii == n - 1 and j % 2 == 1)),
                                    reads=[bpp, bV], writes=[bfo])
                        pipe1.step(stageB)
                pipe1.flush()
                for br, (fa, bfa, fb_, bfb_) in enumerate(((f2, bf2, f3, bf3), (f4, bf4, f5, bf5))):
                    for j in range(4):
                        fo, bfo = (fa, bfa) if j < 2 else (fb_, bfb_)
                        P.op("dve", lambda e, fo=fo, j=j, br=br: e.tensor_scalar(
                            sm[:, 16 + br * 4 + j:17 + br * 4 + j], fo[:, (j % 2) * 129 + 128:(j % 2) * 129 + 129], 1e-30, None, ALU.max),
                            reads=[bfo], writes=[b_sm])
                cf = sm[:, 16:24]
                P.op("dve", lambda e, cf=cf: e.reciprocal(cf, cf), reads=[b_sm], writes=[b_sm])
                P.op("dve", lambda e, cf=cf: e.tensor_tensor(cf, cf, gsg[:, 4:12], ALU.mult), reads=[b_sm, b_gsg], writes=[b_sm])
                for j in range(4):
                    fs_, bfs = (f2, bf2) if j < 2 else (f3, bf3)
                    fw_, bfw = (f4, bf4) if j < 2 else (f5, bf5)
                    o0 = (j % 2) * 129
                    P.op("dve", lambda e, fs_=fs_, j=j, o0=o0, cf=cf: e.scalar_tensor_tensor(
                        ocb[:, j, :], fs_[:, o0:o0 + 128], cf[:, j:j + 1], ocb[:, j, :], ALU.mult, ALU.add),
                        reads=[bfs, b_sm, b_ocb], writes=[b_ocb])
                    P.op("dve", lambda e, fw_=fw_, j=j, o0=o0, cf=cf: e.scalar_tensor_tensor(
                        ob[:, j, :], fw_[:, o0:o0 + 128], cf[:, 4 + j:5 + j], ocb[:, j, :], ALU.mult, ALU.add),
                        reads=[bfw, b_sm, b_ocb], writes=[b_ob])
                for j in range(4):
                    P.op("pe", lambda e, j=j: e.transpose(BK.h[:, j * 128:(j + 1) * 128], ob[:, j, :], identb[:]), reads=[b_ob, b_cb], writes=[BK.bh])
                ot, bot = oTt[k2], b_oTt[k2]
                P.op("act", lambda e, ot=ot: e.copy(ot[:].rearrange("p j q -> p (j q)"), BK.h[:, 0:512]), reads=[BK.bh], writes=[bot])
                ostore(P, T, ot[:], [bot], k2)
            full_barrier(P)


C_OFF_KV_, C_OFF_GATE_ = 1024, 2560


def pack_m1(inp, h1b, g):
    w = inp["c_w_in"][0]
    wQ = w[:, g * 512:(g + 1) * 512]
    kvcols = []
    for br in range(3):
        for kv in range(2):
            o = C_OFF_KV_ + ((br * 2 + kv) * 2 + g) * 128
            kvcols.append(w[:, o:o + 128])
    wKV = np.concatenate(kvcols, axis=1)
    gcols = [C_OFF_GATE_ + br * 8 + g * 4 + j for br in range(3) for j in range(4)]
    wG = w[:, gcols]
    gb = inp["c_gate_b"][0][[c - C_OFF_GATE_ for c in gcols]][None]
    pos = inp["c_cmp_pos"][0]
    posT = np.concatenate([pos[0].T, pos[1].T], axis=1)
    relb = np.concatenate([inp["rel_bias"][:, 4 * g:4 * g + 4], np.full((1, 4), -30000.0, np.float32)], axis=0)
    c = nsa_consts()
    return dict(x=h1b, wQ=np.ascontiguousarray(wQ), wKV=np.ascontiguousarray(wKV), wG=np.ascontiguousarray(wG),
                gb=np.ascontiguousarray(gb.astype(np.float32)), posT=np.ascontiguousarray(posT), w1=inp["c_cmp_w1"][0], w2=inp["c_cmp_w2"][0],
                relb=np.ascontiguousarray(relb), oh1=c["oh1"], ovl=np.ascontiguousarray(c["ovl"].reshape(128, 512)),
                cst1=np.ascontiguousarray(c["cst1"].reshape(128, 512)), qhalf=c["qhalf"], ident=np.eye(128, dtype=np.float32))


U32 = mybir.dt.uint32
GROUPS = [[0, 1], [2, 3], [4, 5], [6, 7]]


def build_fused():
    nc = bass.Bass("TRN2", target_bir_lowering=False)
    ident_d = nc.dram_tensor("ident", [128, 128], F32, kind="ExternalInput").ap()
    gidx_d = nc.dram_tensor("gidx", [128, 16], U32, kind="ExternalInput").ap()
    xres_d = nc.dram_tensor("xres", [TOK, D], F32, kind="ExternalInput").ap()
    out_d = nc.dram_tensor("out", [TOK, D], F32, kind="ExternalOutput").ap()
    io_m0 = declare_mixer0(nc, "m0_")
    io_p0 = declare_post(nc, "p0_")
    io_m1 = declare_mixer1(nc, "m1_")
    io_p1 = declare_post(nc, "p1_")
    y0src = [nc.dram_tensor(f"y0src{i}", [2048, 512], BF16).ap() for i in range(4)]
    y0dst = [nc.dram_tensor(f"y0dst{i}", [4096, 512], BF16).ap() for i in range(4)]
    o1src = [nc.dram_tensor(f"o1src{i}", [2048, 512], BF16).ap() for i in range(4)]
    o1dst = [nc.dram_tensor(f"o1dst{i}", [4096, 512], BF16).ap() for i in range(4)]
    h1src = [nc.dram_tensor(f"h1src{i}", [512, D], F32).ap() for i in range(8)]
    h1dst = [nc.dram_tensor(f"h1dst{i}", [1024, D], F32).ap() for i in range(8)]
    with ExitStack() as es:
        P = Prog(nc, es)
        B = P.buf
        gidx = P.sb("gidx_sb", [128, 16], U32)
        b_gidx = B()
        P.dma("sp", gidx[:], gidx_d[:, :], writes=[b_gidx])

        def phase(pfx, fn, io):
            with ExitStack() as pes:
                P.es = pes
                P.pfx = pfx
                P.in_phase = True
                fn(P, io)
                full_barrier(P)
                P.recycle()
                P.in_phase = False
            P.es = es

        def ag(srcs, dsts):
            P.in_phase = True
            for a, d in zip(srcs, dsts):
                P.all_gather(a, d, GROUPS, reads=[], writes=[B()])
            full_barrier(P)
            P.recycle()
            P.in_phase = False

        def row_store(dst_list, nbuf=4):
            bufs = [[B() for _ in range(nbuf)] for _ in dst_list]
            cnt = [0]

            def ystore(P_, i, blk, sb, reads):
                h, tq = blk // 2, (blk % 2) * 4
                r0 = (h * 8 + tq) * 128
                dst = dst_list[i][r0:r0 + 512, :].rearrange("(t p) c -> p t c", p=128)
                P_.dma("sp", dst, sb.rearrange("p (t c) -> p t c", t=4), reads=reads, writes=[bufs[i][cnt[0] % nbuf]])
                cnt[0] += 1
            return ystore

        def make_load_yt(dsts):
            def load_yt(P_, yt, byt, t4):
                for ec in range(8):
                    i, r = ec // 2, ec % 2
                    P_.gather(yt[:, ec, :], dsts[i], gidx[:, r * 8 + t4:r * 8 + t4 + 1], reads=[b_gidx], writes=[byt[ec]])
            return load_yt

        io_m0.update(ident=ident_d, ystore=row_store(y0src))
        phase("m0_", emit_mixer0, io_m0)
        ag(y0src, y0dst)
        io_p0.update(ident=ident_d, load_yt=make_load_yt(y0dst), xres=lambda t0: xres_d[t0:t0 + 128, :],
                     out=lambda t0: h1src[t0 // 512][t0 % 512:t0 % 512 + 128, :], is_final=False)
        phase("p0_", emit_post, io_p0)
        ag(h1src, h1dst)
        o1bufs = [[B(), B()] for _ in range(4)]

        def ostore(P_, T, ot, reads, slot):
            h, t4 = T // 32, (T % 32) // 4
            r0 = (h * 8 + t4) * 128
            c0 = (T % 4) * 128
            for j in range(4):
                P_.dma("sp", o1src[j][r0:r0 + 128, c0:c0 + 128], ot[:, j, :], reads=reads, writes=[o1bufs[j][slot]])

        def h1tile(t0):
            k, rr, tt = (t0 % TOK) // 512, t0 // TOK, t0 % 512
            return h1dst[k][rr * 512 + tt:rr * 512 + tt + 128, :]

        io_m1.update(ident=ident_d, x=h1tile, ostore=ostore)
        phase("m1_", emit_mixer1, io_m1)
        ag(o1src, o1dst)
        io_p1.update(ident=ident_d, load_yt=make_load_yt(o1dst), xres=lambda t0: h1src[t0 // 512][t0 % 512:t0 % 512 + 128, :],
                     out=lambda t0: out_d[t0:t0 + 128, :], is_final=True)
        phase("p1_", emit_post, io_p1)
        P.finish()
    return nc


def pack_post(inp, layer, wo_perm, pfx):
    wr = np.concatenate([inp["moe_wr_g"][layer]] + [inp["moe_wr_e"][layer][g] for g in range(4)], axis=1)
    br = np.concatenate([inp["moe_br_g"][layer], inp["moe_br_e"][layer].reshape(-1)])[None]
    d = dict(wo=np.ascontiguousarray(wo_perm),
             ln1g=np.ascontiguousarray(inp["ln_g"][layer, 0]), ln1b=np.ascontiguousarray(inp["ln_b"][layer, 0]),
             ln2g=np.ascontiguousarray(inp["ln_g"][layer, 1]), ln2b=np.ascontiguousarray(inp["ln_b"][layer, 1]),
             wg=inp["moe_w_gate"][layer], wu=inp["moe_w_up"][layer], wd=inp["moe_w_down"][layer],
             wr=np.ascontiguousarray(wr.astype(np.float32)), br=np.ascontiguousarray(br.astype(np.float32)))
    return {pfx + k: v for k, v in d.items()}


def kernel(**inputs):
    inp = {k: np.asarray(v) for k, v in inputs.items()}
    nb = inp["x"].shape[0]
    cores = list(range(2 * nb))
    wo0 = inp["ab_w_out"][0]
    ch0 = [(2 * r + i) if i < 2 else (4 + 2 * r + (i - 2)) for i in range(4) for r in range(2)]
    wo0p = np.concatenate([wo0[c * 128:(c + 1) * 128] for c in ch0], axis=0)
    wo1 = inp["c_w_out"][0]
    ch1 = [4 * r + i for i in range(4) for r in range(2)]
    wo1p = np.concatenate([wo1[c * 128:(c + 1) * 128] for c in ch1], axis=0)
    post0 = pack_post(inp, 0, wo0p, "p0_")
    post1 = pack_post(inp, 1, wo1p, "p1_")
    ident = np.eye(128, dtype=np.float32)
    maps = []
    for b in range(nb):
        for c in range(2):
            m = dict(ident=ident, xres=np.ascontiguousarray(inp["x"][b, TOK * c:TOK * (c + 1)]))
            p = np.arange(128, dtype=np.uint32)[:, None]
            rt = np.arange(16, dtype=np.uint32)[None, :]
            m["gidx"] = np.ascontiguousarray(((rt // 8) * 2048 + c * 1024 + (rt % 8) * 128 + p).astype(np.uint32))
            m0 = pack_m0(inp, b, c)
            m0.pop("ident")
            m.update({"m0_" + k: v for k, v in m0.items()})
            m1 = pack_m1(inp, None, c)
            m1.pop("ident")
            m1.pop("x")
            m.update({"m1_" + k: v for k, v in m1.items()})
            m.update(post0)
            m.update(post1)
            maps.append(m)
    nc = build_fused()
    res = run_bass_kernel_spmd(nc, maps, core_ids=cores).results
    out = np.stack([np.concatenate([np.asarray(res[b * 2 + c]["out"]) for c in range(2)], axis=0) for b in range(nb)])
    return out.astype(np.float32)
```

```python
import os
import numpy as np
from contextlib import ExitStack
import concourse.bass as bass
import concourse.mybir as mybir
from concourse.bass_utils import run_bass_kernel_spmd

F32 = mybir.dt.float32
BF16 = mybir.dt.bfloat16
AF = mybir.ActivationFunctionType
ALU = mybir.AluOpType
AX = mybir.AxisListType

D = 1024
ALPHA = (2.0 * 2) ** 0.25
LN_EPS = 1e-5
NEXP = 16
EH = 512


class Buf:
    __slots__ = ("name", "w", "rd", "dsem", "dval", "dkey")

    def __init__(self, name):
        self.name = name
        self.w = None
        self.rd = {}
        self.dsem = None
        self.dval = 0
        self.dkey = None


class Prog:
    def __init__(self, nc, es):
        self.nc = nc
        self.es = es
        self.es_global = es
        self.pfx = ""
        self.eng = dict(pe=nc.tensor, act=nc.scalar, dve=nc.vector, pool=nc.gpsimd, sp=nc.sync)
        self.sem = {k: es.enter_context(nc.semaphore("s_" + k)) for k in self.eng}
        self.cnt = {k: 0 for k in self.eng}
        self.known = {k: {} for k in self.eng}
        self.nsem = 0
        self.outs = []
        self.nbuf = 0
        self.dmasems = {}
        self.issuer = {}
        self.free_sems = []
        self.phase_sems = []
        self.in_phase = False

    def buf(self, name=None):
        self.nbuf += 1
        return Buf(name or f"b{self.nbuf}")

    def sb(self, name, shape, dt):
        return self.es.enter_context(self.nc.sbuf_tensor(self.pfx + name, shape, dt))

    def ps(self, name, shape, dt=F32):
        return self.es.enter_context(self.nc.psum_tensor(self.pfx + name, shape, dt))

    def _collect(self, e, reads, writes):
        deps = {}

        def add(tok, same_ok):
            if tok is None:
                return
            key, sh, val = tok
            if key == e and not same_ok:
                return
            if key in deps and deps[key][1] >= val:
                return
            deps[key] = (sh, val)

        for b in reads:
            add(b.w, e != "pe")
        for b in writes:
            add(b.w, e != "pe")
            for tok in b.rd.values():
                add(tok, e != "pe")
        return deps

    def _wait(self, e, deps):
        kn = self.known[e]
        for key, (sh, val) in deps.items():
            if kn.get(key, 0) >= val:
                continue
            self.eng[e].wait_ge(sh, val)
            kn[key] = val

    def op(self, e, fn, reads=(), writes=()):
        self._wait(e, self._collect(e, reads, writes))
        ins = fn(self.eng[e])
        self.cnt[e] += 1
        ins.then_inc(self.sem[e], 1)
        tok = (e, self.sem[e], self.cnt[e])
        for b in reads:
            b.rd[e] = tok
        for b in writes:
            b.w = tok
            b.rd = {}
        return tok

    def dma(self, q, out, in_, reads=(), writes=(), is_out=False, nowaw=False, **kw):
        wb = writes[0]
        if nowaw:
            saved = wb.w
            if saved is not None and saved[0] == wb.dkey:
                wb.w = None
            deps = self._collect(q, reads, writes)
            wb.w = saved
        else:
            deps = self._collect(q, reads, writes)
        self._wait(q, deps)
        self._ensure_sem(wb)
        ins = self.eng[q].dma_start(out=out, in_=in_, **kw)
        wb.dval += 16
        ins.then_inc(wb.dsem, 16)
        tok = (wb.dkey, wb.dsem, wb.dval)
        self.dmasems[wb.dkey] = tok
        self.issuer[wb.dkey] = q
        for b in reads:
            b.rd[wb.dkey] = tok
        for b in writes:
            b.w = tok
            b.rd = {}
        if is_out and wb not in self.outs:
            self.outs.append(wb)
        return tok

    def _ensure_sem(self, wb):
        if wb.dsem is not None:
            return
        if self.free_sems:
            wb.dkey, wb.dsem, wb.dval = self.free_sems.pop()
        else:
            wb.dkey = f"d{self.nsem}"
            wb.dsem = self.es_global.enter_context(self.nc.semaphore(wb.dkey))
            self.nsem += 1
        if self.in_phase:
            self.phase_sems.append(wb)

    def recycle(self):
        for wb in self.phase_sems:
            self.free_sems.append((wb.dkey, wb.dsem, wb.dval))
            wb.dsem = None
        self.phase_sems = []

    def _tok_async(self, q, wb, ins, inc, reads, writes):
        self._ensure_sem(wb)
        wb.dval += inc
        ins.then_inc(wb.dsem, inc)
        tok = (wb.dkey, wb.dsem, wb.dval)
        self.dmasems[wb.dkey] = tok
        self.issuer[wb.dkey] = q
        for b in reads:
            b.rd[wb.dkey] = tok
        for b in writes:
            b.w = tok
            b.rd = {}
        return tok

    def gather(self, out, in_dram, idx_ap, reads=(), writes=()):
        self._wait("pool", self._collect("pool", reads, writes))
        ins = self.nc.gpsimd.indirect_dma_start(out=out, out_offset=None, in_=in_dram,
                                                in_offset=bass.IndirectOffsetOnAxis(ap=idx_ap, axis=0))
        return self._tok_async("pool", writes[0], ins, 16, reads, writes)

    def all_gather(self, src, dst, groups, reads=(), writes=()):
        self._wait("pool", self._collect("pool", reads, writes))
        ins = self.nc.gpsimd.collective_compute("AllGather", ALU.bypass, replica_groups=groups, ins=[src.opt()], outs=[dst.opt()])
        return self._tok_async("pool", writes[0], ins, 1, reads, writes)

    def finish(self):
        for wb in self.outs:
            if wb.dsem is not None:
                self.eng["sp"].wait_ge(wb.dsem, wb.dval)


def bcast_rows(ap1d_tensor, offset, n, parts=128):
    return bass.AP(ap1d_tensor, offset, [[0, parts], [1, n]])


def emit_tail(P, hmid, hmid_buf, out, out_buf, T, w, layer, ST=1024, banks=None, is_final=True):
    nc = P.nc
    NT = ST // 128
    NK = ST // 512
    ident = P.sb("t_ident", [128, 128], F32)
    ones1 = P.sb("t_ones1", [1, 128], F32)
    brow = P.sb("t_brow", [1, 20], F32)
    wr = P.sb("t_wr", [128, 8, 20], F32)
    gbc = P.sb("t_gbc", [128, D], F32)
    bbc = P.sb("t_bbc", [128, D], F32)
    xin = [P.sb(f"t_xin{i}", [128, D], F32) for i in range(2)]
    acc = P.sb("t_acc", [128, NT, D], F32)
    hT16 = P.sb("t_hT16", [128, 8, ST], BF16)
    hT32 = P.sb("t_hT32", [128, 8, 128], F32)
    gates = P.sb("t_gates", [128, NT, NEXP], F32)
    wgb = [P.sb(f"t_wg{i}", [128, 8, EH], BF16) for i in range(2)]
    wub = [P.sb(f"t_wu{i}", [128, 8, EH], BF16) for i in range(2)]
    wdb = [P.sb(f"t_wd{i}", [128, 4, D], BF16) for i in range(2)]
    sg = [P.sb(f"t_sg{i}", [128, 512], BF16) for i in range(2)]
    hid = [P.sb(f"t_hid{i}", [128, 4, 512], BF16) for i in range(2)]
    obuf = [P.sb(f"t_ob{i}", [128, D], F32) for i in range(2)]
    sm = P.sb("t_sm", [128, 64], F32)
    lg = P.sb("t_lg", [128, 20], F32)
    stats = P.sb("t_stats", [128, 2, 6], F32)

    if banks is None:
        banks = [P.ps(f"t_ps{i}", [128, 512]) for i in range(8)]
    ps_g, ps_u, ps_o, ps_t = banks[0:2], banks[2:4], banks[4:6], banks[6:8]

    B = P.buf
    b_ident, b_ones1, b_brow, b_wr, b_gbc, b_bbc = B(), B(), B(), B(), B(), B()
    b_xin = [B(), B()]
    b_acc = [B() for _ in range(NT)]
    b_hT16 = [B() for _ in range(NT)]
    b_hT32 = B()
    b_gates = [B() for _ in range(NT)]
    b_wg, b_wu, b_wd = [B(), B()], [B(), B()], [B(), B()]
    b_sg = [B(), B()]
    b_hid = [[B() for _ in range(4)] for _ in range(2)]
    b_ob = [B(), B()]
    b_outw = [B(), B()]
    b_sm = B()
    b_lg = B()
    b_stats = B()
    b_psg, b_psu, b_pso, b_pst = [B(), B()], [B(), B()], [B(), B()], [B(), B()]

    P.dma("sp", ident[:], w["ident"][:, :], writes=[b_ident])
    P.dma("sp", brow[:], w["br"][:, :], writes=[b_brow])
    P.dma("sp", wr[:], w["wr"].rearrange("(c p) n -> p c n", p=128), writes=[b_wr])
    P.dma("sp", gbc[:], bcast_rows(w["ln_g"].tensor, w["ln_g"].offset, D), writes=[b_gbc])
    P.dma("sp", bbc[:], bcast_rows(w["ln_b"].tensor, w["ln_b"].offset, D), writes=[b_bbc])
    P.op("dve", lambda e: e.memset(ones1[:], 1.0), writes=[b_ones1])

    nst = T // ST
    wslot = 0
    DBG = ()

    stg = [P.sb(f"t_stg{i}", [128, 4, 512], F32) for i in range(3)]
    b_stg = [B() for _ in range(3)]
    stg_i = [0]

    def load_chunk(dst, bdst, src):
        k = stg_i[0] % 3
        stg_i[0] += 1
        P.dma("sp", stg[k][:], src, writes=[b_stg[k]])
        P.op("act", lambda en, k=k, dst=dst: en.copy(dst, stg[k][:]), reads=[b_stg[k]], writes=[bdst])

    def load_expert(e, slot):
        for h in range(2):
            load_chunk(wgb[slot][:, 4 * h:4 * h + 4, :], b_wg[slot],
                       w["wg"][e][512 * h:512 * h + 512, :].rearrange("(c p) h -> p c h", p=128))
        for h in range(2):
            load_chunk(wub[slot][:, 4 * h:4 * h + 4, :], b_wu[slot],
                       w["wu"][e][512 * h:512 * h + 512, :].rearrange("(c p) h -> p c h", p=128))
        for h in range(2):
            load_chunk(wdb[slot][:, :, 512 * h:512 * h + 512], b_wd[slot],
                       w["wd"][e][:, 512 * h:512 * h + 512].rearrange("(c p) h -> p c h", p=128))

    xi = 0
    oi = 0
    for st in range(nst):
        t0 = st * ST
        if "noload" not in DBG:
            load_expert(0, wslot)
        for tt in range(NT):
            xb, bx = xin[xi % 2], b_xin[xi % 2]
            xi += 1
            P.dma("sp", xb[:], hmid[t0 + tt * 128:t0 + (tt + 1) * 128, :], reads=[hmid_buf], writes=[bx])
            P.op("act", lambda e, xb=xb, tt=tt: e.mul(acc[:, tt, :], xb[:], ALPHA), reads=[bx], writes=[b_acc[tt]])
            if 'notr' in DBG:
                continue
            for half in range(2):
                pt, bpt = ps_t[half], b_pst[half]
                for j in range(4):
                    dc = half * 4 + j
                    P.op("pe", lambda e, pt=pt, j=j, dc=dc, xb=xb: e.transpose(
                        pt[:, j * 128:(j + 1) * 128], xb[:, dc * 128:(dc + 1) * 128], ident[:]),
                        reads=[bx, b_ident], writes=[bpt])
                P.op("act", lambda e, pt=pt, half=half: e.copy(
                    hT32[:, half * 4:(half + 1) * 4, :], pt[:].rearrange("p (c t) -> p c t", c=4)),
                    reads=[bpt], writes=[b_hT32])
                P.op("dve", lambda e, pt=pt, half=half, tt=tt: e.tensor_copy(
                    hT16[:, half * 4:(half + 1) * 4, tt * 128:(tt + 1) * 128],
                    hT32[:, half * 4:(half + 1) * 4, :]),
                    reads=[b_hT32], writes=[b_hT16[tt]])
            if 'norouter' in DBG:
                continue
            pr, bpr = ps_o[0], b_pso[0]
            for dc in range(8):
                P.op("pe", lambda e, dc=dc, pr=pr: e.matmul(pr[:, 0:20], hT32[:, dc, :], wr[:, dc, :],
                                                            start=(dc == 0), stop=False),
                     reads=[b_hT32, b_wr], writes=[bpr])
            P.op("pe", lambda e, pr=pr: e.matmul(pr[:, 0:20], ones1[0:1, :], brow[0:1, :], start=False, stop=True),
                 reads=[b_ones1, b_brow], writes=[bpr])
            P.op("act", lambda e, pr=pr: e.copy(lg[:], pr[:, 0:20]), reads=[bpr], writes=[b_lg])
            emit_router(P, lg, b_lg, sm, b_sm, gates, b_gates[tt], tt)
        units = [(e, tk) for e in range(NEXP) for tk in range(NK)]

        def gu(ui):
            e, tk = units[ui]
            slot = (wslot + e) % 2
            hb = ui % 2
            for hc in range(4):
                pg, bpg = ps_g[hc % 2], b_psg[hc % 2]
                pu, bpu = ps_u[hc % 2], b_psu[hc % 2]
                rds = [b_hT16[tk * 4 + q] for q in range(4)]
                for dc in range(8):
                    P.op("pe", lambda en, pg=pg, dc=dc, hc=hc, slot=slot, tk=tk: en.matmul(
                        pg[:], wgb[slot][:, dc, hc * 128:(hc + 1) * 128], hT16[:, dc, tk * 512:(tk + 1) * 512],
                        start=(dc == 0), stop=(dc == 7)), reads=rds + [b_wg[slot]], writes=[bpg])
                for dc in range(8):
                    P.op("pe", lambda en, pu=pu, dc=dc, hc=hc, slot=slot, tk=tk: en.matmul(
                        pu[:], wub[slot][:, dc, hc * 128:(hc + 1) * 128], hT16[:, dc, tk * 512:(tk + 1) * 512],
                        start=(dc == 0), stop=(dc == 7)), reads=rds + [b_wu[slot]], writes=[bpu])
                s, bs = sg[hc % 2], b_sg[hc % 2]
                P.op("act", lambda en, s=s, pg=pg: en.activation(s[:], pg[:], AF.Silu), reads=[bpg], writes=[bs])
                P.op("dve", lambda en, s=s, pu=pu, hb=hb, hc=hc: en.tensor_tensor(
                    hid[hb][:, hc, :], s[:], pu[:], ALU.mult), reads=[bs, bpu], writes=[b_hid[hb][hc]])

        def down(ui):
            e, tk = units[ui]
            slot = (wslot + e) % 2
            hb = ui % 2
            k = 0
            for tq in range(4):
                tt = tk * 4 + tq
                for dh in range(2):
                    po, bpo = ps_o[k % 2], b_pso[k % 2]
                    k += 1
                    for hc in range(4):
                        P.op("pe", lambda en, po=po, hc=hc, tq=tq, dh=dh, hb=hb, slot=slot: en.matmul(
                            po[:], hid[hb][:, hc, tq * 128:(tq + 1) * 128], wdb[slot][:, hc, dh * 512:(dh + 1) * 512],
                            start=(hc == 0), stop=(hc == 3)), reads=[b_hid[hb][hc], b_wd[slot]], writes=[bpo])
                    P.op("dve", lambda en, po=po, tt=tt, dh=dh, e=e: en.scalar_tensor_tensor(
                        acc[:, tt, dh * 512:(dh + 1) * 512], po[:], gates[:, tt, e:e + 1],
                        acc[:, tt, dh * 512:(dh + 1) * 512], ALU.mult, ALU.add),
                        reads=[bpo, b_gates[tt], b_acc[tt]], writes=[b_acc[tt]])

        if "noexp" in DBG:
            units = []
        if "oneexp" in DBG:
            units = units[:2]
        for ui in range(len(units)):
            e, tk = units[ui]
            gu(ui)
            if ui > 0:
                down(ui - 1)
            if tk == 0 and e + 1 < NEXP:
                load_expert(e + 1, (wslot + e + 1) % 2)
        if units:
            down(len(units) - 1)
        wslot = (wslot + NEXP) % 2
        for tt in range(NT):
            ob, bo = obuf[oi % 2], b_ob[oi % 2]
            oi += 1
            if "nolnorm" in DBG:
                P.op("act", lambda e, ob=ob, tt=tt: e.copy(ob[:], acc[:, tt, :]), reads=[b_acc[tt]], writes=[bo])
            else:
                emit_ln(P, acc[:, tt, :], b_acc[tt], ob, bo, gbc, b_gbc, bbc, b_bbc, stats, b_stats, sm, b_sm)
            P.dma("sp", out(t0 + tt * 128), ob[:], reads=[bo], writes=[b_outw[(oi - 1) % 2]], is_out=is_final)


def emit_ln(P, x, bx, ob, bo, gbc, b_gbc, bbc, b_bbc, stats, b_stats, sm, b_sm):
    mv = sm[:, 40:42]
    rstd = sm[:, 42:43]
    nmr = sm[:, 43:44]
    P.op("dve", lambda e: e.bn_stats(stats[:, 0, :], x[:, 0:512]), reads=[bx], writes=[b_stats])
    P.op("dve", lambda e: e.bn_stats(stats[:, 1, :], x[:, 512:1024]), reads=[bx], writes=[b_stats])
    P.op("dve", lambda e: e.bn_aggr(mv, stats[:]), reads=[b_stats], writes=[b_sm])
    P.op("dve", lambda e: e.tensor_scalar(rstd, sm[:, 41:42], LN_EPS, None, ALU.add), reads=[b_sm], writes=[b_sm])
    P.op("act", lambda e: e.sqrt(rstd, rstd), reads=[b_sm], writes=[b_sm])
    P.op("dve", lambda e: e.reciprocal(rstd, rstd), reads=[b_sm], writes=[b_sm])
    P.op("dve", lambda e: e.scalar_tensor_tensor(nmr, sm[:, 40:41], -1.0, rstd, ALU.mult, ALU.mult), reads=[b_sm], writes=[b_sm])
    P.op("act", lambda e: e.activation(ob[:], x, AF.Identity, bias=nmr, scale=rstd), reads=[bx, b_sm], writes=[bo])
    P.op("dve", lambda e: e.tensor_tensor(ob[:], ob[:], gbc[:], ALU.mult), reads=[bo, b_gbc], writes=[bo])
    P.op("dve", lambda e: e.tensor_tensor(ob[:], ob[:], bbc[:], ALU.add), reads=[bo, b_bbc], writes=[bo])


def emit_router(P, lg, b_lg, sm, b_sm, gates, b_g, tt):
    gmax, ngmax, gsum, gw = sm[:, 0:1], sm[:, 1:2], sm[:, 2:3], sm[:, 3:4]
    ohg, gexp, esel, oh1 = sm[:, 4:8], sm[:, 8:12], sm[:, 12:16], sm[:, 16:20]
    em, oh2 = sm[:, 20:24], sm[:, 24:28]
    m1, m2, dd, ed, den, w1, w2 = (sm[:, 28 + i:29 + i] for i in range(7))
    ew = sm[:, 36:40]
    R = [b_lg, b_sm]
    W = [b_sm]

    def dv(fn, reads=R, writes=W):
        P.op("dve", fn, reads=reads, writes=writes)

    dv(lambda e: e.tensor_reduce(gmax, lg[:, 0:4], AX.X, ALU.max))
    dv(lambda e: e.tensor_scalar(ohg, lg[:, 0:4], gmax, None, ALU.is_equal))
    dv(lambda e: e.tensor_scalar(ngmax, gmax, -1.0, None, ALU.mult))
    P.op("act", lambda e: e.activation(gexp, lg[:, 0:4], AF.Exp, bias=ngmax, scale=1.0), reads=R, writes=W)
    dv(lambda e: e.tensor_reduce(gsum, gexp, AX.X, ALU.add))
    dv(lambda e: e.reciprocal(gw, gsum))
    dv(lambda e: e.tensor_scalar(esel, lg[:, 4:8], ohg[:, 0:1], None, ALU.mult))
    for g in range(1, 4):
        dv(lambda e, g=g: e.scalar_tensor_tensor(esel, lg[:, 4 + 4 * g:8 + 4 * g], ohg[:, g:g + 1], esel, ALU.mult, ALU.add))
    dv(lambda e: e.tensor_reduce(m1, esel, AX.X, ALU.max))
    dv(lambda e: e.tensor_scalar(oh1, esel, m1, None, ALU.is_equal))
    dv(lambda e: e.scalar_tensor_tensor(em, oh1, -1e30, esel, ALU.mult, ALU.add))
    dv(lambda e: e.tensor_reduce(m2, em, AX.X, ALU.max))
    dv(lambda e: e.tensor_scalar(oh2, em, m2, None, ALU.is_equal))
    dv(lambda e: e.tensor_tensor(dd, m2, m1, ALU.subtract))
    P.op("act", lambda e: e.activation(ed, dd, AF.Exp), reads=R, writes=W)
    dv(lambda e: e.tensor_scalar(den, ed, 1.0, None, ALU.add))
    dv(lambda e: e.reciprocal(den, den))
    dv(lambda e: e.tensor_tensor(w1, den, gw, ALU.mult))
    dv(lambda e: e.tensor_tensor(w2, w1, ed, ALU.mult))
    dv(lambda e: e.tensor_scalar(ew, oh1, w1, None, ALU.mult))
    dv(lambda e: e.scalar_tensor_tensor(ew, oh2, w2, ew, ALU.mult, ALU.add))
    for g in range(4):
        dv(lambda e, g=g: e.tensor_scalar(gates[:, tt, 4 * g:4 * g + 4], ew, ohg[:, g:g + 1], None, ALU.mult),
           reads=[b_sm], writes=[b_g])


def t5_bucket_np(dist):
    n = np.maximum(dist, 0)
    nf = np.maximum(n, 1).astype(np.float32)
    large = 16 + (np.log(nf / np.float32(16)) / np.float32(np.log(2048 / 16)) * np.float32(16)).astype(np.int32)
    return np.where(n < 16, n, np.minimum(large, 31))


def dil_onehot():
    oh = np.zeros((33, 3, 384), np.float32)
    for di, d in enumerate((1, 4, 16)):
        for jp in range(384):
            j = jp - 127
            if 0 <= j <= 128:
                oh[t5_bucket_np(np.array(j * d)), di, jp] = 1.0
            else:
                oh[32, di, jp] = 1.0
    return oh


class Banks:
    def __init__(self, P):
        self.f = [P.ps(f"bank{i}", [128, 512], F32) for i in range(7)]
        self.bf = [P.buf() for _ in range(7)]
        self.h = P.ps("bankh", [128, 1024], BF16)
        self.bh = P.buf()


def full_barrier(P):
    for q in ("sp", "pool"):
        eq = P.eng[q]
        for key, (k_, sh, val) in list(P.dmasems.items()):
            if P.issuer.get(key) == q and P.known[q].get(key, 0) < val:
                eq.wait_ge(sh, val)
                P.known[q][key] = val
        eq.sem_inc(P.sem[q], 1)
        P.cnt[q] += 1
    for e in P.eng:
        for f in P.eng:
            if f != e and P.cnt[f] > P.known[e].get(f, 0):
                P.eng[e].wait_ge(P.sem[f], P.cnt[f])
                P.known[e][f] = P.cnt[f]
    for e in P.eng:
        for (key, sh, val) in P.dmasems.values():
            P.known[e][key] = max(P.known[e].get(key, 0), val)


class Pipe:
    def __init__(self, depth=1):
        self.q = []
        self.depth = depth

    def step(self, fn):
        self.q.append(fn)
        if len(self.q) > self.depth:
            self.q.pop(0)()

    def flush(self):
        while self.q:
            self.q.pop(0)()


class Stager:
    def __init__(self, P, n=3, cols=2048):
        self.P = P
        self.cols = cols
        self.t = [P.sb(f"stg{i}", [128, cols], F32) for i in range(n)]
        self.b = [P.buf() for _ in range(n)]
        self.i = 0

    def load(self, dst3, bdst, src2d, eng="act"):
        C, W = dst3.shape[1], dst3.shape[2]
        per = max(1, self.cols // W)
        c0 = 0
        while c0 < C:
            c1 = min(C, c0 + per)
            k = self.i % len(self.t)
            self.i += 1
            st = self.t[k][:, 0:(c1 - c0) * W].rearrange("p (c w) -> p c w", w=W)
            self.P.dma("sp", st, src2d[c0 * 128:c1 * 128, :].rearrange("(c p) w -> p c w", p=128), writes=[self.b[k]])
            self.P.op(eng, lambda en, st=st, c0=c0, c1=c1: en.copy(dst3[:, c0:c1, :], st),
                      reads=[self.b[k]], writes=[bdst])
            c0 = c1


def load_xT(P, BK, x, bx_dram, xin, b_xin, xT, b_xT, ident, b_ident, t0, ntile, ctr):
    for tl in range(ntile):
        k = ctr[0] % 2
        ctr[0] += 1
        xb, bx = xin[k], b_xin[k]
        P.dma("sp", xb[:], x(t0 + tl * 128), reads=[bx_dram], writes=[bx])
        for half in range(2):
            pt, bpt = BK.f[half], BK.bf[half]
            for j in range(4):
                dc = half * 4 + j
                P.op("pe", lambda e, pt=pt, j=j, dc=dc, xb=xb: e.transpose(
                    pt[:, j * 128:(j + 1) * 128], xb[:, dc * 128:(dc + 1) * 128], ident[:]),
                    reads=[bx, b_ident], writes=[bpt])
            eng = "act" if half == 0 else "dve"
            if eng == "act":
                P.op("act", lambda e, pt=pt, half=half, tl=tl: e.copy(
                    xT[:, half * 4:(half + 1) * 4, tl * 128:(tl + 1) * 128], pt[:].rearrange("p (c t) -> p c t", c=4)),
                    reads=[bpt], writes=[b_xT[tl]])
            else:
                P.op("dve", lambda e, pt=pt, half=half, tl=tl: e.tensor_copy(
                    xT[:, half * 4:(half + 1) * 4, tl * 128:(tl + 1) * 128], pt[:].rearrange("p (c t) -> p c t", c=4)),
                    reads=[bpt], writes=[b_xT[tl]])


S_TOT = 8192
NBLK = 4
BLK = 2048


def declare_mixer0(nc, pfx=""):
    din = lambda n, s: nc.dram_tensor(pfx + n, s, F32, kind="ExternalInput").ap()
    return dict(x=din("x", [S_TOT, D]), wA=din("wA", [D, 768]), wB=din("wB", [D, 1028]), gbias=din("gbias", [1, 4]),
                convw=din("convw", [128, 16]), hng=din("hng", [1, 256]), relb=din("relb", [33, 4]), oh=din("oh", [33, 3 * 384]),
                triu=din("triu", [128, 128]),
                scr=nc.dram_tensor(pfx + "scr_e", [128, 12 * 384], F32).ap())


def emit_mixer0(P, io):
    nc = P.nc
    x_d, wA, wB, gbias, convw, hng, relb = io["x"], io["wA"], io["wB"], io["gbias"], io["convw"], io["hng"], io["relb"]
    oh_d, ident_d, triu_d, scr = io["oh"], io["ident"], io["triu"], io["scr"]
    x = lambda t0: x_d[t0:t0 + 128, :]
    ystore = io["ystore"]
    if True:
        B = P.buf
        BK = Banks(P)
        b_x, b_y, b_scr = B(), B(), B()
        ident = P.sb("ident_sb", [128, 128], F32)
        b_ident = B()
        P.dma("sp", ident[:], ident_d[:, :], writes=[b_ident])
        xin = [P.sb(f"xin{i}", [128, D], F32) for i in range(2)]
        b_xin = [B(), B()]
        xT = P.sb("xT", [128, 8, BLK], BF16)
        b_xT = [B() for _ in range(16)]
        stg = Stager(P)
        ctr = [0]
        ones_bf = P.sb("ones_bf", [128, 128], BF16)
        b_ones = B()
        P.op("dve", lambda e: e.memset(ones_bf[:], 1.0), writes=[b_ones])

        with ExitStack() as esA:
            sbA = lambda n, s, d: esA.enter_context(nc.sbuf_tensor(P.pfx + n, s, d))
            Etab = sbA("a_E", [128, 12, 2, 128], BF16)
            with ExitStack() as esT:
                sbA_outer = sbA
                sbA = lambda n, s, d: esT.enter_context(nc.sbuf_tensor(P.pfx + n, s, d))
                tab = sbA("a_tab", [33, 4], F32)
                ohs = sbA("a_oh", [33, 3 * 384], F32)
                tabB = sbA("a_tabB", [33, 128], F32)
                fre = sbA("a_fre", [128, 12 * 384], F32)
                E32 = sbA("a_E32", [128, 12 * 256], F32)
                b_tab, b_oh, b_tabB, b_fre, b_E32, b_E = B(), B(), B(), B(), B(), B()
                P.dma("sp", tab[:], relb[:, :], writes=[b_tab])
                P.dma("sp", ohs[:], oh_d[:, :], writes=[b_oh])
                for h in range(4):
                    P.op("dve", lambda e, h=h: e.tensor_copy(tabB[:], tab[:, h:h + 1].to_broadcast([33, 128])),
                         reads=[b_tab], writes=[b_tabB])
                    for di in range(3):
                        pb, bpb = BK.f[2 + (di % 2)], BK.bf[2 + (di % 2)]
                        P.op("pe", lambda e, pb=pb, di=di: e.matmul(pb[:, 0:384], tabB[:], ohs[:, di * 384:(di + 1) * 384],
                                                                    start=True, stop=True),
                             reads=[b_tabB, b_oh], writes=[bpb])
                        P.op("act", lambda e, pb=pb, di=di, h=h: e.activation(
                            fre[:, (di * 4 + h) * 384:(di * 4 + h + 1) * 384], pb[:, 0:384], AF.Exp),
                            reads=[bpb], writes=[b_fre])
                P.dma("sp", scr[:, :], fre[:], reads=[b_fre], writes=[b_scr])
                L = 12 * 384
                skew = bass.AP(scr.tensor, 127, [[L - 1, 128], [384, 12], [128, 2], [1, 128]])
                P.dma("sp", E32[:].rearrange("p (t s q) -> p t s q", t=12, s=2), skew, reads=[b_scr], writes=[b_E32])
                P.op("dve", lambda e: e.tensor_copy(Etab[:].rearrange("p t s q -> p (t s q)"), E32[:]),
                     reads=[b_E32], writes=[b_E])

                full_barrier(P)
            sbA = sbA_outer
            wAb = sbA("a_w", [128, 8, 384], BF16)
            b_wA = B()
            QT = sbA("a_QT", [128, S_TOT], BF16)
            KT = sbA("a_KT", [128, S_TOT], BF16)
            b_QT = [B() for _ in range(NBLK)]
            b_KT = [B() for _ in range(NBLK)]
            Vd = [sbA(f"a_V{di}", [128, 64, 128], BF16) for di in range(3)]
            b_Vd = [[B() for _ in range(NBLK)] for _ in range(3)]
            numacc = sbA("a_num", [128, BLK], F32)
            denacc = sbA("a_den", [128, BLK], F32)
            b_num, b_den = B(), B()
            Pb = [sbA(f"a_P{i}", [128, 2, 2, 128], BF16) for i in range(2)]
            b_Pb = [B(), B()]
            ybuf = sbA("a_y", [128, BLK], BF16)
            b_yb = B()
            pcount = 0
            pipeA = Pipe()
            for hp in range(2):
                stg.load(wAb[:], b_wA, wA[:, hp * 384:(hp + 1) * 384])
                for blk in range(NBLK):
                    T0 = blk * BLK
                    load_xT(P, BK, x, b_x, xin, b_xin, xT, b_xT, ident, b_ident, T0, 16, ctr)
                    for qtr in range(4):
                        rds = [b_xT[qtr * 4 + i] for i in range(4)] + [b_wA]
                        for sel, (dst, bd) in enumerate(((QT, b_QT[blk]), (KT, b_KT[blk]))):
                            pb, bpb = BK.f[2 + sel], BK.bf[2 + sel]
                            for dc in range(8):
                                P.op("pe", lambda e, pb=pb, dc=dc, sel=sel, qtr=qtr: e.matmul(
                                    pb[:], wAb[:, dc, sel * 128:(sel + 1) * 128], xT[:, dc, qtr * 512:(qtr + 1) * 512],
                                    start=(dc == 0), stop=(dc == 7)), reads=rds, writes=[bpb])
                            if sel == 0:
                                P.op("act", lambda e, pb=pb, dst=dst, T0=T0, qtr=qtr: e.copy(
                                    dst[:, T0 + qtr * 512:T0 + (qtr + 1) * 512], pb[:]), reads=[bpb], writes=[bd])
                            else:
                                P.op("dve", lambda e, pb=pb, dst=dst, T0=T0, qtr=qtr: e.tensor_copy(
                                    dst[:, T0 + qtr * 512:T0 + (qtr + 1) * 512], pb[:]), reads=[bpb], writes=[bd])
                    for di, d in enumerate((1, 4, 16)):
                        npb = 16 // d
                        tiles = [(nl, r) for nl in range(npb) for r in range(d)]
                        for g4 in range(4):
                            pb, bpb = BK.f[2 + (g4 % 2)], BK.bf[2 + (g4 % 2)]
                            for j in range(4):
                                nl, r = tiles[g4 * 4 + j]
                                s0 = r + d * 128 * nl
                                for dc in range(8):
                                    P.op("pe", lambda e, pb=pb, j=j, dc=dc, s0=s0, d=d: e.matmul(
                                        pb[:, j * 128:(j + 1) * 128], xT[:, dc, s0:s0 + 127 * d + 1:d], wAb[:, dc, 256:384],
                                        start=(dc == 0), stop=(dc == 7)), reads=b_xT + [b_wA], writes=[bpb])
                            nl0, r0 = tiles[g4 * 4]
                            idx0 = (blk * npb + nl0) * d + r0
                            P.op("act" if g4 % 2 == 0 else "dve",
                                 (lambda e, pb=pb, di=di, idx0=idx0: e.copy(
                                     Vd[di][:, idx0:idx0 + 4, :], pb[:].rearrange("p (j c) -> p j c", j=4)))
                                 if g4 % 2 == 0 else
                                 (lambda e, pb=pb, di=di, idx0=idx0: e.tensor_copy(
                                     Vd[di][:, idx0:idx0 + 4, :], pb[:].rearrange("p (j c) -> p j c", j=4))),
                                 reads=[bpb], writes=[b_Vd[di][blk]])
                    for di, d in enumerate((1, 4, 16)):
                        npb = 16 // d
                        for nl in range(npb):
                            n = blk * npb + nl
                            nslot = 2 if n > 0 else 1
                            for r in range(d):
                                qs = T0 + r + d * 128 * nl
                                ql = r + d * 128 * nl
                                par = pcount % 2
                                ps_h = [BK.f[2 + 2 * par + h] for h in range(2)]
                                bps_h = [BK.bf[2 + 2 * par + h] for h in range(2)]
                                pb_, bpb_ = Pb[pcount % 2], b_Pb[pcount % 2]
                                pcount += 1
                                kblks = sorted({blk, (qs - d * 128) // BLK}) if n > 0 else [blk]
                                rds = [b_QT[blk]] + [b_KT[kb] for kb in kblks]
                                for h in range(2):
                                    for sl in range(nslot):
                                        ks = qs - sl * d * 128
                                        P.op("pe", lambda e, h=h, sl=sl, ks=ks, qs=qs, d=d, ps_h=ps_h: e.matmul(
                                            ps_h[h][:, sl * 128:(sl + 1) * 128],
                                            KT[h * 64:(h + 1) * 64, ks:ks + 127 * d + 1:d],
                                            QT[h * 64:(h + 1) * 64, qs:qs + 127 * d + 1:d], start=True, stop=True),
                                            reads=rds, writes=[bps_h[h]])
                                for h in range(2):
                                    P.op("act", lambda e, pb_=pb_, h=h, nslot=nslot, ps_h=ps_h: e.activation(
                                        pb_[:, h, 0:nslot, :], ps_h[h][:, 0:nslot * 128].rearrange("p (s q) -> p s q", s=nslot),
                                        AF.Exp, scale=0.125), reads=[bps_h[h]], writes=[bpb_])
                                P.op("dve", lambda e, pb_=pb_, di=di, hp=hp, nslot=nslot: e.tensor_tensor(
                                    pb_[:, :, 0:nslot, :], pb_[:, :, 0:nslot, :],
                                    Etab[:, di * 4 + hp * 2:di * 4 + hp * 2 + 2, 0:nslot, :], ALU.mult),
                                    reads=[bpb_, b_E], writes=[bpb_])
                                def stageB(di=di, d=d, n=n, r=r, nslot=nslot, pb_=pb_, bpb_=bpb_, kblks=kblks, ql=ql, u=pcount):
                                    pv, bpv = (BK.f[6], BK.bf[6]) if u % 2 == 0 else (BK.f[0], BK.bf[0])
                                    vrd = [b_Vd[di][kb] for kb in kblks]
                                    for h in range(2):
                                        for sl in range(nslot):
                                            kidx = (n - sl) * d + r
                                            P.op("pe", lambda e, pv=pv, h=h, sl=sl, kidx=kidx, di=di, pb_=pb_, nslot=nslot: e.matmul(
                                                pv[:, h * 128:(h + 1) * 128], Vd[di][:, kidx, :], pb_[:, h, sl, :],
                                                start=(sl == 0), stop=(sl == nslot - 1)), reads=vrd + [bpb_], writes=[bpv])
                                        for sl in range(nslot):
                                            P.op("pe", lambda e, pv=pv, h=h, sl=sl, pb_=pb_, nslot=nslot: e.matmul(
                                                pv[:, (2 + h) * 128:(3 + h) * 128], ones_bf[:], pb_[:, h, sl, :],
                                                start=(sl == 0), stop=(sl == nslot - 1)), reads=[b_ones, bpb_], writes=[bpv])
                                    for h in range(2):
                                        pr = slice(h * 64, (h + 1) * 64)
                                        qc = slice(ql, ql + 127 * d + 1, d)
                                        if di == 0:
                                            P.op("dve", lambda e, pr=pr, qc=qc, h=h: e.tensor_copy(
                                                numacc[pr, qc], pv[pr, h * 128:(h + 1) * 128]), reads=[bpv], writes=[b_num])
                                            P.op("dve", lambda e, pr=pr, qc=qc, h=h: e.tensor_copy(
                                                denacc[pr, qc], pv[pr, (2 + h) * 128:(3 + h) * 128]), reads=[bpv], writes=[b_den])
                                        else:
                                            P.op("dve", lambda e, pr=pr, qc=qc, h=h: e.tensor_tensor(
                                                numacc[pr, qc], numacc[pr, qc], pv[pr, h * 128:(h + 1) * 128], ALU.add),
                                                reads=[bpv, b_num], writes=[b_num])
                                            P.op("dve", lambda e, pr=pr, qc=qc, h=h: e.tensor_tensor(
                                                denacc[pr, qc], denacc[pr, qc], pv[pr, (2 + h) * 128:(3 + h) * 128], ALU.add),
                                                reads=[bpv, b_den], writes=[b_den])
                                pipeA.step(stageB)
                    pipeA.flush()
                    P.op("dve", lambda e: e.reciprocal(denacc[:], denacc[:]), reads=[b_den], writes=[b_den])
                    P.op("dve", lambda e: e.tensor_tensor(ybuf[:], numacc[:], denacc[:], ALU.mult),
                         reads=[b_num, b_den], writes=[b_yb])
                    ystore(P, hp, blk, ybuf[:], [b_yb])
            full_barrier(P)

        with ExitStack() as esB:
            sbB = lambda n, s, d: esB.enter_context(nc.sbuf_tensor(P.pfx + n, s, d))
            SC = 128.0 ** -0.5
            wBb = sbB("b_w", [128, 8, 1028], BF16)
            b_wB = B()
            stg.load(wBb[:], b_wB, wB[:, :])
            pc = sbB("b_pc", [128, 4, BLK + 3], F32)
            b_pc = [B() for _ in range(4)]
            ctmp = [sbB(f"b_ct{i}", [128, BLK], F32) for i in range(2)]
            b_ct = [B(), B()]
            QKb = sbB("b_qk", [128, 4, BLK], BF16)
            b_qk = [B() for _ in range(4)]
            cw = sbB("b_cw", [128, 16], F32)
            hngb = sbB("b_hng", [128, 256], F32)
            gbb = sbB("b_gb", [128, 4], F32)
            triu32 = sbB("b_triu32", [128, 128], F32)
            triub = sbB("b_triub", [128, 128], BF16)
            ones32 = sbB("b_ones32", [128, 128], F32)
            identb = sbB("b_identb", [128, 128], BF16)
            b_cst = B()
            P.dma("sp", cw[:], convw[:, :], writes=[b_cst])
            b_c2, b_c3, b_c4 = B(), B(), B()
            P.dma("sp", hngb[:], bcast_rows(hng.tensor, hng.offset, 256), writes=[b_c2])
            P.dma("sp", gbb[:], bcast_rows(gbias.tensor, gbias.offset, 4), writes=[b_c3])
            P.dma("sp", triu32[:], triu_d[:, :], writes=[b_c4])
            b_c5 = B()
            P.op("dve", lambda e: e.tensor_copy(triub[:], triu32[:]), reads=[b_c4], writes=[b_c5])
            P.op("dve", lambda e: e.memset(ones32[:], 1.0), writes=[b_c5])
            P.op("dve", lambda e: e.tensor_copy(identb[:], ident[:]), reads=[b_ident], writes=[b_c5])
            CST = [b_cst, b_c2, b_c3, b_c4, b_c5]
            Vaug = [sbB(f"b_va{i}", [128, 2, 129], BF16) for i in range(2)]
            b_va = [B(), B()]
            sgo = [sbB(f"b_sgo{i}", [128, 256], F32) for i in range(2)]
            b_sgo = [B(), B()]
            smB = [sbB(f"b_sm{i}", [128, 64], F32) for i in range(2)]
            b_smB = [B(), B()]
            Sm = [sbB(f"b_Sm{i}", [128, 128], BF16) for i in range(2)]
            b_Sm = [B(), B()]
            hh = [sbB(f"b_hh{i}", [128, 128], F32) for i in range(2)]
            b_hh = [B(), B()]
            yn32 = [sbB(f"b_yn{i}", [128, 128], F32) for i in range(2)]
            b_yn = [B(), B()]
            ynb = [sbB(f"b_ynb{i}", [128, 128], BF16) for i in range(2)]
            b_ynb = [B(), B()]
            Ktm = [sbB(f"b_ktm{i}", [128, 128], BF16) for i in range(2)]
            b_ktm = [B(), B()]
            Vw = [sbB(f"b_vw{i}", [128, 129], BF16) for i in range(2)]
            b_vw = [B(), B()]
            C32 = sbB("b_C32", [128, 2, 129], F32)
            Cb = sbB("b_Cb", [128, 2, 129], BF16)
            b_C32, b_Cb = [B(), B()], [B(), B()]
            hst = [sbB(f"b_hst{i}", [128, 16], F32) for i in range(2)]
            b_hst = [B(), B()]
            ybufB = sbB("b_yb", [128, 2, BLK], BF16)
            b_ybB = B()
            P.op("dve", lambda e: e.memset(C32[:], 0.0), writes=b_C32)
            P.op("dve", lambda e: e.memset(Cb[:], 0.0), writes=b_Cb)
            for k in range(2):
                P.op("dve", lambda e, k=k: e.memset(Vaug[k][:, :, 128:129], 1.0), writes=[b_va[k]])
            for g in range(4):
                P.op("dve", lambda e, g=g: e.memset(pc[:, g, 0:3], 0.0), writes=[b_pc[g]])
            f0, f1, f2, f3, f4, f5, f6 = BK.f
            bf0, bf1, bf2, bf3, bf4, bf5, bf6 = BK.bf
            cti = 0
            for blk in range(NBLK):
                T0 = blk * BLK
                load_xT(P, BK, x, b_x, xin, b_xin, xT, b_xT, ident, b_ident, T0, 16, ctr)
                for g in range(4):
                    for qtr in range(4):
                        pb, bpb = (f0, bf0) if g % 2 == 0 else (f1, bf1)
                        rds = [b_xT[qtr * 4 + i] for i in range(4)] + [b_wB]
                        for dc in range(8):
                            P.op("pe", lambda e, pb=pb, dc=dc, g=g, qtr=qtr: e.matmul(
                                pb[:], wBb[:, dc, g * 128:(g + 1) * 128], xT[:, dc, qtr * 512:(qtr + 1) * 512],
                                start=(dc == 0), stop=(dc == 7)), reads=rds, writes=[bpb])
                        if g % 2 == 0:
                            P.op("act", lambda e, pb=pb, g=g, qtr=qtr: e.copy(pc[:, g, 3 + qtr * 512:3 + (qtr + 1) * 512], pb[:]),
                                 reads=[bpb], writes=[b_pc[g]])
                        else:
                            P.op("dve", lambda e, pb=pb, g=g, qtr=qtr: e.tensor_copy(pc[:, g, 3 + qtr * 512:3 + (qtr + 1) * 512], pb[:]),
                                 reads=[bpb], writes=[b_pc[g]])
                for g in range(4):
                    ct, bct = ctmp[cti % 2], b_ct[cti % 2]
                    cti += 1
                    P.op("dve", lambda e, ct=ct, g=g: e.tensor_scalar(ct[:], pc[:, g, 3:3 + BLK], cw[:, g * 4 + 3:g * 4 + 4], None, ALU.mult),
                         reads=[b_pc[g], b_cst], writes=[bct])
                    for j in (2, 1, 0):
                        P.op("dve", lambda e, ct=ct, g=g, j=j: e.scalar_tensor_tensor(
                            ct[:], pc[:, g, j:j + BLK], cw[:, g * 4 + j:g * 4 + j + 1], ct[:], ALU.mult, ALU.add),
                            reads=[b_pc[g], b_cst, bct], writes=[bct])
                    P.op("act", lambda e, ct=ct, g=g: e.activation(QKb[:, g, :], ct[:], AF.Silu), reads=[bct], writes=[b_qk[g]])
                    P.op("dve", lambda e, g=g: e.tensor_copy(pc[:, g, 0:3], pc[:, g, BLK:BLK + 3]), reads=[b_pc[g]], writes=[b_pc[g]])
                for ch in range(16):
                    k = ch % 2
                    tsl = slice(ch * 128, (ch + 1) * 128)
                    va, bva, sg_, bsg, sm_, bsm = Vaug[k], b_va[k], sgo[k], b_sgo[k], smB[k], b_smB[k]
                    gt, e1, sp_, cs = sm_[:, 0:4], sm_[:, 4:6], sm_[:, 6:8], sm_[:, 8:12]
                    ti, ws, wsq, eb, ebL = sm_[:, 12:14], sm_[:, 14:16], sm_[:, 16:18], sm_[:, 18:20], sm_[:, 20:22]
                    rdx = [b_xT[ch], b_wB]
                    for (bank, bbank, c0, c1, w0) in ((f2, bf2, 0, 256, 512), (f2, bf2, 256, 260, 1024), (f3, bf3, 0, 256, 768)):
                        for dc in range(8):
                            P.op("pe", lambda e, bank=bank, c0=c0, c1=c1, w0=w0, dc=dc, tsl=tsl: e.matmul(
                                bank[:, c0:c1], xT[:, dc, tsl], wBb[:, dc, w0:w0 + (c1 - c0)],
                                start=(dc == 0), stop=(dc == 7)), reads=rdx, writes=[bbank])
                    P.op("dve", lambda e, va=va: e.tensor_copy(va[:, :, 0:128], f2[:, 0:256].rearrange("p (h c) -> p h c", h=2)),
                         reads=[bf2], writes=[bva])
                    P.op("dve", lambda e, gt=gt: e.tensor_tensor(gt, f2[:, 256:260], gbb[:], ALU.add), reads=[bf2] + CST, writes=[bsm])
                    P.op("act", lambda e, sg_=sg_: e.activation(sg_[:], f3[:, 0:256], AF.Sigmoid), reads=[bf3], writes=[bsg])
                    P.op("act", lambda e, e1=e1, gt=gt: e.activation(e1, gt[:, 2:4], AF.Exp, scale=-1.0), reads=[bsm], writes=[bsm])
                    P.op("act", lambda e, e1=e1, sp_=sp_: e.activation(sp_, e1, AF.Ln, bias=1.0), reads=[bsm], writes=[bsm])
                    P.op("pe", lambda e, sp_=sp_: e.matmul(f3[:, 256:258], triu32[:], sp_, start=True, stop=True), reads=[bsm] + CST, writes=[bf3])
                    P.op("pe", lambda e, sp_=sp_: e.matmul(f3[:, 258:260], ones32[:], sp_, start=True, stop=True), reads=[bsm] + CST, writes=[bf3])
                    P.op("act", lambda e, cs=cs: e.copy(cs, f3[:, 256:260]), reads=[bf3], writes=[bsm])
                    P.op("dve", lambda e, ti=ti, gt=gt, cs=cs: e.tensor_tensor(ti, gt[:, 0:2], cs[:, 0:2], ALU.add), reads=[bsm], writes=[bsm])
                    P.op("act", lambda e, ws=ws, ti=ti: e.activation(ws, ti, AF.Exp), reads=[bsm], writes=[bsm])
                    P.op("act", lambda e, eb=eb, cs=cs: e.activation(eb, cs[:, 0:2], AF.Exp), reads=[bsm], writes=[bsm])
                    P.op("act", lambda e, ebL=ebL, cs=cs: e.activation(ebL, cs[:, 2:4], AF.Exp, scale=-1.0), reads=[bsm], writes=[bsm])
                    P.op("dve", lambda e, wsq=wsq, ws=ws: e.tensor_scalar(wsq, ws, SC, None, ALU.mult), reads=[bsm], writes=[bsm])
                    for h in range(2):
                        st_, bst = hst[h], b_hst[h]
                        P.op("pe", lambda e, h=h, tsl=tsl: e.matmul(f4[:, h * 128:(h + 1) * 128], QKb[:, 2 + h, tsl], QKb[:, h, tsl],
                                                                     start=True, stop=True), reads=[b_qk[h], b_qk[2 + h]], writes=[bf4])
                        P.op("dve", lambda e, h=h, wsq=wsq: e.scalar_tensor_tensor(
                            Sm[h][:], f4[:, h * 128:(h + 1) * 128], wsq[:, h:h + 1], triub[:], ALU.mult, ALU.mult),
                            reads=[bf4, bsm] + CST, writes=[b_Sm[h]])
                        xs_ = f5[:, h * 256:h * 256 + 129]
                        P.op("pe", lambda e, h=h, xs_=xs_, va=va: e.matmul(xs_, Sm[h][:], va[:, h, :], start=True, stop=False),
                             reads=[b_Sm[h], bva], writes=[bf5])
                        P.op("pe", lambda e, h=h, xs_=xs_, tsl=tsl: e.matmul(xs_, QKb[:, h, tsl], Cb[:, h, :], start=False, stop=True),
                             reads=[b_qk[h], b_Cb[h]], writes=[bf5])
                        ax, r_, mv, rstd, nmr = st_[:, 0:1], st_[:, 1:2], st_[:, 2:4], st_[:, 4:5], st_[:, 5:6]
                        stat6 = st_[:, 8:14]
                        P.op("dve", lambda e, ax=ax, h=h: e.tensor_scalar(ax, f5[:, h * 256 + 128:h * 256 + 129], -1.0, None, ALU.mult),
                             reads=[bf5], writes=[bst])
                        P.op("dve", lambda e, ax=ax, h=h: e.tensor_tensor(ax, ax, f5[:, h * 256 + 128:h * 256 + 129], ALU.max),
                             reads=[bf5, bst], writes=[bst])
                        P.op("dve", lambda e, ax=ax, eb=eb, h=h: e.tensor_tensor(ax, ax, eb[:, h:h + 1], ALU.max), reads=[bst, bsm], writes=[bst])
                        P.op("dve", lambda e, ax=ax, r_=r_: e.reciprocal(r_, ax), reads=[bst], writes=[bst])
                        P.op("dve", lambda e, h=h, r_=r_: e.tensor_scalar(hh[h][:], f5[:, h * 256:h * 256 + 128], r_, None, ALU.mult),
                             reads=[bf5, bst], writes=[b_hh[h]])
                        P.op("dve", lambda e, h=h, stat6=stat6: e.bn_stats(stat6, hh[h][:]), reads=[b_hh[h]], writes=[bst])
                        P.op("dve", lambda e, mv=mv, stat6=stat6: e.bn_aggr(mv, stat6), reads=[bst], writes=[bst])
                        P.op("dve", lambda e, rstd=rstd, mv=mv: e.tensor_scalar(rstd, mv[:, 1:2], LN_EPS, None, ALU.add), reads=[bst], writes=[bst])
                        P.op("act", lambda e, rstd=rstd: e.sqrt(rstd, rstd), reads=[bst], writes=[bst])
                        P.op("dve", lambda e, rstd=rstd: e.reciprocal(rstd, rstd), reads=[bst], writes=[bst])
                        P.op("dve", lambda e, nmr=nmr, mv=mv, rstd=rstd: e.scalar_tensor_tensor(nmr, mv[:, 0:1], -1.0, rstd, ALU.mult, ALU.mult),
                             reads=[bst], writes=[bst])
                        P.op("act", lambda e, h=h, nmr=nmr, rstd=rstd: e.activation(yn32[h][:], hh[h][:], AF.Identity, bias=nmr, scale=rstd),
                             reads=[b_hh[h], bst], writes=[b_yn[h]])
                        P.op("dve", lambda e, h=h: e.tensor_tensor(yn32[h][:], yn32[h][:], hngb[:, h * 128:(h + 1) * 128], ALU.mult),
                             reads=[b_yn[h]] + CST, writes=[b_yn[h]])
                        P.op("dve", lambda e, h=h, sg_=sg_: e.tensor_tensor(ynb[h][:], yn32[h][:], sg_[:, h * 128:(h + 1) * 128], ALU.mult),
                             reads=[b_yn[h], bsg], writes=[b_ynb[h]])
                        P.op("pe", lambda e, h=h: e.transpose(BK.h[:, h * 256:h * 256 + 128], ynb[h][:], identb[:]),
                             reads=[b_ynb[h]] + CST, writes=[BK.bh])
                        P.op("act", lambda e, h=h, tsl=tsl: e.copy(ybufB[:, h, tsl], BK.h[:, h * 256:h * 256 + 128]),
                             reads=[BK.bh], writes=[b_ybB])
                        P.op("pe", lambda e, h=h, tsl=tsl: e.transpose(BK.h[:, h * 256 + 128:h * 256 + 256], QKb[:, 2 + h, tsl], identb[:]),
                             reads=[b_qk[2 + h]] + CST, writes=[BK.bh])
                        P.op("act", lambda e, h=h: e.copy(Ktm[h][:], BK.h[:, h * 256 + 128:h * 256 + 256]), reads=[BK.bh], writes=[b_ktm[h]])
                        P.op("dve", lambda e, h=h, va=va, ws=ws: e.tensor_scalar(Vw[h][:], va[:, h, :], ws[:, h:h + 1], None, ALU.mult),
                             reads=[bva, bsm], writes=[b_vw[h]])
                        dps = f6[:, h * 256:h * 256 + 129]
                        P.op("pe", lambda e, h=h, dps=dps: e.matmul(dps, Ktm[h][:], Vw[h][:], start=True, stop=True),
                             reads=[b_ktm[h], b_vw[h]], writes=[bf6])
                        P.op("dve", lambda e, h=h, dps=dps: e.tensor_tensor(C32[:, h, :], dps, C32[:, h, :], ALU.add),
                             reads=[bf6, b_C32[h]], writes=[b_C32[h]])
                        P.op("dve", lambda e, h=h, ebL=ebL: e.tensor_scalar(C32[:, h, :], C32[:, h, :], ebL[:, h:h + 1], None, ALU.mult),
                             reads=[b_C32[h], bsm], writes=[b_C32[h]])
                        P.op("act", lambda e, h=h: e.mul(Cb[:, h, :], C32[:, h, :], SC), reads=[b_C32[h]], writes=[b_Cb[h]])
                for h in range(2):
                    ystore(P, 2 + h, blk, ybufB[:, h, :], [b_ybB])
            full_barrier(P)


TOK = 4096


def declare_post(nc, pfx=""):
    din = lambda n, sh: nc.dram_tensor(pfx + n, sh, F32, kind="ExternalInput").ap()
    return dict(wo=din("wo", [D, D]), ln1g=din("ln1g", [D]), ln1b=din("ln1b", [D]),
                w=dict(wg=din("wg", [16, D, EH]), wu=din("wu", [16, D, EH]), wd=din("wd", [16, EH, D]), wr=din("wr", [D, 20]),
                       br=din("br", [1, 20]), ln_g=din("ln2g", [D]), ln_b=din("ln2b", [D])),
                hmid=nc.dram_tensor(pfx + "hmid_scr", [TOK, D], F32).ap())


def emit_post(P, io):
    nc = P.nc
    wo, ln1g, ln1b, w, hmid = io["wo"], io["ln1g"], io["ln1b"], dict(io["w"]), io["hmid"]
    w["ident"] = io["ident"]
    load_yt, xres, out = io["load_yt"], io["xres"], io["out"]
    if True:
        B = P.buf
        banks = [P.ps(f"bank{i}", [128, 512]) for i in range(8)]
        bbank = [B() for _ in range(8)]
        b_hmid, b_out, b_yT = B(), B(), B()
        b_xres = io.get("b_xres") or B()
        b_hmw = [B(), B()]
        with ExitStack() as esF:
            sbF = lambda n, sh, d: esF.enter_context(nc.sbuf_tensor(P.pfx + n, sh, d))
            stg = Stager(P)
            wob = sbF("f_wo", [128, 8, D], BF16)
            b_wo = B()
            stg.load(wob[:], b_wo, wo[:, :])
            g1 = sbF("f_g1", [128, D], F32)
            b1 = sbF("f_b1", [128, D], F32)
            b_g1, b_b1 = B(), B()
            P.dma("sp", g1[:], bcast_rows(ln1g.tensor, ln1g.offset, D), writes=[b_g1])
            P.dma("sp", b1[:], bcast_rows(ln1b.tensor, ln1b.offset, D), writes=[b_b1])
            yts = [sbF(f"f_yt{i}", [128, 8, 512], BF16) for i in range(2)]
            b_yts = [[B() for _ in range(8)] for _ in range(2)]
            xr = [sbF(f"f_xr{i}", [128, D], F32) for i in range(2)]
            b_xr = [B(), B()]
            z = [sbF(f"f_z{i}", [128, D], F32) for i in range(2)]
            b_z = [B(), B()]
            hm = [sbF(f"f_hm{i}", [128, D], F32) for i in range(2)]
            b_hm = [B(), B()]
            stats = sbF("f_stats", [128, 2, 6], F32)
            sm = sbF("f_sm", [128, 64], F32)
            b_stats, b_sm = B(), B()
            ti = 0
            for t4 in range(TOK // 512):
                yt, byt = yts[t4 % 2], b_yts[t4 % 2]
                load_yt(P, yt, byt, t4)
                for tq in range(4):
                    k = ti % 2
                    ti += 1
                    t0 = t4 * 512 + tq * 128
                    P.dma("sp", xr[k][:], xres(t0), reads=[b_xres], writes=[b_xr[k]])
                    for dh in range(2):
                        pb, bpb = banks[(ti * 2 + dh) % 4], bbank[(ti * 2 + dh) % 4]
                        for ec in range(8):
                            P.op("pe", lambda e, pb=pb, ec=ec, yt=yt, tq=tq, dh=dh: e.matmul(
                                pb[:], yt[:, ec, tq * 128:(tq + 1) * 128], wob[:, ec, dh * 512:(dh + 1) * 512],
                                start=(ec == 0), stop=(ec == 7)), reads=[byt[ec], b_wo], writes=[bpb])
                        P.op("dve", lambda e, pb=pb, k=k, dh=dh: e.scalar_tensor_tensor(
                            z[k][:, dh * 512:(dh + 1) * 512], xr[k][:, dh * 512:(dh + 1) * 512], ALPHA, pb[:], ALU.mult, ALU.add),
                            reads=[bpb, b_xr[k]], writes=[b_z[k]])
                    emit_ln(P, z[k][:], b_z[k], hm[k], b_hm[k], g1, b_g1, b1, b_b1, stats, b_stats, sm, b_sm)
                    P.dma("sp", hmid[t0:t0 + 128, :], hm[k][:], reads=[b_hm[k]], writes=[b_hmw[k]])
            full_barrier(P)
        emit_tail(P, hmid, b_hmid, out, b_out, TOK, w, 0, banks=banks, is_final=io.get("is_final", True))
        full_barrier(P)


def pack_m0(inp, b, c):
    w = inp["ab_w_in"][0]
    cols = []
    for hp in range(2):
        h0 = 4 * c + 2 * hp
        for sec in (0, 512, 1024):
            cols.append(w[:, sec + h0 * 64: sec + (h0 + 2) * 64])
    wA = np.concatenate(cols, axis=1)
    hb = [2 * c, 2 * c + 1]
    BQ, BKK, BV, BO, BI, BF = 1536, 2048, 2560, 3072, 3584, 3588
    wB = np.concatenate([w[:, BQ + h * 128: BQ + (h + 1) * 128] for h in hb] + [w[:, BKK + h * 128: BKK + (h + 1) * 128] for h in hb]
                        + [w[:, BV + h * 128: BV + (h + 1) * 128] for h in hb] + [w[:, BO + h * 128: BO + (h + 1) * 128] for h in hb]
                        + [w[:, BI + h: BI + h + 1] for h in hb] + [w[:, BF + h: BF + h + 1] for h in hb], axis=1)
    gb = inp["ab_gate_b"][0]
    gbias = np.array([[gb[hb[0]], gb[hb[1]], gb[4 + hb[0]], gb[4 + hb[1]]]], np.float32)
    cw = inp["ab_conv"][0]
    convw = np.zeros((128, 4, 4), np.float32)
    for g, off in enumerate([hb[0] * 128, hb[1] * 128, 512 + hb[0] * 128, 512 + hb[1] * 128]):
        convw[:, g, :] = cw[:, off:off + 128].T
    hng = np.concatenate([inp["ab_head_norm"][0][h * 128:(h + 1) * 128] for h in hb])[None]
    relb = np.concatenate([inp["rel_bias"][:, 4 * c:4 * c + 4], np.full((1, 4), -30000.0, np.float32)], axis=0)
    return dict(x=np.ascontiguousarray(inp["x"][b]), wA=np.ascontiguousarray(wA), wB=np.ascontiguousarray(wB), gbias=gbias,
                convw=np.ascontiguousarray(convw.reshape(128, 16)), hng=np.ascontiguousarray(hng),
                relb=np.ascontiguousarray(relb), oh=np.ascontiguousarray(dil_onehot().reshape(33, -1)),
                ident=np.eye(128, dtype=np.float32), triu=np.triu(np.ones((128, 128), np.float32)))


L1 = 5632
OFF1 = 2063


def nsa_consts():
    dist = np.arange(L1) - OFF1
    oh = np.zeros((33, L1), np.float32)
    bk = t5_bucket_np(dist)
    for jp in range(L1):
        if dist[jp] >= 0:
            oh[bk[jp], jp] = 1.0
        else:
            oh[32, jp] = 1.0
    n = np.arange(512)
    m = np.arange(128)
    cs, ce = n * 16, n * 16 + 31
    ov = ((cs[:, None] < m[None, :] * 64 + 64) & (ce[:, None] >= m[None, :] * 64)).astype(np.float32)
    ov[511] = 0.0
    ovl = ov.reshape(4, 128, 128).transpose(1, 0, 2)
    mrow = np.broadcast_to(m[None, :].astype(np.float32), (128, 128))
    k = np.arange(128)
    cst = np.stack([mrow, (mrow >= 1).astype(np.float32), (mrow == 0).astype(np.float32),
                    (k[:, None] > k[None, :]).astype(np.float32)], axis=1)
    qhalf = (np.arange(128) >= 64).astype(np.float32)[:, None]
    return dict(oh1=oh, ovl=np.ascontiguousarray(ovl), cst1=np.ascontiguousarray(cst.astype(np.float32)), qhalf=qhalf)


def declare_mixer1(nc, pfx=""):
    din = lambda n, s: nc.dram_tensor(pfx + n, s, F32, kind="ExternalInput").ap()
    return dict(wQ=din("wQ", [D, 512]), wKV=din("wKV", [D, 768]), wG=din("wG", [D, 12]), gb=din("gb", [1, 12]),
                posT=din("posT", [128, 64]), w1=din("w1", [2, 4096, 256]), w2=din("w2", [2, 256, 128]), relb=din("relb", [33, 4]),
                oh1=din("oh1", [33, L1]), ovl=din("ovl", [128, 512]), cst1=din("cst1", [128, 512]), qhalf=din("qhalf", [128, 1]),
                fsc=nc.dram_tensor(pfx + "frep_scr", [128, 4 * L1], BF16).ap())


def emit_mixer1(P, io):
    nc = P.nc
    wQ, wKV, wG, gbd, posT, w1d, w2d, relb = io["wQ"], io["wKV"], io["wG"], io["gb"], io["posT"], io["w1"], io["w2"], io["relb"]
    oh_d, ovl_d, cst_d, qh_d, ident_d, fsc = io["oh1"], io["ovl"], io["cst1"], io["qhalf"], io["ident"], io["fsc"]
    x, ostore = io["x"], io["ostore"]
    NT = S_TOT // 128
    SCQ = 128.0 ** -0.5
    if True:
        B = P.buf
        BK = Banks(P)
        f0, f1, f2, f3, f4, f5, f6 = BK.f
        bf0, bf1, bf2, bf3, bf4, bf5, bf6 = BK.bf
        b_x, b_o, b_fsc = io.get("b_x") or B(), B(), B()
        ident = P.sb("ident_sb", [128, 128], F32)
        identb = P.sb("identb", [128, 128], BF16)
        ones_bf = P.sb("ones_bf", [128, 128], BF16)
        b_ident, b_cb = B(), B()
        P.dma("sp", ident[:], ident_d[:, :], writes=[b_ident])
        P.op("dve", lambda e: e.tensor_copy(identb[:], ident[:]), reads=[b_ident], writes=[b_cb])
        P.op("dve", lambda e: e.memset(ones_bf[:], 1.0), writes=[b_cb])
        xin = [P.sb(f"xin{i}", [128, D], F32) for i in range(2)]
        b_xin = [B(), B()]
        stg = Stager(P, n=2)
        ctr = [0]
        wQb = P.sb("wQb", [128, 8, 512], BF16)
        wGb = P.sb("wGb", [128, 8, 12], BF16)
        b_wQ, b_wG = B(), B()
        stg.load(wQb[:], b_wQ, wQ[:, :])
        stg.load(wGb[:], b_wG, wG[:, :])
        KsT = P.sb("KsT", [128, S_TOT], BF16)
        KwT = P.sb("KwT", [128, S_TOT], BF16)
        Vs = P.sb("Vs", [128, NT, 129], BF16)
        Vw = P.sb("Vw", [128, NT, 129], BF16)
        b_KsT, b_KwT, b_Vs, b_Vw = B(), B(), B(), B()
        kcT = P.sb("kcT", [128, 512], BF16)
        rhsC = P.sb("rhsC", [128, 4, 256], BF16)
        b_kcT, b_rhsC = B(), B()
        P.op("dve", lambda e: e.memset(Vs[:, :, 128:129], 1.0), writes=[b_Vs])
        P.op("dve", lambda e: e.memset(Vw[:, :, 128:129], 1.0), writes=[b_Vw])
        with ExitStack() as es12:
            sb12 = lambda n, s, d: es12.enter_context(nc.sbuf_tensor(P.pfx + n, s, d))
            KcT = sb12("KcT", [128, 2, S_TOT], BF16)
            b_KcT = [B(), B()]
            with ExitStack() as es1:
                sb1 = lambda n, s, d: es1.enter_context(nc.sbuf_tensor(P.pfx + n, s, d))
                hT = sb1("hT", [128, 8, BLK], BF16)
                b_hT = [B() for _ in range(16)]
                wKVb = sb1("wKVb", [128, 8, 768], BF16)
                b_wKV = B()
                stg.load(wKVb[:], b_wKV, wKV[:, :])
                for blk in range(NBLK):
                    T0 = blk * BLK
                    load_xT(P, BK, x, b_x, xin, b_xin, hT, b_hT, ident, b_ident, T0, 16, ctr)
                    fm = [(0, KcT[:, 0, :], b_KcT[0]), (1, KcT[:, 1, :], b_KcT[1]), (2, KsT[:], b_KsT), (4, KwT[:], b_KwT)]
                    for fi, (wi, dst, bd) in enumerate(fm):
                        for qtr in range(4):
                            pb, bpb = (f2, bf2) if (fi + qtr) % 2 == 0 else (f3, bf3)
                            rds = [b_hT[qtr * 4 + i] for i in range(4)] + [b_wKV]
                            for dc in range(8):
                                P.op("pe", lambda e, pb=pb, dc=dc, wi=wi, qtr=qtr: e.matmul(
                                    pb[:], wKVb[:, dc, wi * 128:(wi + 1) * 128], hT[:, dc, qtr * 512:(qtr + 1) * 512],
                                    start=(dc == 0), stop=(dc == 7)), reads=rds, writes=[bpb])
                            if (fi + qtr) % 2 == 0:
                                P.op("act", lambda e, pb=pb, dst=dst, T0=T0, qtr=qtr: e.copy(
                                    dst[:, T0 + qtr * 512:T0 + (qtr + 1) * 512], pb[:]), reads=[bpb], writes=[bd])
                            else:
                                P.op("dve", lambda e, pb=pb, dst=dst, T0=T0, qtr=qtr: e.tensor_copy(
                                    dst[:, T0 + qtr * 512:T0 + (qtr + 1) * 512], pb[:]), reads=[bpb], writes=[bd])
                    for tl in range(16):
                        for (wi, dst, bd, pb, bpb, eng) in ((3, Vs, b_Vs, f4, bf4, "act"), (5, Vw, b_Vw, f5, bf5, "dve")):
                            for dc in range(8):
                                P.op("pe", lambda e, pb=pb, dc=dc, wi=wi, tl=tl: e.matmul(
                                    pb[:, 0:128], hT[:, dc, tl * 128:(tl + 1) * 128], wKVb[:, dc, wi * 128:(wi + 1) * 128],
                                    start=(dc == 0), stop=(dc == 7)), reads=[b_hT[tl], b_wKV], writes=[bpb])
                            if eng == "act":
                                P.op("act", lambda e, pb=pb, dst=dst, tl=tl, blk=blk: e.copy(dst[:, blk * 16 + tl, 0:128], pb[:, 0:128]),
                                     reads=[bpb], writes=[bd])
                            else:
                                P.op("dve", lambda e, pb=pb, dst=dst, tl=tl, blk=blk: e.tensor_copy(dst[:, blk * 16 + tl, 0:128], pb[:, 0:128]),
                                     reads=[bpb], writes=[bd])
                full_barrier(P)
            with ExitStack() as es2:
                sb2 = lambda n, s, d: es2.enter_context(nc.sbuf_tensor(P.pfx + n, s, d))
                w1b = sb2("w1b", [128, 32, 256], BF16)
                w2b = sb2("w2b", [128, 2, 128], BF16)
                posb = sb2("posb", [128, 64], BF16)
                pos32 = sb2("pos32", [128, 64], F32)
                ovl32 = sb2("ovl32", [128, 512], F32)
                pbias = sb2("pbias", [128, 2], F32)
                xg = sb2("xg", [128, 512], F32)
                ug = sb2("ug", [128, 512], F32)
                hid = sb2("hid_sb", [128, 2, 512], BF16)
                b_w1, b_w2, b_pos, b_ovl, b_pb, b_xg, b_ug, b_hid = B(), B(), B(), B(), B(), B(), B(), [B(), B()]
                P.dma("sp", pos32[:], posT[:, :], writes=[b_pos])
                P.op("dve", lambda e: e.tensor_copy(posb[:], pos32[:]), reads=[b_pos], writes=[b_pos])
                P.dma("sp", ovl32[:], ovl_d[:, :], writes=[b_ovl])
                P.op("dve", lambda e: e.tensor_copy(rhsC[:, :, 128:256], ovl32[:].rearrange("p (c m) -> p c m", c=4)),
                     reads=[b_ovl], writes=[b_rhsC])
                P.op("dve", lambda e: e.memset(hid[:, :, 511:512], 0.0), writes=b_hid)
                for kv in range(2):
                    stg.load(w1b[:], b_w1, w1d[kv])
                    stg.load(w2b[:], b_w2, w2d[kv])
                    for hc in range(2):
                        for p_ in range(32):
                            P.op("pe", lambda e, hc=hc, p_=p_, kv=kv: e.matmul(
                                f6[:, 0:1], w1b[:, p_, hc * 128:(hc + 1) * 128], posb[:, kv * 32 + p_:kv * 32 + p_ + 1],
                                start=(p_ == 0), stop=(p_ == 31)), reads=[b_w1, b_pos], writes=[bf6])
                        P.op("act", lambda e, hc=hc: e.copy(pbias[:, hc:hc + 1], f6[:, 0:1]), reads=[bf6], writes=[b_pb])
                        for p_ in range(32):
                            P.op("pe", lambda e, hc=hc, p_=p_, kv=kv: e.matmul(
                                f2[:, 0:511], w1b[:, p_, hc * 128:(hc + 1) * 128], KcT[:, kv, p_:p_ + 16 * 510 + 1:16],
                                start=(p_ == 0), stop=(p_ == 31)), reads=[b_w1, b_KcT[kv]], writes=[bf2])
                        P.op("act", lambda e, hc=hc: e.activation(xg[:, 0:511], f2[:, 0:511], AF.Identity, bias=pbias[:, hc:hc + 1], scale=1.0),
                             reads=[bf2, b_pb], writes=[b_xg])
                        P.op("dve", lambda e: e.tensor_tensor(ug[:, 0:511], xg[:, 0:511], xg[:, 0:511], ALU.mult), reads=[b_xg], writes=[b_ug])
                        P.op("dve", lambda e: e.tensor_scalar(ug[:, 0:511], ug[:, 0:511], 0.044715, 1.0, ALU.mult, ALU.add), reads=[b_ug], writes=[b_ug])
                        P.op("dve", lambda e: e.tensor_tensor(ug[:, 0:511], ug[:, 0:511], xg[:, 0:511], ALU.mult), reads=[b_ug, b_xg], writes=[b_ug])
                        P.op("act", lambda e: e.activation(ug[:, 0:511], ug[:, 0:511], AF.Sigmoid, scale=1.5957691216057308), reads=[b_ug], writes=[b_ug])
                        P.op("dve", lambda e, hc=hc: e.tensor_tensor(hid[:, hc, 0:511], xg[:, 0:511], ug[:, 0:511], ALU.mult),
                             reads=[b_ug, b_xg], writes=[b_hid[hc]])
                    if kv == 0:
                        for hc in range(2):
                            P.op("pe", lambda e, hc=hc: e.matmul(f3[:, 0:512], w2b[:, hc, :], hid[:, hc, :], start=(hc == 0), stop=(hc == 1)),
                                 reads=[b_w2] + b_hid, writes=[bf3])
                        P.op("act", lambda e: e.copy(kcT[:], f3[:, 0:512]), reads=[bf3], writes=[b_kcT])
                    else:
                        for c in range(4):
                            for hc in range(2):
                                P.op("pe", lambda e, hc=hc, c=c: e.matmul(f3[:, c * 128:(c + 1) * 128], hid[:, hc, c * 128:(c + 1) * 128], w2b[:, hc, :],
                                                                          start=(hc == 0), stop=(hc == 1)), reads=[b_w2] + b_hid, writes=[bf3])
                        P.op("act", lambda e: e.copy(rhsC[:, :, 0:128], f3[:].rearrange("p (c m) -> p c m", c=4)), reads=[bf3], writes=[b_rhsC])
                full_barrier(P)
        with ExitStack() as es3:
            sb3 = lambda n, s, d: es3.enter_context(nc.sbuf_tensor(P.pfx + n, s, d))
            tab = sb3("tab", [33, 4], F32)
            tabB = sb3("tabB", [33, 128], F32)
            ohp = [sb3(f"ohp{i}", [33, 512], F32) for i in range(2)]
            frp = [sb3(f"frp{i}", [128, 512], BF16) for i in range(2)]
            b_tab, b_tabB, b_ohp, b_frp = B(), B(), [B(), B()], [B(), B()]
            b_fscw = [B(), B()]
            P.dma("sp", tab[:], relb[:, :], writes=[b_tab])
            ki = 0
            for h in range(4):
                P.op("dve", lambda e, h=h: e.tensor_copy(tabB[:], tab[:, h:h + 1].to_broadcast([33, 128])), reads=[b_tab], writes=[b_tabB])
                for pc_ in range(L1 // 512):
                    k = ki % 2
                    ki += 1
                    P.dma("sp", ohp[k][:], oh_d[:, pc_ * 512:(pc_ + 1) * 512], writes=[b_ohp[k]])
                    pb, bpb = (f2, bf2) if k == 0 else (f3, bf3)
                    P.op("pe", lambda e, pb=pb, k=k: e.matmul(pb[:], tabB[:], ohp[k][:], start=True, stop=True), reads=[b_tabB, b_ohp[k]], writes=[bpb])
                    P.op("act", lambda e, pb=pb, k=k: e.activation(frp[k][:], pb[:], AF.Exp), reads=[bpb], writes=[b_frp[k]])
                    P.dma("sp", fsc[:, h * L1 + pc_ * 512:h * L1 + (pc_ + 1) * 512], frp[k][:], reads=[b_frp[k]], writes=[b_fscw[k]])
            full_barrier(P)
            RS = 4 * L1
            Esel = sb3("Esel", [128, 14, 4, 128], BF16)
            Ew4 = sb3("Ew4", [128, 4, 128], BF16)
            b_Esel = B()
            for dl in range(14):
                P.dma("sp", Esel[:, dl], bass.AP(fsc.tensor, 128 * dl + OFF1, [[RS - 1, 128], [L1, 4], [1, 128]]),
                      reads=[b_fsc], writes=[b_Esel])
            cst = sb3("cst", [128, 4, 128], F32)
            qhalf = sb3("qhalf_sb", [128, 1], F32)
            gbb = sb3("gbb", [128, 12], F32)
            m4b = sb3("m4b", [128, 128], BF16)
            b_cst = B()
            P.dma("sp", cst[:], cst_d[:, :].rearrange("p (c m) -> p c m", c=4), writes=[b_cst])
            b_q2, b_g2 = B(), B()
            P.dma("sp", qhalf[:], qh_d[:, :], writes=[b_q2])
            P.dma("sp", gbb[:], bcast_rows(gbd.tensor, gbd.offset, 12), writes=[b_g2])
            CST = [b_cst, b_q2, b_g2, b_cb, b_Esel]
            P.op("dve", lambda e: e.tensor_copy(m4b[:], cst[:, 3, :]), reads=[b_cst], writes=[b_Esel])
            P.op("dve", lambda e: e.tensor_tensor(Ew4[:], Esel[:, 4], m4b[:].unsqueeze(1).to_broadcast([128, 4, 128]), ALU.mult),
                 reads=[b_Esel], writes=[b_Esel])
            mrow, mge1, meq0 = cst[:, 0, :], cst[:, 1, :], cst[:, 2, :]
            Expand = sb3("Expand", [128, NT, 128], BF16)
            for Tk in range(NT):
                P.op("dve", lambda e, Tk=Tk: e.tensor_copy(Expand[:, Tk, :].rearrange("m (h k) -> m h k", h=2),
                                                            identb[:, 2 * Tk:2 * Tk + 2].unsqueeze(2).to_broadcast([128, 2, 64])),
                     reads=[b_cb], writes=[b_Esel])
            hTt = sb3("hTt", [128, 8, 128], BF16)
            b_hTt = [B()]
            QT = [sb3(f"QT{i}", [128, 4, 128], BF16) for i in range(2)]
            b_QT = [B(), B()]
            gsg = sb3("gsg", [128, 12], F32)
            b_gsg = B()
            ecb = [sb3(f"ecb{i}", [128, 4, 128], BF16) for i in range(2)]
            b_ecb = [B(), B()]
            psb = [sb3(f"psb{i}", [128, 4, 128], BF16) for i in range(5)]
            b_psb = [B() for _ in range(5)]
            ocb = sb3("ocb", [128, 4, 128], F32)
            b_ocb = B()
            sm = sb3("sm3", [128, 64], F32)
            b_sm = B()
            imp = sb3("imp", [128, 128], F32)
            M1 = sb3("M1", [128, 128], F32)
            Fm = sb3("Fm", [128, 128], F32)
            sa = sb3("sa", [128, 128], F32)
            rp = sb3("rp", [128, 128], F32)
            sel = sb3("sel", [128, 128], F32)
            nsb = sb3("nsb", [128, 128], BF16)
            nsT = sb3("nsT", [128, 128], BF16)
            mx = sb3("mx", [128, 16], F32)
            b_imp, b_M1, b_Fm, b_sa, b_rp, b_sel, b_nsb, b_nsT, b_mx = (B() for _ in range(9))
            b_ow = [B(), B()]
            ob = sb3("ob", [128, 4, 128], BF16)
            oTt = [sb3(f"oTt{i}", [128, 4, 128], BF16) for i in range(2)]
            b_ob, b_oTt = B(), [B(), B()]
            si = 0
            pipe1 = Pipe(depth=2)
            for T in range(NT):
                k2 = T % 2
                load_xT(P, BK, x, b_x, xin, b_xin, hTt, b_hTt, ident, b_ident, T * 128, 1, ctr)
                for j in range(4):
                    for dc in range(8):
                        P.op("pe", lambda e, j=j, dc=dc: e.matmul(f6[:, j * 128:(j + 1) * 128], wQb[:, dc, j * 128:(j + 1) * 128], hTt[:, dc, :],
                                                                  start=(dc == 0), stop=(dc == 7)), reads=[b_hTt[0], b_wQ], writes=[bf6])
                qt, bqt = QT[k2], b_QT[k2]
                P.op("act", lambda e, qt=qt: e.mul(qt[:].rearrange("p j q -> p (j q)"), f6[:], SCQ), reads=[bf6], writes=[bqt])
                for dc in range(8):
                    P.op("pe", lambda e, dc=dc: e.matmul(f4[:, 16:28], hTt[:, dc, :], wGb[:, dc, :], start=(dc == 0), stop=(dc == 7)),
                         reads=[b_hTt[0], b_wG], writes=[bf4])
                P.op("dve", lambda e: e.tensor_tensor(gsg[:], f4[:, 16:28], gbb[:], ALU.add), reads=[bf4] + CST, writes=[b_gsg])
                P.op("act", lambda e: e.activation(gsg[:], gsg[:], AF.Sigmoid), reads=[b_gsg], writes=[b_gsg])
                qrhs = qt[:].rearrange("p j q -> p (j q)")
                ncmp = (8 * T + 6) // 128 + 1
                for c in range(ncmp):
                    dl = min(T - 16 * c, 28)
                    ke = si % 2
                    kp = si % 5
                    si += 1
                    P.dma("sp", ecb[ke][:], bass.AP(fsc.tensor, 128 * dl - 31 + OFF1, [[RS - 16, 128], [L1, 4], [1, 128]]),
                          reads=[b_fsc], writes=[b_ecb[ke]])
                    P.op("pe", lambda e, c=c, qrhs=qrhs: e.matmul(f6[:], kcT[:, c * 128:(c + 1) * 128], qrhs, start=True, stop=True),
                         reads=[b_kcT, bqt], writes=[bf6])
                    pp, bpp = psb[kp], b_psb[kp]
                    P.op("act", lambda e, pp=pp: e.activation(pp[:].rearrange("p j q -> p (j q)"), f6[:], AF.Exp), reads=[bf6], writes=[bpp])
                    P.op("dve", lambda e, pp=pp, ke=ke: e.tensor_tensor(pp[:], pp[:], ecb[ke][:], ALU.mult), reads=[bpp, b_ecb[ke]], writes=[bpp])
                    for j in range(4):
                        fb, bfb = (f2, bf2) if j < 2 else (f3, bf3)
                        P.op("pe", lambda e, fb=fb, j=j, c=c, pp=pp, ncmp=ncmp: e.matmul(
                            fb[:, (j % 2) * 256:(j % 2) * 256 + 256], pp[:, j, :], rhsC[:, c, :],
                            start=(c == 0 and j % 2 == 0), stop=(c == ncmp - 1 and j % 2 == 1)),
                            reads=[bpp, b_rhsC], writes=[bfb])
                        P.op("pe", lambda e, j=j, c=c, pp=pp, ncmp=ncmp: e.matmul(
                            f4[:, j:j + 1], pp[:, j, :], ones_bf[:, 0:1], start=(c == 0 and j == 0), stop=(c == ncmp - 1 and j == 3)),
                            reads=[bpp, b_cb], writes=[bf4])
                rc, cc = sm[:, 0:4], sm[:, 4:8]
                P.op("dve", lambda e, rc=rc: e.tensor_scalar(rc, f4[:, 0:4], 1e-30, None, ALU.max), reads=[bf4], writes=[b_sm])
                P.op("dve", lambda e, rc=rc: e.reciprocal(rc, rc), reads=[b_sm], writes=[b_sm])
                P.op("dve", lambda e, rc=rc, cc=cc: e.tensor_tensor(cc, rc, gsg[:, 0:4], ALU.mult), reads=[b_sm, b_gsg], writes=[b_sm])
                for j in range(4):
                    fb, bfb = (f2, bf2) if j < 2 else (f3, bf3)
                    o0 = (j % 2) * 256
                    P.op("dve", lambda e, fb=fb, j=j, o0=o0, cc=cc: e.tensor_scalar(ocb[:, j, :], fb[:, o0:o0 + 128], cc[:, j:j + 1], None, ALU.mult),
                         reads=[bfb, b_sm], writes=[b_ocb])
                    if j == 0:
                        P.op("dve", lambda e, fb=fb, o0=o0, rc=rc: e.tensor_scalar(imp[:], fb[:, o0 + 128:o0 + 256], rc[:, 0:1], None, ALU.mult),
                             reads=[bfb, b_sm], writes=[b_imp])
                    else:
                        P.op("dve", lambda e, fb=fb, o0=o0, rc=rc, j=j: e.scalar_tensor_tensor(
                            imp[:], fb[:, o0 + 128:o0 + 256], rc[:, j:j + 1], imp[:], ALU.mult, ALU.add), reads=[bfb, b_sm, b_imp], writes=[b_imp])
                qb, qb1, qb2, tau = sm[:, 8:9], sm[:, 9:10], sm[:, 10:11], sm[:, 11:12]
                P.op("dve", lambda e, qb=qb, T=T: e.tensor_scalar(qb, qhalf[:], float(2 * T), None, ALU.add), reads=CST, writes=[b_sm])
                P.op("dve", lambda e, qb=qb, qb1=qb1: e.tensor_scalar(qb1, qb, -1.0, None, ALU.add), reads=[b_sm], writes=[b_sm])
                P.op("dve", lambda e, qb=qb, qb2=qb2: e.tensor_scalar(qb2, qb, -2.0, None, ALU.add), reads=[b_sm], writes=[b_sm])
                P.op("dve", lambda e, qb2=qb2: e.tensor_scalar(M1[:], mrow, qb2, None, ALU.is_le), reads=[b_sm] + CST, writes=[b_M1])
                P.op("dve", lambda e: e.tensor_tensor(M1[:], M1[:], mge1, ALU.mult), reads=[b_M1] + CST, writes=[b_M1])
                P.op("dve", lambda e, qb=qb: e.tensor_scalar(Fm[:], mrow, qb, None, ALU.is_equal), reads=[b_sm] + CST, writes=[b_Fm])
                P.op("dve", lambda e, qb1=qb1: e.tensor_scalar(sel[:], mrow, qb1, None, ALU.is_equal), reads=[b_sm] + CST, writes=[b_sel])
                P.op("dve", lambda e: e.tensor_tensor(Fm[:], Fm[:], sel[:], ALU.add), reads=[b_Fm, b_sel], writes=[b_Fm])
                P.op("dve", lambda e: e.tensor_tensor(Fm[:], Fm[:], meq0, ALU.max), reads=[b_Fm] + CST, writes=[b_Fm])
                P.op("dve", lambda e: e.tensor_tensor(sa[:], imp[:], M1[:], ALU.mult), reads=[b_imp, b_M1], writes=[b_sa])
                P.op("dve", lambda e: e.max(mx[:, 0:8], sa[:]), reads=[b_sa], writes=[b_mx])
                P.op("dve", lambda e: e.match_replace(rp[:], mx[:, 0:8], sa[:], -1.0), reads=[b_sa, b_mx], writes=[b_rp])
                P.op("dve", lambda e: e.max(mx[:, 8:16], rp[:]), reads=[b_rp], writes=[b_mx])
                P.op("dve", lambda e: e.tensor_scalar(sel[:], sa[:], mx[:, 12:13], None, ALU.is_ge), reads=[b_sa, b_mx], writes=[b_sel])
                P.op("dve", lambda e: e.tensor_tensor(sel[:], sel[:], M1[:], ALU.mult), reads=[b_sel, b_M1], writes=[b_sel])
                P.op("dve", lambda e: e.tensor_tensor(sel[:], sel[:], Fm[:], ALU.max), reads=[b_sel, b_Fm], writes=[b_sel])
                P.op("dve", lambda e: e.tensor_scalar(nsb[:], sel[:], -1.0, 30000.0, ALU.add, ALU.mult), reads=[b_sel], writes=[b_nsb])
                P.op("pe", lambda e: e.transpose(BK.h[:, 512:640], nsb[:], identb[:]), reads=[b_nsb, b_cb], writes=[BK.bh])
                P.op("act", lambda e: e.copy(nsT[:], BK.h[:, 512:640]), reads=[BK.bh], writes=[b_nsT])
                nsrhs = nsT[:].unsqueeze(1).to_broadcast([128, 4, 128])
                for br, (KT_, bKT, V_, bV, fa, bfa, fb_, bfb_, tks) in enumerate((
                        (KsT, b_KsT, Vs, b_Vs, f2, bf2, f3, bf3, list(range(0, T + 1))),
                        (KwT, b_KwT, Vw, b_Vw, f4, bf4, f5, bf5, list(range(max(0, T - 4), T + 1))))):
                    for ii, Tk in enumerate(tks):
                        dl = T - Tk
                        kp = si % 5
                        ks_ = si % 3
                        si += 1
                        st, bst = ((f0, bf0), (f1, bf1), (f6, bf6))[ks_]
                        P.op("pe", lambda e, st=st, KT_=KT_, Tk=Tk, qrhs=qrhs, br=br: e.matmul(
                            st[:], KT_[:, Tk * 128:(Tk + 1) * 128], qrhs, start=True, stop=(br == 1)),
                            reads=[bKT, bqt], writes=[bst])
                        if br == 0:
                            P.op("pe", lambda e, st=st, Tk=Tk, nsrhs=nsrhs: e.matmul(
                                st[:].rearrange("p (j q) -> p j q", j=4), Expand[:, Tk, :], nsrhs, start=False, stop=True),
                                reads=[b_nsT, b_Esel], writes=[bst])
                        pp, bpp = psb[kp], b_psb[kp]
                        P.op("act", lambda e, pp=pp, st=st: e.activation(pp[:].rearrange("p j q -> p (j q)"), st[:], AF.Exp), reads=[bst], writes=[bpp])
                        if br == 1 and dl == 4:
                            et = Ew4[:]
                        else:
                            et = Esel[:, min(dl, 13)]
                        P.op("dve", lambda e, pp=pp, et=et: e.tensor_tensor(pp[:], pp[:], et, ALU.mult), reads=[bpp, b_Esel], writes=[bpp])
                        def stageB(fa=fa, bfa=bfa, fb_=fb_, bfb_=bfb_, pp=pp, bpp=bpp, V_=V_, bV=bV, Tk=Tk, ii=ii, n=len(tks)):
                            for j in range(4):
                                fo, bfo = (fa, bfa) if j < 2 else (fb_, bfb_)
                                P.op("pe", lambda e, fo=fo, j=j: e.matmul(
                                    fo[:, (j % 2) * 129:(j % 2) * 129 + 129], pp[:, j, :], V_[:, Tk, :],
                                    start=(ii == 0 and j % 2 == 0), stop=(ii == n - 1 and j % 2 == 1)),
                                    reads=[bpp, bV], writes=[bfo])
                        pipe1.step(stageB)
                pipe1.flush()
                for br, (fa, bfa, fb_, bfb_) in enumerate(((f2, bf2, f3, bf3), (f4, bf4, f5, bf5))):
                    for j in range(4):
                        fo, bfo = (fa, bfa) if j < 2 else (fb_, bfb_)
                        P.op("dve", lambda e, fo=fo, j=j, br=br: e.tensor_scalar(
                            sm[:, 16 + br * 4 + j:17 + br * 4 + j], fo[:, (j % 2) * 129 + 128:(j % 2) * 129 + 129], 1e-30, None, ALU.max),
                            reads=[bfo], writes=[b_sm])
                cf = sm[:, 16:24]
                P.op("dve", lambda e, cf=cf: e.reciprocal(cf, cf), reads=[b_sm], writes=[b_sm])
                P.op("dve", lambda e, cf=cf: e.tensor_tensor(cf, cf, gsg[:, 4:12], ALU.mult), reads=[b_sm, b_gsg], writes=[b_sm])
                for j in range(4):
                    fs_, bfs = (f2, bf2) if j < 2 else (f3, bf3)
                    fw_, bfw = (f4, bf4) if j < 2 else (f5, bf5)
                    o0 = (j % 2) * 129
                    P.op("dve", lambda e, fs_=fs_, j=j, o0=o0, cf=cf: e.scalar_tensor_tensor(
                        ocb[:, j, :], fs_[:, o0:o0 + 128], cf[:, j:j + 1], ocb[:, j, :], ALU.mult, ALU.add),
                        reads=[bfs, b_sm, b_ocb], writes=[b_ocb])
                    P.op("dve", lambda e, fw_=fw_, j=j, o0=o0, cf=cf: e.scalar_tensor_tensor(
                        ob[:, j, :], fw_[:, o0:o0 + 128], cf[:, 4 + j:5 + j], ocb[:, j, :], ALU.mult, ALU.add),
                        reads=[bfw, b_sm, b_ocb], writes=[b_ob])
                for j in range(4):
                    P.op("pe", lambda e, j=j: e.transpose(BK.h[:, j * 128:(j + 1) * 128], ob[:, j, :], identb[:]), reads=[b_ob, b_cb], writes=[BK.bh])
                ot, bot = oTt[k2], b_oTt[k2]
                P.op("act", lambda e, ot=ot: e.copy(ot[:].rearrange("p j q -> p (j q)"), BK.h[:, 0:512]), reads=[BK.bh], writes=[bot])
                ostore(P, T, ot[:], [bot], k2)
            full_barrier(P)


C_OFF_KV_, C_OFF_GATE_ = 1024, 2560


def pack_m1(inp, h1b, g):
    w = inp["c_w_in"][0]
    wQ = w[:, g * 512:(g + 1) * 512]
    kvcols = []
    for br in range(3):
        for kv in range(2):
            o = C_OFF_KV_ + ((br * 2 + kv) * 2 + g) * 128
            kvcols.append(w[:, o:o + 128])
    wKV = np.concatenate(kvcols, axis=1)
    gcols = [C_OFF_GATE_ + br * 8 + g * 4 + j for br in range(3) for j in range(4)]
    wG = w[:, gcols]
    gb = inp["c_gate_b"][0][[c - C_OFF_GATE_ for c in gcols]][None]
    pos = inp["c_cmp_pos"][0]
    posT = np.concatenate([pos[0].T, pos[1].T], axis=1)
    relb = np.concatenate([inp["rel_bias"][:, 4 * g:4 * g + 4], np.full((1, 4), -30000.0, np.float32)], axis=0)
    c = nsa_consts()
    return dict(x=h1b, wQ=np.ascontiguousarray(wQ), wKV=np.ascontiguousarray(wKV), wG=np.ascontiguousarray(wG),
                gb=np.ascontiguousarray(gb.astype(np.float32)), posT=np.ascontiguousarray(posT), w1=inp["c_cmp_w1"][0], w2=inp["c_cmp_w2"][0],
                relb=np.ascontiguousarray(relb), oh1=c["oh1"], ovl=np.ascontiguousarray(c["ovl"].reshape(128, 512)),
                cst1=np.ascontiguousarray(c["cst1"].reshape(128, 512)), qhalf=c["qhalf"], ident=np.eye(128, dtype=np.float32))


U32 = mybir.dt.uint32
GROUPS = [[0, 1], [2, 3], [4, 5], [6, 7]]


def build_fused():
    nc = bass.Bass("TRN2", target_bir_lowering=False)
    ident_d = nc.dram_tensor("ident", [128, 128], F32, kind="ExternalInput").ap()
    gidx_d = nc.dram_tensor("gidx", [128, 16], U32, kind="ExternalInput").ap()
    xres_d = nc.dram_tensor("xres", [TOK, D], F32, kind="ExternalInput").ap()
    out_d = nc.dram_tensor("out", [TOK, D], F32, kind="ExternalOutput").ap()
    io_m0 = declare_mixer0(nc, "m0_")
    io_p0 = declare_post(nc, "p0_")
    io_m1 = declare_mixer1(nc, "m1_")
    io_p1 = declare_post(nc, "p1_")
    y0src = [nc.dram_tensor(f"y0src{i}", [2048, 512], BF16).ap() for i in range(4)]
    y0dst = [nc.dram_tensor(f"y0dst{i}", [4096, 512], BF16).ap() for i in range(4)]
    o1src = [nc.dram_tensor(f"o1src{i}", [2048, 512], BF16).ap() for i in range(4)]
    o1dst = [nc.dram_tensor(f"o1dst{i}", [4096, 512], BF16).ap() for i in range(4)]
    h1src = [nc.dram_tensor(f"h1src{i}", [512, D], F32).ap() for i in range(8)]
    h1dst = [nc.dram_tensor(f"h1dst{i}", [1024, D], F32).ap() for i in range(8)]
    with ExitStack() as es:
        P = Prog(nc, es)
        B = P.buf
        gidx = P.sb("gidx_sb", [128, 16], U32)
        b_gidx = B()
        P.dma("sp", gidx[:], gidx_d[:, :], writes=[b_gidx])

        def phase(pfx, fn, io):
            with ExitStack() as pes:
                P.es = pes
                P.pfx = pfx
                P.in_phase = True
                fn(P, io)
                full_barrier(P)
                P.recycle()
                P.in_phase = False
            P.es = es

        def ag(srcs, dsts):
            P.in_phase = True
            for a, d in zip(srcs, dsts):
                P.all_gather(a, d, GROUPS, reads=[], writes=[B()])
            full_barrier(P)
            P.recycle()
            P.in_phase = False

        def row_store(dst_list, nbuf=4):
            bufs = [[B() for _ in range(nbuf)] for _ in dst_list]
            cnt = [0]

            def ystore(P_, i, blk, sb, reads):
                h, tq = blk // 2, (blk % 2) * 4
                r0 = (h * 8 + tq) * 128
                dst = dst_list[i][r0:r0 + 512, :].rearrange("(t p) c -> p t c", p=128)
                P_.dma("sp", dst, sb.rearrange("p (t c) -> p t c", t=4), reads=reads, writes=[bufs[i][cnt[0] % nbuf]])
                cnt[0] += 1
            return ystore

        def make_load_yt(dsts):
            def load_yt(P_, yt, byt, t4):
                for ec in range(8):
                    i, r = ec // 2, ec % 2
                    P_.gather(yt[:, ec, :], dsts[i], gidx[:, r * 8 + t4:r * 8 + t4 + 1], reads=[b_gidx], writes=[byt[ec]])
            return load_yt

        io_m0.update(ident=ident_d, ystore=row_store(y0src))
        phase("m0_", emit_mixer0, io_m0)
        ag(y0src, y0dst)
        io_p0.update(ident=ident_d, load_yt=make_load_yt(y0dst), xres=lambda t0: xres_d[t0:t0 + 128, :],
                     out=lambda t0: h1src[t0 // 512][t0 % 512:t0 % 512 + 128, :], is_final=False)
        phase("p0_", emit_post, io_p0)
        ag(h1src, h1dst)
        o1bufs = [[B(), B()] for _ in range(4)]

        def ostore(P_, T, ot, reads, slot):
            h, t4 = T // 32, (T % 32) // 4
            r0 = (h * 8 + t4) * 128
            c0 = (T % 4) * 128
            for j in range(4):
                P_.dma("sp", o1src[j][r0:r0 + 128, c0:c0 + 128], ot[:, j, :], reads=reads, writes=[o1bufs[j][slot]])

        def h1tile(t0):
            k, rr, tt = (t0 % TOK) // 512, t0 // TOK, t0 % 512
            return h1dst[k][rr * 512 + tt:rr * 512 + tt + 128, :]

        io_m1.update(ident=ident_d, x=h1tile, ostore=ostore)
        phase("m1_", emit_mixer1, io_m1)
        ag(o1src, o1dst)
        io_p1.update(ident=ident_d, load_yt=make_load_yt(o1dst), xres=lambda t0: h1src[t0 // 512][t0 % 512:t0 % 512 + 128, :],
                     out=lambda t0: out_d[t0:t0 + 128, :], is_final=True)
        phase("p1_", emit_post, io_p1)
        P.finish()
    return nc


def pack_post(inp, layer, wo_perm, pfx):
    wr = np.concatenate([inp["moe_wr_g"][layer]] + [inp["moe_wr_e"][layer][g] for g in range(4)], axis=1)
    br = np.concatenate([inp["moe_br_g"][layer], inp["moe_br_e"][layer].reshape(-1)])[None]
    d = dict(wo=np.ascontiguousarray(wo_perm),
             ln1g=np.ascontiguousarray(inp["ln_g"][layer, 0]), ln1b=np.ascontiguousarray(inp["ln_b"][layer, 0]),
             ln2g=np.ascontiguousarray(inp["ln_g"][layer, 1]), ln2b=np.ascontiguousarray(inp["ln_b"][layer, 1]),
             wg=inp["moe_w_gate"][layer], wu=inp["moe_w_up"][layer], wd=inp["moe_w_down"][layer],
             wr=np.ascontiguousarray(wr.astype(np.float32)), br=np.ascontiguousarray(br.astype(np.float32)))
    return {pfx + k: v for k, v in d.items()}


def kernel(**inputs):
    inp = {k: np.asarray(v) for k, v in inputs.items()}
    nb = inp["x"].shape[0]
    cores = list(range(2 * nb))
    wo0 = inp["ab_w_out"][0]
    ch0 = [(2 * r + i) if i < 2 else (4 + 2 * r + (i - 2)) for i in range(4) for r in range(2)]
    wo0p = np.concatenate([wo0[c * 128:(c + 1) * 128] for c in ch0], axis=0)
    wo1 = inp["c_w_out"][0]
    ch1 = [4 * r + i for i in range(4) for r in range(2)]
    wo1p = np.concatenate([wo1[c * 128:(c + 1) * 128] for c in ch1], axis=0)
    post0 = pack_post(inp, 0, wo0p, "p0_")
    post1 = pack_post(inp, 1, wo1p, "p1_")
    ident = np.eye(128, dtype=np.float32)
    maps = []
    for b in range(nb):
        for c in range(2):
            m = dict(ident=ident, xres=np.ascontiguousarray(inp["x"][b, TOK * c:TOK * (c + 1)]))
            p = np.arange(128, dtype=np.uint32)[:, None]
            rt = np.arange(16, dtype=np.uint32)[None, :]
            m["gidx"] = np.ascontiguousarray(((rt // 8) * 2048 + c * 1024 + (rt % 8) * 128 + p).astype(np.uint32))
            m0 = pack_m0(inp, b, c)
            m0.pop("ident")
            m.update({"m0_" + k: v for k, v in m0.items()})
            m1 = pack_m1(inp, None, c)
            m1.pop("ident")
            m1.pop("x")
            m.update({"m1_" + k: v for k, v in m1.items()})
            m.update(post0)
            m.update(post1)
            maps.append(m)
    nc = build_fused()
    res = run_bass_kernel_spmd(nc, maps, core_ids=cores).results
    out = np.stack([np.concatenate([np.asarray(res[b * 2 + c]["out"]) for c in range(2)], axis=0) for b in range(nb)])
    return out.astype(np.float32)
```

```python
import os
import numpy as np
from contextlib import ExitStack
import concourse.bass as bass
import concourse.mybir as mybir
from concourse.bass_utils import run_bass_kernel_spmd

F32 = mybir.dt.float32
BF16 = mybir.dt.bfloat16
AF = mybir.ActivationFunctionType
ALU = mybir.AluOpType
AX = mybir.AxisListType

D = 1024
ALPHA = (2.0 * 2) ** 0.25
LN_EPS = 1e-5
NEXP = 16
EH = 512


class Buf:
    __slots__ = ("name", "w", "rd", "dsem", "dval", "dkey")

    def __init__(self, name):
        self.name = name
        self.w = None
        self.rd = {}
        self.dsem = None
        self.dval = 0
        self.dkey = None


class Prog:
    def __init__(self, nc, es):
        self.nc = nc
        self.es = es
        self.es_global = es
        self.pfx = ""
        self.eng = dict(pe=nc.tensor, act=nc.scalar, dve=nc.vector, pool=nc.gpsimd, sp=nc.sync)
        self.sem = {k: es.enter_context(nc.semaphore("s_" + k)) for k in self.eng}
        self.cnt = {k: 0 for k in self.eng}
        self.known = {k: {} for k in self.eng}
        self.nsem = 0
        self.outs = []
        self.nbuf = 0
        self.dmasems = {}
        self.issuer = {}
        self.free_sems = []
        self.phase_sems = []
        self.in_phase = False

    def scope(self, name):
        if getattr(self, "_scope_cm", None) is not None:
            self._scope_cm.__exit__(None, None, None)
            self._scope_cm = None
        if name is not None:
            self._scope_cm = self.nc.named_scope(name)
            self._scope_cm.__enter__()

    def buf(self, name=None):
        self.nbuf += 1
        return Buf(name or f"b{self.nbuf}")

    def sb(self, name, shape, dt):
        return self.es.enter_context(self.nc.sbuf_tensor(self.pfx + name, shape, dt))

    def ps(self, name, shape, dt=F32):
        return self.es.enter_context(self.nc.psum_tensor(self.pfx + name, shape, dt))

    def _collect(self, e, reads, writes):
        deps = {}

        def add(tok, same_ok):
            if tok is None:
                return
            key, sh, val = tok
            if key == e and not same_ok:
                return
            if key in deps and deps[key][1] >= val:
                return
            deps[key] = (sh, val)

        for b in reads:
            add(b.w, e != "pe")
        for b in writes:
            add(b.w, e != "pe")
            for tok in b.rd.values():
                add(tok, e != "pe")
        return deps

    def _wait(self, e, deps):
        kn = self.known[e]
        for key, (sh, val) in deps.items():
            if kn.get(key, 0) >= val:
                continue
            self.eng[e].wait_ge(sh, val)
            kn[key] = val

    def op(self, e, fn, reads=(), writes=()):
        self._wait(e, self._collect(e, reads, writes))
        ins = fn(self.eng[e])
        self.cnt[e] += 1
        ins.then_inc(self.sem[e], 1)
        tok = (e, self.sem[e], self.cnt[e])
        for b in reads:
            b.rd[e] = tok
        for b in writes:
            b.w = tok
            b.rd = {}
        return tok

    def dma(self, q, out, in_, reads=(), writes=(), is_out=False, nowaw=False, **kw):
        wb = writes[0]
        if nowaw:
            saved = wb.w
            if saved is not None and saved[0] == wb.dkey:
                wb.w = None
            deps = self._collect(q, reads, writes)
            wb.w = saved
        else:
            deps = self._collect(q, reads, writes)
        self._wait(q, deps)
        self._ensure_sem(wb)
        ins = self.eng[q].dma_start(out=out, in_=in_, **kw)
        wb.dval += 16
        ins.then_inc(wb.dsem, 16)
        tok = (wb.dkey, wb.dsem, wb.dval)
        self.dmasems[wb.dkey] = tok
        self.issuer[wb.dkey] = q
        for b in reads:
            b.rd[wb.dkey] = tok
        for b in writes:
            b.w = tok
            b.rd = {}
        if is_out and wb not in self.outs:
            self.outs.append(wb)
        return tok

    def _ensure_sem(self, wb):
        if wb.dsem is not None:
            return
        if self.free_sems:
            wb.dkey, wb.dsem, wb.dval = self.free_sems.pop()
        else:
            wb.dkey = f"d{self.nsem}"
            wb.dsem = self.es_global.enter_context(self.nc.semaphore(wb.dkey))
            self.nsem += 1
        if self.in_phase:
            self.phase_sems.append(wb)

    def recycle(self):
        for wb in self.phase_sems:
            self.free_sems.append((wb.dkey, wb.dsem, wb.dval))
            wb.dsem = None
        self.phase_sems = []

    def _tok_async(self, q, wb, ins, inc, reads, writes):
        self._ensure_sem(wb)
        wb.dval += inc
        ins.then_inc(wb.dsem, inc)
        tok = (wb.dkey, wb.dsem, wb.dval)
        self.dmasems[wb.dkey] = tok
        self.issuer[wb.dkey] = q
        for b in reads:
            b.rd[wb.dkey] = tok
        for b in writes:
            b.w = tok
            b.rd = {}
        return tok

    def gather(self, out, in_dram, idx_ap, reads=(), writes=()):
        self._wait("pool", self._collect("pool", reads, writes))
        ins = self.nc.gpsimd.indirect_dma_start(out=out, out_offset=None, in_=in_dram,
                                                in_offset=bass.IndirectOffsetOnAxis(ap=idx_ap, axis=0))
        return self._tok_async("pool", writes[0], ins, 16, reads, writes)

    def all_gather(self, src, dst, groups, reads=(), writes=()):
        self._wait("pool", self._collect("pool", reads, writes))
        ins = self.nc.gpsimd.collective_compute("AllGather", ALU.bypass, replica_groups=groups, ins=[src.opt()], outs=[dst.opt()])
        return self._tok_async("pool", writes[0], ins, 1, reads, writes)

    def finish(self):
        for wb in self.outs:
            if wb.dsem is not None:
                self.eng["sp"].wait_ge(wb.dsem, wb.dval)


def bcast_rows(ap1d_tensor, offset, n, parts=128):
    return bass.AP(ap1d_tensor, offset, [[0, parts], [1, n]])


def emit_tail(P, hmid, hmid_buf, out, out_buf, T, w, layer, ST=1024, banks=None, is_final=True):
    nc = P.nc
    NT = ST // 128
    NK = ST // 512
    ident = P.sb("t_ident", [128, 128], F32)
    ones1 = P.sb("t_ones1", [1, 128], F32)
    brow = P.sb("t_brow", [1, 20], F32)
    wr = P.sb("t_wr", [128, 8, 20], F32)
    gbc = P.sb("t_gbc", [128, D], F32)
    bbc = P.sb("t_bbc", [128, D], F32)
    xin = [P.sb(f"t_xin{i}", [128, D], F32) for i in range(2)]
    acc = P.sb("t_acc", [128, NT, D], F32)
    hT16 = P.sb("t_hT16", [128, 8, ST], BF16)
    hT32 = P.sb("t_hT32", [128, 8, 128], F32)
    gates = P.sb("t_gates", [128, NT, NEXP], F32)
    wgb = [P.sb(f"t_wg{i}", [128, 8, EH], BF16) for i in range(2)]
    wub = [P.sb(f"t_wu{i}", [128, 8, EH], BF16) for i in range(2)]
    wdb = [P.sb(f"t_wd{i}", [128, 4, D], BF16) for i in range(2)]
    sg = [P.sb(f"t_sg{i}", [128, 512], BF16) for i in range(2)]
    hid = [P.sb(f"t_hid{i}", [128, 4, 512], BF16) for i in range(2)]
    obuf = [P.sb(f"t_ob{i}", [128, D], F32) for i in range(2)]
    sm = P.sb("t_sm", [128, 64], F32)
    lg = P.sb("t_lg", [128, 20], F32)
    stats = P.sb("t_stats", [128, 2, 6], F32)

    if banks is None:
        banks = [P.ps(f"t_ps{i}", [128, 512]) for i in range(8)]
    ps_g, ps_u, ps_o, ps_t = banks[0:2], banks[2:4], banks[4:6], banks[6:8]

    B = P.buf
    b_ident, b_ones1, b_brow, b_wr, b_gbc, b_bbc = B(), B(), B(), B(), B(), B()
    b_xin = [B(), B()]
    b_acc = [B() for _ in range(NT)]
    b_hT16 = [B() for _ in range(NT)]
    b_hT32 = B()
    b_gates = [B() for _ in range(NT)]
    b_wg, b_wu, b_wd = [B(), B()], [B(), B()], [B(), B()]
    b_sg = [B(), B()]
    b_hid = [[B() for _ in range(4)] for _ in range(2)]
    b_ob = [B(), B()]
    b_outw = [B(), B()]
    b_sm = B()
    b_lg = B()
    b_stats = B()
    b_psg, b_psu, b_pso, b_pst = [B(), B()], [B(), B()], [B(), B()], [B(), B()]

    P.dma("sp", ident[:], w["ident"][:, :], writes=[b_ident])
    P.dma("sp", brow[:], w["br"][:, :], writes=[b_brow])
    P.dma("sp", wr[:], w["wr"].rearrange("(c p) n -> p c n", p=128), writes=[b_wr])
    P.dma("sp", gbc[:], bcast_rows(w["ln_g"].tensor, w["ln_g"].offset, D), writes=[b_gbc])
    P.dma("sp", bbc[:], bcast_rows(w["ln_b"].tensor, w["ln_b"].offset, D), writes=[b_bbc])
    P.op("dve", lambda e: e.memset(ones1[:], 1.0), writes=[b_ones1])

    nst = T // ST
    wslot = 0
    DBG = ()

    stg = [P.sb(f"t_stg{i}", [128, 4, 512], F32) for i in range(3)]
    b_stg = [B() for _ in range(3)]
    stg_i = [0]

    def load_chunk(dst, bdst, src):
        k = stg_i[0] % 3
        stg_i[0] += 1
        P.dma("sp", stg[k][:], src, writes=[b_stg[k]])
        P.op("act", lambda en, k=k, dst=dst: en.copy(dst, stg[k][:]), reads=[b_stg[k]], writes=[bdst])

    def load_expert(e, slot):
        for h in range(2):
            load_chunk(wgb[slot][:, 4 * h:4 * h + 4, :], b_wg[slot],
                       w["wg"][e][512 * h:512 * h + 512, :].rearrange("(c p) h -> p c h", p=128))
        for h in range(2):
            load_chunk(wub[slot][:, 4 * h:4 * h + 4, :], b_wu[slot],
                       w["wu"][e][512 * h:512 * h + 512, :].rearrange("(c p) h -> p c h", p=128))
        for h in range(2):
            load_chunk(wdb[slot][:, :, 512 * h:512 * h + 512], b_wd[slot],
                       w["wd"][e][:, 512 * h:512 * h + 512].rearrange("(c p) h -> p c h", p=128))

    xi = 0
    oi = 0
    for st in range(nst):
        t0 = st * ST
        if "noload" not in DBG:
            load_expert(0, wslot)
        for tt in range(NT):
            xb, bx = xin[xi % 2], b_xin[xi % 2]
            xi += 1
            P.dma("sp", xb[:], hmid[t0 + tt * 128:t0 + (tt + 1) * 128, :], reads=[hmid_buf], writes=[bx])
            P.op("act", lambda e, xb=xb, tt=tt: e.mul(acc[:, tt, :], xb[:], ALPHA), reads=[bx], writes=[b_acc[tt]])
            if 'notr' in DBG:
                continue
            for half in range(2):
                pt, bpt = ps_t[half], b_pst[half]
                for j in range(4):
                    dc = half * 4 + j
                    P.op("pe", lambda e, pt=pt, j=j, dc=dc, xb=xb: e.transpose(
                        pt[:, j * 128:(j + 1) * 128], xb[:, dc * 128:(dc + 1) * 128], ident[:]),
                        reads=[bx, b_ident], writes=[bpt])
                P.op("act", lambda e, pt=pt, half=half: e.copy(
                    hT32[:, half * 4:(half + 1) * 4, :], pt[:].rearrange("p (c t) -> p c t", c=4)),
                    reads=[bpt], writes=[b_hT32])
                P.op("dve", lambda e, pt=pt, half=half, tt=tt: e.tensor_copy(
                    hT16[:, half * 4:(half + 1) * 4, tt * 128:(tt + 1) * 128],
                    hT32[:, half * 4:(half + 1) * 4, :]),
                    reads=[b_hT32], writes=[b_hT16[tt]])
            if 'norouter' in DBG:
                continue
            pr, bpr = ps_o[0], b_pso[0]
            for dc in range(8):
                P.op("pe", lambda e, dc=dc, pr=pr: e.matmul(pr[:, 0:20], hT32[:, dc, :], wr[:, dc, :],
                                                            start=(dc == 0), stop=False),
                     reads=[b_hT32, b_wr], writes=[bpr])
            P.op("pe", lambda e, pr=pr: e.matmul(pr[:, 0:20], ones1[0:1, :], brow[0:1, :], start=False, stop=True),
                 reads=[b_ones1, b_brow], writes=[bpr])
            P.op("act", lambda e, pr=pr: e.copy(lg[:], pr[:, 0:20]), reads=[bpr], writes=[b_lg])
            emit_router(P, lg, b_lg, sm, b_sm, gates, b_gates[tt], tt)
        units = [(e, tk) for e in range(NEXP) for tk in range(NK)]

        def gu(ui):
            e, tk = units[ui]
            slot = (wslot + e) % 2
            hb = ui % 2
            for hc in range(4):
                pg, bpg = ps_g[hc % 2], b_psg[hc % 2]
                pu, bpu = ps_u[hc % 2], b_psu[hc % 2]
                rds = [b_hT16[tk * 4 + q] for q in range(4)]
                for dc in range(8):
                    P.op("pe", lambda en, pg=pg, dc=dc, hc=hc, slot=slot, tk=tk: en.matmul(
                        pg[:], wgb[slot][:, dc, hc * 128:(hc + 1) * 128], hT16[:, dc, tk * 512:(tk + 1) * 512],
                        start=(dc == 0), stop=(dc == 7)), reads=rds + [b_wg[slot]], writes=[bpg])
                for dc in range(8):
                    P.op("pe", lambda en, pu=pu, dc=dc, hc=hc, slot=slot, tk=tk: en.matmul(
                        pu[:], wub[slot][:, dc, hc * 128:(hc + 1) * 128], hT16[:, dc, tk * 512:(tk + 1) * 512],
                        start=(dc == 0), stop=(dc == 7)), reads=rds + [b_wu[slot]], writes=[bpu])
                s, bs = sg[hc % 2], b_sg[hc % 2]
                P.op("act", lambda en, s=s, pg=pg: en.activation(s[:], pg[:], AF.Silu), reads=[bpg], writes=[bs])
                P.op("dve", lambda en, s=s, pu=pu, hb=hb, hc=hc: en.tensor_tensor(
                    hid[hb][:, hc, :], s[:], pu[:], ALU.mult), reads=[bs, bpu], writes=[b_hid[hb][hc]])

        def down(ui):
            e, tk = units[ui]
            slot = (wslot + e) % 2
            hb = ui % 2
            k = 0
            for tq in range(4):
                tt = tk * 4 + tq
                for dh in range(2):
                    po, bpo = ps_o[k % 2], b_pso[k % 2]
                    k += 1
                    for hc in range(4):
                        P.op("pe", lambda en, po=po, hc=hc, tq=tq, dh=dh, hb=hb, slot=slot: en.matmul(
                            po[:], hid[hb][:, hc, tq * 128:(tq + 1) * 128], wdb[slot][:, hc, dh * 512:(dh + 1) * 512],
                            start=(hc == 0), stop=(hc == 3)), reads=[b_hid[hb][hc], b_wd[slot]], writes=[bpo])
                    P.op("dve", lambda en, po=po, tt=tt, dh=dh, e=e: en.scalar_tensor_tensor(
                        acc[:, tt, dh * 512:(dh + 1) * 512], po[:], gates[:, tt, e:e + 1],
                        acc[:, tt, dh * 512:(dh + 1) * 512], ALU.mult, ALU.add),
                        reads=[bpo, b_gates[tt], b_acc[tt]], writes=[b_acc[tt]])

        if "noexp" in DBG:
            units = []
        if "oneexp" in DBG:
            units = units[:2]
        for ui in range(len(units)):
            e, tk = units[ui]
            gu(ui)
            if ui > 0:
                down(ui - 1)
            if tk == 0 and e + 1 < NEXP:
                load_expert(e + 1, (wslot + e + 1) % 2)
        if units:
            down(len(units) - 1)
        wslot = (wslot + NEXP) % 2
        for tt in range(NT):
            ob, bo = obuf[oi % 2], b_ob[oi % 2]
            oi += 1
            if "nolnorm" in DBG:
                P.op("act", lambda e, ob=ob, tt=tt: e.copy(ob[:], acc[:, tt, :]), reads=[b_acc[tt]], writes=[bo])
            else:
                emit_ln(P, acc[:, tt, :], b_acc[tt], ob, bo, gbc, b_gbc, bbc, b_bbc, stats, b_stats, sm, b_sm)
            P.dma("sp", out(t0 + tt * 128), ob[:], reads=[bo], writes=[b_outw[(oi - 1) % 2]], is_out=is_final)


def emit_ln(P, x, bx, ob, bo, gbc, b_gbc, bbc, b_bbc, stats, b_stats, sm, b_sm):
    mv = sm[:, 40:42]
    rstd = sm[:, 42:43]
    nmr = sm[:, 43:44]
    P.op("dve", lambda e: e.bn_stats(stats[:, 0, :], x[:, 0:512]), reads=[bx], writes=[b_stats])
    P.op("dve", lambda e: e.bn_stats(stats[:, 1, :], x[:, 512:1024]), reads=[bx], writes=[b_stats])
    P.op("dve", lambda e: e.bn_aggr(mv, stats[:]), reads=[b_stats], writes=[b_sm])
    P.op("dve", lambda e: e.tensor_scalar(rstd, sm[:, 41:42], LN_EPS, None, ALU.add), reads=[b_sm], writes=[b_sm])
    P.op("act", lambda e: e.sqrt(rstd, rstd), reads=[b_sm], writes=[b_sm])
    P.op("dve", lambda e: e.reciprocal(rstd, rstd), reads=[b_sm], writes=[b_sm])
    P.op("dve", lambda e: e.scalar_tensor_tensor(nmr, sm[:, 40:41], -1.0, rstd, ALU.mult, ALU.mult), reads=[b_sm], writes=[b_sm])
    P.op("act", lambda e: e.activation(ob[:], x, AF.Identity, bias=nmr, scale=rstd), reads=[bx, b_sm], writes=[bo])
    P.op("dve", lambda e: e.tensor_tensor(ob[:], ob[:], gbc[:], ALU.mult), reads=[bo, b_gbc], writes=[bo])
    P.op("dve", lambda e: e.tensor_tensor(ob[:], ob[:], bbc[:], ALU.add), reads=[bo, b_bbc], writes=[bo])


def emit_router(P, lg, b_lg, sm, b_sm, gates, b_g, tt):
    gmax, ngmax, gsum, gw = sm[:, 0:1], sm[:, 1:2], sm[:, 2:3], sm[:, 3:4]
    ohg, gexp, esel, oh1 = sm[:, 4:8], sm[:, 8:12], sm[:, 12:16], sm[:, 16:20]
    em, oh2 = sm[:, 20:24], sm[:, 24:28]
    m1, m2, dd, ed, den, w1, w2 = (sm[:, 28 + i:29 + i] for i in range(7))
    ew = sm[:, 36:40]
    R = [b_lg, b_sm]
    W = [b_sm]

    def dv(fn, reads=R, writes=W):
        P.op("dve", fn, reads=reads, writes=writes)

    dv(lambda e: e.tensor_reduce(gmax, lg[:, 0:4], AX.X, ALU.max))
    dv(lambda e: e.tensor_scalar(ohg, lg[:, 0:4], gmax, None, ALU.is_equal))
    dv(lambda e: e.tensor_scalar(ngmax, gmax, -1.0, None, ALU.mult))
    P.op("act", lambda e: e.activation(gexp, lg[:, 0:4], AF.Exp, bias=ngmax, scale=1.0), reads=R, writes=W)
    dv(lambda e: e.tensor_reduce(gsum, gexp, AX.X, ALU.add))
    dv(lambda e: e.reciprocal(gw, gsum))
    dv(lambda e: e.tensor_scalar(esel, lg[:, 4:8], ohg[:, 0:1], None, ALU.mult))
    for g in range(1, 4):
        dv(lambda e, g=g: e.scalar_tensor_tensor(esel, lg[:, 4 + 4 * g:8 + 4 * g], ohg[:, g:g + 1], esel, ALU.mult, ALU.add))
    dv(lambda e: e.tensor_reduce(m1, esel, AX.X, ALU.max))
    dv(lambda e: e.tensor_scalar(oh1, esel, m1, None, ALU.is_equal))
    dv(lambda e: e.scalar_tensor_tensor(em, oh1, -1e30, esel, ALU.mult, ALU.add))
    dv(lambda e: e.tensor_reduce(m2, em, AX.X, ALU.max))
    dv(lambda e: e.tensor_scalar(oh2, em, m2, None, ALU.is_equal))
    dv(lambda e: e.tensor_tensor(dd, m2, m1, ALU.subtract))
    P.op("act", lambda e: e.activation(ed, dd, AF.Exp), reads=R, writes=W)
    dv(lambda e: e.tensor_scalar(den, ed, 1.0, None, ALU.add))
    dv(lambda e: e.reciprocal(den, den))
    dv(lambda e: e.tensor_tensor(w1, den, gw, ALU.mult))
    dv(lambda e: e.tensor_tensor(w2, w1, ed, ALU.mult))
    dv(lambda e: e.tensor_scalar(ew, oh1, w1, None, ALU.mult))
    dv(lambda e: e.scalar_tensor_tensor(ew, oh2, w2, ew, ALU.mult, ALU.add))
    for g in range(4):
        dv(lambda e, g=g: e.tensor_scalar(gates[:, tt, 4 * g:4 * g + 4], ew, ohg[:, g:g + 1], None, ALU.mult),
           reads=[b_sm], writes=[b_g])


def t5_bucket_np(dist):
    n = np.maximum(dist, 0)
    nf = np.maximum(n, 1).astype(np.float32)
    large = 16 + (np.log(nf / np.float32(16)) / np.float32(np.log(2048 / 16)) * np.float32(16)).astype(np.int32)
    return np.where(n < 16, n, np.minimum(large, 31))


def dil_onehot():
    oh = np.zeros((33, 3, 384), np.float32)
    for di, d in enumerate((1, 4, 16)):
        for jp in range(384):
            j = jp - 127
            if 0 <= j <= 128:
                oh[t5_bucket_np(np.array(j * d)), di, jp] = 1.0
            else:
                oh[32, di, jp] = 1.0
    return oh


class Banks:
    def __init__(self, P):
        self.f = [P.ps(f"bank{i}", [128, 512], F32) for i in range(7)]
        self.bf = [P.buf() for _ in range(7)]
        self.h = P.ps("bankh", [128, 1024], BF16)
        self.bh = P.buf()


def full_barrier(P):
    for q in ("sp", "pool"):
        eq = P.eng[q]
        for key, (k_, sh, val) in list(P.dmasems.items()):
            if P.issuer.get(key) == q and P.known[q].get(key, 0) < val:
                eq.wait_ge(sh, val)
                P.known[q][key] = val
        eq.sem_inc(P.sem[q], 1)
        P.cnt[q] += 1
    for e in P.eng:
        for f in P.eng:
            if f != e and P.cnt[f] > P.known[e].get(f, 0):
                P.eng[e].wait_ge(P.sem[f], P.cnt[f])
                P.known[e][f] = P.cnt[f]
    for e in P.eng:
        for (key, sh, val) in P.dmasems.values():
            P.known[e][key] = max(P.known[e].get(key, 0), val)


class Pipe:
    def __init__(self, depth=1):
        self.q = []
        self.depth = depth

    def step(self, fn):
        self.q.append(fn)
        if len(self.q) > self.depth:
            self.q.pop(0)()

    def flush(self):
        while self.q:
            self.q.pop(0)()


class Stager:
    def __init__(self, P, n=3, cols=2048):
        self.P = P
        self.cols = cols
        self.t = [P.sb(f"stg{i}", [128, cols], F32) for i in range(n)]
        self.b = [P.buf() for _ in range(n)]
        self.i = 0

    def load(self, dst3, bdst, src2d, eng="act"):
        C, W = dst3.shape[1], dst3.shape[2]
        per = max(1, self.cols // W)
        c0 = 0
        while c0 < C:
            c1 = min(C, c0 + per)
            k = self.i % len(self.t)
            self.i += 1
            st = self.t[k][:, 0:(c1 - c0) * W].rearrange("p (c w) -> p c w", w=W)
            self.P.dma("sp", st, src2d[c0 * 128:c1 * 128, :].rearrange("(c p) w -> p c w", p=128), writes=[self.b[k]])
            self.P.op(eng, lambda en, st=st, c0=c0, c1=c1: en.copy(dst3[:, c0:c1, :], st),
                      reads=[self.b[k]], writes=[bdst])
            c0 = c1


def load_xT(P, BK, x, bx_dram, xin, b_xin, xT, b_xT, ident, b_ident, t0, ntile, ctr):
    for tl in range(ntile):
        k = ctr[0] % 2
        ctr[0] += 1
        xb, bx = xin[k], b_xin[k]
        P.dma("sp", xb[:], x(t0 + tl * 128), reads=[bx_dram], writes=[bx])
        for half in range(2):
            pt, bpt = BK.f[half], BK.bf[half]
            for j in range(4):
                dc = half * 4 + j
                P.op("pe", lambda e, pt=pt, j=j, dc=dc, xb=xb: e.transpose(
                    pt[:, j * 128:(j + 1) * 128], xb[:, dc * 128:(dc + 1) * 128], ident[:]),
                    reads=[bx, b_ident], writes=[bpt])
            eng = "act" if half == 0 else "dve"
            if eng == "act":
                P.op("act", lambda e, pt=pt, half=half, tl=tl: e.copy(
                    xT[:, half * 4:(half + 1) * 4, tl * 128:(tl + 1) * 128], pt[:].rearrange("p (c t) -> p c t", c=4)),
                    reads=[bpt], writes=[b_xT[tl]])
            else:
                P.op("dve", lambda e, pt=pt, half=half, tl=tl: e.tensor_copy(
                    xT[:, half * 4:(half + 1) * 4, tl * 128:(tl + 1) * 128], pt[:].rearrange("p (c t) -> p c t", c=4)),
                    reads=[bpt], writes=[b_xT[tl]])


S_TOT = 8192
NBLK = 4
BLK = 2048


def declare_mixer0(nc, pfx=""):
    din = lambda n, s: nc.dram_tensor(pfx + n, s, F32, kind="ExternalInput").ap()
    return dict(x=din("x", [S_TOT, D]), wA=din("wA", [D, 768]), wB=din("wB", [D, 1028]), gbias=din("gbias", [1, 4]),
                convw=din("convw", [128, 16]), hng=din("hng", [1, 256]), relb=din("relb", [33, 4]), oh=din("oh", [33, 3 * 384]),
                triu=din("triu", [128, 128]),
                scr=nc.dram_tensor(pfx + "scr_e", [128, 12 * 384], F32).ap())


def emit_mixer0(P, io):
    nc = P.nc
    x_d, wA, wB, gbias, convw, hng, relb = io["x"], io["wA"], io["wB"], io["gbias"], io["convw"], io["hng"], io["relb"]
    oh_d, ident_d, triu_d, scr = io["oh"], io["ident"], io["triu"], io["scr"]
    x = lambda t0: x_d[t0:t0 + 128, :]
    ystore = io["ystore"]
    if True:
        B = P.buf
        BK = Banks(P)
        b_x, b_y, b_scr = B(), B(), B()
        ident = P.sb("ident_sb", [128, 128], F32)
        b_ident = B()
        P.dma("sp", ident[:], ident_d[:, :], writes=[b_ident])
        xin = [P.sb(f"xin{i}", [128, D], F32) for i in range(2)]
        b_xin = [B(), B()]
        xT = P.sb("xT", [128, 8, BLK], BF16)
        b_xT = [B() for _ in range(16)]
        stg = Stager(P)
        ctr = [0]
        ones_bf = P.sb("ones_bf", [128, 128], BF16)
        b_ones = B()
        P.op("dve", lambda e: e.memset(ones_bf[:], 1.0), writes=[b_ones])

        with ExitStack() as esA:
            sbA = lambda n, s, d: esA.enter_context(nc.sbuf_tensor(P.pfx + n, s, d))
            Etab = sbA("a_E", [128, 12, 2, 128], BF16)
            with ExitStack() as esT:
                sbA_outer = sbA
                sbA = lambda n, s, d: esT.enter_context(nc.sbuf_tensor(P.pfx + n, s, d))
                tab = sbA("a_tab", [33, 4], F32)
                ohs = sbA("a_oh", [33, 3 * 384], F32)
                tabB = sbA("a_tabB", [33, 128], F32)
                fre = sbA("a_fre", [128, 12 * 384], F32)
                E32 = sbA("a_E32", [128, 12 * 256], F32)
                b_tab, b_oh, b_tabB, b_fre, b_E32, b_E = B(), B(), B(), B(), B(), B()
                P.dma("sp", tab[:], relb[:, :], writes=[b_tab])
                P.dma("sp", ohs[:], oh_d[:, :], writes=[b_oh])
                for h in range(4):
                    P.op("dve", lambda e, h=h: e.tensor_copy(tabB[:], tab[:, h:h + 1].to_broadcast([33, 128])),
                         reads=[b_tab], writes=[b_tabB])
                    for di in range(3):
                        pb, bpb = BK.f[2 + (di % 2)], BK.bf[2 + (di % 2)]
                        P.op("pe", lambda e, pb=pb, di=di: e.matmul(pb[:, 0:384], tabB[:], ohs[:, di * 384:(di + 1) * 384],
                                                                    start=True, stop=True),
                             reads=[b_tabB, b_oh], writes=[bpb])
                        P.op("act", lambda e, pb=pb, di=di, h=h: e.activation(
                            fre[:, (di * 4 + h) * 384:(di * 4 + h + 1) * 384], pb[:, 0:384], AF.Exp),
                            reads=[bpb], writes=[b_fre])
                P.dma("sp", scr[:, :], fre[:], reads=[b_fre], writes=[b_scr])
                L = 12 * 384
                skew = bass.AP(scr.tensor, 127, [[L - 1, 128], [384, 12], [128, 2], [1, 128]])
                P.dma("sp", E32[:].rearrange("p (t s q) -> p t s q", t=12, s=2), skew, reads=[b_scr], writes=[b_E32])
                P.op("dve", lambda e: e.tensor_copy(Etab[:].rearrange("p t s q -> p (t s q)"), E32[:]),
                     reads=[b_E32], writes=[b_E])

                full_barrier(P)
            sbA = sbA_outer
            wAb = sbA("a_w", [128, 8, 384], BF16)
            b_wA = B()
            QT = sbA("a_QT", [128, S_TOT], BF16)
            KT = sbA("a_KT", [128, S_TOT], BF16)
            b_QT = [B() for _ in range(NBLK)]
            b_KT = [B() for _ in range(NBLK)]
            Vd = [sbA(f"a_V{di}", [128, 64, 128], BF16) for di in range(3)]
            b_Vd = [[B() for _ in range(NBLK)] for _ in range(3)]
            numacc = sbA("a_num", [128, BLK], F32)
            denacc = sbA("a_den", [128, BLK], F32)
            b_num, b_den = B(), B()
            Pb = [sbA(f"a_P{i}", [128, 2, 2, 128], BF16) for i in range(2)]
            b_Pb = [B(), B()]
            ybuf = sbA("a_y", [128, BLK], BF16)
            b_yb = B()
            pcount = 0
            pipeA = Pipe()
            for hp in range(2):
                stg.load(wAb[:], b_wA, wA[:, hp * 384:(hp + 1) * 384])
                for blk in range(NBLK):
                    T0 = blk * BLK
                    load_xT(P, BK, x, b_x, xin, b_xin, xT, b_xT, ident, b_ident, T0, 16, ctr)
                    for qtr in range(4):
                        rds = [b_xT[qtr * 4 + i] for i in range(4)] + [b_wA]
                        for sel, (dst, bd) in enumerate(((QT, b_QT[blk]), (KT, b_KT[blk]))):
                            pb, bpb = BK.f[2 + sel], BK.bf[2 + sel]
                            for dc in range(8):
                                P.op("pe", lambda e, pb=pb, dc=dc, sel=sel, qtr=qtr: e.matmul(
                                    pb[:], wAb[:, dc, sel * 128:(sel + 1) * 128], xT[:, dc, qtr * 512:(qtr + 1) * 512],
                                    start=(dc == 0), stop=(dc == 7)), reads=rds, writes=[bpb])
                            if sel == 0:
                                P.op("act", lambda e, pb=pb, dst=dst, T0=T0, qtr=qtr: e.copy(
                                    dst[:, T0 + qtr * 512:T0 + (qtr + 1) * 512], pb[:]), reads=[bpb], writes=[bd])
                            else:
                                P.op("dve", lambda e, pb=pb, dst=dst, T0=T0, qtr=qtr: e.tensor_copy(
                                    dst[:, T0 + qtr * 512:T0 + (qtr + 1) * 512], pb[:]), reads=[bpb], writes=[bd])
                    for di, d in enumerate((1, 4, 16)):
                        npb = 16 // d
                        tiles = [(nl, r) for nl in range(npb) for r in range(d)]
                        for g4 in range(4):
                            pb, bpb = BK.f[2 + (g4 % 2)], BK.bf[2 + (g4 % 2)]
                            for j in range(4):
                                nl, r = tiles[g4 * 4 + j]
                                s0 = r + d * 128 * nl
                                for dc in range(8):
                                    P.op("pe", lambda e, pb=pb, j=j, dc=dc, s0=s0, d=d: e.matmul(
                                        pb[:, j * 128:(j + 1) * 128], xT[:, dc, s0:s0 + 127 * d + 1:d], wAb[:, dc, 256:384],
                                        start=(dc == 0), stop=(dc == 7)), reads=b_xT + [b_wA], writes=[bpb])
                            nl0, r0 = tiles[g4 * 4]
                            idx0 = (blk * npb + nl0) * d + r0
                            P.op("act" if g4 % 2 == 0 else "dve",
                                 (lambda e, pb=pb, di=di, idx0=idx0: e.copy(
                                     Vd[di][:, idx0:idx0 + 4, :], pb[:].rearrange("p (j c) -> p j c", j=4)))
                                 if g4 % 2 == 0 else
                                 (lambda e, pb=pb, di=di, idx0=idx0: e.tensor_copy(
                                     Vd[di][:, idx0:idx0 + 4, :], pb[:].rearrange("p (j c) -> p j c", j=4))),
                                 reads=[bpb], writes=[b_Vd[di][blk]])
                    for di, d in enumerate((1, 4, 16)):
                        npb = 16 // d
                        for nl in range(npb):
                            n = blk * npb + nl
                            nslot = 2 if n > 0 else 1
                            for r in range(d):
                                qs = T0 + r + d * 128 * nl
                                ql = r + d * 128 * nl
                                par = pcount % 2
                                ps_h = [BK.f[2 + 2 * par + h] for h in range(2)]
                                bps_h = [BK.bf[2 + 2 * par + h] for h in range(2)]
                                pb_, bpb_ = Pb[pcount % 2], b_Pb[pcount % 2]
                                pcount += 1
                                kblks = sorted({blk, (qs - d * 128) // BLK}) if n > 0 else [blk]
                                rds = [b_QT[blk]] + [b_KT[kb] for kb in kblks]
                                for h in range(2):
                                    for sl in range(nslot):
                                        ks = qs - sl * d * 128
                                        P.op("pe", lambda e, h=h, sl=sl, ks=ks, qs=qs, d=d, ps_h=ps_h: e.matmul(
                                            ps_h[h][:, sl * 128:(sl + 1) * 128],
                                            KT[h * 64:(h + 1) * 64, ks:ks + 127 * d + 1:d],
                                            QT[h * 64:(h + 1) * 64, qs:qs + 127 * d + 1:d], start=True, stop=True),
                                            reads=rds, writes=[bps_h[h]])
                                for h in range(2):
                                    P.op("act", lambda e, pb_=pb_, h=h, nslot=nslot, ps_h=ps_h: e.activation(
                                        pb_[:, h, 0:nslot, :], ps_h[h][:, 0:nslot * 128].rearrange("p (s q) -> p s q", s=nslot),
                                        AF.Exp, scale=0.125), reads=[bps_h[h]], writes=[bpb_])
                                P.op("dve", lambda e, pb_=pb_, di=di, hp=hp, nslot=nslot: e.tensor_tensor(
                                    pb_[:, :, 0:nslot, :], pb_[:, :, 0:nslot, :],
                                    Etab[:, di * 4 + hp * 2:di * 4 + hp * 2 + 2, 0:nslot, :], ALU.mult),
                                    reads=[bpb_, b_E], writes=[bpb_])
                                def stageB(di=di, d=d, n=n, r=r, nslot=nslot, pb_=pb_, bpb_=bpb_, kblks=kblks, ql=ql, u=pcount):
                                    pv, bpv = (BK.f[6], BK.bf[6]) if u % 2 == 0 else (BK.f[0], BK.bf[0])
                                    vrd = [b_Vd[di][kb] for kb in kblks]
                                    for h in range(2):
                                        for sl in range(nslot):
                                            kidx = (n - sl) * d + r
                                            P.op("pe", lambda e, pv=pv, h=h, sl=sl, kidx=kidx, di=di, pb_=pb_, nslot=nslot: e.matmul(
                                                pv[:, h * 128:(h + 1) * 128], Vd[di][:, kidx, :], pb_[:, h, sl, :],
                                                start=(sl == 0), stop=(sl == nslot - 1)), reads=vrd + [bpb_], writes=[bpv])
                                        for sl in range(nslot):
                                            P.op("pe", lambda e, pv=pv, h=h, sl=sl, pb_=pb_, nslot=nslot: e.matmul(
                                                pv[:, (2 + h) * 128:(3 + h) * 128], ones_bf[:], pb_[:, h, sl, :],
                                                start=(sl == 0), stop=(sl == nslot - 1)), reads=[b_ones, bpb_], writes=[bpv])
                                    for h in range(2):
                                        pr = slice(h * 64, (h + 1) * 64)
                                        qc = slice(ql, ql + 127 * d + 1, d)
                                        if di == 0:
                                            P.op("dve", lambda e, pr=pr, qc=qc, h=h: e.tensor_copy(
                                                numacc[pr, qc], pv[pr, h * 128:(h + 1) * 128]), reads=[bpv], writes=[b_num])
                                            P.op("dve", lambda e, pr=pr, qc=qc, h=h: e.tensor_copy(
                                                denacc[pr, qc], pv[pr, (2 + h) * 128:(3 + h) * 128]), reads=[bpv], writes=[b_den])
                                        else:
                                            P.op("dve", lambda e, pr=pr, qc=qc, h=h: e.tensor_tensor(
                                                numacc[pr, qc], numacc[pr, qc], pv[pr, h * 128:(h + 1) * 128], ALU.add),
                                                reads=[bpv, b_num], writes=[b_num])
                                            P.op("dve", lambda e, pr=pr, qc=qc, h=h: e.tensor_tensor(
                                                denacc[pr, qc], denacc[pr, qc], pv[pr, (2 + h) * 128:(3 + h) * 128], ALU.add),
                                                reads=[bpv, b_den], writes=[b_den])
                                pipeA.step(stageB)
                    pipeA.flush()
                    P.op("dve", lambda e: e.reciprocal(denacc[:], denacc[:]), reads=[b_den], writes=[b_den])
                    P.op("dve", lambda e: e.tensor_tensor(ybuf[:], numacc[:], denacc[:], ALU.mult),
                         reads=[b_num, b_den], writes=[b_yb])
                    ystore(P, hp, blk, ybuf[:], [b_yb])
            full_barrier(P)

        P.scope(P.pfx + "mlstm")
        with ExitStack() as esB:
            sbB = lambda n, s, d: esB.enter_context(nc.sbuf_tensor(P.pfx + n, s, d))
            SC = 128.0 ** -0.5
            wBb = sbB("b_w", [128, 8, 1028], BF16)
            b_wB = B()
            stg.load(wBb[:], b_wB, wB[:, :])
            pc = sbB("b_pc", [128, 4, BLK + 3], F32)
            b_pc = [B() for _ in range(4)]
            ctmp = [sbB(f"b_ct{i}", [128, BLK], F32) for i in range(2)]
            b_ct = [B(), B()]
            QKb = sbB("b_qk", [128, 4, BLK], BF16)
            b_qk = [B() for _ in range(4)]
            cw = sbB("b_cw", [128, 16], F32)
            hngb = sbB("b_hng", [128, 256], F32)
            gbb = sbB("b_gb", [128, 4], F32)
            triu32 = sbB("b_triu32", [128, 128], F32)
            triub = sbB("b_triub", [128, 128], BF16)
            ones32 = sbB("b_ones32", [128, 128], F32)
            identb = sbB("b_identb", [128, 128], BF16)
            b_cst = B()
            P.dma("sp", cw[:], convw[:, :], writes=[b_cst])
            b_c2, b_c3, b_c4 = B(), B(), B()
            P.dma("sp", hngb[:], bcast_rows(hng.tensor, hng.offset, 256), writes=[b_c2])
            P.dma("sp", gbb[:], bcast_rows(gbias.tensor, gbias.offset, 4), writes=[b_c3])
            P.dma("sp", triu32[:], triu_d[:, :], writes=[b_c4])
            b_c5 = B()
            P.op("dve", lambda e: e.tensor_copy(triub[:], triu32[:]), reads=[b_c4], writes=[b_c5])
            P.op("dve", lambda e: e.memset(ones32[:], 1.0), writes=[b_c5])
            P.op("dve", lambda e: e.tensor_copy(identb[:], ident[:]), reads=[b_ident], writes=[b_c5])
            CST = [b_cst, b_c2, b_c3, b_c4, b_c5]
            Vaug = [sbB(f"b_va{i}", [128, 2, 129], BF16) for i in range(2)]
            b_va = [B(), B()]
            sgo = [sbB(f"b_sgo{i}", [128, 256], F32) for i in range(2)]
            b_sgo = [B(), B()]
            smB = [sbB(f"b_sm{i}", [128, 64], F32) for i in range(2)]
            b_smB = [B(), B()]
            Sm = [sbB(f"b_Sm{i}", [128, 128], BF16) for i in range(4)]
            b_Sm = [B() for _ in range(4)]
            hh = [sbB(f"b_hh{i}", [128, 128], F32) for i in range(2)]
            b_hh = [B(), B()]
            yn32 = [sbB(f"b_yn{i}", [128, 128], F32) for i in range(2)]
            b_yn = [B(), B()]
            ynb = [sbB(f"b_ynb{i}", [128, 128], BF16) for i in range(2)]
            b_ynb = [B(), B()]
            Ktm = [sbB(f"b_ktm{i}", [128, 128], BF16) for i in range(4)]
            b_ktm = [B() for _ in range(4)]
            Vw = [sbB(f"b_vw{i}", [128, 129], BF16) for i in range(4)]
            b_vw = [B() for _ in range(4)]
            C32 = sbB("b_C32", [128, 2, 129], F32)
            Cb = sbB("b_Cb", [128, 2, 129], BF16)
            b_C32, b_Cb = [B(), B()], [B(), B()]
            hst = [sbB(f"b_hst{i}", [128, 16], F32) for i in range(2)]
            b_hst = [B(), B()]
            ybufB = sbB("b_yb", [128, 2, BLK], BF16)
            b_ybB = B()
            P.op("dve", lambda e: e.memset(C32[:], 0.0), writes=b_C32)
            P.op("dve", lambda e: e.memset(Cb[:], 0.0), writes=b_Cb)
            for k in range(2):
                P.op("dve", lambda e, k=k: e.memset(Vaug[k][:, :, 128:129], 1.0), writes=[b_va[k]])
            for g in range(4):
                P.op("dve", lambda e, g=g: e.memset(pc[:, g, 0:3], 0.0), writes=[b_pc[g]])
            f0, f1, f2, f3, f4, f5, f6 = BK.f
            bf0, bf1, bf2, bf3, bf4, bf5, bf6 = BK.bf
            cti = 0
            pipeB = Pipe(depth=1)
            for blk in range(NBLK):
                T0 = blk * BLK
                load_xT(P, BK, x, b_x, xin, b_xin, xT, b_xT, ident, b_ident, T0, 16, ctr)
                for g in range(4):
                    for qtr in range(4):
                        pb, bpb = (f0, bf0) if g % 2 == 0 else (f1, bf1)
                        rds = [b_xT[qtr * 4 + i] for i in range(4)] + [b_wB]
                        for dc in range(8):
                            P.op("pe", lambda e, pb=pb, dc=dc, g=g, qtr=qtr: e.matmul(
                                pb[:], wBb[:, dc, g * 128:(g + 1) * 128], xT[:, dc, qtr * 512:(qtr + 1) * 512],
                                start=(dc == 0), stop=(dc == 7)), reads=rds, writes=[bpb])
                        if g % 2 == 0:
                            P.op("act", lambda e, pb=pb, g=g, qtr=qtr: e.copy(pc[:, g, 3 + qtr * 512:3 + (qtr + 1) * 512], pb[:]),
                                 reads=[bpb], writes=[b_pc[g]])
                        else:
                            P.op("dve", lambda e, pb=pb, g=g, qtr=qtr: e.tensor_copy(pc[:, g, 3 + qtr * 512:3 + (qtr + 1) * 512], pb[:]),
                                 reads=[bpb], writes=[b_pc[g]])
                for g in range(4):
                    ct, bct = ctmp[cti % 2], b_ct[cti % 2]
                    cti += 1
                    P.op("dve", lambda e, ct=ct, g=g: e.tensor_scalar(ct[:], pc[:, g, 3:3 + BLK], cw[:, g * 4 + 3:g * 4 + 4], None, ALU.mult),
                         reads=[b_pc[g], b_cst], writes=[bct])
                    for j in (2, 1, 0):
                        P.op("dve", lambda e, ct=ct, g=g, j=j: e.scalar_tensor_tensor(
                            ct[:], pc[:, g, j:j + BLK], cw[:, g * 4 + j:g * 4 + j + 1], ct[:], ALU.mult, ALU.add),
                            reads=[b_pc[g], b_cst, bct], writes=[bct])
                    P.op("act", lambda e, ct=ct, g=g: e.activation(QKb[:, g, :], ct[:], AF.Silu), reads=[bct], writes=[b_qk[g]])
                    P.op("dve", lambda e, g=g: e.tensor_copy(pc[:, g, 0:3], pc[:, g, BLK:BLK + 3]), reads=[b_pc[g]], writes=[b_pc[g]])
                for ch in range(16):
                    k = ch % 2
                    tsl = slice(ch * 128, (ch + 1) * 128)
                    va, bva, sg_, bsg, sm_, bsm = Vaug[k], b_va[k], sgo[k], b_sgo[k], smB[k], b_smB[k]
                    gt, e1, sp_, cs = sm_[:, 0:4], sm_[:, 4:6], sm_[:, 6:8], sm_[:, 8:12]
                    ti, ws, wsq, eb, ebL = sm_[:, 12:14], sm_[:, 14:16], sm_[:, 16:18], sm_[:, 18:20], sm_[:, 20:22]
                    rdx = [b_xT[ch], b_wB]
                    for (bank, bbank, c0, c1, w0) in ((f2, bf2, 0, 256, 512), (f2, bf2, 256, 260, 1024), (f3, bf3, 0, 256, 768)):
                        for dc in range(8):
                            P.op("pe", lambda e, bank=bank, c0=c0, c1=c1, w0=w0, dc=dc, tsl=tsl: e.matmul(
                                bank[:, c0:c1], xT[:, dc, tsl], wBb[:, dc, w0:w0 + (c1 - c0)],
                                start=(dc == 0), stop=(dc == 7)), reads=rdx, writes=[bbank])
                    P.op("dve", lambda e, va=va: e.tensor_copy(va[:, :, 0:128], f2[:, 0:256].rearrange("p (h c) -> p h c", h=2)),
                         reads=[bf2], writes=[bva])
                    P.op("dve", lambda e, gt=gt: e.tensor_tensor(gt, f2[:, 256:260], gbb[:], ALU.add), reads=[bf2] + CST, writes=[bsm])
                    P.op("act", lambda e, sg_=sg_: e.activation(sg_[:], f3[:, 0:256], AF.Sigmoid), reads=[bf3], writes=[bsg])
                    P.op("act", lambda e, e1=e1, gt=gt: e.activation(e1, gt[:, 2:4], AF.Exp, scale=-1.0), reads=[bsm], writes=[bsm])
                    P.op("act", lambda e, e1=e1, sp_=sp_: e.activation(sp_, e1, AF.Ln, bias=1.0), reads=[bsm], writes=[bsm])
                    P.op("pe", lambda e, sp_=sp_: e.matmul(f3[:, 256:258], triu32[:], sp_, start=True, stop=True), reads=[bsm] + CST, writes=[bf3])
                    P.op("pe", lambda e, sp_=sp_: e.matmul(f3[:, 258:260], ones32[:], sp_, start=True, stop=True), reads=[bsm] + CST, writes=[bf3])
                    P.op("act", lambda e, cs=cs: e.copy(cs, f3[:, 256:260]), reads=[bf3], writes=[bsm])
                    P.op("dve", lambda e, ti=ti, gt=gt, cs=cs: e.tensor_tensor(ti, gt[:, 0:2], cs[:, 0:2], ALU.add), reads=[bsm], writes=[bsm])
                    P.op("act", lambda e, ws=ws, ti=ti: e.activation(ws, ti, AF.Exp), reads=[bsm], writes=[bsm])
                    P.op("act", lambda e, eb=eb, cs=cs: e.activation(eb, cs[:, 0:2], AF.Exp), reads=[bsm], writes=[bsm])
                    P.op("act", lambda e, ebL=ebL, cs=cs: e.activation(ebL, cs[:, 2:4], AF.Exp, scale=-1.0), reads=[bsm], writes=[bsm])
                    P.op("dve", lambda e, wsq=wsq, ws=ws: e.tensor_scalar(wsq, ws, SC, None, ALU.mult), reads=[bsm], writes=[bsm])
                    for h in range(2):
                        kk = k * 2 + h
                        P.op("pe", lambda e, h=h, tsl=tsl: e.matmul(f4[:, h * 128:(h + 1) * 128], QKb[:, 2 + h, tsl], QKb[:, h, tsl],
                                                                     start=True, stop=True), reads=[b_qk[h], b_qk[2 + h]], writes=[bf4])
                        P.op("dve", lambda e, h=h, wsq=wsq, kk=kk: e.scalar_tensor_tensor(
                            Sm[kk][:], f4[:, h * 128:(h + 1) * 128], wsq[:, h:h + 1], triub[:], ALU.mult, ALU.mult),
                            reads=[bf4, bsm] + CST, writes=[b_Sm[kk]])
                        P.op("pe", lambda e, h=h, tsl=tsl: e.transpose(BK.h[:, h * 256 + 128:h * 256 + 256], QKb[:, 2 + h, tsl], identb[:]),
                             reads=[b_qk[2 + h]] + CST, writes=[BK.bh])
                        P.op("act", lambda e, h=h, kk=kk: e.copy(Ktm[kk][:], BK.h[:, h * 256 + 128:h * 256 + 256]), reads=[BK.bh], writes=[b_ktm[kk]])
                        P.op("dve", lambda e, h=h, va=va, ws=ws, kk=kk: e.tensor_scalar(Vw[kk][:], va[:, h, :], ws[:, h:h + 1], None, ALU.mult),
                             reads=[bva, bsm], writes=[b_vw[kk]])

                    def stageB(k=k, tsl=tsl, va=va, bva=bva, sg_=sg_, bsg=bsg, bsm=bsm, eb=eb, ebL=ebL):
                        for h in range(2):
                            kk = k * 2 + h
                            st_, bst = hst[h], b_hst[h]
                            xs_ = f5[:, h * 256:h * 256 + 129]
                            P.op("pe", lambda e, h=h, xs_=xs_, kk=kk: e.matmul(xs_, Sm[kk][:], va[:, h, :], start=True, stop=False),
                                 reads=[b_Sm[kk], bva], writes=[bf5])
                            P.op("pe", lambda e, h=h, xs_=xs_: e.matmul(xs_, QKb[:, h, tsl], Cb[:, h, :], start=False, stop=True),
                                 reads=[b_qk[h], b_Cb[h]], writes=[bf5])
                            ax, r_, mv, rstd, nmr = st_[:, 0:1], st_[:, 1:2], st_[:, 2:4], st_[:, 4:5], st_[:, 5:6]
                            stat6 = st_[:, 8:14]
                            P.op("dve", lambda e, ax=ax, h=h: e.tensor_scalar(ax, f5[:, h * 256 + 128:h * 256 + 129], -1.0, None, ALU.mult),
                                 reads=[bf5], writes=[bst])
                            P.op("dve", lambda e, ax=ax, h=h: e.tensor_tensor(ax, ax, f5[:, h * 256 + 128:h * 256 + 129], ALU.max),
                                 reads=[bf5, bst], writes=[bst])
                            P.op("dve", lambda e, ax=ax, h=h: e.tensor_tensor(ax, ax, eb[:, h:h + 1], ALU.max), reads=[bst, bsm], writes=[bst])
                            P.op("dve", lambda e, ax=ax, r_=r_: e.reciprocal(r_, ax), reads=[bst], writes=[bst])
                            P.op("dve", lambda e, h=h, r_=r_: e.tensor_scalar(hh[h][:], f5[:, h * 256:h * 256 + 128], r_, None, ALU.mult),
                                 reads=[bf5, bst], writes=[b_hh[h]])
                            P.op("dve", lambda e, h=h, stat6=stat6: e.bn_stats(stat6, hh[h][:]), reads=[b_hh[h]], writes=[bst])
                            P.op("dve", lambda e, mv=mv, stat6=stat6: e.bn_aggr(mv, stat6), reads=[bst], writes=[bst])
                            P.op("dve", lambda e, rstd=rstd, mv=mv: e.tensor_scalar(rstd, mv[:, 1:2], LN_EPS, None, ALU.add), reads=[bst], writes=[bst])
                            P.op("act", lambda e, rstd=rstd: e.sqrt(rstd, rstd), reads=[bst], writes=[bst])
                            P.op("dve", lambda e, rstd=rstd: e.reciprocal(rstd, rstd), reads=[bst], writes=[bst])
                            P.op("dve", lambda e, nmr=nmr, mv=mv, rstd=rstd: e.scalar_tensor_tensor(nmr, mv[:, 0:1], -1.0, rstd, ALU.mult, ALU.mult),
                                 reads=[bst], writes=[bst])
                            P.op("act", lambda e, h=h, nmr=nmr, rstd=rstd: e.activation(yn32[h][:], hh[h][:], AF.Identity, bias=nmr, scale=rstd),
                                 reads=[b_hh[h], bst], writes=[b_yn[h]])
                            P.op("dve", lambda e, h=h: e.tensor_tensor(yn32[h][:], yn32[h][:], hngb[:, h * 128:(h + 1) * 128], ALU.mult),
                                 reads=[b_yn[h]] + CST, writes=[b_yn[h]])
                            P.op("dve", lambda e, h=h: e.tensor_tensor(ynb[h][:], yn32[h][:], sg_[:, h * 128:(h + 1) * 128], ALU.mult),
                                 reads=[b_yn[h], bsg], writes=[b_ynb[h]])
                            P.op("pe", lambda e, h=h: e.transpose(BK.h[:, h * 256:h * 256 + 128], ynb[h][:], identb[:]),
                                 reads=[b_ynb[h]] + CST, writes=[BK.bh])
                            P.op("act", lambda e, h=h: e.copy(ybufB[:, h, tsl], BK.h[:, h * 256:h * 256 + 128]),
                                 reads=[BK.bh], writes=[b_ybB])
                            dps = f6[:, h * 256:h * 256 + 129]
                            P.op("pe", lambda e, h=h, dps=dps, kk=kk: e.matmul(dps, Ktm[kk][:], Vw[kk][:], start=True, stop=True),
                                 reads=[b_ktm[kk], b_vw[kk]], writes=[bf6])
                            P.op("dve", lambda e, h=h, dps=dps: e.tensor_tensor(C32[:, h, :], dps, C32[:, h, :], ALU.add),
                                 reads=[bf6, b_C32[h]], writes=[b_C32[h]])
                            P.op("dve", lambda e, h=h: e.tensor_scalar(C32[:, h, :], C32[:, h, :], ebL[:, h:h + 1], None, ALU.mult),
                                 reads=[b_C32[h], bsm], writes=[b_C32[h]])
                            P.op("act", lambda e, h=h: e.mul(Cb[:, h, :], C32[:, h, :], SC), reads=[b_C32[h]], writes=[b_Cb[h]])
                    pipeB.step(stageB)
                pipeB.flush()
                for h in range(2):
                    ystore(P, 2 + h, blk, ybufB[:, h, :], [b_ybB])
            full_barrier(P)


TOK = 4096


def declare_post(nc, pfx=""):
    din = lambda n, sh: nc.dram_tensor(pfx + n, sh, F32, kind="ExternalInput").ap()
    return dict(wo=din("wo", [D, D]), ln1g=din("ln1g", [D]), ln1b=din("ln1b", [D]),
                w=dict(wg=din("wg", [16, D, EH]), wu=din("wu", [16, D, EH]), wd=din("wd", [16, EH, D]), wr=din("wr", [D, 20]),
                       br=din("br", [1, 20]), ln_g=din("ln2g", [D]), ln_b=din("ln2b", [D])),
                hmid=nc.dram_tensor(pfx + "hmid_scr", [TOK, D], F32).ap())


def emit_post(P, io):
    nc = P.nc
    wo, ln1g, ln1b, w, hmid = io["wo"], io["ln1g"], io["ln1b"], dict(io["w"]), io["hmid"]
    w["ident"] = io["ident"]
    load_yt, xres, out = io["load_yt"], io["xres"], io["out"]
    if True:
        B = P.buf
        banks = [P.ps(f"bank{i}", [128, 512]) for i in range(8)]
        bbank = [B() for _ in range(8)]
        b_hmid, b_out, b_yT = B(), B(), B()
        b_xres = io.get("b_xres") or B()
        b_hmw = [B(), B()]
        with ExitStack() as esF:
            sbF = lambda n, sh, d: esF.enter_context(nc.sbuf_tensor(P.pfx + n, sh, d))
            stg = Stager(P)
            wob = sbF("f_wo", [128, 8, D], BF16)
            b_wo = B()
            stg.load(wob[:], b_wo, wo[:, :])
            g1 = sbF("f_g1", [128, D], F32)
            b1 = sbF("f_b1", [128, D], F32)
            b_g1, b_b1 = B(), B()
            P.dma("sp", g1[:], bcast_rows(ln1g.tensor, ln1g.offset, D), writes=[b_g1])
            P.dma("sp", b1[:], bcast_rows(ln1b.tensor, ln1b.offset, D), writes=[b_b1])
            yts = [sbF(f"f_yt{i}", [128, 8, 512], BF16) for i in range(2)]
            b_yts = [[B() for _ in range(8)] for _ in range(2)]
            xr = [sbF(f"f_xr{i}", [128, D], F32) for i in range(2)]
            b_xr = [B(), B()]
            z = [sbF(f"f_z{i}", [128, D], F32) for i in range(2)]
            b_z = [B(), B()]
            hm = [sbF(f"f_hm{i}", [128, D], F32) for i in range(2)]
            b_hm = [B(), B()]
            stats = sbF("f_stats", [128, 2, 6], F32)
            sm = sbF("f_sm", [128, 64], F32)
            b_stats, b_sm = B(), B()
            ti = 0
            for t4 in range(TOK // 512):
                yt, byt = yts[t4 % 2], b_yts[t4 % 2]
                load_yt(P, yt, byt, t4)
                for tq in range(4):
                    k = ti % 2
                    ti += 1
                    t0 = t4 * 512 + tq * 128
                    P.dma("sp", xr[k][:], xres(t0), reads=[b_xres], writes=[b_xr[k]])
                    for dh in range(2):
                        pb, bpb = banks[(ti * 2 + dh) % 4], bbank[(ti * 2 + dh) % 4]
                        for ec in range(8):
                            P.op("pe", lambda e, pb=pb, ec=ec, yt=yt, tq=tq, dh=dh: e.matmul(
                                pb[:], yt[:, ec, tq * 128:(tq + 1) * 128], wob[:, ec, dh * 512:(dh + 1) * 512],
                                start=(ec == 0), stop=(ec == 7)), reads=[byt[ec], b_wo], writes=[bpb])
                        P.op("dve", lambda e, pb=pb, k=k, dh=dh: e.scalar_tensor_tensor(
                            z[k][:, dh * 512:(dh + 1) * 512], xr[k][:, dh * 512:(dh + 1) * 512], ALPHA, pb[:], ALU.mult, ALU.add),
                            reads=[bpb, b_xr[k]], writes=[b_z[k]])
                    emit_ln(P, z[k][:], b_z[k], hm[k], b_hm[k], g1, b_g1, b1, b_b1, stats, b_stats, sm, b_sm)
                    P.dma("sp", hmid[t0:t0 + 128, :], hm[k][:], reads=[b_hm[k]], writes=[b_hmw[k]])
            full_barrier(P)
        P.scope(P.pfx + "moe")
        emit_tail(P, hmid, b_hmid, out, b_out, TOK, w, 0, banks=banks, is_final=io.get("is_final", True))
        full_barrier(P)


def pack_m0(inp, b, c):
    w = inp["ab_w_in"][0]
    cols = []
    for hp in range(2):
        h0 = 4 * c + 2 * hp
        for sec in (0, 512, 1024):
            cols.append(w[:, sec + h0 * 64: sec + (h0 + 2) * 64])
    wA = np.concatenate(cols, axis=1)
    hb = [2 * c, 2 * c + 1]
    BQ, BKK, BV, BO, BI, BF = 1536, 2048, 2560, 3072, 3584, 3588
    wB = np.concatenate([w[:, BQ + h * 128: BQ + (h + 1) * 128] for h in hb] + [w[:, BKK + h * 128: BKK + (h + 1) * 128] for h in hb]
                        + [w[:, BV + h * 128: BV + (h + 1) * 128] for h in hb] + [w[:, BO + h * 128: BO + (h + 1) * 128] for h in hb]
                        + [w[:, BI + h: BI + h + 1] for h in hb] + [w[:, BF + h: BF + h + 1] for h in hb], axis=1)
    gb = inp["ab_gate_b"][0]
    gbias = np.array([[gb[hb[0]], gb[hb[1]], gb[4 + hb[0]], gb[4 + hb[1]]]], np.float32)
    cw = inp["ab_conv"][0]
    convw = np.zeros((128, 4, 4), np.float32)
    for g, off in enumerate([hb[0] * 128, hb[1] * 128, 512 + hb[0] * 128, 512 + hb[1] * 128]):
        convw[:, g, :] = cw[:, off:off + 128].T
    hng = np.concatenate([inp["ab_head_norm"][0][h * 128:(h + 1) * 128] for h in hb])[None]
    relb = np.concatenate([inp["rel_bias"][:, 4 * c:4 * c + 4], np.full((1, 4), -30000.0, np.float32)], axis=0)
    return dict(x=np.ascontiguousarray(inp["x"][b]), wA=np.ascontiguousarray(wA), wB=np.ascontiguousarray(wB), gbias=gbias,
                convw=np.ascontiguousarray(convw.reshape(128, 16)), hng=np.ascontiguousarray(hng),
                relb=np.ascontiguousarray(relb), oh=np.ascontiguousarray(dil_onehot().reshape(33, -1)),
                ident=np.eye(128, dtype=np.float32), triu=np.triu(np.ones((128, 128), np.float32)))


L1 = 5632
OFF1 = 2063


def nsa_consts():
    dist = np.arange(L1) - OFF1
    oh = np.zeros((33, L1), np.float32)
    bk = t5_bucket_np(dist)
    for jp in range(L1):
        if dist[jp] >= 0:
            oh[bk[jp], jp] = 1.0
        else:
            oh[32, jp] = 1.0
    n = np.arange(512)
    m = np.arange(128)
    cs, ce = n * 16, n * 16 + 31
    ov = ((cs[:, None] < m[None, :] * 64 + 64) & (ce[:, None] >= m[None, :] * 64)).astype(np.float32)
    ov[511] = 0.0
    ovl = ov.reshape(4, 128, 128).transpose(1, 0, 2)
    mrow = np.broadcast_to(m[None, :].astype(np.float32), (128, 128))
    k = np.arange(128)
    cst = np.stack([mrow, (mrow >= 1).astype(np.float32), (mrow == 0).astype(np.float32),
                    (k[:, None] > k[None, :]).astype(np.float32)], axis=1)
    qhalf = (np.arange(128) >= 64).astype(np.float32)[:, None]
    return dict(oh1=oh, ovl=np.ascontiguousarray(ovl), cst1=np.ascontiguousarray(cst.astype(np.float32)), qhalf=qhalf)


def declare_mixer1(nc, pfx=""):
    din = lambda n, s: nc.dram_tensor(pfx + n, s, F32, kind="ExternalInput").ap()
    return dict(wQ=din("wQ", [D, 512]), wKV=din("wKV", [D, 768]), wG=din("wG", [D, 12]), gb=din("gb", [1, 12]),
                posT=din("posT", [128, 64]), w1=din("w1", [2, 4096, 256]), w2=din("w2", [2, 256, 128]), relb=din("relb", [33, 4]),
                oh1=din("oh1", [33, L1]), ovl=din("ovl", [128, 512]), cst1=din("cst1", [128, 512]), qhalf=din("qhalf", [128, 1]),
                fsc=nc.dram_tensor(pfx + "frep_scr", [128, 4 * L1], BF16).ap())


def emit_mixer1(P, io):
    nc = P.nc
    wQ, wKV, wG, gbd, posT, w1d, w2d, relb = io["wQ"], io["wKV"], io["wG"], io["gb"], io["posT"], io["w1"], io["w2"], io["relb"]
    oh_d, ovl_d, cst_d, qh_d, ident_d, fsc = io["oh1"], io["ovl"], io["cst1"], io["qhalf"], io["ident"], io["fsc"]
    x, ostore = io["x"], io["ostore"]
    NT = S_TOT // 128
    SCQ = 128.0 ** -0.5
    if True:
        B = P.buf
        BK = Banks(P)
        f0, f1, f2, f3, f4, f5, f6 = BK.f
        bf0, bf1, bf2, bf3, bf4, bf5, bf6 = BK.bf
        b_x, b_o, b_fsc = io.get("b_x") or B(), B(), B()
        ident = P.sb("ident_sb", [128, 128], F32)
        identb = P.sb("identb", [128, 128], BF16)
        ones_bf = P.sb("ones_bf", [128, 128], BF16)
        b_ident, b_cb = B(), B()
        P.dma("sp", ident[:], ident_d[:, :], writes=[b_ident])
        P.op("dve", lambda e: e.tensor_copy(identb[:], ident[:]), reads=[b_ident], writes=[b_cb])
        P.op("dve", lambda e: e.memset(ones_bf[:], 1.0), writes=[b_cb])
        xin = [P.sb(f"xin{i}", [128, D], F32) for i in range(2)]
        b_xin = [B(), B()]
        stg = Stager(P, n=2)
        ctr = [0]
        wQb = P.sb("wQb", [128, 8, 512], BF16)
        wGb = P.sb("wGb", [128, 8, 12], BF16)
        b_wQ, b_wG = B(), B()
        stg.load(wQb[:], b_wQ, wQ[:, :])
        stg.load(wGb[:], b_wG, wG[:, :])
        KsT = P.sb("KsT", [128, S_TOT], BF16)
        KwT = P.sb("KwT", [128, S_TOT], BF16)
        Vs = P.sb("Vs", [128, NT, 129], BF16)
        Vw = P.sb("Vw", [128, NT, 129], BF16)
        b_KsT, b_KwT, b_Vs, b_Vw = B(), B(), B(), B()
        kcT = P.sb("kcT", [128, 512], BF16)
        rhsC = P.sb("rhsC", [128, 4, 256], BF16)
        b_kcT, b_rhsC = B(), B()
        P.op("dve", lambda e: e.memset(Vs[:, :, 128:129], 1.0), writes=[b_Vs])
        P.op("dve", lambda e: e.memset(Vw[:, :, 128:129], 1.0), writes=[b_Vw])
        with ExitStack() as es12:
            sb12 = lambda n, s, d: es12.enter_context(nc.sbuf_tensor(P.pfx + n, s, d))
            KcT = sb12("KcT", [128, 2, S_TOT], BF16)
            b_KcT = [B(), B()]
            with ExitStack() as es1:
                sb1 = lambda n, s, d: es1.enter_context(nc.sbuf_tensor(P.pfx + n, s, d))
                hT = sb1("hT", [128, 8, BLK], BF16)
                b_hT = [B() for _ in range(16)]
                wKVb = sb1("wKVb", [128, 8, 768], BF16)
                b_wKV = B()
                stg.load(wKVb[:], b_wKV, wKV[:, :])
                for blk in range(NBLK):
                    T0 = blk * BLK
                    load_xT(P, BK, x, b_x, xin, b_xin, hT, b_hT, ident, b_ident, T0, 16, ctr)
                    fm = [(0, KcT[:, 0, :], b_KcT[0]), (1, KcT[:, 1, :], b_KcT[1]), (2, KsT[:], b_KsT), (4, KwT[:], b_KwT)]
                    for fi, (wi, dst, bd) in enumerate(fm):
                        for qtr in range(4):
                            pb, bpb = (f2, bf2) if (fi + qtr) % 2 == 0 else (f3, bf3)
                            rds = [b_hT[qtr * 4 + i] for i in range(4)] + [b_wKV]
                            for dc in range(8):
                                P.op("pe", lambda e, pb=pb, dc=dc, wi=wi, qtr=qtr: e.matmul(
                                    pb[:], wKVb[:, dc, wi * 128:(wi + 1) * 128], hT[:, dc, qtr * 512:(qtr + 1) * 512],
                                    start=(dc == 0), stop=(dc == 7)), reads=rds, writes=[bpb])
                            if (fi + qtr) % 2 == 0:
                                P.op("act", lambda e, pb=pb, dst=dst, T0=T0, qtr=qtr: e.copy(
                                    dst[:, T0 + qtr * 512:T0 + (qtr + 1) * 512], pb[:]), reads=[bpb], writes=[bd])
                            else:
                                P.op("dve", lambda e, pb=pb, dst=dst, T0=T0, qtr=qtr: e.tensor_copy(
                                    dst[:, T0 + qtr * 512:T0 + (qtr + 1) * 512], pb[:]), reads=[bpb], writes=[bd])
                    for tl in range(16):
                        for (wi, dst, bd, pb, bpb, eng) in ((3, Vs, b_Vs, f4, bf4, "act"), (5, Vw, b_Vw, f5, bf5, "dve")):
                            for dc in range(8):
                                P.op("pe", lambda e, pb=pb, dc=dc, wi=wi, tl=tl: e.matmul(
                                    pb[:, 0:128], hT[:, dc, tl * 128:(tl + 1) * 128], wKVb[:, dc, wi * 128:(wi + 1) * 128],
                                    start=(dc == 0), stop=(dc == 7)), reads=[b_hT[tl], b_wKV], writes=[bpb])
                            if eng == "act":
                                P.op("act", lambda e, pb=pb, dst=dst, tl=tl, blk=blk: e.copy(dst[:, blk * 16 + tl, 0:128], pb[:, 0:128]),
                                     reads=[bpb], writes=[bd])
                            else:
                                P.op("dve", lambda e, pb=pb, dst=dst, tl=tl, blk=blk: e.tensor_copy(dst[:, blk * 16 + tl, 0:128], pb[:, 0:128]),
                                     reads=[bpb], writes=[bd])
                full_barrier(P)
            P.scope(P.pfx + "compress")
            with ExitStack() as es2:
                sb2 = lambda n, s, d: es2.enter_context(nc.sbuf_tensor(P.pfx + n, s, d))
                w1b = sb2("w1b", [128, 32, 256], BF16)
                w2b = sb2("w2b", [128, 2, 128], BF16)
                posb = sb2("posb", [128, 64], BF16)
                pos32 = sb2("pos32", [128, 64], F32)
                ovl32 = sb2("ovl32", [128, 512], F32)
                pbias = sb2("pbias", [128, 2], F32)
                xg = sb2("xg", [128, 512], F32)
                ug = sb2("ug", [128, 512], F32)
                hid = sb2("hid_sb", [128, 2, 512], BF16)
                b_w1, b_w2, b_pos, b_ovl, b_pb, b_xg, b_ug, b_hid = B(), B(), B(), B(), B(), B(), B(), [B(), B()]
                P.dma("sp", pos32[:], posT[:, :], writes=[b_pos])
                P.op("dve", lambda e: e.tensor_copy(posb[:], pos32[:]), reads=[b_pos], writes=[b_pos])
                P.dma("sp", ovl32[:], ovl_d[:, :], writes=[b_ovl])
                P.op("dve", lambda e: e.tensor_copy(rhsC[:, :, 128:256], ovl32[:].rearrange("p (c m) -> p c m", c=4)),
                     reads=[b_ovl], writes=[b_rhsC])
                P.op("dve", lambda e: e.memset(hid[:, :, 511:512], 0.0), writes=b_hid)
                for kv in range(2):
                    stg.load(w1b[:], b_w1, w1d[kv])
                    stg.load(w2b[:], b_w2, w2d[kv])
                    for hc in range(2):
                        for p_ in range(32):
                            P.op("pe", lambda e, hc=hc, p_=p_, kv=kv: e.matmul(
                                f6[:, 0:1], w1b[:, p_, hc * 128:(hc + 1) * 128], posb[:, kv * 32 + p_:kv * 32 + p_ + 1],
                                start=(p_ == 0), stop=(p_ == 31)), reads=[b_w1, b_pos], writes=[bf6])
                        P.op("act", lambda e, hc=hc: e.copy(pbias[:, hc:hc + 1], f6[:, 0:1]), reads=[bf6], writes=[b_pb])
                        for p_ in range(32):
                            P.op("pe", lambda e, hc=hc, p_=p_, kv=kv: e.matmul(
                                f2[:, 0:511], w1b[:, p_, hc * 128:(hc + 1) * 128], KcT[:, kv, p_:p_ + 16 * 510 + 1:16],
                                start=(p_ == 0), stop=(p_ == 31)), reads=[b_w1, b_KcT[kv]], writes=[bf2])
                        P.op("act", lambda e, hc=hc: e.activation(xg[:, 0:511], f2[:, 0:511], AF.Identity, bias=pbias[:, hc:hc + 1], scale=1.0),
                             reads=[bf2, b_pb], writes=[b_xg])
                        P.op("dve", lambda e: e.tensor_tensor(ug[:, 0:511], xg[:, 0:511], xg[:, 0:511], ALU.mult), reads=[b_xg], writes=[b_ug])
                        P.op("dve", lambda e: e.tensor_scalar(ug[:, 0:511], ug[:, 0:511], 0.044715, 1.0, ALU.mult, ALU.add), reads=[b_ug], writes=[b_ug])
                        P.op("dve", lambda e: e.tensor_tensor(ug[:, 0:511], ug[:, 0:511], xg[:, 0:511], ALU.mult), reads=[b_ug, b_xg], writes=[b_ug])
                        P.op("act", lambda e: e.activation(ug[:, 0:511], ug[:, 0:511], AF.Sigmoid, scale=1.5957691216057308), reads=[b_ug], writes=[b_ug])
                        P.op("dve", lambda e, hc=hc: e.tensor_tensor(hid[:, hc, 0:511], xg[:, 0:511], ug[:, 0:511], ALU.mult),
                             reads=[b_ug, b_xg], writes=[b_hid[hc]])
                    if kv == 0:
                        for hc in range(2):
                            P.op("pe", lambda e, hc=hc: e.matmul(f3[:, 0:512], w2b[:, hc, :], hid[:, hc, :], start=(hc == 0), stop=(hc == 1)),
                                 reads=[b_w2] + b_hid, writes=[bf3])
                        P.op("act", lambda e: e.copy(kcT[:], f3[:, 0:512]), reads=[bf3], writes=[b_kcT])
                    else:
                        for c in range(4):
                            for hc in range(2):
                                P.op("pe", lambda e, hc=hc, c=c: e.matmul(f3[:, c * 128:(c + 1) * 128], hid[:, hc, c * 128:(c + 1) * 128], w2b[:, hc, :],
                                                                          start=(hc == 0), stop=(hc == 1)), reads=[b_w2] + b_hid, writes=[bf3])
                        P.op("act", lambda e: e.copy(rhsC[:, :, 0:128], f3[:].rearrange("p (c m) -> p c m", c=4)), reads=[bf3], writes=[b_rhsC])
                full_barrier(P)
        P.scope(P.pfx + "tables")
        with ExitStack() as es3:
            sb3 = lambda n, s, d: es3.enter_context(nc.sbuf_tensor(P.pfx + n, s, d))
            tab = sb3("tab", [33, 4], F32)
            tabB = sb3("tabB", [33, 128], F32)
            ohp = [sb3(f"ohp{i}", [33, 512], F32) for i in range(2)]
            frp = [sb3(f"frp{i}", [128, 512], BF16) for i in range(2)]
            b_tab, b_tabB, b_ohp, b_frp = B(), B(), [B(), B()], [B(), B()]
            b_fscw = [B(), B()]
            P.dma("sp", tab[:], relb[:, :], writes=[b_tab])
            ki = 0
            for h in range(4):
                P.op("dve", lambda e, h=h: e.tensor_copy(tabB[:], tab[:, h:h + 1].to_broadcast([33, 128])), reads=[b_tab], writes=[b_tabB])
                for pc_ in range(L1 // 512):
                    k = ki % 2
                    ki += 1
                    P.dma("sp", ohp[k][:], oh_d[:, pc_ * 512:(pc_ + 1) * 512], writes=[b_ohp[k]])
                    pb, bpb = (f2, bf2) if k == 0 else (f3, bf3)
                    P.op("pe", lambda e, pb=pb, k=k: e.matmul(pb[:], tabB[:], ohp[k][:], start=True, stop=True), reads=[b_tabB, b_ohp[k]], writes=[bpb])
                    P.op("act", lambda e, pb=pb, k=k: e.activation(frp[k][:], pb[:], AF.Exp), reads=[bpb], writes=[b_frp[k]])
                    P.dma("sp", fsc[:, h * L1 + pc_ * 512:h * L1 + (pc_ + 1) * 512], frp[k][:], reads=[b_frp[k]], writes=[b_fscw[k]])
            full_barrier(P)
            RS = 4 * L1
            Esel = sb3("Esel", [128, 14, 4, 128], BF16)
            Ew4 = sb3("Ew4", [128, 4, 128], BF16)
            b_Esel = B()
            for dl in range(14):
                P.dma("sp", Esel[:, dl], bass.AP(fsc.tensor, 128 * dl + OFF1, [[RS - 1, 128], [L1, 4], [1, 128]]),
                      reads=[b_fsc], writes=[b_Esel])
            cst = sb3("cst", [128, 4, 128], F32)
            qhalf = sb3("qhalf_sb", [128, 1], F32)
            gbb = sb3("gbb", [128, 12], F32)
            m4b = sb3("m4b", [128, 128], BF16)
            b_cst = B()
            P.dma("sp", cst[:], cst_d[:, :].rearrange("p (c m) -> p c m", c=4), writes=[b_cst])
            b_q2, b_g2 = B(), B()
            P.dma("sp", qhalf[:], qh_d[:, :], writes=[b_q2])
            P.dma("sp", gbb[:], bcast_rows(gbd.tensor, gbd.offset, 12), writes=[b_g2])
            CST = [b_cst, b_q2, b_g2, b_cb, b_Esel]
            P.op("dve", lambda e: e.tensor_copy(m4b[:], cst[:, 3, :]), reads=[b_cst], writes=[b_Esel])
            P.op("dve", lambda e: e.tensor_tensor(Ew4[:], Esel[:, 4], m4b[:].unsqueeze(1).to_broadcast([128, 4, 128]), ALU.mult),
                 reads=[b_Esel], writes=[b_Esel])
            mrow, mge1, meq0 = cst[:, 0, :], cst[:, 1, :], cst[:, 2, :]
            Expand = sb3("Expand", [128, NT, 128], BF16)
            for Tk in range(NT):
                P.op("dve", lambda e, Tk=Tk: e.tensor_copy(Expand[:, Tk, :].rearrange("m (h k) -> m h k", h=2),
                                                            identb[:, 2 * Tk:2 * Tk + 2].unsqueeze(2).to_broadcast([128, 2, 64])),
                     reads=[b_cb], writes=[b_Esel])
            hTt = sb3("hTt", [128, 8, 128], BF16)
            b_hTt = [B()]
            QT = [sb3(f"QT{i}", [128, 4, 128], BF16) for i in range(2)]
            b_QT = [B(), B()]
            gsg = sb3("gsg", [128, 12], F32)
            b_gsg = B()
            ecb = [sb3(f"ecb{i}", [128, 4, 128], BF16) for i in range(2)]
            b_ecb = [B(), B()]
            psb = [sb3(f"psb{i}", [128, 4, 128], BF16) for i in range(5)]
            b_psb = [B() for _ in range(5)]
            ocb = sb3("ocb", [128, 4, 128], F32)
            b_ocb = B()
            sm = sb3("sm3", [128, 64], F32)
            b_sm = B()
            imp = sb3("imp", [128, 128], F32)
            M1 = sb3("M1", [128, 128], F32)
            Fm = sb3("Fm", [128, 128], F32)
            sa = sb3("sa", [128, 128], F32)
            rp = sb3("rp", [128, 128], F32)
            sel = sb3("sel", [128, 128], F32)
            nsb = sb3("nsb", [128, 128], BF16)
            nsT = sb3("nsT", [128, 128], BF16)
            mx = sb3("mx", [128, 16], F32)
            b_imp, b_M1, b_Fm, b_sa, b_rp, b_sel, b_nsb, b_nsT, b_mx = (B() for _ in range(9))
            b_ow = [B(), B()]
            ob = sb3("ob", [128, 4, 128], BF16)
            oTt = [sb3(f"oTt{i}", [128, 4, 128], BF16) for i in range(2)]
            b_ob, b_oTt = B(), [B(), B()]
            si = 0
            P.scope(P.pfx + "attn")
            pipe1 = Pipe(depth=2)
            for T in range(NT):
                k2 = T % 2
                load_xT(P, BK, x, b_x, xin, b_xin, hTt, b_hTt, ident, b_ident, T * 128, 1, ctr)
                for j in range(4):
                    for dc in range(8):
                        P.op("pe", lambda e, j=j, dc=dc: e.matmul(f6[:, j * 128:(j + 1) * 128], wQb[:, dc, j * 128:(j + 1) * 128], hTt[:, dc, :],
                                                                  start=(dc == 0), stop=(dc == 7)), reads=[b_hTt[0], b_wQ], writes=[bf6])
                qt, bqt = QT[k2], b_QT[k2]
                P.op("act", lambda e, qt=qt: e.mul(qt[:].rearrange("p j q -> p (j q)"), f6[:], SCQ), reads=[bf6], writes=[bqt])
                for dc in range(8):
                    P.op("pe", lambda e, dc=dc: e.matmul(f4[:, 16:28], hTt[:, dc, :], wGb[:, dc, :], start=(dc == 0), stop=(dc == 7)),
                         reads=[b_hTt[0], b_wG], writes=[bf4])
                P.op("dve", lambda e: e.tensor_tensor(gsg[:], f4[:, 16:28], gbb[:], ALU.add), reads=[bf4] + CST, writes=[b_gsg])
                P.op("act", lambda e: e.activation(gsg[:], gsg[:], AF.Sigmoid), reads=[b_gsg], writes=[b_gsg])
                qrhs = qt[:].rearrange("p j q -> p (j q)")
                ncmp = (8 * T + 6) // 128 + 1
                for c in range(ncmp):
                    dl = min(T - 16 * c, 28)
                    ke = si % 2
                    kp = si % 5
                    si += 1
                    P.dma("sp", ecb[ke][:], bass.AP(fsc.tensor, 128 * dl - 31 + OFF1, [[RS - 16, 128], [L1, 4], [1, 128]]),
                          reads=[b_fsc], writes=[b_ecb[ke]])
                    P.op("pe", lambda e, c=c, qrhs=qrhs: e.matmul(f6[:], kcT[:, c * 128:(c + 1) * 128], qrhs, start=True, stop=True),
                         reads=[b_kcT, bqt], writes=[bf6])
                    pp, bpp = psb[kp], b_psb[kp]
                    P.op("act", lambda e, pp=pp: e.activation(pp[:].rearrange("p j q -> p (j q)"), f6[:], AF.Exp), reads=[bf6], writes=[bpp])
                    P.op("dve", lambda e, pp=pp, ke=ke: e.tensor_tensor(pp[:], pp[:], ecb[ke][:], ALU.mult), reads=[bpp, b_ecb[ke]], writes=[bpp])
                    for j in range(4):
                        fb, bfb = (f2, bf2) if j < 2 else (f3, bf3)
                        P.op("pe", lambda e, fb=fb, j=j, c=c, pp=pp, ncmp=ncmp: e.matmul(
                            fb[:, (j % 2) * 256:(j % 2) * 256 + 256], pp[:, j, :], rhsC[:, c, :],
                            start=(c == 0 and j % 2 == 0), stop=(c == ncmp - 1 and j % 2 == 1)),
                            reads=[bpp, b_rhsC], writes=[bfb])
                        P.op("pe", lambda e, j=j, c=c, pp=pp, ncmp=ncmp: e.matmul(
                            f4[:, j:j + 1], pp[:, j, :], ones_bf[:, 0:1], start=(c == 0 and j == 0), stop=(c == ncmp - 1 and j == 3)),
                            reads=[bpp, b_cb], writes=[bf4])
                rc, cc = sm[:, 0:4], sm[:, 4:8]
                P.op("dve", lambda e, rc=rc: e.tensor_scalar(rc, f4[:, 0:4], 1e-30, None, ALU.max), reads=[bf4], writes=[b_sm])
                P.op("dve", lambda e, rc=rc: e.reciprocal(rc, rc), reads=[b_sm], writes=[b_sm])
                P.op("dve", lambda e, rc=rc, cc=cc: e.tensor_tensor(cc, rc, gsg[:, 0:4], ALU.mult), reads=[b_sm, b_gsg], writes=[b_sm])
                for j in range(4):
                    fb, bfb = (f2, bf2) if j < 2 else (f3, bf3)
                    o0 = (j % 2) * 256
                    P.op("dve", lambda e, fb=fb, j=j, o0=o0, cc=cc: e.tensor_scalar(ocb[:, j, :], fb[:, o0:o0 + 128], cc[:, j:j + 1], None, ALU.mult),
                         reads=[bfb, b_sm], writes=[b_ocb])
                    if j == 0:
                        P.op("dve", lambda e, fb=fb, o0=o0, rc=rc: e.tensor_scalar(imp[:], fb[:, o0 + 128:o0 + 256], rc[:, 0:1], None, ALU.mult),
                             reads=[bfb, b_sm], writes=[b_imp])
                    else:
                        P.op("dve", lambda e, fb=fb, o0=o0, rc=rc, j=j: e.scalar_tensor_tensor(
                            imp[:], fb[:, o0 + 128:o0 + 256], rc[:, j:j + 1], imp[:], ALU.mult, ALU.add), reads=[bfb, b_sm, b_imp], writes=[b_imp])
                qb, qb1, qb2, tau = sm[:, 8:9], sm[:, 9:10], sm[:, 10:11], sm[:, 11:12]
                P.op("dve", lambda e, qb=qb, T=T: e.tensor_scalar(qb, qhalf[:], float(2 * T), None, ALU.add), reads=CST, writes=[b_sm])
                P.op("dve", lambda e, qb=qb, qb1=qb1: e.tensor_scalar(qb1, qb, -1.0, None, ALU.add), reads=[b_sm], writes=[b_sm])
                P.op("dve", lambda e, qb=qb, qb2=qb2: e.tensor_scalar(qb2, qb, -2.0, None, ALU.add), reads=[b_sm], writes=[b_sm])
                P.op("dve", lambda e, qb2=qb2: e.tensor_scalar(M1[:], mrow, qb2, None, ALU.is_le), reads=[b_sm] + CST, writes=[b_M1])
                P.op("dve", lambda e: e.tensor_tensor(M1[:], M1[:], mge1, ALU.mult), reads=[b_M1] + CST, writes=[b_M1])
                P.op("dve", lambda e, qb=qb: e.tensor_scalar(Fm[:], mrow, qb, None, ALU.is_equal), reads=[b_sm] + CST, writes=[b_Fm])
                P.op("dve", lambda e, qb1=qb1: e.tensor_scalar(sel[:], mrow, qb1, None, ALU.is_equal), reads=[b_sm] + CST, writes=[b_sel])
                P.op("dve", lambda e: e.tensor_tensor(Fm[:], Fm[:], sel[:], ALU.add), reads=[b_Fm, b_sel], writes=[b_Fm])
                P.op("dve", lambda e: e.tensor_tensor(Fm[:], Fm[:], meq0, ALU.max), reads=[b_Fm] + CST, writes=[b_Fm])
                P.op("dve", lambda e: e.tensor_tensor(sa[:], imp[:], M1[:], ALU.mult), reads=[b_imp, b_M1], writes=[b_sa])
                P.op("dve", lambda e: e.max(mx[:, 0:8], sa[:]), reads=[b_sa], writes=[b_mx])
                P.op("dve", lambda e: e.match_replace(rp[:], mx[:, 0:8], sa[:], -1.0), reads=[b_sa, b_mx], writes=[b_rp])
                P.op("dve", lambda e: e.max(mx[:, 8:16], rp[:]), reads=[b_rp], writes=[b_mx])
                P.op("dve", lambda e: e.tensor_scalar(sel[:], sa[:], mx[:, 12:13], None, ALU.is_ge), reads=[b_sa, b_mx], writes=[b_sel])
                P.op("dve", lambda e: e.tensor_tensor(sel[:], sel[:], M1[:], ALU.mult), reads=[b_sel, b_M1], writes=[b_sel])
                P.op("dve", lambda e: e.tensor_tensor(sel[:], sel[:], Fm[:], ALU.max), reads=[b_sel, b_Fm], writes=[b_sel])
                P.op("dve", lambda e: e.tensor_scalar(nsb[:], sel[:], -1.0, 30000.0, ALU.add, ALU.mult), reads=[b_sel], writes=[b_nsb])
                P.op("pe", lambda e: e.transpose(BK.h[:, 512:640], nsb[:], identb[:]), reads=[b_nsb, b_cb], writes=[BK.bh])
                P.op("act", lambda e: e.copy(nsT[:], BK.h[:, 512:640]), reads=[BK.bh], writes=[b_nsT])
                nsrhs = nsT[:].unsqueeze(1).to_broadcast([128, 4, 128])
                for br, (KT_, bKT, V_, bV, fa, bfa, fb_, bfb_, tks) in enumerate((
                        (KsT, b_KsT, Vs, b_Vs, f2, bf2, f3, bf3, list(range(0, T + 1))),
                        (KwT, b_KwT, Vw, b_Vw, f4, bf4, f5, bf5, list(range(max(0, T - 4), T + 1))))):
                    for ii, Tk in enumerate(tks):
                        dl = T - Tk
                        kp = si % 5
                        ks_ = si % 3
                        si += 1
                        st, bst = ((f0, bf0), (f1, bf1), (f6, bf6))[ks_]
                        P.op("pe", lambda e, st=st, KT_=KT_, Tk=Tk, qrhs=qrhs, br=br: e.matmul(
                            st[:], KT_[:, Tk * 128:(Tk + 1) * 128], qrhs, start=True, stop=(br == 1)),
                            reads=[bKT, bqt], writes=[bst])
                        if br == 0:
                            P.op("pe", lambda e, st=st, Tk=Tk, nsrhs=nsrhs: e.matmul(
                                st[:].rearrange("p (j q) -> p j q", j=4), Expand[:, Tk, :], nsrhs, start=False, stop=True),
                                reads=[b_nsT, b_Esel], writes=[bst])
                        pp, bpp = psb[kp], b_psb[kp]
                        P.op("act", lambda e, pp=pp, st=st: e.activation(pp[:].rearrange("p j q -> p (j q)"), st[:], AF.Exp), reads=[bst], writes=[bpp])
                        if br == 1 and dl == 4:
                            et = Ew4[:]
                        else:
                            et = Esel[:, min(dl, 13)]
                        P.op("dve", lambda e, pp=pp, et=et: e.tensor_tensor(pp[:], pp[:], et, ALU.mult), reads=[bpp, b_Esel], writes=[bpp])
                        def stageB(fa=fa, bfa=bfa, fb_=fb_, bfb_=bfb_, pp=pp, bpp=bpp, V_=V_, bV=bV, Tk=Tk, ii=ii, n=len(tks)):
                            for j in range(4):
                                fo, bfo = (fa, bfa) if j < 2 else (fb_, bfb_)
                                P.op("pe", lambda e, fo=fo, j=j: e.matmul(
                                    fo[:, (j % 2) * 129:(j % 2) * 129 + 129], pp[:, j, :], V_[:, Tk, :],
                                    start=(ii == 0 and j % 2 == 0), stop=(ii == n - 1 and j % 2 == 1)),
                                    reads=[bpp, bV], writes=[bfo])
                        pipe1.step(stageB)
                pipe1.flush()
                for br, (fa, bfa, fb_, bfb_) in enumerate(((f2, bf2, f3, bf3), (f4, bf4, f5, bf5))):
                    for j in range(4):
                        fo, bfo = (fa, bfa) if j < 2 else (fb_, bfb_)
                        P.op("dve", lambda e, fo=fo, j=j, br=br: e.tensor_scalar(
                            sm[:, 16 + br * 4 + j:17 + br * 4 + j], fo[:, (j % 2) * 129 + 128:(j % 2) * 129 + 129], 1e-30, None, ALU.max),
                            reads=[bfo], writes=[b_sm])
                cf = sm[:, 16:24]
                P.op("dve", lambda e, cf=cf: e.reciprocal(cf, cf), reads=[b_sm], writes=[b_sm])
                P.op("dve", lambda e, cf=cf: e.tensor_tensor(cf, cf, gsg[:, 4:12], ALU.mult), reads=[b_sm, b_gsg], writes=[b_sm])
                for j in range(4):
                    fs_, bfs = (f2, bf2) if j < 2 else (f3, bf3)
                    fw_, bfw = (f4, bf4) if j < 2 else (f5, bf5)
                    o0 = (j % 2) * 129
                    P.op("dve", lambda e, fs_=fs_, j=j, o0=o0, cf=cf: e.scalar_tensor_tensor(
                        ocb[:, j, :], fs_[:, o0:o0 + 128], cf[:, j:j + 1], ocb[:, j, :], ALU.mult, ALU.add),
                        reads=[bfs, b_sm, b_ocb], writes=[b_ocb])
                    P.op("dve", lambda e, fw_=fw_, j=j, o0=o0, cf=cf: e.scalar_tensor_tensor(
                        ob[:, j, :], fw_[:, o0:o0 + 128], cf[:, 4 + j:5 + j], ocb[:, j, :], ALU.mult, ALU.add),
                        reads=[bfw, b_sm, b_ocb], writes=[b_ob])
                for j in range(4):
                    P.op("pe", lambda e, j=j: e.transpose(BK.h[:, j * 128:(j + 1) * 128], ob[:, j, :], identb[:]), reads=[b_ob, b_cb], writes=[BK.bh])
                ot, bot = oTt[k2], b_oTt[k2]
                P.op("act", lambda e, ot=ot: e.copy(ot[:].rearrange("p j q -> p (j q)"), BK.h[:, 0:512]), reads=[BK.bh], writes=[bot])
                ostore(P, T, ot[:], [bot], k2)
            full_barrier(P)


C_OFF_KV_, C_OFF_GATE_ = 1024, 2560


def pack_m1(inp, h1b, g):
    w = inp["c_w_in"][0]
    wQ = w[:, g * 512:(g + 1) * 512]
    kvcols = []
    for br in range(3):
        for kv in range(2):
            o = C_OFF_KV_ + ((br * 2 + kv) * 2 + g) * 128
            kvcols.append(w[:, o:o + 128])
    wKV = np.concatenate(kvcols, axis=1)
    gcols = [C_OFF_GATE_ + br * 8 + g * 4 + j for br in range(3) for j in range(4)]
    wG = w[:, gcols]
    gb = inp["c_gate_b"][0][[c - C_OFF_GATE_ for c in gcols]][None]
    pos = inp["c_cmp_pos"][0]
    posT = np.concatenate([pos[0].T, pos[1].T], axis=1)
    relb = np.concatenate([inp["rel_bias"][:, 4 * g:4 * g + 4], np.full((1, 4), -30000.0, np.float32)], axis=0)
    c = nsa_consts()
    return dict(x=h1b, wQ=np.ascontiguousarray(wQ), wKV=np.ascontiguousarray(wKV), wG=np.ascontiguousarray(wG),
                gb=np.ascontiguousarray(gb.astype(np.float32)), posT=np.ascontiguousarray(posT), w1=inp["c_cmp_w1"][0], w2=inp["c_cmp_w2"][0],
                relb=np.ascontiguousarray(relb), oh1=c["oh1"], ovl=np.ascontiguousarray(c["ovl"].reshape(128, 512)),
                cst1=np.ascontiguousarray(c["cst1"].reshape(128, 512)), qhalf=c["qhalf"], ident=np.eye(128, dtype=np.float32))


U32 = mybir.dt.uint32
GROUPS = [[0, 1], [2, 3], [4, 5], [6, 7]]


def build_fused():
    nc = bass.Bass("TRN2", target_bir_lowering=False)
    ident_d = nc.dram_tensor("ident", [128, 128], F32, kind="ExternalInput").ap()
    gidx_d = nc.dram_tensor("gidx", [128, 16], U32, kind="ExternalInput").ap()
    xres_d = nc.dram_tensor("xres", [TOK, D], F32, kind="ExternalInput").ap()
    out_d = nc.dram_tensor("out", [TOK, D], F32, kind="ExternalOutput").ap()
    io_m0 = declare_mixer0(nc, "m0_")
    io_p0 = declare_post(nc, "p0_")
    io_m1 = declare_mixer1(nc, "m1_")
    io_p1 = declare_post(nc, "p1_")
    y0src = [nc.dram_tensor(f"y0src{i}", [2048, 512], BF16).ap() for i in range(4)]
    y0dst = [nc.dram_tensor(f"y0dst{i}", [4096, 512], BF16).ap() for i in range(4)]
    o1src = [nc.dram_tensor(f"o1src{i}", [2048, 512], BF16).ap() for i in range(4)]
    o1dst = [nc.dram_tensor(f"o1dst{i}", [4096, 512], BF16).ap() for i in range(4)]
    h1src = [nc.dram_tensor(f"h1src{i}", [512, D], F32).ap() for i in range(8)]
    h1dst = [nc.dram_tensor(f"h1dst{i}", [1024, D], F32).ap() for i in range(8)]
    with ExitStack() as es:
        P = Prog(nc, es)
        B = P.buf
        gidx = P.sb("gidx_sb", [128, 16], U32)
        b_gidx = B()
        P.dma("sp", gidx[:], gidx_d[:, :], writes=[b_gidx])

        def phase(pfx, fn, io):
            with ExitStack() as pes:
                P.es = pes
                P.pfx = pfx
                P.in_phase = True
                P.scope(pfx + "main")
                fn(P, io)
                P.scope(None)
                full_barrier(P)
                P.recycle()
                P.in_phase = False
            P.es = es

        def ag(srcs, dsts):
            P.in_phase = True
            P.scope("allgather")
            for a, d in zip(srcs, dsts):
                P.all_gather(a, d, GROUPS, reads=[], writes=[B()])
            full_barrier(P)
            P.scope(None)
            P.recycle()
            P.in_phase = False

        def row_store(dst_list, nbuf=4):
            bufs = [[B() for _ in range(nbuf)] for _ in dst_list]
            cnt = [0]

            def ystore(P_, i, blk, sb, reads):
                h, tq = blk // 2, (blk % 2) * 4
                r0 = (h * 8 + tq) * 128
                dst = dst_list[i][r0:r0 + 512, :].rearrange("(t p) c -> p t c", p=128)
                P_.dma("sp", dst, sb.rearrange("p (t c) -> p t c", t=4), reads=reads, writes=[bufs[i][cnt[0] % nbuf]])
                cnt[0] += 1
            return ystore

        def make_load_yt(dsts):
            def load_yt(P_, yt, byt, t4):
                for ec in range(8):
                    i, r = ec // 2, ec % 2
                    P_.gather(yt[:, ec, :], dsts[i], gidx[:, r * 8 + t4:r * 8 + t4 + 1], reads=[b_gidx], writes=[byt[ec]])
            return load_yt

        io_m0.update(ident=ident_d, ystore=row_store(y0src))
        phase("m0_", emit_mixer0, io_m0)
        ag(y0src, y0dst)
        io_p0.update(ident=ident_d, load_yt=make_load_yt(y0dst), xres=lambda t0: xres_d[t0:t0 + 128, :],
                     out=lambda t0: h1src[t0 // 512][t0 % 512:t0 % 512 + 128, :], is_final=False)
        phase("p0_", emit_post, io_p0)
        ag(h1src, h1dst)
        o1bufs = [[B(), B()] for _ in range(4)]

        def ostore(P_, T, ot, reads, slot):
            h, t4 = T // 32, (T % 32) // 4
            r0 = (h * 8 + t4) * 128
            c0 = (T % 4) * 128
            for j in range(4):
                P_.dma("sp", o1src[j][r0:r0 + 128, c0:c0 + 128], ot[:, j, :], reads=reads, writes=[o1bufs[j][slot]])

        def h1tile(t0):
            k, rr, tt = (t0 % TOK) // 512, t0 // TOK, t0 % 512
            return h1dst[k][rr * 512 + tt:rr * 512 + tt + 128, :]

        io_m1.update(ident=ident_d, x=h1tile, ostore=ostore)
        phase("m1_", emit_mixer1, io_m1)
        ag(o1src, o1dst)
        io_p1.update(ident=ident_d, load_yt=make_load_yt(o1dst), xres=lambda t0: h1src[t0 // 512][t0 % 512:t0 % 512 + 128, :],
                     out=lambda t0: out_d[t0:t0 + 128, :], is_final=True)
        phase("p1_", emit_post, io_p1)
        P.finish()
    return nc


def pack_post(inp, layer, wo_perm, pfx):
    wr = np.concatenate([inp["moe_wr_g"][layer]] + [inp["moe_wr_e"][layer][g] for g in range(4)], axis=1)
    br = np.concatenate([inp["moe_br_g"][layer], inp["moe_br_e"][layer].reshape(-1)])[None]
    d = dict(wo=np.ascontiguousarray(wo_perm),
             ln1g=np.ascontiguousarray(inp["ln_g"][layer, 0]), ln1b=np.ascontiguousarray(inp["ln_b"][layer, 0]),
             ln2g=np.ascontiguousarray(inp["ln_g"][layer, 1]), ln2b=np.ascontiguousarray(inp["ln_b"][layer, 1]),
             wg=inp["moe_w_gate"][layer], wu=inp["moe_w_up"][layer], wd=inp["moe_w_down"][layer],
             wr=np.ascontiguousarray(wr.astype(np.float32)), br=np.ascontiguousarray(br.astype(np.float32)))
    return {pfx + k: v for k, v in d.items()}


def kernel(**inputs):
    inp = {k: np.asarray(v) for k, v in inputs.items()}
    nb = inp["x"].shape[0]
    cores = list(range(2 * nb))
    wo0 = inp["ab_w_out"][0]
    ch0 = [(2 * r + i) if i < 2 else (4 + 2 * r + (i - 2)) for i in range(4) for r in range(2)]
    wo0p = np.concatenate([wo0[c * 128:(c + 1) * 128] for c in ch0], axis=0)
    wo1 = inp["c_w_out"][0]
    ch1 = [4 * r + i for i in range(4) for r in range(2)]
    wo1p = np.concatenate([wo1[c * 128:(c + 1) * 128] for c in ch1], axis=0)
    post0 = pack_post(inp, 0, wo0p, "p0_")
    post1 = pack_post(inp, 1, wo1p, "p1_")
    ident = np.eye(128, dtype=np.float32)
    maps = []
    for b in range(nb):
        for c in range(2):
            m = dict(ident=ident, xres=np.ascontiguousarray(inp["x"][b, TOK * c:TOK * (c + 1)]))
            p = np.arange(128, dtype=np.uint32)[:, None]
            rt = np.arange(16, dtype=np.uint32)[None, :]
            m["gidx"] = np.ascontiguousarray(((rt // 8) * 2048 + c * 1024 + (rt % 8) * 128 + p).astype(np.uint32))
            m0 = pack_m0(inp, b, c)
            m0.pop("ident")
            m.update({"m0_" + k: v for k, v in m0.items()})
            m1 = pack_m1(inp, None, c)
            m1.pop("ident")
            m1.pop("x")
            m.update({"m1_" + k: v for k, v in m1.items()})
            m.update(post0)
            m.update(post1)
            maps.append(m)
    nc = build_fused()
    res = run_bass_kernel_spmd(nc, maps, core_ids=cores).results
    out = np.stack([np.concatenate([np.asarray(res[b * 2 + c]["out"]) for c in range(2)], axis=0) for b in range(nb)])
    return out.astype(np.float32)
```
